# Optimizing a Trainium2 kernel written in Bass

```python
import jax, jax.numpy as jnp
from jax import lax
import numpy as np

D_MODEL = 1024
BATCH = 8
SEQ = 2048
DEPTH = 1

N_MEM = 256
D_MIX = D_MODEL
HEAD_DIM = 64
RET_WIDTH = D_MIX // 2
RWKV_WIDTH = D_MIX - RET_WIDTH
RET_HEADS = RET_WIDTH // HEAD_DIM
RWKV_HEADS = RWKV_WIDTH // HEAD_DIM
RET_CHUNK = 128
ROPE_BASE = 10000.0
DECAY_LORA = 64
AAA_LORA = 64
GATE_LORA = 160
RET_PROJ = 4 * RET_WIDTH
RWKV_PROJ = 3 * RWKV_WIDTH + DECAY_LORA + AAA_LORA + GATE_LORA
D_IN_PROJ = RET_PROJ + RWKV_PROJ
XATTN_HEADS = 4
XATTN_HEAD_DIM = D_MODEL // XATTN_HEADS
D_FF = 4 * D_MODEL
RMS_EPS = 1e-6
GN_EPS_RET = 1e-5
GN_EPS_RWKV = 64e-5

kernel_name = "hybrid_retention_rwkv7_memxattn_block"


def rmsnorm(x, g):
    xf = x.astype(jnp.float32)
    y = xf * lax.rsqrt(jnp.mean(jnp.square(xf), axis=-1, keepdims=True) + RMS_EPS)
    return (y * g.astype(jnp.float32)).astype(x.dtype)


def head_group_norm(y, w, b, eps):
    B, S, H, d = y.shape
    yf = y.astype(jnp.float32)
    mu = jnp.mean(yf, axis=-1, keepdims=True)
    var = jnp.mean(jnp.square(yf - mu), axis=-1, keepdims=True)
    yn = ((yf - mu) * lax.rsqrt(var + eps)).reshape(B, S, H * d)
    return yn * w.astype(jnp.float32) + b.astype(jnp.float32)


def rope(x, positions):
    d = x.shape[-1]
    half = d // 2
    inv_freq = ROPE_BASE ** (-jnp.arange(half, dtype=jnp.float32) / half)
    ang = positions.astype(jnp.float32)[..., None] * inv_freq
    cos = jnp.cos(ang)[:, :, None, :]
    sin = jnp.sin(ang)[:, :, None, :]
    xf = x.astype(jnp.float32)
    x1, x2 = xf[..., :half], xf[..., half:]
    return jnp.concatenate([x1 * cos - x2 * sin, x2 * cos + x1 * sin], axis=-1)


def retention_chunkwise(q, k, v):
    B, S, H, d = q.shape
    C = RET_CHUNK
    N = S // C
    log_g = jnp.log(1.0 - 2.0 ** (-5.0 - jnp.arange(H, dtype=jnp.float32)))
    idx = jnp.arange(C, dtype=jnp.float32)
    diff = idx[:, None] - idx[None, :]
    causal = diff >= 0
    dmask = jnp.where(causal[None], jnp.exp(log_g[:, None, None] * jnp.where(causal, diff, 0.0)[None]), 0.0)
    xi = jnp.exp(log_g[:, None] * (idx + 1.0)[None])
    zeta = jnp.exp(log_g[:, None] * (C - 1.0 - idx)[None])
    g_chunk = jnp.exp(log_g * C)
    qc = q.reshape(B, N, C, H, d)
    kc = k.reshape(B, N, C, H, d)
    vc = v.reshape(B, N, C, H, d)
    s = jnp.einsum('bnihd,bnjhd->bnhij', qc, kc) * dmask[None, None]
    intra = jnp.einsum('bnhij,bnjhe->bnihe', s, vc)
    kv = jnp.einsum('bnjhd,hj,bnjhe->bnhde', kc, zeta, vc)

    def step(R, kv_n):
        return R * g_chunk[None, :, None, None] + kv_n, R

    _, R_prev = lax.scan(step, jnp.zeros((B, H, d, d), jnp.float32), jnp.moveaxis(kv, 1, 0))
    R_prev = jnp.moveaxis(R_prev, 0, 1)
    inter = jnp.einsum('bnihd,hi,bnhde->bnihe', qc, xi, R_prev)
    return (intra + inter).reshape(B, S, H, d)


def retention_mixer(p, positions, gn_w, gn_b):
    B, S, _ = p.shape
    q, k, v, g = jnp.split(p, 4, axis=-1)
    q = rope(q.reshape(B, S, RET_HEADS, HEAD_DIM), positions)
    k = rope(k.reshape(B, S, RET_HEADS, HEAD_DIM), positions) * (HEAD_DIM ** -0.5)
    v = v.reshape(B, S, RET_HEADS, HEAD_DIM).astype(jnp.float32)
    y = retention_chunkwise(q, k, v)
    return jax.nn.silu(g.astype(jnp.float32)) * head_group_norm(y, gn_w, gn_b, GN_EPS_RET)


def token_shift(p, mu):
    prev = jnp.pad(p, ((0, 0), (1, 0), (0, 0)))[:, :-1]
    return p + (prev - p) * mu


def wkv7_scan(r, w, k, v, a_vec, b_vec):
    B, S, H, d = r.shape

    def step(state, inp):
        r_t, w_t, k_t, v_t, a_t, b_t = inp
        sa = jnp.einsum('bhvk,bhk->bhv', state, a_t)
        state = (state * w_t[:, :, None, :] + sa[..., None] * b_t[:, :, None, :]
                 + v_t[..., None] * k_t[:, :, None, :])
        return state, jnp.einsum('bhvk,bhk->bhv', state, r_t)

    xs = (jnp.moveaxis(r, 1, 0), jnp.moveaxis(w, 1, 0), jnp.moveaxis(k, 1, 0),
          jnp.moveaxis(v, 1, 0), jnp.moveaxis(a_vec, 1, 0), jnp.moveaxis(b_vec, 1, 0))
    _, y = lax.scan(step, jnp.zeros((B, H, d, d), jnp.float32), xs)
    return jnp.moveaxis(y, 0, 1)


def rwkv7_mixer(p, mu, w0, w_up, a0, a_up, g_up, k_k, k_a, r_k, gn_w, gn_b):
    B, S, _ = p.shape
    H, d, W = RWKV_HEADS, HEAD_DIM, RWKV_WIDTH
    p = token_shift(p.astype(jnp.float32), mu.astype(jnp.float32))
    r, k, v, w_lr, a_lr, g_lr = jnp.split(
        p, [W, 2 * W, 3 * W, 3 * W + DECAY_LORA, 3 * W + DECAY_LORA + AAA_LORA], axis=-1)
    w_log = -jax.nn.softplus(-(w0 + jnp.tanh(w_lr) @ w_up)) - 0.5
    decay = jnp.exp(-jnp.exp(w_log))
    a = jax.nn.sigmoid(a0 + a_lr @ a_up)
    g = jax.nn.sigmoid(g_lr) @ g_up
    kk = (k * k_k).reshape(B, S, H, d)
    kk = kk / jnp.maximum(jnp.sqrt(jnp.sum(jnp.square(kk), axis=-1, keepdims=True)), 1e-12)
    k = k * (1.0 + (a - 1.0) * k_a)
    rh = r.reshape(B, S, H, d)
    kh = k.reshape(B, S, H, d)
    vh = v.reshape(B, S, H, d)
    ah = a.reshape(B, S, H, d)
    y = wkv7_scan(rh, decay.reshape(B, S, H, d), kh, vh, -kk, kk * ah)
    y = head_group_norm(y, gn_w, gn_b, GN_EPS_RWKV)
    bonus = jnp.sum(rh * kh * r_k.astype(jnp.float32), axis=-1, keepdims=True) * vh
    return (y + bonus.reshape(B, S, W)) * g


def memory_cross_attention(h, mem_n, w_q, w_kv, w_o):
    B, S, _ = h.shape
    M = mem_n.shape[1]
    q = (h @ w_q).reshape(B, S, XATTN_HEADS, XATTN_HEAD_DIM)
    k, v = jnp.split(mem_n @ w_kv, 2, axis=-1)
    k = k.reshape(B, M, XATTN_HEADS, XATTN_HEAD_DIM)
    v = v.reshape(B, M, XATTN_HEADS, XATTN_HEAD_DIM)
    s = jnp.einsum('bshd,bmhd->bhsm', q.astype(jnp.float32), k.astype(jnp.float32)) * (XATTN_HEAD_DIM ** -0.5)
    prob = jax.nn.softmax(s, axis=-1).astype(v.dtype)
    o = jnp.einsum('bhsm,bmhd->bshd', prob, v).reshape(B, S, D_MODEL)
    return o @ w_o


def setup_inputs(seed: int = 0) -> dict:
    key = jax.random.key(seed)
    ks = jax.random.split(key, 32)
    f32 = jnp.float32
    L = DEPTH

    def nrm(k, shape, scale):
        return jax.random.normal(k, shape, f32) * scale

    def gain(k, shape):
        return 1.0 + 0.05 * jax.random.normal(k, shape, f32)

    x = jax.random.normal(ks[0], (BATCH, SEQ, D_MODEL), f32)
    mem = jax.random.normal(ks[1], (BATCH, N_MEM, D_MODEL), f32)
    positions = jnp.broadcast_to(jnp.arange(SEQ, dtype=jnp.int32)[None], (BATCH, SEQ))
    return {
        "x": x,
        "mem": mem,
        "positions": positions,
        "norm_mix": gain(ks[2], (L, D_MODEL)),
        "w_in": nrm(ks[3], (L, D_MODEL, D_IN_PROJ), D_MODEL ** -0.5),
        "ret_gn_w": gain(ks[4], (L, RET_WIDTH)),
        "ret_gn_b": nrm(ks[5], (L, RET_WIDTH), 0.02),
        "rwkv_mu": jax.random.uniform(ks[6], (L, RWKV_PROJ), f32),
        "rwkv_w0": jax.random.uniform(ks[7], (L, RWKV_WIDTH), f32, -6.5, -1.5),
        "rwkv_w_up": nrm(ks[8], (L, DECAY_LORA, RWKV_WIDTH), 0.1 * DECAY_LORA ** -0.5),
        "rwkv_a0": nrm(ks[9], (L, RWKV_WIDTH), 0.1),
        "rwkv_a_up": nrm(ks[10], (L, AAA_LORA, RWKV_WIDTH), 0.1 * AAA_LORA ** -0.5),
        "rwkv_g_up": nrm(ks[11], (L, GATE_LORA, RWKV_WIDTH), GATE_LORA ** -0.5),
        "rwkv_k_k": 0.85 + 0.05 * jax.random.normal(ks[12], (L, RWKV_WIDTH), f32),
        "rwkv_k_a": gain(ks[13], (L, RWKV_WIDTH)),
        "rwkv_r_k": nrm(ks[14], (L, RWKV_HEADS, HEAD_DIM), 0.1),
        "rwkv_gn_w": gain(ks[15], (L, RWKV_WIDTH)),
        "rwkv_gn_b": nrm(ks[16], (L, RWKV_WIDTH), 0.02),
        "w_out": nrm(ks[17], (L, D_MIX, D_MODEL), D_MIX ** -0.5),
        "norm_xattn": gain(ks[18], (L, D_MODEL)),
        "norm_mem": gain(ks[19], (L, D_MODEL)),
        "xattn_w_q": nrm(ks[20], (L, D_MODEL, D_MODEL), D_MODEL ** -0.5),
        "xattn_w_kv": nrm(ks[21], (L, D_MODEL, 2 * D_MODEL), D_MODEL ** -0.5),
        "xattn_w_o": nrm(ks[22], (L, D_MODEL, D_MODEL), D_MODEL ** -0.5),
        "norm_mlp": gain(ks[23], (L, D_MODEL)),
        "mlp_w_up": nrm(ks[24], (L, D_MODEL, D_FF), D_MODEL ** -0.5),
        "mlp_w_down": nrm(ks[25], (L, D_FF, D_MODEL), D_FF ** -0.5),
        "norm_final": gain(ks[26], (D_MODEL,)),
    }


def reference(x, mem, positions, norm_mix, w_in, ret_gn_w, ret_gn_b, rwkv_mu, rwkv_w0,
              rwkv_w_up, rwkv_a0, rwkv_a_up, rwkv_g_up, rwkv_k_k, rwkv_k_a, rwkv_r_k,
              rwkv_gn_w, rwkv_gn_b, w_out, norm_xattn, norm_mem, xattn_w_q, xattn_w_kv,
              xattn_w_o, norm_mlp, mlp_w_up, mlp_w_down, norm_final):
    dt = x.dtype
    for l in range(DEPTH):
        h = rmsnorm(x, norm_mix[l])
        p = h @ w_in[l]
        y_ret = retention_mixer(p[..., :RET_PROJ], positions, ret_gn_w[l], ret_gn_b[l])
        y_rwkv = rwkv7_mixer(p[..., RET_PROJ:], rwkv_mu[l], rwkv_w0[l], rwkv_w_up[l], rwkv_a0[l],
                             rwkv_a_up[l], rwkv_g_up[l], rwkv_k_k[l], rwkv_k_a[l], rwkv_r_k[l],
                             rwkv_gn_w[l], rwkv_gn_b[l])
        y = jnp.concatenate([y_ret, y_rwkv], axis=-1).astype(dt)
        x = x + y @ w_out[l]
        x = x + memory_cross_attention(rmsnorm(x, norm_xattn[l]), rmsnorm(mem, norm_mem[l]),
                                       xattn_w_q[l], xattn_w_kv[l], xattn_w_o[l])
        h = rmsnorm(x, norm_mlp[l])
        x = x + jnp.square(jax.nn.relu(h @ mlp_w_up[l])) @ mlp_w_down[l]
    return rmsnorm(x, norm_final)
```

```python
import contextlib
import math
import numpy as np
import concourse.bass as bass
import concourse.mybir as mybir
from concourse.bass_utils import run_bass_kernel_spmd

F32 = mybir.dt.float32
BF16 = mybir.dt.bfloat16
I32 = mybir.dt.int32
AF = mybir.ActivationFunctionType
ALU = mybir.AluOpType
AX = mybir.AxisListType

T = 2048
D = 1024
NT = 16
KC = 8
NCOLS = 135
NCQ = 1024
CUT = 99
COMPUTE = ("pe", "act", "dve", "pool")
ISSUERS = ("pe", "act", "dve", "pool", "sp")


class Prog:
    def __init__(self, nc, st, n_dma=12, self_sync=True):
        self.nc = nc
        self.n_dma = n_dma
        self.self_sync = self_sync
        self.chans = list(COMPUTE) + [f"d{i}" for i in range(n_dma)]
        self.sems = {c: st.enter_context(nc.semaphore(f"s_{c}")) for c in self.chans}
        self.streams = {e: [] for e in ISSUERS}
        self.count = {c: 0 for c in self.chans}
        self.clock = {e: {} for e in ISSUERS}
        self.snap = {}
        self.wr = {}
        self.rd = {}
        self.rr = 0
        self.nops = 0
        self.nwaits = 0

    @staticmethod
    def _need(needs, d):
        for c, n in d.items():
            if needs.get(c, 0) < n:
                needs[c] = n

    def op(self, eng, fn, reads=(), writes=(), dma=False):
        needs = {}
        for k in reads:
            self._need(needs, self.wr.get(k, {}))
        for k in writes:
            self._need(needs, self.wr.get(k, {}))
            self._need(needs, self.rd.get(k, {}))
        if dma:
            chan = f"d{self.rr % self.n_dma}"
            self.rr += 1
            if self.count[chan]:
                self._need(needs, {chan: self.count[chan]})
        else:
            chan = eng
        clk = self.clock[eng]
        waits = []
        for c, n in needs.items():
            if c == eng and (eng == "pe" or not self.self_sync):
                continue
            if clk.get(c, 0) < n:
                waits.append((c, n))
                for c2, n2 in self.snap[(c, n)].items():
                    if clk.get(c2, 0) < n2:
                        clk[c2] = n2
        self.nwaits += len(waits)
        self.nops += 1
        n_new = self.count[chan] + 1
        self.count[chan] = n_new
        s = dict(clk)
        s[chan] = n_new
        self.snap[(chan, n_new)] = s
        if eng == "pe" and chan == "pe":
            clk["pe"] = n_new
        wset = set(writes)
        for k in wset:
            self.wr[k] = {chan: n_new}
            self.rd[k] = {}
        for k in reads:
            if k in wset:
                continue
            self.rd.setdefault(k, {})[chan] = n_new
        self.streams[eng].append((waits, fn, chan))

    def barrier(self):
        allk = {c: n for c, n in self.count.items() if n}
        for e in ISSUERS:
            clk = self.clock[e]
            waits = []
            for c, n in allk.items():
                if c == e and e == "pe":
                    continue
                if clk.get(c, 0) < n:
                    waits.append((c, n))
            if waits:
                self.streams[e].append((waits, None, None))
        for e in ISSUERS:
            for c, n in allk.items():
                if self.clock[e].get(c, 0) < n:
                    self.clock[e][c] = n

    def flush(self):
        nc = self.nc
        streams = self.streams
        if not any(streams[e] for e in ISSUERS):
            return
        self.streams = {e: [] for e in ISSUERS}
        sems = self.sems

        def val(c, n):
            return n * 16 if c.startswith("d") else n

        def run(engname):
            def body(e):
                for waits, fn, chan in streams[engname]:
                    for c, n in waits:
                        e.wait_ge(sems[c], val(c, n))
                    if fn is not None:
                        fn(e).then_inc(sems[chan], 16 if chan.startswith("d") else 1)
            return body

        with nc.Block() as block:
            block.tensor(run("pe"))
            block.scalar(run("act"))
            block.vector(run("dve"))
            block.gpsimd(run("pool"))
            block.sync(run("sp"))


class Ring:
    def __init__(self, kb, name, n, shape, dtype, st=None, psum=False):
        st = st or kb.st
        alloc = kb.nc.psum_tensor if psum else kb.nc.sbuf_tensor
        self.name = name
        self.t = [st.enter_context(alloc(f"rg_{name}{i}", shape, dtype)) for i in range(n)]
        self.i = 0

    def next(self):
        i = self.i % len(self.t)
        self.i += 1
        return self.t[i], f"{self.name}.{i}"


class ViewRing:
    def __init__(self, aps, keys):
        self.t, self.k, self.i = aps, keys, 0

    def next(self):
        i = self.i % len(self.t)
        self.i += 1
        return self.t[i], self.k[i]


class Banks:
    def __init__(self, pp):
        import collections
        self.pp = pp
        self.free = collections.deque(range(8))

    def get(self):
        return self.free.popleft()

    def put(self, i):
        self.free.append(i)

    def f32(self, i):
        return self.pp[:, i, :]

    def bf(self, i):
        return self.pp[:, i, :].bitcast(BF16)

    @staticmethod
    def key(i):
        return f"ps{i}"


def interleave(gens):
    gens = list(gens)
    while gens:
        for g in list(gens):
            try:
                next(g)
            except StopIteration:
                gens.remove(g)


class KB:
    def __init__(self, nc, P, st):
        self.nc, self.P, self.st = nc, P, st
        self.flip = 0

    def sb(self, name, shape, dtype, st=None):
        return (st or self.st).enter_context(self.nc.sbuf_tensor("sb_" + name, shape, dtype))

    def mm(self, out, lhsT, rhs, start, stop, r, w):
        self.P.op("pe", lambda e: e.matmul(out, lhsT=lhsT, rhs=rhs, start=start, stop=stop), r, w)

    def tr(self, out, in_, r, w):
        idb = self.identb[:]
        self.P.op("pe", lambda e: e.transpose(out=out, in_=in_, identity=idb), list(r) + ["identb"], w)

    def act(self, out, in_, func, r, w, scale=1.0, bias=0.0, accum=None):
        kw = {}
        if accum is not None:
            kw["accum_out"] = accum
        self.P.op("act", lambda e: e.activation(out=out, in_=in_, func=func, bias=bias, scale=scale, **kw), r, w)

    def tt(self, eng, out, in0, in1, op, r, w):
        self.P.op(eng, lambda e: e.tensor_tensor(out=out, in0=in0, in1=in1, op=op), r, w)

    def ts(self, eng, out, in0, s1, s2, op0, op1, r, w):
        if s2 is None:
            self.P.op(eng, lambda e: e.tensor_scalar(out=out, in0=in0, scalar1=s1, scalar2=None, op0=op0), r, w)
        else:
            self.P.op(eng, lambda e: e.tensor_scalar(out=out, in0=in0, scalar1=s1, scalar2=s2, op0=op0, op1=op1), r, w)

    def stt(self, out, in0, sc, in1, op0, op1, r, w):
        self.P.op("dve", lambda e: e.scalar_tensor_tensor(out=out, in0=in0, scalar=sc, in1=in1, op0=op0, op1=op1), r, w)

    def cp(self, eng, out, in_, r, w):
        if eng == "act":
            self.P.op("act", lambda e: e.activation(out=out, in_=in_, func=AF.Copy), r, w)
        else:
            self.P.op(eng, lambda e: e.tensor_copy(out=out, in_=in_), r, w)

    def cpalt(self, out, in_, r, w):
        self.flip ^= 1
        self.cp("act" if self.flip else "dve", out, in_, r, w)

    def red(self, out, in_, op, r, w):
        self.P.op("dve", lambda e: e.tensor_reduce(out=out, in_=in_, axis=AX.X, op=op), r, w)

    def dma(self, eng, out, in_, r, w):
        self.P.op(eng, lambda e: e.dma_start(out=out, in_=in_), r, w, dma=True)


def build(stages="ABCD", dbg=False):
    nc = bass.Bass("TRN2", target_bir_lowering=False)

    def dram(n, s, d, kind="ExternalInput"):
        return nc.dram_tensor(n, s, d, kind=kind).ap()

    x_d = dram("x", [T, D], F32)
    mem_d = dram("mem", [256, D], F32)
    pos_d = dram("pos", [128, NT], I32)
    cols_d = dram("cols", [128, NCOLS], F32)
    cq_d = dram("cq", [128, NCQ], F32)
    gfin_d = dram("gfin", [D], F32)
    w_in_d = dram("w_in", [D, 3872], F32)
    wup_d = dram("w_up", [64, 512], F32)
    aup_d = dram("a_up", [64, 512], F32)
    gup_d = dram("g_up", [160, 512], F32)
    w_out_d = dram("w_out", [D, D], F32)
    wq_d = dram("wq", [D, D], F32)
    wkv_d = dram("wkv", [D, 2 * D], F32)
    wo_d = dram("wo", [D, D], F32)
    wupm_d = dram("mlp_up", [D, 4 * D], F32)
    wdn_d = dram("mlp_down", [4 * D, D], F32)
    out_d = dram("out", [T, D], F32, kind="ExternalOutput")
    if dbg:
        dbg_yT = dram("dbg_yT", [128, KC, T], BF16, kind="ExternalOutput")
        dbg_hT = dram("dbg_hT", [128, KC, T], BF16, kind="ExternalOutput")

    def wview(w):
        return w.rearrange("(kc p) n -> p kc n", p=128)

    with contextlib.ExitStack() as st:
        P = Prog(nc, st)
        kb = KB(nc, P, st)
        cols = kb.sb("cols", [128, NCOLS], F32)
        dc = kb.sb("dcols", [128, 40], F32)
        cqf = kb.sb("cqf", [128, NCQ], F32)
        identb = kb.sb("identb", [128, 128], BF16)
        bonesb = kb.sb("bonesb", [128, 128], BF16)
        onesb = kb.sb("onesb", [128, 128], BF16)
        mhalf = kb.sb("mhalf", [128, 256], F32)
        hT = kb.sb("hT", [128, KC, T], BF16)
        yT = kb.sb("yT", [128, 4, T], BF16)
        WA = kb.sb("WA", [128, 16384], BF16)
        ss = kb.sb("ss", [128, 3 * NT], F32)
        kb.identb = identb
        pp = st.enter_context(nc.psum_tensor("pp", [128, 8, 512], F32))
        L_BK = Banks(pp)
        psA = ViewRing([pp[:, i, :] for i in range(6)], [f"ps{i}" for i in range(6)])
        psT = ViewRing([pp[:, i, :].bitcast(BF16) for i in (6, 7)], ["ps6", "ps7"])
        mask4 = cqf[:, 128:640]
        maskI = cqf[:, 256:384]
        maskLs = cqf[:, 640:768]
        identf = cqf[:, 0:128]

        kb.dma("sp", cols[:], cols_d, [], ["cols"])
        kb.dma("sp", cqf[:], cq_d, [], ["cqf"])
        kb.dma("pool", identb[:], cq_d[:, 0:128], [], ["identb"])
        kb.dma("pool", bonesb[:], cq_d[:, 768:896], [], ["bonesb"])
        kb.dma("pool", onesb[:], cq_d[:, 896:1024], [], ["onesb"])
        P.op("dve", lambda e: e.memset(mhalf[:], -0.5), [], ["mhalf"])
        kb.ts("dve", dc[:, 0:15], cols[:, 40:55], -1.0, 1.0, ALU.mult, ALU.add, ["cols"], ["dc"])
        kb.ts("dve", dc[:, 15:19], cols[:, 55:59], 0.5, None, ALU.mult, None, ["cols"], ["dc"])
        kb.ts("dve", dc[:, 19:23], cols[:, 59:63], 0.5, None, ALU.mult, None, ["cols"], ["dc"])
        kb.ts("dve", dc[:, 23:27], cols[:, 67:71], -1.0, None, ALU.mult, None, ["cols"], ["dc"])
        kb.ts("dve", dc[:, 27:31], cols[:, 67:71], -1.0, 1.0, ALU.mult, ALU.add, ["cols"], ["dc"])

        def norm_T(src, nblk, tpb, gcol0, dst, dstkey, sscol0, hb, sqj):
            for blk in range(nblk):
                hbs = []
                for tl in range(tpb):
                    t = blk * tpb + tl
                    xa, xk = src(t)
                    c = sscol0 + t
                    kb.act(sqj[:], xa, AF.Square, [xk], ["sqj", f"ss{c}"], accum=ss[:, c:c + 1])
                    kb.ts("dve", ss[:, c:c + 1], ss[:, c:c + 1], 1.0 / D, 1e-6, ALU.mult, ALU.add, [f"ss{c}"], [f"ss{c}"])
                    kb.tt("pool", ss[:, c:c + 1], ss[:, c:c + 1], mhalf[:, 0:1], ALU.pow, [f"ss{c}", "mhalf"], [f"ss{c}"])
                    h, hk = hb.next()
                    kb.act(h[:], xa, AF.Copy, [xk, f"ss{c}"], [hk], scale=ss[:, c:c + 1])
                    hbs.append((h, hk))
                for kc in range(KC):
                    bank, bk = psT.next()
                    for tl, (h, hk) in enumerate(hbs):
                        kb.tr(bank[:, tl * 128:(tl + 1) * 128], h[:, kc * 128:(kc + 1) * 128], [hk], [bk])
                    n = tpb * 128
                    o = dst[:, kc, blk * n:(blk + 1) * n]
                    kb.flip ^= 1
                    if kb.flip:
                        kb.act(o, bank[:, 0:n], AF.Copy, [bk, "cols"], [dstkey(blk)], scale=cols[:, gcol0 + kc:gcol0 + kc + 1])
                    else:
                        kb.ts("dve", o, bank[:, 0:n], cols[:, gcol0 + kc:gcol0 + kc + 1], None, ALU.mult, None, [bk, "cols"], [dstkey(blk)])

        def load_w(dst, src, key, eng="pool"):
            kb.dma(eng, dst, src, [], [key])

        Wv8 = lambda ncol: WA[:, 0:KC * ncol].rearrange("p (k n) -> p k n", k=KC)

        Wr = Wv8(2048)
        BK = L_BK

        def acquire(n=1):
            spins = 0
            while len(BK.free) < n:
                spins += 1
                assert spins < 100000, "PSUM bank pool deadlock"
                yield
            return [BK.get() for _ in range(n)]

        def rr(gens):
            gens = list(gens)
            while gens:
                for g in list(gens):
                    try:
                        next(g)
                    except StopIteration:
                        gens.remove(g)
                yield

        def run(g):
            for _ in g:
                pass

        if "B" not in stages:
            with contextlib.ExitStack() as sa:
                xring = Ring(kb, "xr", 3, [128, D], F32, st=sa)
                hb = Ring(kb, "hb", 8, [128, D], BF16, st=sa)
                sqj = kb.sb("sqj", [128, D], BF16, st=sa)

                def srcx(t):
                    xa, xk = xring.next()
                    kb.dma("sp", xa[:], x_d[t * 128:(t + 1) * 128, :], [], [xk])
                    return xa[:], xk
                norm_T(srcx, 4, 4, 0, hT, lambda b: f"hT.{b}", 0, hb, sqj)
                P.barrier()
                P.flush()
            P.op("dve", lambda e: e.memset(yT[:, 0:4, :], 0.0), [], [f"yT.{t}.0" for t in range(NT)])
        else:
            for gi in range(4):
                load_w(Wr[:, :, gi * 512:(gi + 1) * 512], wview(w_in_d)[:, :, gi * 512:(gi + 1) * 512], f"W.{gi}")
            with contextlib.ExitStack() as sbk:
                cos_t = kb.sb("cos_t", [128, NT, 32], F32, st=sbk)
                sin_t = kb.sb("sin_t", [128, NT, 32], F32, st=sbk)
                srope = sbk
                if True:
                    posi = kb.sb("posi", [128, NT], I32, st=srope)
                    posf = kb.sb("posf", [128, NT], F32, st=srope)
                    ang = kb.sb("ang", [128, NT, 32], F32, st=srope)
                    ra = kb.sb("ra", [128, NT, 32], F32, st=srope)
                    rb_ = kb.sb("rb_", [128, NT, 32], F32, st=srope)
                    ri = kb.sb("ri", [128, NT, 32], I32, st=srope)
                    kb.dma("sp", posi[:], pos_d, [], ["posi"])
                    kb.cp("dve", posf[:], posi[:], ["posi"], ["posf"])
                    kb.tt("dve", ang[:], posf[:].unsqueeze(2).to_broadcast([128, NT, 32]),
                          cols[:, 103:135].unsqueeze(1).to_broadcast([128, NT, 32]), ALU.mult, ["posf", "cols"], ["ang"])
                    C1 = 6.28125
                    C2 = 2.0 * math.pi - C1
                    for tab, shift, nm in ((sin_t, 0.0, "sin"), (cos_t, 0.5 * math.pi, "cos")):
                        kb.ts("dve", ra[:], ang[:], shift, 1.0 / (2.0 * math.pi), ALU.add, ALU.mult, ["ang"], ["ra"])
                        kb.cp("dve", ri[:], ra[:], ["ra"], ["ri"])
                        kb.cp("dve", rb_[:], ri[:], ["ri"], ["rb"])
                        kb.ts("dve", ra[:], ang[:], shift, None, ALU.add, None, ["ang"], ["ra"])
                        kb.stt(ra[:], rb_[:], -C1, ra[:], ALU.mult, ALU.add, ["rb", "ra"], ["ra"])
                        kb.stt(ra[:], rb_[:], -C2, ra[:], ALU.mult, ALU.add, ["rb", "ra"], ["ra"])
                        kb.ts("dve", rb_[:], ra[:], math.pi, -2.0 * math.pi, ALU.is_gt, ALU.mult, ["ra"], ["rb"])
                        kb.tt("dve", ra[:], ra[:], rb_[:], ALU.add, ["ra", "rb"], ["ra"])
                        kb.ts("dve", rb_[:], ra[:], -math.pi, 2.0 * math.pi, ALU.is_lt, ALU.mult, ["ra"], ["rb"])
                        kb.tt("dve", ra[:], ra[:], rb_[:], ALU.add, ["ra", "rb"], ["ra"])
                        kb.act(tab[:], ra[:], AF.Sin, ["ra"], [nm])

                sb_ = lambda n, shp, d: kb.sb(n, shp, d, st=sbk)
                W = 2
                xr_p = [sb_(f"xr{i}", [128, D], F32) for i in range(2)]
                hb_p = [sb_(f"hb{i}", [128, D], BF16) for i in range(4)]
                sqj = sb_("sqj", [128, D], BF16)
                qT_p = [sb_(f"qT{i}", [128, 4, 2, 512], BF16) for i in range(2)]
                for i in range(2):
                    P.op("dve", lambda e, i=i: e.memset(qT_p[i][:], 0.0), [], [f"qT{i}.{j}" for j in range(4)])
                kT_p = [sb_(f"kT{i}", [128, 4, 512], BF16) for i in range(2)]
                ktok_p = [sb_(f"ktok{i}", [128, 4, 512], BF16) for i in range(2)]
                vtok_p = [sb_(f"vtok{i}", [128, 4, 512], BF16) for i in range(2)]
                gateT_p = [sb_(f"gateT{i}", [128, 4, 512], BF16) for i in range(2)]
                xs_p = [sb_(f"xs{i}", [128, 512], F32) for i in range(W)]
                A_p = [sb_(f"rA{i}", [128, 512], F32) for i in range(W)]
                B_p = [sb_(f"rB{i}", [128, 512], F32) for i in range(W)]
                qtok_p = [sb_(f"qtok{i}", [128, 512], BF16) for i in range(W)]
                th_p = [sb_(f"thr{i}", [128, 512], F32) for i in range(2)]
                sT_p = [sb_(f"sT{i}", [128, 8, 128], BF16) for i in range(W)]
                y32_p = [sb_(f"y32{i}", [128, 512], F32) for i in range(W)]
                sq_p = [sb_(f"sqr{i}", [128, 512], F32) for i in range(W)]
                ynb_p = [sb_(f"ynb{i}", [128, 512], BF16) for i in range(W)]
                yaff_p = [sb_(f"yaff{i}", [128, 2, 128], F32) for i in range(W)]
                stat_p = [sb_(f"stat{i}", [128, 64], F32) for i in range(W)]
                R32 = sb_("R32", [128, 4, 64], F32)
                Rbf = sb_("Rbf", [128, 5, 4, 64], BF16)
                P.op("dve", lambda e: e.memset(R32[:], 0.0), [], ["R32"])

                def gen_normA(blk):
                    for tl in range(4):
                        t = blk * 4 + tl
                        xa, xk = xr_p[t % 2], f"xr.{t % 2}"
                        kb.dma("sp", xa[:], x_d[t * 128:(t + 1) * 128, :], [], [xk])
                        kb.act(sqj[:], xa[:], AF.Square, [xk], ["sqj", f"ss{t}"], accum=ss[:, t:t + 1])
                        yield
                        kb.ts("dve", ss[:, t:t + 1], ss[:, t:t + 1], 1.0 / D, 1e-6, ALU.mult, ALU.add, [f"ss{t}"], [f"ss{t}"])
                        yield
                        kb.tt("pool", ss[:, t:t + 1], ss[:, t:t + 1], mhalf[:, 0:1], ALU.pow, [f"ss{t}", "mhalf"], [f"ss{t}"])
                        yield
                        kb.act(hb_p[tl][:], xa[:], AF.Copy, [xk, f"ss{t}"], [f"hb.{tl}"], scale=ss[:, t:t + 1])
                        yield
                    for kc in range(KC):
                        (b,) = yield from acquire(1)
                        bankT, bk = BK.bf(b), BK.key(b)
                        for tl in range(4):
                            kb.tr(bankT[:, tl * 128:(tl + 1) * 128], hb_p[tl][:, kc * 128:(kc + 1) * 128], [f"hb.{tl}"], [bk])
                        yield
                        o = hT[:, kc, blk * 512:(blk + 1) * 512]
                        if kc % 2:
                            kb.act(o, bankT[:, 0:512], AF.Copy, [bk, "cols"], [f"hT.{blk}"], scale=cols[:, kc:kc + 1])
                        else:
                            kb.ts("dve", o, bankT[:, 0:512], cols[:, kc:kc + 1], None, ALU.mult, None, [bk, "cols"], [f"hT.{blk}"])
                        BK.put(b)
                        yield

                def post_gen(slot, b, eps, wcol0, bcol0, finish):
                    bank, bk = BK.f32(b), BK.key(b)
                    y32, k1 = y32_p[slot], f"y32.{slot}"
                    kb.cp("act", y32[:], bank, [bk], [k1])
                    BK.put(b)
                    yield
                    stt_, ks = stat_p[slot], f"stat.{slot}"
                    v3 = lambda a: a.rearrange("p (h d) -> p h d", d=64)
                    bc = lambda a: a.unsqueeze(2).to_broadcast([128, 8, 64])
                    kb.red(stt_[:, 0:8], v3(y32[:]), ALU.add, [k1], [ks])
                    sq, k2 = sq_p[slot], f"sq.{slot}"
                    kb.act(sq[:], y32[:], AF.Square, [k1], [k2])
                    yield
                    kb.red(stt_[:, 8:16], v3(sq[:]), ALU.add, [k2], [ks])
                    yield
                    kb.ts("dve", stt_[:, 16:24], stt_[:, 0:8], 1.0 / 64, None, ALU.mult, None, [ks], [ks])
                    yield
                    kb.tt("dve", stt_[:, 24:32], stt_[:, 16:24], stt_[:, 16:24], ALU.mult, [ks], [ks])
                    kb.ts("dve", stt_[:, 32:40], stt_[:, 8:16], 1.0 / 64, eps, ALU.mult, ALU.add, [ks], [ks])
                    yield
                    kb.tt("dve", stt_[:, 40:48], stt_[:, 32:40], stt_[:, 24:32], ALU.subtract, [ks], [ks])
                    yield
                    kb.tt("pool", stt_[:, 48:56], stt_[:, 40:48], mhalf[:, 0:8], ALU.pow, [ks, "mhalf"], [ks])
                    yield
                    kb.stt(stt_[:, 56:64], stt_[:, 16:24], -1.0, stt_[:, 48:56], ALU.mult, ALU.mult, [ks], [ks])
                    kb.tt("pool", v3(y32[:]), v3(y32[:]), bc(stt_[:, 48:56]), ALU.mult, [k1, ks], [k1])
                    yield
                    ynb, k3 = ynb_p[slot], f"ynb.{slot}"
                    kb.tt("dve", v3(ynb[:]), v3(y32[:]), bc(stt_[:, 56:64]), ALU.add, [k1, ks], [k3])
                    yield
                    (b2,) = yield from acquire(1)
                    bankT, kt = BK.bf(b2), BK.key(b2)
                    for ct in range(4):
                        kb.tr(bankT[:, ct * 128:(ct + 1) * 128], ynb[:, ct * 128:(ct + 1) * 128], [k3], [kt])
                    yield
                    for ct in range(4):
                        ya, k4 = yaff_p[slot][:, ct % 2, :], f"yaff.{slot}.{ct % 2}"
                        kb.act(ya, bankT[:, ct * 128:(ct + 1) * 128], AF.Identity, [kt, "cols"], [k4],
                               scale=cols[:, wcol0 + ct:wcol0 + ct + 1], bias=cols[:, bcol0 + ct:bcol0 + ct + 1])
                        finish(ct, ya, k4)
                        if ct == 3:
                            BK.put(b2)
                        yield

                def qk_chain(slot, blk, tl, wi):
                    par = blk % 2
                    t = blk * 4 + tl
                    tok = slice(t * 128, (t + 1) * 128)
                    hk = f"hT.{blk}"
                    tabcol = 83 if wi == 0 else 91
                    (b,) = yield from acquire(1)
                    bank, bk = BK.f32(b), BK.key(b)
                    for kc in range(KC):
                        kb.mm(bank, hT[:, kc, tok], Wr[:, kc, wi * 512:(wi + 1) * 512], kc == 0, kc == KC - 1, [hk, f"W.{wi}"], [bk])
                    yield
                    xs, kx = xs_p[slot], f"xs.{slot}"
                    kb.cp("act", xs[:], bank, [bk], [kx])
                    BK.put(b)
                    yield
                    A, ka = A_p[slot], f"rA.{slot}"
                    B, kbk = B_p[slot], f"rB.{slot}"
                    v4 = lambda a: a.rearrange("p (h two f) -> p h two f", two=2, f=32)
                    cs4 = cos_t[:, t, :].unsqueeze(1).unsqueeze(1).to_broadcast([128, 8, 2, 32])
                    sn3 = sin_t[:, t, :].unsqueeze(1).to_broadcast([128, 8, 32])
                    kb.tt("dve", v4(A[:]), v4(xs[:]), cs4, ALU.mult, [kx, "cos"], [ka])
                    kb.tt("pool", v4(B[:])[:, :, 0, :], v4(xs[:])[:, :, 1, :], sn3, ALU.mult, [kx, "sin"], [kbk])
                    kb.tt("pool", v4(B[:])[:, :, 1, :], v4(xs[:])[:, :, 0, :], sn3, ALU.mult, [kx, "sin"], [kbk])
                    yield
                    kb.tt("dve", v4(A[:])[:, :, 0, :], v4(A[:])[:, :, 0, :], v4(B[:])[:, :, 0, :], ALU.subtract, [ka, kbk], [ka])
                    kb.tt("dve", v4(A[:])[:, :, 1, :], v4(A[:])[:, :, 1, :], v4(B[:])[:, :, 1, :], ALU.add, [ka, kbk], [ka])
                    yield
                    scl = cols[:, tabcol:tabcol + 8].unsqueeze(2).to_broadcast([128, 8, 64])
                    A3 = A[:].rearrange("p (h d) -> p h d", d=64)
                    if wi == 0:
                        qtv, kq = qtok_p[slot][:], f"qtok.{slot}"
                    else:
                        qtv, kq = ktok_p[par][:, tl, :], f"ktok{par}.{tl}"
                    kb.tt("dve", qtv.rearrange("p (h d) -> p h d", d=64), A3, scl, ALU.mult, [ka, "cols"], [kq])
                    yield
                    (b2,) = yield from acquire(1)
                    bankT, kt = BK.bf(b2), BK.key(b2)
                    for ct in range(4):
                        kb.tr(bankT[:, ct * 128:(ct + 1) * 128], qtv[:, ct * 128:(ct + 1) * 128], [kq], [kt])
                    yield
                    bv = bankT[:, 0:512].rearrange("p (c n) -> p c n", c=4)
                    if wi == 0:
                        kb.cp("act", qT_p[par][0:64, :, 0, tl * 128:(tl + 1) * 128], bv[0:64], [kt], [f"qT{par}.{tl}"])
                        kb.cp("dve", qT_p[par][64:128, :, 1, tl * 128:(tl + 1) * 128], bv[64:128], [kt], [f"qT{par}.{tl}"])
                    else:
                        kb.cpalt(kT_p[par][:, :, tl * 128:(tl + 1) * 128], bv, [kt], [f"kT{par}.{tl}"])
                    BK.put(b2)
                    yield

                def v_chain(blk, tl):
                    par = blk % 2
                    t = blk * 4 + tl
                    tok = slice(t * 128, (t + 1) * 128)
                    (b,) = yield from acquire(1)
                    bank, bk = BK.f32(b), BK.key(b)
                    for kc in range(KC):
                        kb.mm(bank, hT[:, kc, tok], Wr[:, kc, 1024:1536], kc == 0, kc == KC - 1, [f"hT.{blk}", "W.2"], [bk])
                    yield
                    kb.cpalt(vtok_p[par][:, tl, :], bank, [bk], [f"vtok{par}.{tl}"])
                    BK.put(b)
                    yield

                def g_chain(blk, ct):
                    par = blk % 2
                    (b,) = yield from acquire(1)
                    bank, bk = BK.f32(b), BK.key(b)
                    for kc in range(KC):
                        kb.mm(bank, Wr[:, kc, 1536 + ct * 128:1536 + (ct + 1) * 128], hT[:, kc, blk * 512:(blk + 1) * 512],
                              kc == 0, kc == KC - 1, [f"hT.{blk}", "W.3"], [bk])
                    yield
                    th, kth = th_p[ct % 2], f"thr.{ct % 2}"
                    kb.act(th[:], bank, AF.Tanh, [bk], [kth], scale=0.5)
                    yield
                    kb.ts("pool", th[:], th[:], 0.5, 0.5, ALU.mult, ALU.add, [kth], [kth])
                    yield
                    kb.tt("dve", gateT_p[par][:, ct, :], th[:], bank, ALU.mult, [kth, bk], [f"gateT{par}.{ct}"])
                    BK.put(b)
                    yield

                def gen_proj(blk):
                    for k in range(4):
                        yield from rr([qk_chain((2 * k) % W, blk, k, 0), qk_chain((2 * k + 1) % W, blk, k, 1), v_chain(blk, k), g_chain(blk, k)])

                def chunk_chain(slot, blk, tl):
                    par = blk % 2
                    qT, kT_, vtok, gateT = qT_p[par], kT_p[par], vtok_p[par], gateT_p[par]
                    t = blk * 4 + tl
                    tok = slice(t * 128, (t + 1) * 128)
                    tl_ = slice(tl * 128, (tl + 1) * 128)
                    sT, ksT = sT_p[slot], f"sT.{slot}"
                    bs = yield from acquire(2)
                    for g4 in range(2):
                        bank, bk = BK.f32(bs[g4]), BK.key(bs[g4])
                        for hh in range(4):
                            h = 4 * g4 + hh
                            kb.mm(bank[:, hh * 128:(hh + 1) * 128], kT_[:, h // 2, tl_], qT[:, h // 2, h % 2, tl_], True, True,
                                  [f"kT{par}.{tl}", f"qT{par}.{tl}"], [bk])
                    yield
                    for g4 in range(2):
                        bank, bk = BK.f32(bs[g4]), BK.key(bs[g4])
                        kb.tt("dve", sT[:, 4 * g4:4 * g4 + 4, :], bank.rearrange("p (c n) -> p c n", c=4),
                              maskI.unsqueeze(1).to_broadcast([128, 4, 128]), ALU.mult, [bk, "cqf"], [ksT])
                        BK.put(bs[g4])
                        yield
                    (bo,) = yield from acquire(1)
                    bankO, ko = BK.f32(bo), BK.key(bo)
                    for h in range(8):
                        kb.mm(bankO[:, 64 * h:64 * h + 64], sT[:, h, :], vtok[:, tl, 64 * h:64 * h + 64], True, t == 0,
                              [ksT, f"vtok{par}.{tl}"], [ko])
                        if t > 0:
                            kb.mm(bankO[:, 64 * h:64 * h + 64], qT[:, h // 2, h % 2, tl_], Rbf[:, t % 5, h // 2, :], False, True,
                                  [f"qT{par}.{tl}", f"Rbf.{t % 5}"], [ko])
                    yield

                    def fin(ct, ya, k4):
                        kb.tt("dve", yT[:, ct, tok], ya, gateT[:, ct, tl_], ALU.mult, [k4, f"gateT{par}.{ct}"], [f"yT.{t}.0"])
                    yield from post_gen(slot, bo, 1e-5, 32, 36, fin)

                def gen_chunks(blk):
                    par = blk % 2
                    ktok, vtok = ktok_p[par], vtok_p[par]
                    for tl in range(4):
                        t = blk * 4 + tl
                        if t == NT - 1:
                            continue
                        (b,) = yield from acquire(1)
                        bankK, kk_ = BK.f32(b), BK.key(b)
                        for ct in range(4):
                            kb.mm(bankK[:, ct * 128:(ct + 1) * 128], ktok[:, tl, ct * 128:(ct + 1) * 128], vtok[:, tl, ct * 128:(ct + 1) * 128],
                                  True, True, [f"ktok{par}.{tl}", f"vtok{par}.{tl}"], [kk_])
                        yield
                        for hh in range(2):
                            b0 = 64 * hh
                            kb.tt("dve", R32[b0:b0 + 64, :, :], bankK[b0:b0 + 64, :].rearrange("p (c n) -> p c n", c=4)[:, :, b0:b0 + 64],
                                  R32[b0:b0 + 64, :, :], ALU.add, [kk_, "R32"], ["R32"])
                        BK.put(b)
                        yield
                        kb.tt("dve", R32[:], R32[:], cols[:, 99:103].unsqueeze(2).to_broadcast([128, 4, 64]), ALU.mult, ["R32", "cols"], ["R32"])
                        yield
                        kb.cp("act", Rbf[:, (t + 1) % 5, :, :], R32[:], ["R32"], [f"Rbf.{(t + 1) % 5}"])
                        yield
                    yield from rr([chunk_chain(0, blk, 0), chunk_chain(1, blk, 1)])
                    yield from rr([chunk_chain(0, blk, 2), chunk_chain(1, blk, 3)])

                run(gen_normA(0))
                interleave([gen_proj(0), gen_normA(1)])
                for blk in range(4):
                    if blk == 3 and "C" in stages:
                        Wk = Wv8(1824)
                        kb.dma("pool", Wk[:, :, :], wview(w_in_d)[:, :, 2048:3872], [], ["Wk", "W.0", "W.1", "W.2", "W.3"])
                    gs = [gen_chunks(blk)]
                    if blk + 1 < 4:
                        gs.append(gen_proj(blk + 1))
                    if blk + 2 < 4:
                        gs.append(gen_normA(blk + 2))
                    interleave(gs)
                if dbg:
                    kb.dma("sp", dbg_hT, hT[:], [f"hT.{b}" for b in range(4)], [])
                P.barrier()
                P.flush()

        if "C" in stages:
            stage_C(nc, P, kb, locals())
        else:
            P.op("dve", lambda e: e.memset(hT[:, 0:4, :], 0.0), [f"hT.{b}" for b in range(4)], [f"yT.{t}.1" for t in range(NT)])

        if dbg:
            kb.dma("sp", dbg_yT[:, 0:4, :], yT[:], [f"yT.{t}.0" for t in range(NT)], [])
            kb.dma("sp", dbg_yT[:, 4:8, :], hT[:, 0:4, :], [f"yT.{t}.1" for t in range(NT)], [])

        if "D" in stages:
            stage_D(nc, P, kb, locals())
        P.barrier()
        P.flush()
    return nc


def stage_C(nc, P, kb, L):
    cols, dc, cqf, hT, WA, mhalf, bonesb, BK = (L[k] for k in ("cols", "dc", "cqf", "hT", "WA", "mhalf", "bonesb", "L_BK"))
    wup_d, aup_d, gup_d, w_in_d = L["wup_d"], L["aup_d"], L["gup_d"], L["w_in_d"]
    mask4, maskLs, identf = L["mask4"], L["maskLs"], L["identf"]
    onesf = cqf[:, 896:1024]
    Wk = WA[:, 0:KC * 1824].rearrange("p (k n) -> p k n", k=KC)
    CL = 0.5 * math.exp(-0.5)
    NB = 256
    NBLK = T // NB
    with contextlib.ExitStack() as sc:
        if "B" not in L["stages"]:
            kb.dma("pool", Wk[:, :, :], w_in_d.rearrange("(kc p) n -> p kc n", p=128)[:, :, 2048:3872], [], ["Wk"])
        sb = lambda n, s, d: kb.sb(n, s, d, st=sc)
        wupz = sb("wupz", [128, 512], BF16)
        aupz = sb("aupz", [128, 512], BF16)
        gup1b = sb("gup1b", [128, 512], BF16)
        gup2z = sb("gup2z", [128, 512], BF16)
        for tz in (wupz, aupz, gup2z):
            P.op("dve", lambda e, tz=tz: e.memset(tz[:], 0.0), [], ["wupb", "gupb"])
        kb.dma("pool", wupz[0:64, :], wup_d, [], ["wupb"])
        kb.dma("pool", aupz[64:128, :], aup_d, [], ["wupb"])
        kb.dma("pool", gup1b[:], gup_d[0:128, :], [], ["gupb"])
        kb.dma("pool", gup2z[96:128, :], gup_d[128:160, :], [], ["gupb"])
        RKblk = sb("RKblk", [128, 4, 128], BF16)
        for ct in range(4):
            kb.ts("dve", RKblk[:, ct, :], cqf[:, 768:896], cols[:, 71 + ct:72 + ct], None, ALU.mult, None, ["cqf", "cols"], ["RKblk"])
        pT = sb("pT", [128, 15, NB + 1], F32)
        P.op("dve", lambda e: e.memset(pT[:], 0.0), [], [f"pT.{i}" for i in range(15)])
        carry = sb("carry", [128, 15], F32)
        NTMP = 10
        T_p = [[sb(f"T{c}_{i}", [128, NB], F32) for i in range(NTMP)] for c in range(2)]
        sh_p = [sb(f"shT{i}", [128, NB], F32) for i in range(2)]
        kk2_p = [sb(f"kk2{i}", [128, NB], BF16) for i in range(2)]
        twa = sb("twa", [128, NB], BF16)
        sg1 = sb("sg1", [128, NB], BF16)
        sg2 = sb("sg2", [128, NB], BF16)
        ARt = sb("ARt", [128, 4, 2, 2, 2, 128], BF16)
        P.op("dve", lambda e: e.memset(ARt[:], 0.0), [], [f"ARt.{i}" for i in range(4)])
        KT = sb("KT", [128, 4, NB], BF16)
        BnT = sb("BnT", [128, 4, NB], BF16)
        rkb = sb("rkb", [128, 4, NB], BF16)
        vbf_p = [sb(f"vbf{i}", [128, 4, NB], BF16) for i in range(2)]
        gT = sb("gT", [128, 4, NB], BF16)
        Ktok = sb("Ktok", [128, 2, 512], BF16)
        Btok = sb("Btok", [128, 2, 512], BF16)
        Vtok = sb("Vtok", [128, 2, 512], BF16)
        PCt = sb("PCt", [128, 4, 2], F32)
        MTs = [sb(f"MT{i}", [128, 8, 512], BF16) for i in range(2)]
        mraw_p = [sb(f"mraw{i}", [128, 512], BF16) for i in range(2)]
        mask4b = sb("mask4b", [128, 512], BF16)
        kb.dma("pool", mask4b[:], L["cq_d"][:, 128:640], [], ["mask4b"])
        Xm = [sb(f"Xm{i}", [128, 8, 128], BF16) for i in range(2)]
        Nm = [sb(f"Nm{i}", [128, 8, 128], BF16) for i in range(2)]
        Lm = [sb(f"Lm{i}", [128, 8, 128], BF16) for i in range(2)]
        TTs = [sb(f"TT{i}", [128, 8, 128], BF16) for i in range(2)]
        RHSs = [sb(f"RHSb{i}", [128, 512], BF16) for i in range(2)]
        Ubs = [sb(f"Ub{i}", [128, 512], BF16) for i in range(2)]
        S32 = sb("S32", [128, 4, 64], F32)
        Sbf = sb("Sbf", [128, 4, 64], BF16)
        P.op("dve", lambda e: e.memset(S32[:], 0.0), [], ["S32"])
        y32 = sb("cy32", [128, 512], F32)
        sq = sb("csq", [128, 512], F32)
        ynb = sb("cynb", [128, 512], BF16)
        yaff = sb("cyaff", [128, 4, 128], F32)
        t1b = sb("ct1", [128, 4, 128], F32)
        stt_ = sb("cstat", [128, 64], F32)

        def gen_AB(blk):
            tok0 = blk * NB
            hk = f"hT.{tok0 // 512}"
            for i0 in range(0, 15, 2):
                b = BK.get()
                bank, bk = BK.f32(b), BK.key(b)
                idxs = [i for i in (i0, i0 + 1) if i < 15]
                for j, idx in enumerate(idxs):
                    c0 = idx * 128 if idx < 14 else 1696
                    for kc in range(KC):
                        kb.mm(bank[:, j * NB:(j + 1) * NB], Wk[:, kc, c0:c0 + 128], hT[:, kc, tok0:tok0 + NB],
                              kc == 0, kc == KC - 1, [hk, "Wk"] + (["hTu"] if kc >= 4 else []), [bk])
                yield
                for j, idx in enumerate(idxs):
                    kb.cp("act", pT[:, idx, 1:NB + 1], bank[:, j * NB:(j + 1) * NB], [bk], [f"pT.{idx}"])
                BK.put(b)
                yield
            allp = [f"pT.{i}" for i in range(15)]
            kb.cp("dve", carry[:], pT[:, :, NB], allp, ["carry"])
            yield
            for idx in range(15):
                tmp, ktm = sh_p[idx % 2], f"shT.{idx % 2}"
                kb.act(tmp[:], pT[:, idx, 0:NB], AF.Copy, [f"pT.{idx}", "cols"], [ktm], scale=cols[:, 40 + idx:41 + idx])
                yield
                if 8 <= idx < 12:
                    kb.stt(vbf_p[blk % 2][:, idx - 8, :], pT[:, idx, 1:NB + 1], dc[:, idx:idx + 1], tmp[:], ALU.mult, ALU.add,
                           [f"pT.{idx}", "dc", ktm, "carry"], [f"vbf{blk % 2}.{idx - 8}"])
                else:
                    kb.stt(pT[:, idx, 1:NB + 1], pT[:, idx, 1:NB + 1], dc[:, idx:idx + 1], tmp[:], ALU.mult, ALU.add,
                           [f"pT.{idx}", "dc", ktm, "carry"], [f"pT.{idx}"])
                yield
            kb.cp("dve", pT[:, :, 0], carry[:], ["carry"], allp)
            yield

        ps = lambda idx: pT[:, idx, 1:NB + 1]

        def gen_lora(blk):
            kb.act(twa[0:64, :], pT[0:64, 12, 1:NB + 1], AF.Tanh, ["pT.12"], ["twa"])
            kb.cp("dve", twa[64:128, :], pT[64:128, 12, 1:NB + 1], ["pT.12"], ["twa"])
            yield
            th, kth = sh_p[0], "shT.0"
            kb.act(th[:], ps(13), AF.Tanh, ["pT.13"], [kth], scale=0.5)
            th2, kth2 = sh_p[1], "shT.1"
            kb.act(th2[:], ps(14), AF.Tanh, ["pT.14"], [kth2], scale=0.5)
            yield
            kb.ts("dve", sg1[:], th[:], 0.5, 0.5, ALU.mult, ALU.add, [kth], ["sg1"])
            kb.ts("pool", sg2[:], th2[:], 0.5, 0.5, ALU.mult, ALU.add, [kth2], ["sg2"])
            yield

        def gen_ct(ch, blk, ct):
            Tb = T_p[ch]
            tk = lambda i: f"T{ch}.{i}"
            cs = slice(ct * 128, (ct + 1) * 128)
            r_, k_, v_ = ps(ct), ps(4 + ct), ps(8 + ct)
            kr, kk_, kv = f"pT.{ct}", f"pT.{4 + ct}", f"pT.{8 + ct}"
            bz, bg = BK.get(), BK.get()
            bankZ, kz = BK.f32(bz), BK.key(bz)
            bankG, kg = BK.f32(bg), BK.key(bg)
            kb.mm(bankZ[:, 0:NB], wupz[:, cs], twa[:], True, True, ["wupb", "twa"], [kz])
            kb.mm(bankZ[:, NB:2 * NB], aupz[:, cs], twa[:], True, True, ["wupb", "twa"], [kz])
            kb.mm(bankG[:, 0:NB], gup1b[:, cs], sg1[:], True, False, ["gupb", "sg1"], [kg])
            kb.mm(bankG[:, 0:NB], gup2z[:, cs], sg2[:], False, True, ["gupb", "sg2"], [kg])
            yield
            thw, tha = Tb[0], Tb[1]
            kb.act(thw[:], bankZ[:, 0:NB], AF.Tanh, [kz, "dc"], [tk(0)], scale=0.5, bias=dc[:, 15 + ct:16 + ct])
            kb.act(tha[:], bankZ[:, NB:2 * NB], AF.Tanh, [kz, "dc"], [tk(1)], scale=0.5, bias=dc[:, 19 + ct:20 + ct])
            BK.put(bz)
            kk = Tb[4]
            kb.ts("pool", kk[:], k_, cols[:, 63 + ct:64 + ct], 0.0, ALU.mult, ALU.add, [kk_, "cols"], [tk(4)])
            yield
            kb.cp("act", gT[:, ct, :], bankG[:, 0:NB], [kg], [f"gT.{ct}"])
            BK.put(bg)
            kk2, k_kk2 = kk2_p[ch], f"kk2.{ch}"
            kb.act(kk2[:], kk[:], AF.Square, [tk(4)], [k_kk2])
            ld = Tb[0]
            kb.ts("dve", ld[:], thw[:], -CL, -CL, ALU.mult, ALU.add, [tk(0)], [tk(0)])
            yield
            bn_ = BK.get()
            bankN, kn = BK.f32(bn_), BK.key(bn_)
            kb.mm(bankN[:, 0:NB], bonesb[:], kk2[:], True, True, ["bonesb", k_kk2], [kn])
            cum = Tb[2]
            for c in range(2):
                ld_c, cum_c = ld[:, c * 128:(c + 1) * 128], cum[:, c * 128:(c + 1) * 128]
                P.op("dve", lambda e, cum_c=cum_c, ld_c=ld_c: e.tensor_tensor_scan(out=cum_c, data0=onesf, data1=ld_c, initial=0.0, op0=ALU.mult, op1=ALU.add),
                     [tk(0), "cqf"], [tk(2)])
            an = Tb[1]
            kb.ts("pool", an[:], tha[:], -0.5, -0.5, ALU.mult, ALU.add, [tk(1)], [tk(1)])
            yield
            sn = Tb[5]
            kb.act(sn[:], bankN[:, 0:NB], AF.Sqrt, [kn], [tk(5)])
            BK.put(bn_)
            cumex = Tb[3]
            kb.tt("pool", cumex[:], cum[:], ld[:], ALU.subtract, [tk(2), tk(0)], [tk(3)])
            yield
            ep, en, ex = Tb[6], Tb[7], Tb[8]
            kb.act(ep[:], cum[:], AF.Exp, [tk(2)], [tk(6)])
            kb.act(en[:], cum[:], AF.Exp, [tk(2)], [tk(7)], scale=-1.0)
            kb.ts("dve", sn[:], sn[:], 1e-12, None, ALU.max, None, [tk(5)], [tk(5)])
            tmp2 = Tb[9]
            kb.ts("pool", tmp2[:], an[:], dc[:, 23 + ct:24 + ct], dc[:, 27 + ct:28 + ct], ALU.mult, ALU.add, [tk(1), "dc"], [tk(9)])
            yield
            kb.act(ex[:], cumex[:], AF.Exp, [tk(3)], [tk(8)])
            P.op("dve", lambda e: e.reciprocal(out=sn[:], in_=sn[:]), [tk(5)], [tk(5)])
            k2 = Tb[9]
            kb.tt("pool", k2[:], k_, tmp2[:], ALU.mult, [kk_, tk(9)], [tk(9)])
            yield
            kkn = Tb[4]
            kb.tt("dve", kkn[:], kk[:], sn[:], ALU.mult, [tk(4), tk(5)], [tk(4)])
            c3 = lambda a: a.rearrange("p (c n) -> p c n", c=2)
            kb.cp("pool", PCt[:, ct, :], c3(ep[:])[:, :, 127], [tk(6)], [f"PCt.{ct}"])
            yield
            bn = Tb[5]
            kb.tt("pool", bn[:], kkn[:], an[:], ALU.mult, [tk(4), tk(1), tk(5)], [tk(5)])
            kb.tt("dve", KT[:, ct, :], k2[:], en[:], ALU.mult, [tk(9), tk(7)], [f"KT.{ct}"])
            yield
            for hh in range(2):
                pp = slice(64 * hh, 64 * hh + 64)
                kb.tt("dve", ARt[pp, ct, :, hh, 1, :], c3(r_)[pp], c3(ep[:])[pp], ALU.mult, [kr, tk(6)], [f"ARt.{ct}"])
                kb.tt("dve" if hh == 0 else "pool", ARt[pp, ct, :, hh, 0, :], c3(kkn[:])[pp], c3(ex[:])[pp], ALU.mult, [tk(4), tk(8)], [f"ARt.{ct}"])
                yield
            kb.tt("dve", BnT[:, ct, :], bn[:], en[:], ALU.mult, [tk(5), tk(7)], [f"BnT.{ct}"])
            kb.tt("pool", rkb[:, ct, :], r_, k2[:], ALU.mult, [kr, tk(9)], [f"rkb.{ct}"])
            yield

        def gen_tok(blk):
            for c in range(2):
                cs = slice(c * 128, (c + 1) * 128)
                for src, dst, nm, sk in ((KT, Ktok, "KT", "KT"), (BnT, Btok, "BnT", "BnT"), (vbf_p[blk % 2], Vtok, "vbf", f"vbf{blk % 2}")):
                    b = BK.get()
                    bankT, kt = BK.bf(b), BK.key(b)
                    for ct in range(4):
                        kb.tr(bankT[:, ct * 128:(ct + 1) * 128], src[:, ct, cs], [f"{sk}.{ct}"], [kt])
                    yield
                    kb.cpalt(dst[:, c, :], bankT[:, 0:512], [kt], [f"{nm}tok.{c}"])
                    BK.put(b)
                    yield

        def gen_F1(blk, c):
            cs = slice(c * 128, (c + 1) * 128)
            MT = MTs[c]
            for h in range(8):
                ct = h // 2
                b = BK.get()
                bankM, km = BK.f32(b), BK.key(b)
                rhsAR = ARt[:, ct, c, h % 2, :, :].rearrange("p a b -> p (a b)")
                kb.mm(bankM[:, 0:256], BnT[:, ct, cs], rhsAR, True, True, [f"BnT.{ct}", f"ARt.{ct}"], [km])
                kb.mm(bankM[:, 256:512], KT[:, ct, cs], rhsAR, True, True, [f"KT.{ct}", f"ARt.{ct}"], [km])
                yield
                if h % 2 == 0:
                    kb.tt("dve", MT[:, h, :], bankM, mask4, ALU.mult, [km, "cqf"], [f"MT{c}.{h}"])
                    BK.put(b)
                else:
                    mr, kmr = mraw_p[(h // 2) % 2], f"mraw.{(h // 2) % 2}"
                    kb.cp("act", mr[:], bankM, [km], [kmr])
                    BK.put(b)
                    yield
                    kb.tt("pool", MT[:, h, :], mr[:], mask4b[:], ALU.mult, [kmr, "mask4b"], [f"MT{c}.{h}"])
                yield
            for g4 in range(2):
                b = BK.get()
                bankL, kl = BK.f32(b), BK.key(b)
                for hh in range(4):
                    h = 4 * g4 + hh
                    kb.mm(bankL[:, hh * 128:(hh + 1) * 128], ARt[:, h // 2, c, h % 2, 0, :], BnT[:, h // 2, cs], True, True,
                          [f"ARt.{h // 2}", f"BnT.{h // 2}"], [kl])
                yield
                kb.tt("dve", Lm[0][:, 4 * g4:4 * g4 + 4, :], bankL.rearrange("p (c n) -> p c n", c=4),
                      maskLs.unsqueeze(1).to_broadcast([128, 4, 128]), ALU.mult, [kl, "cqf"], [f"Lm0.{g4}"])
                BK.put(b)
                hs = slice(4 * g4, 4 * g4 + 4)
                mtk = [f"MT{c}.{h}" for h in range(4 * g4, 4 * g4 + 4)]
                kb.cp("act", Nm[0][:, hs, :], MT[:, hs, 0:128], mtk, [f"Nm0.{g4}"])
                kb.tt("pool", Xm[0][:, hs, :], MT[:, hs, 0:128], identf.unsqueeze(1).to_broadcast([128, 4, 128]), ALU.add,
                      mtk + ["cqf"], [f"Xm0.{g4}"])
                yield

        def gen_inv_group(c, g4):
            hs = slice(4 * g4, 4 * g4 + 4)
            v4 = lambda bank: bank.rearrange("p (c n) -> p c n", c=4)
            cur = 0
            for lvl in range(7):
                nxt = cur ^ 1
                kN, kL, kX = f"Nm{cur}.{g4}", f"Lm{cur}.{g4}", f"Xm{cur}.{g4}"
                last = lvl == 6
                Xdst, kXd = (TTs[c], f"TT{c}.{g4}") if last else (Xm[nxt], f"Xm{nxt}.{g4}")
                bx = ba = bb = None
                if lvl >= 1:
                    bx = BK.get()
                    bankX, kbx = BK.f32(bx), BK.key(bx)
                    for hh in range(4):
                        h = 4 * g4 + hh
                        kb.mm(bankX[:, hh * 128:(hh + 1) * 128], Lm[cur][:, h, :], Xm[cur][:, h, :], True, True, [kL, kX], [kbx])
                if not last:
                    ba, bb = BK.get(), BK.get()
                    bankA, kba = BK.f32(ba), BK.key(ba)
                    bankB, kbb = BK.f32(bb), BK.key(bb)
                    for hh in range(4):
                        h = 4 * g4 + hh
                        kb.mm(bankA[:, hh * 128:(hh + 1) * 128], Lm[cur][:, h, :], Nm[cur][:, h, :], True, True, [kL, kN], [kba])
                    for hh in range(4):
                        h = 4 * g4 + hh
                        kb.mm(bankB[:, hh * 128:(hh + 1) * 128], Nm[cur][:, h, :], Lm[cur][:, h, :], True, True, [kL, kN], [kbb])
                yield
                if lvl >= 1:
                    kb.tt("dve", Xdst[:, hs, :], v4(bankX), Xm[cur][:, hs, :], ALU.add, [kbx, kX], [kXd])
                    BK.put(bx)
                else:
                    kb.cp("pool", Xdst[:, hs, :], Xm[cur][:, hs, :], [kX], [kXd])
                if not last:
                    kb.cp("act", Nm[nxt][:, hs, :], v4(bankA), [kba], [f"Nm{nxt}.{g4}"])
                    BK.put(ba)
                    if g4 == 0:
                        kb.cp("act", Lm[nxt][:, hs, :], v4(bankB), [kbb], [f"Lm{nxt}.{g4}"])
                    else:
                        kb.cp("dve", Lm[nxt][:, hs, :], v4(bankB), [kbb], [f"Lm{nxt}.{g4}"])
                    BK.put(bb)
                yield
                cur = nxt

        def gen_inv(blk, c):
            gs = [gen_inv_group(c, 0), gen_inv_group(c, 1)]
            while gs:
                for g in list(gs):
                    try:
                        next(g)
                    except StopIteration:
                        gs.remove(g)
                yield

        def gen_seqpost(blk, c):
            n_ch = blk * 2 + c
            cs = slice(c * 128, (c + 1) * 128)
            tok = slice(blk * NB + c * 128, blk * NB + (c + 1) * 128)
            MT, TT, RHSb, Ub = MTs[c], TTs[c], RHSs[c], Ubs[c]
            kTT = [f"TT{c}.0", f"TT{c}.1"]
            kRH, kUb = f"RHSb{c}", f"Ub{c}"
            br = BK.get()
            bankR, kr_ = BK.f32(br), BK.key(br)
            for h in range(8):
                ct = h // 2
                o = bankR[:, 64 * h:64 * h + 64]
                if n_ch > 0:
                    kb.mm(o, ARt[:, ct, c, h % 2, 0, :], Sbf[:, ct, :], True, False, [f"ARt.{ct}", "Sbf"], [kr_])
                kb.mm(o, MT[:, h, 256:384], Vtok[:, c, 64 * h:64 * h + 64], n_ch == 0, True, [f"MT{c}.{h}", f"vbftok.{c}"], [kr_])
            yield
            kb.cp("act", RHSb[:], bankR, [kr_], [kRH])
            BK.put(br)
            yield
            bu = BK.get()
            bankU, ku = BK.f32(bu), BK.key(bu)
            for h in range(8):
                kb.mm(bankU[:, 64 * h:64 * h + 64], TT[:, h, :], RHSb[:, 64 * h:64 * h + 64], True, True, kTT + [kRH], [ku])
            yield
            kb.cp("dve", Ub[:], bankU, [ku], [kUb])
            BK.put(bu)
            yield
            if n_ch < NT - 1:
                bs_ = BK.get()
                bankS, ks_ = BK.f32(bs_), BK.key(bs_)
                for ct in range(4):
                    o = bankS[:, ct * 128:(ct + 1) * 128]
                    kb.mm(o, Btok[:, c, ct * 128:(ct + 1) * 128], Ub[:, ct * 128:(ct + 1) * 128], True, False, [f"BnTtok.{c}", kUb], [ks_])
                    kb.mm(o, Ktok[:, c, ct * 128:(ct + 1) * 128], Vtok[:, c, ct * 128:(ct + 1) * 128], False, True, [f"KTtok.{c}", f"vbftok.{c}"], [ks_])
            by = BK.get()
            bankY, ky = BK.f32(by), BK.key(by)
            for h in range(8):
                ct = h // 2
                o = bankY[:, 64 * h:64 * h + 64]
                if n_ch > 0:
                    kb.mm(o, ARt[:, ct, c, h % 2, 1, :], Sbf[:, ct, :], True, False, [f"ARt.{ct}", "Sbf"], [ky])
                kb.mm(o, MT[:, h, 128:256], Ub[:, 64 * h:64 * h + 64], n_ch == 0, False, [f"MT{c}.{h}", kUb], [ky])
                kb.mm(o, MT[:, h, 384:512], Vtok[:, c, 64 * h:64 * h + 64], False, True, [f"MT{c}.{h}", f"vbftok.{c}"], [ky])
            yield
            if n_ch < NT - 1:
                for hh in range(2):
                    b0 = 64 * hh
                    kb.tt("dve", S32[b0:b0 + 64, :, :], bankS[b0:b0 + 64, :].rearrange("p (c n) -> p c n", c=4)[:, :, b0:b0 + 64],
                          S32[b0:b0 + 64, :, :], ALU.add, [ks_, "S32"], ["S32"])
                BK.put(bs_)
                kb.tt("dve", S32[:], S32[:], PCt[:, :, c].unsqueeze(2).to_broadcast([128, 4, 64]), ALU.mult,
                      ["S32"] + [f"PCt.{ct}" for ct in range(4)], ["S32"])
                kb.cp("act", Sbf[:], S32[:], ["S32"], ["Sbf"])
                yield
            k1, k2, ks, k3 = "cy32", "csq", "cstat", "cynb"
            kb.cp("act", y32[:], bankY, [ky], [k1])
            BK.put(by)
            yield
            v3 = lambda a: a.rearrange("p (h d) -> p h d", d=64)
            bc = lambda a: a.unsqueeze(2).to_broadcast([128, 8, 64])
            kb.red(stt_[:, 0:8], v3(y32[:]), ALU.add, [k1], [ks])
            kb.act(sq[:], y32[:], AF.Square, [k1], [k2])
            yield
            kb.red(stt_[:, 8:16], v3(sq[:]), ALU.add, [k2], [ks])
            yield
            kb.ts("dve", stt_[:, 16:24], stt_[:, 0:8], 1.0 / 64, None, ALU.mult, None, [ks], [ks])
            yield
            kb.tt("dve", stt_[:, 24:32], stt_[:, 16:24], stt_[:, 16:24], ALU.mult, [ks], [ks])
            kb.ts("dve", stt_[:, 32:40], stt_[:, 8:16], 1.0 / 64, 64e-5, ALU.mult, ALU.add, [ks], [ks])
            yield
            kb.tt("dve", stt_[:, 40:48], stt_[:, 32:40], stt_[:, 24:32], ALU.subtract, [ks], [ks])
            yield
            kb.tt("pool", stt_[:, 48:56], stt_[:, 40:48], mhalf[:, 0:8], ALU.pow, [ks, "mhalf"], [ks])
            yield
            kb.stt(stt_[:, 56:64], stt_[:, 16:24], -1.0, stt_[:, 48:56], ALU.mult, ALU.mult, [ks], [ks])
            kb.tt("pool", v3(y32[:]), v3(y32[:]), bc(stt_[:, 48:56]), ALU.mult, [k1, ks], [k1])
            yield
            kb.tt("dve", v3(ynb[:]), v3(y32[:]), bc(stt_[:, 56:64]), ALU.add, [k1, ks], [k3])
            yield
            b2 = BK.get()
            bankT, kt = BK.bf(b2), BK.key(b2)
            for ct in range(4):
                kb.tr(bankT[:, ct * 128:(ct + 1) * 128], ynb[:, ct * 128:(ct + 1) * 128], [k3], [kt])
            bbo = BK.get()
            bankBo, kbo = BK.f32(bbo), BK.key(bbo)
            for ct in range(4):
                kb.mm(bankBo[:, ct * 128:(ct + 1) * 128], RKblk[:, ct, :], rkb[:, ct, cs], True, True, ["RKblk", f"rkb.{ct}"], [kbo])
            yield
            for ct in range(4):
                kb.act(yaff[:, ct, :], bankT[:, ct * 128:(ct + 1) * 128], AF.Identity, [kt, "cols"], ["cyaff"],
                       scale=cols[:, 75 + ct:76 + ct], bias=cols[:, 79 + ct:80 + ct])
            kb.tt("dve", t1b[:], bankBo.rearrange("p (c n) -> p c n", c=4), vbf_p[blk % 2][:, :, cs], ALU.mult,
                  [kbo] + [f"vbf{blk % 2}.{ct}" for ct in range(4)], ["ct1"])
            BK.put(b2)
            BK.put(bbo)
            yield
            kb.tt("pool", t1b[:], t1b[:], yaff[:], ALU.add, ["ct1", "cyaff"], ["ct1"])
            yield
            kb.tt("dve", hT[:, 0:4, tok], t1b[:], gT[:, :, cs], ALU.mult, ["ct1"] + [f"gT.{ct}" for ct in range(4)],
                  [f"yT.{n_ch}.1", f"hT.{n_ch // 4}"])
            yield

        def gen_CD(blk):
            yield from gen_lora(blk)
            for pair in range(2):
                gs = [gen_ct(0, blk, 2 * pair), gen_ct(1, blk, 2 * pair + 1)]
                while gs:
                    for g in list(gs):
                        try:
                            next(g)
                        except StopIteration:
                            gs.remove(g)
                    yield
            yield from gen_tok(blk)

        def run(g):
            for _ in g:
                pass

        run(gen_AB(0))
        run(gen_CD(0))
        for blk in range(NBLK):
            run(gen_F1(blk, 0))
            interleave([gen_inv(blk, 0), gen_F1(blk, 1)])
            interleave([gen_seqpost(blk, 0), gen_inv(blk, 1)])
            if blk + 1 < NBLK:
                interleave([gen_seqpost(blk, 1), gen_AB(blk + 1)])
                if blk + 1 == NBLK - 1 and "D" in L["stages"]:
                    wv = lambda w: w.rearrange("(kc p) n -> p kc n", p=128)
                    Wout = hT[:, 4:8, :].rearrange("p a (b n) -> p (a b) n", n=1024)
                    kb.dma("pool", Wout, wv(L["w_out_d"]), [], ["Wout", "hTu"])
                    Wkv = WA[:, 0:16384].rearrange("p (k n) -> p k n", k=KC)
                    for hf in range(2):
                        kb.dma("pool", Wkv[:, :, hf * 1024:(hf + 1) * 1024], wv(L["wkv_d"])[:, :, hf * 1024:(hf + 1) * 1024], [], ["WA.0", "WA.1", "Wk"])
                run(gen_CD(blk + 1))
            else:
                run(gen_seqpost(blk, 1))
        P.barrier()
        P.flush()


def stage_D(nc, P, kb, L):
    cols, cqf, hT, yT, WA, mhalf, onesb, ss, BK = (L[k] for k in ("cols", "cqf", "hT", "yT", "WA", "mhalf", "onesb", "ss", "L_BK"))
    x_d, mem_d, gfin_d, out_d = L["x_d"], L["mem_d"], L["gfin_d"], L["out_d"]
    w_out_d, wq_d, wkv_d, wo_d, wupm_d, wdn_d = (L[k] for k in ("w_out_d", "wq_d", "wkv_d", "wo_d", "wupm_d", "wdn_d"))
    wview = lambda w: w.rearrange("(kc p) n -> p kc n", p=128)

    def run(g):
        for _ in g:
            pass

    with contextlib.ExitStack() as sd:
        sb = lambda n, s, d, st=sd: kb.sb(n, s, d, st=st)
        xres = sb("xres", [128, NT, D], F32)
        gfin = sb("gfin", [128, D], F32)
        kTm = sb("kTm", [128, KC, 256], BF16)
        vmem = sb("vmem", [128, 2, D], BF16)
        kmx = sb("kmx", [128, 4], F32)
        hb_p = [sb(f"dhb{i}", [128, D], BF16) for i in range(4)]
        sqj = sb("dsqj", [128, D], BF16)
        Wkv = WA[:, 0:16384].rearrange("p (k n) -> p k n", k=KC)
        W0 = WA[:, 0:8192].rearrange("p (k n) -> p k n", k=KC)
        W1 = WA[:, 8192:16384].rearrange("p (k n) -> p k n", k=KC)
        Wout = hT[:, 4:8, :].rearrange("p a (b n) -> p (a b) n", n=1024)
        xv = x_d.rearrange("(t p) d -> p t d", p=128)
        kb.dma("sp", xres[:, 0:4, :], xv[:, 0:4, :], [], [f"xres.{t}" for t in range(0, 4)])
        if "C" not in L["stages"]:
            kb.dma("pool", Wout, wview(w_out_d), [], ["Wout"])
            for hf in range(2):
                kb.dma("pool", Wkv[:, :, hf * 1024:(hf + 1) * 1024], wview(wkv_d)[:, :, hf * 1024:(hf + 1) * 1024], [], ["WA.0", "WA.1"])
        kb.dma("sp", gfin[:], gfin_d.partition_broadcast(128), [], ["gfin"])

        def gen_norm(tiles, src, gcol0, dst_fn, dkey, sscol0):
            for tl, t in enumerate(tiles):
                xa, xk = src(t)
                c = sscol0 + t
                kb.act(sqj[:], xa, AF.Square, [xk], ["sqj", f"ss{c}"], accum=ss[:, c:c + 1])
                yield
                kb.ts("dve", ss[:, c:c + 1], ss[:, c:c + 1], 1.0 / D, 1e-6, ALU.mult, ALU.add, [f"ss{c}"], [f"ss{c}"])
                yield
                kb.tt("pool", ss[:, c:c + 1], ss[:, c:c + 1], mhalf[:, 0:1], ALU.pow, [f"ss{c}", "mhalf"], [f"ss{c}"])
                yield
                kb.act(hb_p[tl][:], xa, AF.Copy, [xk, f"ss{c}"], [f"dhb.{tl}"], scale=ss[:, c:c + 1])
                yield
            n = len(tiles) * 128
            for kc in range(KC):
                b = BK.get()
                bankT, bk = BK.bf(b), BK.key(b)
                for tl in range(len(tiles)):
                    kb.tr(bankT[:, tl * 128:(tl + 1) * 128], hb_p[tl][:, kc * 128:(kc + 1) * 128], [f"dhb.{tl}"], [bk])
                yield
                o = dst_fn(kc)
                if kc % 2:
                    kb.act(o, bankT[:, 0:n], AF.Copy, [bk, "cols"], [dkey], scale=cols[:, gcol0 + kc:gcol0 + kc + 1])
                else:
                    kb.ts("dve", o, bankT[:, 0:n], cols[:, gcol0 + kc:gcol0 + kc + 1], None, ALU.mult, None, [bk, "cols"], [dkey])
                BK.put(b)
                yield

        with contextlib.ExitStack() as s0:
            memx = [sb(f"memx{i}", [128, D], F32, st=s0) for i in range(2)]
            memT = sb("memT", [128, KC, 256], BF16, st=s0)
            k2m = sb("k2m", [128, KC, 256], BF16, st=s0)
            for i in range(2):
                kb.dma("sp", memx[i][:], mem_d[i * 128:(i + 1) * 128, :], [], [f"memx.{i}"])
            for i in range(1, 4):
                kb.dma("sp", xres[:, 4 * i:4 * i + 4, :], xv[:, 4 * i:4 * i + 4, :], [], [f"xres.{t}" for t in range(4 * i, 4 * i + 4)])

            def gen_memkv():
                yield from gen_norm([0, 1], lambda t: (memx[t][:], f"memx.{t}"), 16, lambda kc: memT[:, kc, :], "memT", 16)
                for ft in range(KC):
                    b = BK.get()
                    bank, bk = BK.f32(b), BK.key(b)
                    for kc in range(KC):
                        kb.mm(bank[:, 0:256], Wkv[:, kc, ft * 128:(ft + 1) * 128], memT[:, kc, :], kc == 0, kc == KC - 1, ["WA.0", "memT"], [bk])
                    yield
                    kb.cpalt(kTm[:, ft, :], bank[:, 0:256], [bk], ["kTm"])
                    BK.put(b)
                    yield
                for mt in range(2):
                    for hf in range(2):
                        b = BK.get()
                        bank, bk = BK.f32(b), BK.key(b)
                        for kc in range(KC):
                            kb.mm(bank, memT[:, kc, mt * 128:(mt + 1) * 128], Wkv[:, kc, 1024 + hf * 512:1024 + (hf + 1) * 512],
                                  kc == 0, kc == KC - 1, ["WA.1", "memT"], [bk])
                        yield
                        kb.cpalt(vmem[:, mt, hf * 512:(hf + 1) * 512], bank, [bk], ["vmem"])
                        BK.put(b)
                        yield
                kb.act(k2m[:], kTm[:], AF.Square, ["kTm"], ["k2m"])
                yield
                for h in range(4):
                    b = BK.get()
                    bank, bk = BK.f32(b), BK.key(b)
                    kb.mm(bank[:, 0:256], onesb[:], k2m[:, 2 * h, :], True, False, ["onesb", "k2m"], [bk])
                    kb.mm(bank[:, 0:256], onesb[:], k2m[:, 2 * h + 1, :], False, True, ["onesb", "k2m"], [bk])
                    yield
                    kb.red(kmx[:, h:h + 1], bank[:, 0:256], ALU.max, [bk], ["kmx"])
                    BK.put(b)
                    yield

            def gen_wout():
                for t in range(NT):
                    tok = slice(t * 128, (t + 1) * 128)
                    for hf in range(2):
                        b = BK.get()
                        bank, bk = BK.f32(b), BK.key(b)
                        for kc in range(KC):
                            ysrc = yT[:, kc, tok] if kc < 4 else hT[:, kc - 4, tok]
                            kb.mm(bank, ysrc, Wout[:, kc, hf * 512:(hf + 1) * 512], kc == 0, kc == KC - 1,
                                  [f"yT.{t}.0", f"yT.{t}.1", "Wout"], [bk])
                        yield
                        xs = xres[:, t, hf * 512:(hf + 1) * 512]
                        kb.tt("dve", xs, bank, xs, ALU.add, [bk, f"xres.{t}"], [f"xres.{t}"])
                        BK.put(b)
                        yield

            interleave([gen_memkv(), gen_wout()])
            kb.dma("pool", W1, wview(wq_d), [], ["WA.0", "WA.1"])
            kb.dma("pool", W0, wview(wo_d), [], ["WA.0", "WA.1"])
            P.barrier()
            P.flush()

        srcx = lambda t: (xres[:, t, :], f"xres.{t}")
        norm_x = lambda blk: gen_norm(list(range(4 * blk, 4 * blk + 4)), srcx, 8, lambda kc: hT[:, kc, blk * 512:(blk + 1) * 512], f"hT.{blk}", 0)
        norm_m = lambda blk: gen_norm(list(range(4 * blk, 4 * blk + 4)), srcx, 24, lambda kc: hT[:, kc, blk * 512:(blk + 1) * 512], f"hT.{blk}", 16)
        with contextlib.ExitStack() as s2:
            qTb = yT[:, 0:2, :].rearrange("p a (b n) -> p (a b) n", n=512)
            PT = yT[:, 2:4, :].rearrange("p a (b n) -> p (a b) n", n=512)
            oTb_t = sb("oTb", [128, KC * 512], BF16, st=s2)
            oTb = oTb_t[:].rearrange("p (k n) -> p k n", k=KC)
            q2b_p = [sb(f"q2b{i}", [128, 2, 512], BF16, st=s2) for i in range(2)]
            Bs_p = [sb(f"Bs{i}", [128, 512], F32, st=s2) for i in range(2)]
            sh_p = [sb(f"shd{i}", [128, 512], F32, st=s2) for i in range(2)]
            rc_p = [sb(f"rc{i}", [128, 512], F32, st=s2) for i in range(2)]

            def gen_head(sl, h):
                qk = [f"qTb.{2 * h}", f"qTb.{2 * h + 1}"]
                q2b, kq2 = q2b_p[sl], f"q2b.{sl}"
                kb.act(q2b[:], qTb[:, 2 * h:2 * h + 2, :], AF.Square, qk, [kq2])
                yield
                b = BK.get()
                bankQ, kq = BK.f32(b), BK.key(b)
                kb.mm(bankQ, onesb[:], q2b[:, 0, :], True, False, ["onesb", kq2], [kq])
                kb.mm(bankQ, onesb[:], q2b[:, 1, :], False, True, ["onesb", kq2], [kq])
                yield
                Bs, kbs = Bs_p[sl], f"Bs.{sl}"
                kb.ts("dve", Bs[:], bankQ, kmx[:, h:h + 1], 1.0 / 32, ALU.add, ALU.mult, [kq, "kmx"], [kbs])
                BK.put(b)
                yield
                for mt in range(2):
                    b = BK.get()
                    bankS, ksb = BK.f32(b), BK.key(b)
                    ms = slice(mt * 128, (mt + 1) * 128)
                    kb.mm(bankS, kTm[:, 2 * h, ms], qTb[:, 2 * h, :], True, False, ["kTm", qk[0]], [ksb])
                    kb.mm(bankS, kTm[:, 2 * h + 1, ms], qTb[:, 2 * h + 1, :], False, True, ["kTm", qk[1]], [ksb])
                    yield
                    sh, ksh = sh_p[sl], f"shd.{sl}"
                    kb.stt(sh[:], bankS, 1.0 / 16, Bs[:], ALU.mult, ALU.subtract, [ksb, kbs], [ksh])
                    BK.put(b)
                    yield
                    kb.act(PT[:, 2 * h + mt, :], sh[:], AF.Exp, [ksh], [f"PT.{2 * h + mt}"])
                    yield
                pk = [f"PT.{2 * h}", f"PT.{2 * h + 1}"]
                b = BK.get()
                bankRs, krs = BK.f32(b), BK.key(b)
                kb.mm(bankRs, onesb[:], PT[:, 2 * h, :], True, False, ["onesb", pk[0]], [krs])
                kb.mm(bankRs, onesb[:], PT[:, 2 * h + 1, :], False, True, ["onesb", pk[1]], [krs])
                yield
                rc, krc = rc_p[sl], f"rc.{sl}"
                P.op("dve", lambda e: e.reciprocal(out=rc[:], in_=bankRs), [krs], [krc])
                BK.put(b)
                yield
                for dt_ in range(2):
                    ft = 2 * h + dt_
                    b = BK.get()
                    bankO, kob = BK.f32(b), BK.key(b)
                    kb.mm(bankO, vmem[:, 0, ft * 128:(ft + 1) * 128], PT[:, 2 * h, :], True, False, ["vmem", pk[0]], [kob])
                    kb.mm(bankO, vmem[:, 1, ft * 128:(ft + 1) * 128], PT[:, 2 * h + 1, :], False, True, ["vmem", pk[1]], [kob])
                    yield
                    kb.tt("dve", oTb[:, ft, :], bankO, rc[:], ALU.mult, [kob, krc], [f"oTb.{ft}"])
                    BK.put(b)
                    yield

            def gen_attn(blk):
                bs = slice(blk * 512, (blk + 1) * 512)
                for ft in range(KC):
                    b = BK.get()
                    bank, bk = BK.f32(b), BK.key(b)
                    for kc in range(KC):
                        kb.mm(bank, W1[:, kc, ft * 128:(ft + 1) * 128], hT[:, kc, bs], kc == 0, kc == KC - 1, ["WA.1", f"hT.{blk}"], [bk])
                    yield
                    kb.cpalt(qTb[:, ft, :], bank, [bk], [f"qTb.{ft}"])
                    BK.put(b)
                    yield
                if blk == 3:
                    mlp_load(0)
                for pair in range(2):
                    gs = [gen_head(0, 2 * pair), gen_head(1, 2 * pair + 1)]
                    while gs:
                        for g in list(gs):
                            try:
                                next(g)
                            except StopIteration:
                                gs.remove(g)
                        yield
                for tl in range(4):
                    t = blk * 4 + tl
                    for hf in range(2):
                        b = BK.get()
                        bank, bk = BK.f32(b), BK.key(b)
                        for kc in range(KC):
                            kb.mm(bank, oTb[:, kc, tl * 128:(tl + 1) * 128], W0[:, kc, hf * 512:(hf + 1) * 512], kc == 0, kc == KC - 1,
                                  [f"oTb.{kc}", "WA.0"], [bk])
                        yield
                        xs = xres[:, t, hf * 512:(hf + 1) * 512]
                        kb.tt("dve", xs, bank, xs, ALU.add, [bk, f"xres.{t}"], [f"xres.{t}"])
                        BK.put(b)
                        yield

            def norm_worker(blk):
                if blk + 1 < 4:
                    yield from norm_x(blk + 1)
                if blk - 1 >= 0:
                    yield from norm_m(blk - 1)

            s3 = s2
            aT_all = sb("aT_all", [128, 8, 512], BF16, st=s3)[:]
            rl_p = [sb(f"rl{i}", [128, 512], BF16, st=s3) for i in range(2)]
            o_p = [oTb_t[:, 2048 * i:2048 * (i + 1)].bitcast(F32) for i in range(2)]
            upv = wupm_d.rearrange("(kc p) n -> p kc n", p=128)
            dnv = wdn_d.rearrange("(f p) n -> p f n", p=128)
            ov = out_d.rearrange("(t p) d -> p t d", p=128)

            def mlp_w(j):
                s = (j + 1) % 2
                Wu = WA[:, s * 8192:s * 8192 + 4096].rearrange("p (k n) -> p k n", k=KC)
                Wd = WA[:, s * 8192 + 4096:(s + 1) * 8192].rearrange("p (f n) -> p f n", f=4)
                return s, Wu, Wd

            def mlp_load(j):
                s, Wu, Wd = mlp_w(j)
                kb.dma("pool", Wu, upv[:, :, j * 512:(j + 1) * 512], [], [f"WA.{s}"])
                kb.dma("pool", Wd, dnv[:, 4 * j:4 * j + 4, :], [], [f"WA.{s}"])

            run(norm_x(0))
            for blk in range(4):
                interleave([gen_attn(blk), norm_worker(blk)])
            run(norm_m(3))

            NJ = 8
            ai = 0
            ri = 0
            for j in range(NJ):
                s, Wu, Wd = mlp_w(j)
                if j > 0:
                    mlp_load(j)
                for blk in range(4):
                    bs = slice(blk * 512, (blk + 1) * 512)
                    ab = (ai % 2) * 4
                    ai += 1
                    for fft in range(4):
                        b = BK.get()
                        bank, bk = BK.f32(b), BK.key(b)
                        for kc in range(KC):
                            kb.mm(bank, Wu[:, kc, fft * 128:(fft + 1) * 128], hT[:, kc, bs], kc == 0, kc == KC - 1, [f"WA.{s}", f"hT.{blk}"], [bk])
                        rl, krl = rl_p[ri % 2], f"rl.{ri % 2}"
                        ri += 1
                        kb.act(rl[:], bank, AF.Relu, [bk], [krl])
                        BK.put(b)
                        kb.tt("dve", aT_all[:, ab + fft, :], rl[:], rl[:], ALU.mult, [krl], [f"aT.{ab + fft}"])
                    for tl in range(4):
                        t = blk * 4 + tl
                        for hf in range(2):
                            b = BK.get()
                            bank, bk = BK.f32(b), BK.key(b)
                            for fft in range(4):
                                kb.mm(bank, aT_all[:, ab + fft, tl * 128:(tl + 1) * 128], Wd[:, fft, hf * 512:(hf + 1) * 512], fft == 0, fft == 3,
                                      [f"aT.{ab + fft}", f"WA.{s}"], [bk])
                            xs = xres[:, t, hf * 512:(hf + 1) * 512]
                            kb.tt("dve", xs, bank, xs, ALU.add, [bk, f"xres.{t}"], [f"xres.{t}"])
                            BK.put(b)
                        if j == NJ - 1:
                            c = 32 + t
                            kb.act(sqj[:], xres[:, t, :], AF.Square, [f"xres.{t}"], ["sqj", f"ss{c}"], accum=ss[:, c:c + 1])
                            kb.ts("dve", ss[:, c:c + 1], ss[:, c:c + 1], 1.0 / D, 1e-6, ALU.mult, ALU.add, [f"ss{c}"], [f"ss{c}"])
                            kb.tt("pool", ss[:, c:c + 1], ss[:, c:c + 1], mhalf[:, 0:1], ALU.pow, [f"ss{c}", "mhalf"], [f"ss{c}"])
                            ot, ko = o_p[t % 2], f"ot.{t % 2}"
                            kb.stt(ot, xres[:, t, :], ss[:, c:c + 1], gfin[:], ALU.mult, ALU.mult, [f"xres.{t}", f"ss{c}", "gfin"],
                                   [ko] + [f"oTb.{k}" for k in range(KC)])
                            kb.dma("sp", ov[:, t, :], ot, [ko], [f"out.{t}"])
            P.barrier()
            P.flush()


def _consts():
    cq = np.zeros((128, NCQ), np.float32)
    i = np.arange(128)
    cq[:, 0:128] = np.eye(128, dtype=np.float32)
    strict = (i[:, None] < i[None, :]).astype(np.float32)
    incl = (i[:, None] <= i[None, :]).astype(np.float32)
    cq[:, 128:256] = strict
    cq[:, 256:384] = incl
    cq[:, 384:512] = strict
    cq[:, 512:640] = incl
    cq[:, 640:768] = (i[:, None] > i[None, :]).astype(np.float32)
    cq[:, 768:896] = ((i[:, None] // 64) == (i[None, :] // 64)).astype(np.float32)
    cq[:, 896:1024] = 1.0
    gam = 1.0 - 2.0 ** (-5.0 - np.arange(8, dtype=np.float64))
    xi = gam[None, :] ** (i[:, None] + 1.0)
    kz = 0.125 * gam[None, :] ** (-(i[:, None] + 1.0))
    gC = np.zeros((128, 4))
    for ct in range(4):
        for p in range(128):
            gC[p, ct] = gam[2 * ct + p // 64] ** 128.0
    invf = (10000.0 ** (-np.arange(32, dtype=np.float32) / 32.0)).astype(np.float32)
    return cq, xi.astype(np.float32), kz.astype(np.float32), gC.astype(np.float32), np.broadcast_to(invf[None], (128, 32))


def _col(v):
    v = np.asarray(v, np.float32).reshape(-1)
    return v.reshape(-1, 128).T


def make_inputs(inp):
    cq, xi, kz, gC, invf = _consts()
    cols = np.zeros((128, NCOLS), np.float32)
    cols[:, 0:8] = _col(inp["norm_mix"][0])
    cols[:, 8:16] = _col(inp["norm_xattn"][0])
    cols[:, 16:24] = _col(inp["norm_mem"][0])
    cols[:, 24:32] = _col(inp["norm_mlp"][0])
    cols[:, 32:36] = _col(inp["ret_gn_w"][0])
    cols[:, 36:40] = _col(inp["ret_gn_b"][0])
    mu = np.asarray(inp["rwkv_mu"][0], np.float32)
    cols[:, 40:54] = _col(mu[:1792])
    cols[:, 54] = mu[1696:1824]
    cols[:, 55:59] = _col(inp["rwkv_w0"][0])
    cols[:, 59:63] = _col(inp["rwkv_a0"][0])
    cols[:, 63:67] = _col(inp["rwkv_k_k"][0])
    cols[:, 67:71] = _col(inp["rwkv_k_a"][0])
    cols[:, 71:75] = _col(inp["rwkv_r_k"][0])
    cols[:, 75:79] = _col(inp["rwkv_gn_w"][0])
    cols[:, 79:83] = _col(inp["rwkv_gn_b"][0])
    cols[:, 83:91] = xi
    cols[:, 91:99] = kz
    cols[:, 99:103] = gC
    cols[:, 103:135] = invf
    shared = {
        "cols": cols, "cq": cq,
        "gfin": np.ascontiguousarray(inp["norm_final"], np.float32),
        "w_in": np.ascontiguousarray(inp["w_in"][0]),
        "w_up": np.ascontiguousarray(inp["rwkv_w_up"][0]),
        "a_up": np.ascontiguousarray(inp["rwkv_a_up"][0]),
        "g_up": np.ascontiguousarray(inp["rwkv_g_up"][0]),
        "w_out": np.ascontiguousarray(inp["w_out"][0]),
        "wq": np.ascontiguousarray(inp["xattn_w_q"][0]),
        "wkv": np.ascontiguousarray(inp["xattn_w_kv"][0]),
        "wo": np.ascontiguousarray(inp["xattn_w_o"][0]),
        "mlp_up": np.ascontiguousarray(inp["mlp_w_up"][0]),
        "mlp_down": np.ascontiguousarray(inp["mlp_w_down"][0]),
    }
    maps = []
    for b in range(8):
        m = dict(shared)
        m["x"] = np.ascontiguousarray(inp["x"][b], np.float32)
        m["mem"] = np.ascontiguousarray(inp["mem"][b], np.float32)
        m["pos"] = np.ascontiguousarray(np.asarray(inp["positions"][b], np.int32).reshape(NT, 128).T)
        maps.append(m)
    return maps


_NC_CACHE = {}


def kernel(**inputs):
    inp = {k: np.asarray(v) for k, v in inputs.items()}
    maps = make_inputs(inp)
    if "nc" not in _NC_CACHE:
        _NC_CACHE["nc"] = build()
    res = run_bass_kernel_spmd(_NC_CACHE["nc"], maps, core_ids=list(range(8)))
    return np.stack([np.asarray(r["out"], np.float32) for r in res.results], axis=0)
```

```python
import contextlib
import math
import numpy as np
import concourse.bass as bass
import concourse.mybir as mybir
from concourse.bass_utils import run_bass_kernel_spmd

F32 = mybir.dt.float32
BF16 = mybir.dt.bfloat16
I32 = mybir.dt.int32
AF = mybir.ActivationFunctionType
ALU = mybir.AluOpType
AX = mybir.AxisListType

T = 2048
D = 1024
NT = 16
KC = 8
NCOLS = 135
NCQ = 1024
CUT = 99
COMPUTE = ("pe", "act", "dve", "pool")
ISSUERS = ("pe", "act", "dve", "pool", "sp")


class Prog:
    def __init__(self, nc, st, n_dma=12, self_sync=True):
        self.nc = nc
        self.n_dma = n_dma
        self.self_sync = self_sync
        self.chans = list(COMPUTE) + [f"d{i}" for i in range(n_dma)]
        self.sems = {c: st.enter_context(nc.semaphore(f"s_{c}")) for c in self.chans}
        self.streams = {e: [] for e in ISSUERS}
        self.count = {c: 0 for c in self.chans}
        self.clock = {e: {} for e in ISSUERS}
        self.snap = {}
        self.wr = {}
        self.rd = {}
        self.rr = 0
        self.nops = 0
        self.nwaits = 0

    @staticmethod
    def _need(needs, d):
        for c, n in d.items():
            if needs.get(c, 0) < n:
                needs[c] = n

    def op(self, eng, fn, reads=(), writes=(), dma=False):
        needs = {}
        for k in reads:
            self._need(needs, self.wr.get(k, {}))
        for k in writes:
            self._need(needs, self.wr.get(k, {}))
            self._need(needs, self.rd.get(k, {}))
        if dma:
            chan = f"d{self.rr % self.n_dma}"
            self.rr += 1
            if self.count[chan]:
                self._need(needs, {chan: self.count[chan]})
        else:
            chan = eng
        clk = self.clock[eng]
        waits = []
        for c, n in needs.items():
            if c == eng and (eng == "pe" or not self.self_sync):
                continue
            if clk.get(c, 0) < n:
                waits.append((c, n))
                for c2, n2 in self.snap[(c, n)].items():
                    if clk.get(c2, 0) < n2:
                        clk[c2] = n2
        self.nwaits += len(waits)
        self.nops += 1
        n_new = self.count[chan] + 1
        self.count[chan] = n_new
        s = dict(clk)
        s[chan] = n_new
        self.snap[(chan, n_new)] = s
        if eng == "pe" and chan == "pe":
            clk["pe"] = n_new
        wset = set(writes)
        for k in wset:
            self.wr[k] = {chan: n_new}
            self.rd[k] = {}
        for k in reads:
            if k in wset:
                continue
            self.rd.setdefault(k, {})[chan] = n_new
        self.streams[eng].append((waits, fn, chan))

    def barrier(self):
        allk = {c: n for c, n in self.count.items() if n}
        for e in ISSUERS:
            clk = self.clock[e]
            waits = []
            for c, n in allk.items():
                if c == e and e == "pe":
                    continue
                if clk.get(c, 0) < n:
                    waits.append((c, n))
            if waits:
                self.streams[e].append((waits, None, None))
        for e in ISSUERS:
            for c, n in allk.items():
                if self.clock[e].get(c, 0) < n:
                    self.clock[e][c] = n

    def flush(self):
        nc = self.nc
        streams = self.streams
        if not any(streams[e] for e in ISSUERS):
            return
        self.streams = {e: [] for e in ISSUERS}
        sems = self.sems

        def val(c, n):
            return n * 16 if c.startswith("d") else n

        def run(engname):
            def body(e):
                for waits, fn, chan in streams[engname]:
                    for c, n in waits:
                        e.wait_ge(sems[c], val(c, n))
                    if fn is not None:
                        fn(e).then_inc(sems[chan], 16 if chan.startswith("d") else 1)
            return body

        with nc.Block() as block:
            block.tensor(run("pe"))
            block.scalar(run("act"))
            block.vector(run("dve"))
            block.gpsimd(run("pool"))
            block.sync(run("sp"))


class Ring:
    def __init__(self, kb, name, n, shape, dtype, st=None, psum=False):
        st = st or kb.st
        alloc = kb.nc.psum_tensor if psum else kb.nc.sbuf_tensor
        self.name = name
        self.t = [st.enter_context(alloc(f"rg_{name}{i}", shape, dtype)) for i in range(n)]
        self.i = 0

    def next(self):
        i = self.i % len(self.t)
        self.i += 1
        return self.t[i], f"{self.name}.{i}"


class ViewRing:
    def __init__(self, aps, keys):
        self.t, self.k, self.i = aps, keys, 0

    def next(self):
        i = self.i % len(self.t)
        self.i += 1
        return self.t[i], self.k[i]


class Banks:
    def __init__(self, pp):
        import collections
        self.pp = pp
        self.free = collections.deque(range(8))

    def get(self):
        return self.free.popleft()

    def put(self, i):
        self.free.append(i)

    def f32(self, i):
        return self.pp[:, i, :]

    def bf(self, i):
        return self.pp[:, i, :].bitcast(BF16)

    @staticmethod
    def key(i):
        return f"ps{i}"


def interleave(gens):
    gens = list(gens)
    while gens:
        for g in list(gens):
            try:
                next(g)
            except StopIteration:
                gens.remove(g)


class KB:
    def __init__(self, nc, P, st):
        self.nc, self.P, self.st = nc, P, st
        self.flip = 0

    def sb(self, name, shape, dtype, st=None):
        return (st or self.st).enter_context(self.nc.sbuf_tensor("sb_" + name, shape, dtype))

    def mm(self, out, lhsT, rhs, start, stop, r, w):
        self.P.op("pe", lambda e: e.matmul(out, lhsT=lhsT, rhs=rhs, start=start, stop=stop), r, w)

    def tr(self, out, in_, r, w):
        idb = self.identb[:]
        self.P.op("pe", lambda e: e.transpose(out=out, in_=in_, identity=idb), list(r) + ["identb"], w)

    def act(self, out, in_, func, r, w, scale=1.0, bias=0.0, accum=None):
        kw = {}
        if accum is not None:
            kw["accum_out"] = accum
        self.P.op("act", lambda e: e.activation(out=out, in_=in_, func=func, bias=bias, scale=scale, **kw), r, w)

    def tt(self, eng, out, in0, in1, op, r, w):
        self.P.op(eng, lambda e: e.tensor_tensor(out=out, in0=in0, in1=in1, op=op), r, w)

    def ts(self, eng, out, in0, s1, s2, op0, op1, r, w):
        if s2 is None:
            self.P.op(eng, lambda e: e.tensor_scalar(out=out, in0=in0, scalar1=s1, scalar2=None, op0=op0), r, w)
        else:
            self.P.op(eng, lambda e: e.tensor_scalar(out=out, in0=in0, scalar1=s1, scalar2=s2, op0=op0, op1=op1), r, w)

    def stt(self, out, in0, sc, in1, op0, op1, r, w):
        self.P.op("dve", lambda e: e.scalar_tensor_tensor(out=out, in0=in0, scalar=sc, in1=in1, op0=op0, op1=op1), r, w)

    def cp(self, eng, out, in_, r, w):
        if eng == "act":
            self.P.op("act", lambda e: e.activation(out=out, in_=in_, func=AF.Copy), r, w)
        else:
            self.P.op(eng, lambda e: e.tensor_copy(out=out, in_=in_), r, w)

    def cpalt(self, out, in_, r, w):
        self.flip ^= 1
        self.cp("act" if self.flip else "dve", out, in_, r, w)

    def red(self, out, in_, op, r, w):
        self.P.op("dve", lambda e: e.tensor_reduce(out=out, in_=in_, axis=AX.X, op=op), r, w)

    def dma(self, eng, out, in_, r, w):
        self.P.op(eng, lambda e: e.dma_start(out=out, in_=in_), r, w, dma=True)


def build(stages="ABCD", dbg=False):
    nc = bass.Bass("TRN2", target_bir_lowering=False)

    def dram(n, s, d, kind="ExternalInput"):
        return nc.dram_tensor(n, s, d, kind=kind).ap()

    x_d = dram("x", [T, D], F32)
    mem_d = dram("mem", [256, D], F32)
    pos_d = dram("pos", [128, NT], I32)
    cols_d = dram("cols", [128, NCOLS], F32)
    cq_d = dram("cq", [128, NCQ], F32)
    gfin_d = dram("gfin", [D], F32)
    w_in_d = dram("w_in", [D, 3872], F32)
    wup_d = dram("w_up", [64, 512], F32)
    aup_d = dram("a_up", [64, 512], F32)
    gup_d = dram("g_up", [160, 512], F32)
    w_out_d = dram("w_out", [D, D], F32)
    wq_d = dram("wq", [D, D], F32)
    wkv_d = dram("wkv", [D, 2 * D], F32)
    wo_d = dram("wo", [D, D], F32)
    wupm_d = dram("mlp_up", [D, 4 * D], F32)
    wdn_d = dram("mlp_down", [4 * D, D], F32)
    out_d = dram("out", [T, D], F32, kind="ExternalOutput")
    if dbg:
        dbg_yT = dram("dbg_yT", [128, KC, T], BF16, kind="ExternalOutput")
        dbg_hT = dram("dbg_hT", [128, KC, T], BF16, kind="ExternalOutput")

    def wview(w):
        return w.rearrange("(kc p) n -> p kc n", p=128)

    with contextlib.ExitStack() as st:
        P = Prog(nc, st)
        kb = KB(nc, P, st)
        cols = kb.sb("cols", [128, NCOLS], F32)
        dc = kb.sb("dcols", [128, 40], F32)
        cqf = kb.sb("cqf", [128, NCQ], F32)
        identb = kb.sb("identb", [128, 128], BF16)
        bonesb = kb.sb("bonesb", [128, 128], BF16)
        onesb = kb.sb("onesb", [128, 128], BF16)
        mhalf = kb.sb("mhalf", [128, 256], F32)
        hT = kb.sb("hT", [128, KC, T], BF16)
        yT = kb.sb("yT", [128, 4, T], BF16)
        WA = kb.sb("WA", [128, 16384], BF16)
        ss = kb.sb("ss", [128, 3 * NT], F32)
        kb.identb = identb
        pp = st.enter_context(nc.psum_tensor("pp", [128, 8, 512], F32))
        L_BK = Banks(pp)
        psA = ViewRing([pp[:, i, :] for i in range(6)], [f"ps{i}" for i in range(6)])
        psT = ViewRing([pp[:, i, :].bitcast(BF16) for i in (6, 7)], ["ps6", "ps7"])
        mask4 = cqf[:, 128:640]
        maskI = cqf[:, 256:384]
        maskLs = cqf[:, 640:768]
        identf = cqf[:, 0:128]

        kb.dma("sp", cols[:], cols_d, [], ["cols"])
        kb.dma("sp", cqf[:], cq_d, [], ["cqf"])
        kb.dma("pool", identb[:], cq_d[:, 0:128], [], ["identb"])
        kb.dma("pool", bonesb[:], cq_d[:, 768:896], [], ["bonesb"])
        kb.dma("pool", onesb[:], cq_d[:, 896:1024], [], ["onesb"])
        P.op("dve", lambda e: e.memset(mhalf[:], -0.5), [], ["mhalf"])
        kb.ts("dve", dc[:, 0:15], cols[:, 40:55], -1.0, 1.0, ALU.mult, ALU.add, ["cols"], ["dc"])
        kb.ts("dve", dc[:, 15:19], cols[:, 55:59], 0.5, None, ALU.mult, None, ["cols"], ["dc"])
        kb.ts("dve", dc[:, 19:23], cols[:, 59:63], 0.5, None, ALU.mult, None, ["cols"], ["dc"])
        kb.ts("dve", dc[:, 23:27], cols[:, 67:71], -1.0, None, ALU.mult, None, ["cols"], ["dc"])
        kb.ts("dve", dc[:, 27:31], cols[:, 67:71], -1.0, 1.0, ALU.mult, ALU.add, ["cols"], ["dc"])

        def norm_T(src, nblk, tpb, gcol0, dst, dstkey, sscol0, hb, sqj):
            for blk in range(nblk):
                hbs = []
                for tl in range(tpb):
                    t = blk * tpb + tl
                    xa, xk = src(t)
                    c = sscol0 + t
                    kb.act(sqj[:], xa, AF.Square, [xk], ["sqj", f"ss{c}"], accum=ss[:, c:c + 1])
                    kb.ts("dve", ss[:, c:c + 1], ss[:, c:c + 1], 1.0 / D, 1e-6, ALU.mult, ALU.add, [f"ss{c}"], [f"ss{c}"])
                    kb.tt("pool", ss[:, c:c + 1], ss[:, c:c + 1], mhalf[:, 0:1], ALU.pow, [f"ss{c}", "mhalf"], [f"ss{c}"])
                    h, hk = hb.next()
                    kb.act(h[:], xa, AF.Copy, [xk, f"ss{c}"], [hk], scale=ss[:, c:c + 1])
                    hbs.append((h, hk))
                for kc in range(KC):
                    bank, bk = psT.next()
                    for tl, (h, hk) in enumerate(hbs):
                        kb.tr(bank[:, tl * 128:(tl + 1) * 128], h[:, kc * 128:(kc + 1) * 128], [hk], [bk])
                    n = tpb * 128
                    o = dst[:, kc, blk * n:(blk + 1) * n]
                    kb.flip ^= 1
                    if kb.flip:
                        kb.act(o, bank[:, 0:n], AF.Copy, [bk, "cols"], [dstkey(blk)], scale=cols[:, gcol0 + kc:gcol0 + kc + 1])
                    else:
                        kb.ts("dve", o, bank[:, 0:n], cols[:, gcol0 + kc:gcol0 + kc + 1], None, ALU.mult, None, [bk, "cols"], [dstkey(blk)])

        def load_w(dst, src, key, eng="pool"):
            kb.dma(eng, dst, src, [], [key])

        Wv8 = lambda ncol: WA[:, 0:KC * ncol].rearrange("p (k n) -> p k n", k=KC)

        Wr = Wv8(2048)
        BK = L_BK

        def acquire(n=1):
            spins = 0
            while len(BK.free) < n:
                spins += 1
                assert spins < 100000, "PSUM bank pool deadlock"
                yield
            return [BK.get() for _ in range(n)]

        def rr(gens):
            gens = list(gens)
            while gens:
                for g in list(gens):
                    try:
                        next(g)
                    except StopIteration:
                        gens.remove(g)
                yield

        def run(g):
            for _ in g:
                pass

        if "B" not in stages:
            with contextlib.ExitStack() as sa:
                xring = Ring(kb, "xr", 3, [128, D], F32, st=sa)
                hb = Ring(kb, "hb", 8, [128, D], BF16, st=sa)
                sqj = kb.sb("sqj", [128, D], BF16, st=sa)

                def srcx(t):
                    xa, xk = xring.next()
                    kb.dma("sp", xa[:], x_d[t * 128:(t + 1) * 128, :], [], [xk])
                    return xa[:], xk
                norm_T(srcx, 4, 4, 0, hT, lambda b: f"hT.{b}", 0, hb, sqj)
                P.barrier()
                P.flush()
            P.op("dve", lambda e: e.memset(yT[:, 0:4, :], 0.0), [], [f"yT.{t}.0" for t in range(NT)])
        else:
            for gi in range(4):
                load_w(Wr[:, :, gi * 512:(gi + 1) * 512], wview(w_in_d)[:, :, gi * 512:(gi + 1) * 512], f"W.{gi}")
            with contextlib.ExitStack() as sbk:
                cos_t = kb.sb("cos_t", [128, NT, 32], F32, st=sbk)
                sin_t = kb.sb("sin_t", [128, NT, 32], F32, st=sbk)
                srope = sbk
                if True:
                    posi = kb.sb("posi", [128, NT], I32, st=srope)
                    posf = kb.sb("posf", [128, NT], F32, st=srope)
                    ang = kb.sb("ang", [128, NT, 32], F32, st=srope)
                    ra = kb.sb("ra", [128, NT, 32], F32, st=srope)
                    rb_ = kb.sb("rb_", [128, NT, 32], F32, st=srope)
                    ri = kb.sb("ri", [128, NT, 32], I32, st=srope)
                    kb.dma("sp", posi[:], pos_d, [], ["posi"])
                    kb.cp("dve", posf[:], posi[:], ["posi"], ["posf"])
                    kb.tt("dve", ang[:], posf[:].unsqueeze(2).to_broadcast([128, NT, 32]),
                          cols[:, 103:135].unsqueeze(1).to_broadcast([128, NT, 32]), ALU.mult, ["posf", "cols"], ["ang"])
                    C1 = 6.28125
                    C2 = 2.0 * math.pi - C1
                    for tab, shift, nm in ((sin_t, 0.0, "sin"), (cos_t, 0.5 * math.pi, "cos")):
                        kb.ts("dve", ra[:], ang[:], shift, 1.0 / (2.0 * math.pi), ALU.add, ALU.mult, ["ang"], ["ra"])
                        kb.cp("dve", ri[:], ra[:], ["ra"], ["ri"])
                        kb.cp("dve", rb_[:], ri[:], ["ri"], ["rb"])
                        kb.ts("dve", ra[:], ang[:], shift, None, ALU.add, None, ["ang"], ["ra"])
                        kb.stt(ra[:], rb_[:], -C1, ra[:], ALU.mult, ALU.add, ["rb", "ra"], ["ra"])
                        kb.stt(ra[:], rb_[:], -C2, ra[:], ALU.mult, ALU.add, ["rb", "ra"], ["ra"])
                        kb.ts("dve", rb_[:], ra[:], math.pi, -2.0 * math.pi, ALU.is_gt, ALU.mult, ["ra"], ["rb"])
                        kb.tt("dve", ra[:], ra[:], rb_[:], ALU.add, ["ra", "rb"], ["ra"])
                        kb.ts("dve", rb_[:], ra[:], -math.pi, 2.0 * math.pi, ALU.is_lt, ALU.mult, ["ra"], ["rb"])
                        kb.tt("dve", ra[:], ra[:], rb_[:], ALU.add, ["ra", "rb"], ["ra"])
                        kb.act(tab[:], ra[:], AF.Sin, ["ra"], [nm])

                sb_ = lambda n, shp, d: kb.sb(n, shp, d, st=sbk)
                W = 2
                xr_p = [sb_(f"xr{i}", [128, D], F32) for i in range(2)]
                hb_p = [sb_(f"hb{i}", [128, D], BF16) for i in range(4)]
                sqj = sb_("sqj", [128, D], BF16)
                qT_p = [sb_(f"qT{i}", [128, 4, 2, 512], BF16) for i in range(2)]
                for i in range(2):
                    P.op("dve", lambda e, i=i: e.memset(qT_p[i][:], 0.0), [], [f"qT{i}.{j}" for j in range(4)])
                kT_p = [sb_(f"kT{i}", [128, 4, 512], BF16) for i in range(2)]
                ktok_p = [sb_(f"ktok{i}", [128, 4, 512], BF16) for i in range(2)]
                vtok_p = [sb_(f"vtok{i}", [128, 4, 512], BF16) for i in range(2)]
                gateT_p = [sb_(f"gateT{i}", [128, 4, 512], BF16) for i in range(2)]
                xs_p = [sb_(f"xs{i}", [128, 512], F32) for i in range(W)]
                A_p = [sb_(f"rA{i}", [128, 512], F32) for i in range(W)]
                B_p = [sb_(f"rB{i}", [128, 512], F32) for i in range(W)]
                qtok_p = [sb_(f"qtok{i}", [128, 512], BF16) for i in range(W)]
                th_p = [sb_(f"thr{i}", [128, 512], F32) for i in range(2)]
                sT_p = [sb_(f"sT{i}", [128, 8, 128], BF16) for i in range(W)]
                y32_p = [sb_(f"y32{i}", [128, 512], F32) for i in range(W)]
                sq_p = [sb_(f"sqr{i}", [128, 512], F32) for i in range(W)]
                ynb_p = [sb_(f"ynb{i}", [128, 512], BF16) for i in range(W)]
                yaff_p = [sb_(f"yaff{i}", [128, 2, 128], F32) for i in range(W)]
                stat_p = [sb_(f"stat{i}", [128, 64], F32) for i in range(W)]
                R32 = sb_("R32", [128, 4, 64], F32)
                Rbf = sb_("Rbf", [128, 5, 4, 64], BF16)
                P.op("dve", lambda e: e.memset(R32[:], 0.0), [], ["R32"])

                def gen_normA(blk):
                    for tl in range(4):
                        t = blk * 4 + tl
                        xa, xk = xr_p[t % 2], f"xr.{t % 2}"
                        kb.dma("sp", xa[:], x_d[t * 128:(t + 1) * 128, :], [], [xk])
                        kb.act(sqj[:], xa[:], AF.Square, [xk], ["sqj", f"ss{t}"], accum=ss[:, t:t + 1])
                        yield
                        kb.ts("dve", ss[:, t:t + 1], ss[:, t:t + 1], 1.0 / D, 1e-6, ALU.mult, ALU.add, [f"ss{t}"], [f"ss{t}"])
                        yield
                        kb.tt("pool", ss[:, t:t + 1], ss[:, t:t + 1], mhalf[:, 0:1], ALU.pow, [f"ss{t}", "mhalf"], [f"ss{t}"])
                        yield
                        kb.act(hb_p[tl][:], xa[:], AF.Copy, [xk, f"ss{t}"], [f"hb.{tl}"], scale=ss[:, t:t + 1])
                        yield
                    for kc in range(KC):
                        (b,) = yield from acquire(1)
                        bankT, bk = BK.bf(b), BK.key(b)
                        for tl in range(4):
                            kb.tr(bankT[:, tl * 128:(tl + 1) * 128], hb_p[tl][:, kc * 128:(kc + 1) * 128], [f"hb.{tl}"], [bk])
                        yield
                        o = hT[:, kc, blk * 512:(blk + 1) * 512]
                        if kc % 2:
                            kb.act(o, bankT[:, 0:512], AF.Copy, [bk, "cols"], [f"hT.{blk}"], scale=cols[:, kc:kc + 1])
                        else:
                            kb.ts("dve", o, bankT[:, 0:512], cols[:, kc:kc + 1], None, ALU.mult, None, [bk, "cols"], [f"hT.{blk}"])
                        BK.put(b)
                        yield

                def post_gen(slot, b, eps, wcol0, bcol0, finish):
                    bank, bk = BK.f32(b), BK.key(b)
                    y32, k1 = y32_p[slot], f"y32.{slot}"
                    kb.cp("act", y32[:], bank, [bk], [k1])
                    BK.put(b)
                    yield
                    stt_, ks = stat_p[slot], f"stat.{slot}"
                    v3 = lambda a: a.rearrange("p (h d) -> p h d", d=64)
                    bc = lambda a: a.unsqueeze(2).to_broadcast([128, 8, 64])
                    kb.red(stt_[:, 0:8], v3(y32[:]), ALU.add, [k1], [ks])
                    sq, k2 = sq_p[slot], f"sq.{slot}"
                    kb.act(sq[:], y32[:], AF.Square, [k1], [k2])
                    yield
                    kb.red(stt_[:, 8:16], v3(sq[:]), ALU.add, [k2], [ks])
                    yield
                    kb.ts("dve", stt_[:, 16:24], stt_[:, 0:8], 1.0 / 64, None, ALU.mult, None, [ks], [ks])
                    yield
                    kb.tt("dve", stt_[:, 24:32], stt_[:, 16:24], stt_[:, 16:24], ALU.mult, [ks], [ks])
                    kb.ts("dve", stt_[:, 32:40], stt_[:, 8:16], 1.0 / 64, eps, ALU.mult, ALU.add, [ks], [ks])
                    yield
                    kb.tt("dve", stt_[:, 40:48], stt_[:, 32:40], stt_[:, 24:32], ALU.subtract, [ks], [ks])
                    yield
                    kb.tt("pool", stt_[:, 48:56], stt_[:, 40:48], mhalf[:, 0:8], ALU.pow, [ks, "mhalf"], [ks])
                    yield
                    kb.stt(stt_[:, 56:64], stt_[:, 16:24], -1.0, stt_[:, 48:56], ALU.mult, ALU.mult, [ks], [ks])
                    kb.tt("pool", v3(y32[:]), v3(y32[:]), bc(stt_[:, 48:56]), ALU.mult, [k1, ks], [k1])
                    yield
                    ynb, k3 = ynb_p[slot], f"ynb.{slot}"
                    kb.tt("dve", v3(ynb[:]), v3(y32[:]), bc(stt_[:, 56:64]), ALU.add, [k1, ks], [k3])
                    yield
                    (b2,) = yield from acquire(1)
                    bankT, kt = BK.bf(b2), BK.key(b2)
                    for ct in range(4):
                        kb.tr(bankT[:, ct * 128:(ct + 1) * 128], ynb[:, ct * 128:(ct + 1) * 128], [k3], [kt])
                    yield
                    for ct in range(4):
                        ya, k4 = yaff_p[slot][:, ct % 2, :], f"yaff.{slot}.{ct % 2}"
                        kb.act(ya, bankT[:, ct * 128:(ct + 1) * 128], AF.Identity, [kt, "cols"], [k4],
                               scale=cols[:, wcol0 + ct:wcol0 + ct + 1], bias=cols[:, bcol0 + ct:bcol0 + ct + 1])
                        finish(ct, ya, k4)
                        if ct == 3:
                            BK.put(b2)
                        yield

                def qk_chain(slot, blk, tl, wi):
                    par = blk % 2
                    t = blk * 4 + tl
                    tok = slice(t * 128, (t + 1) * 128)
                    hk = f"hT.{blk}"
                    tabcol = 83 if wi == 0 else 91
                    (b,) = yield from acquire(1)
                    bank, bk = BK.f32(b), BK.key(b)
                    for kc in range(KC):
                        kb.mm(bank, hT[:, kc, tok], Wr[:, kc, wi * 512:(wi + 1) * 512], kc == 0, kc == KC - 1, [hk, f"W.{wi}"], [bk])
                    yield
                    xs, kx = xs_p[slot], f"xs.{slot}"
                    kb.cp("act", xs[:], bank, [bk], [kx])
                    BK.put(b)
                    yield
                    A, ka = A_p[slot], f"rA.{slot}"
                    B, kbk = B_p[slot], f"rB.{slot}"
                    v4 = lambda a: a.rearrange("p (h two f) -> p h two f", two=2, f=32)
                    cs4 = cos_t[:, t, :].unsqueeze(1).unsqueeze(1).to_broadcast([128, 8, 2, 32])
                    sn3 = sin_t[:, t, :].unsqueeze(1).to_broadcast([128, 8, 32])
                    kb.tt("dve", v4(A[:]), v4(xs[:]), cs4, ALU.mult, [kx, "cos"], [ka])
                    kb.tt("pool", v4(B[:])[:, :, 0, :], v4(xs[:])[:, :, 1, :], sn3, ALU.mult, [kx, "sin"], [kbk])
                    kb.tt("pool", v4(B[:])[:, :, 1, :], v4(xs[:])[:, :, 0, :], sn3, ALU.mult, [kx, "sin"], [kbk])
                    yield
                    kb.tt("dve", v4(A[:])[:, :, 0, :], v4(A[:])[:, :, 0, :], v4(B[:])[:, :, 0, :], ALU.subtract, [ka, kbk], [ka])
                    kb.tt("dve", v4(A[:])[:, :, 1, :], v4(A[:])[:, :, 1, :], v4(B[:])[:, :, 1, :], ALU.add, [ka, kbk], [ka])
                    yield
                    scl = cols[:, tabcol:tabcol + 8].unsqueeze(2).to_broadcast([128, 8, 64])
                    A3 = A[:].rearrange("p (h d) -> p h d", d=64)
                    if wi == 0:
                        qtv, kq = qtok_p[slot][:], f"qtok.{slot}"
                    else:
                        qtv, kq = ktok_p[par][:, tl, :], f"ktok{par}.{tl}"
                    kb.tt("dve", qtv.rearrange("p (h d) -> p h d", d=64), A3, scl, ALU.mult, [ka, "cols"], [kq])
                    yield
                    (b2,) = yield from acquire(1)
                    bankT, kt = BK.bf(b2), BK.key(b2)
                    for ct in range(4):
                        kb.tr(bankT[:, ct * 128:(ct + 1) * 128], qtv[:, ct * 128:(ct + 1) * 128], [kq], [kt])
                    yield
                    bv = bankT[:, 0:512].rearrange("p (c n) -> p c n", c=4)
                    if wi == 0:
                        kb.cp("act", qT_p[par][0:64, :, 0, tl * 128:(tl + 1) * 128], bv[0:64], [kt], [f"qT{par}.{tl}"])
                        kb.cp("dve", qT_p[par][64:128, :, 1, tl * 128:(tl + 1) * 128], bv[64:128], [kt], [f"qT{par}.{tl}"])
                    else:
                        kb.cpalt(kT_p[par][:, :, tl * 128:(tl + 1) * 128], bv, [kt], [f"kT{par}.{tl}"])
                    BK.put(b2)
                    yield

                def v_chain(blk, tl):
                    par = blk % 2
                    t = blk * 4 + tl
                    tok = slice(t * 128, (t + 1) * 128)
                    (b,) = yield from acquire(1)
                    bank, bk = BK.f32(b), BK.key(b)
                    for kc in range(KC):
                        kb.mm(bank, hT[:, kc, tok], Wr[:, kc, 1024:1536], kc == 0, kc == KC - 1, [f"hT.{blk}", "W.2"], [bk])
                    yield
                    kb.cpalt(vtok_p[par][:, tl, :], bank, [bk], [f"vtok{par}.{tl}"])
                    BK.put(b)
                    yield

                def g_chain(blk, ct):
                    par = blk % 2
                    (b,) = yield from acquire(1)
                    bank, bk = BK.f32(b), BK.key(b)
                    for kc in range(KC):
                        kb.mm(bank, Wr[:, kc, 1536 + ct * 128:1536 + (ct + 1) * 128], hT[:, kc, blk * 512:(blk + 1) * 512],
                              kc == 0, kc == KC - 1, [f"hT.{blk}", "W.3"], [bk])
                    yield
                    th, kth = th_p[ct % 2], f"thr.{ct % 2}"
                    kb.act(th[:], bank, AF.Tanh, [bk], [kth], scale=0.5)
                    yield
                    kb.ts("pool", th[:], th[:], 0.5, 0.5, ALU.mult, ALU.add, [kth], [kth])
                    yield
                    kb.tt("dve", gateT_p[par][:, ct, :], th[:], bank, ALU.mult, [kth, bk], [f"gateT{par}.{ct}"])
                    BK.put(b)
                    yield

                def gen_proj(blk):
                    for k in range(4):
                        yield from rr([qk_chain((2 * k) % W, blk, k, 0), qk_chain((2 * k + 1) % W, blk, k, 1), v_chain(blk, k), g_chain(blk, k)])

                def chunk_chain(slot, blk, tl):
                    par = blk % 2
                    qT, kT_, vtok, gateT = qT_p[par], kT_p[par], vtok_p[par], gateT_p[par]
                    t = blk * 4 + tl
                    tok = slice(t * 128, (t + 1) * 128)
                    tl_ = slice(tl * 128, (tl + 1) * 128)
                    sT, ksT = sT_p[slot], f"sT.{slot}"
                    bs = yield from acquire(2)
                    for g4 in range(2):
                        bank, bk = BK.f32(bs[g4]), BK.key(bs[g4])
                        for hh in range(4):
                            h = 4 * g4 + hh
                            kb.mm(bank[:, hh * 128:(hh + 1) * 128], kT_[:, h // 2, tl_], qT[:, h // 2, h % 2, tl_], True, True,
                                  [f"kT{par}.{tl}", f"qT{par}.{tl}"], [bk])
                    yield
                    for g4 in range(2):
                        bank, bk = BK.f32(bs[g4]), BK.key(bs[g4])
                        kb.tt("dve", sT[:, 4 * g4:4 * g4 + 4, :], bank.rearrange("p (c n) -> p c n", c=4),
                              maskI.unsqueeze(1).to_broadcast([128, 4, 128]), ALU.mult, [bk, "cqf"], [ksT])
                        BK.put(bs[g4])
                        yield
                    (bo,) = yield from acquire(1)
                    bankO, ko = BK.f32(bo), BK.key(bo)
                    for h in range(8):
                        kb.mm(bankO[:, 64 * h:64 * h + 64], sT[:, h, :], vtok[:, tl, 64 * h:64 * h + 64], True, t == 0,
                              [ksT, f"vtok{par}.{tl}"], [ko])
                        if t > 0:
                            kb.mm(bankO[:, 64 * h:64 * h + 64], qT[:, h // 2, h % 2, tl_], Rbf[:, t % 5, h // 2, :], False, True,
                                  [f"qT{par}.{tl}", f"Rbf.{t % 5}"], [ko])
                    yield

                    def fin(ct, ya, k4):
                        kb.tt("dve", yT[:, ct, tok], ya, gateT[:, ct, tl_], ALU.mult, [k4, f"gateT{par}.{ct}"], [f"yT.{t}.0"])
                    yield from post_gen(slot, bo, 1e-5, 32, 36, fin)

                def gen_chunks(blk):
                    par = blk % 2
                    ktok, vtok = ktok_p[par], vtok_p[par]
                    for tl in range(4):
                        t = blk * 4 + tl
                        if t == NT - 1:
                            continue
                        (b,) = yield from acquire(1)
                        bankK, kk_ = BK.f32(b), BK.key(b)
                        for ct in range(4):
                            kb.mm(bankK[:, ct * 128:(ct + 1) * 128], ktok[:, tl, ct * 128:(ct + 1) * 128], vtok[:, tl, ct * 128:(ct + 1) * 128],
                                  True, True, [f"ktok{par}.{tl}", f"vtok{par}.{tl}"], [kk_])
                        yield
                        for hh in range(2):
                            b0 = 64 * hh
                            kb.tt("dve", R32[b0:b0 + 64, :, :], bankK[b0:b0 + 64, :].rearrange("p (c n) -> p c n", c=4)[:, :, b0:b0 + 64],
                                  R32[b0:b0 + 64, :, :], ALU.add, [kk_, "R32"], ["R32"])
                        BK.put(b)
                        yield
                        kb.tt("dve", R32[:], R32[:], cols[:, 99:103].unsqueeze(2).to_broadcast([128, 4, 64]), ALU.mult, ["R32", "cols"], ["R32"])
                        yield
                        kb.cp("act", Rbf[:, (t + 1) % 5, :, :], R32[:], ["R32"], [f"Rbf.{(t + 1) % 5}"])
                        yield
                    yield from rr([chunk_chain(0, blk, 0), chunk_chain(1, blk, 1)])
                    yield from rr([chunk_chain(0, blk, 2), chunk_chain(1, blk, 3)])

                run(gen_normA(0))
                interleave([gen_proj(0), gen_normA(1)])
                for blk in range(4):
                    if blk == 3 and "C" in stages:
                        Wk = Wv8(1824)
                        kb.dma("pool", Wk[:, :, :], wview(w_in_d)[:, :, 2048:3872], [], ["Wk", "W.0", "W.1", "W.2", "W.3"])
                    gs = [gen_chunks(blk)]
                    if blk + 1 < 4:
                        gs.append(gen_proj(blk + 1))
                    if blk + 2 < 4:
                        gs.append(gen_normA(blk + 2))
                    interleave(gs)
                if dbg:
                    kb.dma("sp", dbg_hT, hT[:], [f"hT.{b}" for b in range(4)], [])
                P.barrier()
                P.flush()

        if "C" in stages:
            stage_C(nc, P, kb, locals())
        else:
            P.op("dve", lambda e: e.memset(hT[:, 0:4, :], 0.0), [f"hT.{b}" for b in range(4)], [f"yT.{t}.1" for t in range(NT)])

        if dbg:
            kb.dma("sp", dbg_yT[:, 0:4, :], yT[:], [f"yT.{t}.0" for t in range(NT)], [])
            kb.dma("sp", dbg_yT[:, 4:8, :], hT[:, 0:4, :], [f"yT.{t}.1" for t in range(NT)], [])

        if "D" in stages:
            stage_D(nc, P, kb, locals())
        P.barrier()
        P.flush()
    return nc


def stage_C(nc, P, kb, L):
    cols, dc, cqf, hT, WA, mhalf, bonesb, BK = (L[k] for k in ("cols", "dc", "cqf", "hT", "WA", "mhalf", "bonesb", "L_BK"))
    wup_d, aup_d, gup_d, w_in_d = L["wup_d"], L["aup_d"], L["gup_d"], L["w_in_d"]
    mask4, maskLs, identf = L["mask4"], L["maskLs"], L["identf"]
    onesf = cqf[:, 896:1024]
    Wk = WA[:, 0:KC * 1824].rearrange("p (k n) -> p k n", k=KC)
    CL = 0.5 * math.exp(-0.5)
    NB = 256
    NBLK = T // NB
    with contextlib.ExitStack() as sc:
        if "B" not in L["stages"]:
            kb.dma("pool", Wk[:, :, :], w_in_d.rearrange("(kc p) n -> p kc n", p=128)[:, :, 2048:3872], [], ["Wk"])
        sb = lambda n, s, d: kb.sb(n, s, d, st=sc)
        wupz = sb("wupz", [128, 512], BF16)
        aupz = sb("aupz", [128, 512], BF16)
        gup1b = sb("gup1b", [128, 512], BF16)
        gup2z = sb("gup2z", [128, 512], BF16)
        for tz in (wupz, aupz, gup2z):
            P.op("dve", lambda e, tz=tz: e.memset(tz[:], 0.0), [], ["wupb", "gupb"])
        kb.dma("pool", wupz[0:64, :], wup_d, [], ["wupb"])
        kb.dma("pool", aupz[64:128, :], aup_d, [], ["wupb"])
        kb.dma("pool", gup1b[:], gup_d[0:128, :], [], ["gupb"])
        kb.dma("pool", gup2z[96:128, :], gup_d[128:160, :], [], ["gupb"])
        RKblk = sb("RKblk", [128, 4, 128], BF16)
        for ct in range(4):
            kb.ts("dve", RKblk[:, ct, :], cqf[:, 768:896], cols[:, 71 + ct:72 + ct], None, ALU.mult, None, ["cqf", "cols"], ["RKblk"])
        pT = sb("pT", [128, 15, NB + 1], F32)
        P.op("dve", lambda e: e.memset(pT[:], 0.0), [], [f"pT.{i}" for i in range(15)])
        carry = sb("carry", [128, 15], F32)
        NTMP = 10
        T_p = [[sb(f"T{c}_{i}", [128, NB], F32) for i in range(NTMP)] for c in range(2)]
        sh_p = [sb(f"shT{i}", [128, NB], F32) for i in range(2)]
        kk2_p = [sb(f"kk2{i}", [128, NB], BF16) for i in range(2)]
        twa = sb("twa", [128, NB], BF16)
        sg1 = sb("sg1", [128, NB], BF16)
        sg2 = sb("sg2", [128, NB], BF16)
        ARt = sb("ARt", [128, 4, 2, 2, 2, 128], BF16)
        P.op("dve", lambda e: e.memset(ARt[:], 0.0), [], [f"ARt.{i}" for i in range(4)])
        KT = sb("KT", [128, 4, NB], BF16)
        BnT = sb("BnT", [128, 4, NB], BF16)
        rkb = sb("rkb", [128, 4, NB], BF16)
        vbf_p = [sb(f"vbf{i}", [128, 4, NB], BF16) for i in range(2)]
        gT = sb("gT", [128, 4, NB], BF16)
        Ktok = sb("Ktok", [128, 2, 512], BF16)
        Btok = sb("Btok", [128, 2, 512], BF16)
        Vtok = sb("Vtok", [128, 2, 512], BF16)
        PCt = sb("PCt", [128, 4, 2], F32)
        MTs = [sb(f"MT{i}", [128, 8, 512], BF16) for i in range(2)]
        mraw_p = [sb(f"mraw{i}", [128, 512], BF16) for i in range(2)]
        mask4b = sb("mask4b", [128, 512], BF16)
        kb.dma("pool", mask4b[:], L["cq_d"][:, 128:640], [], ["mask4b"])
        Xm = [sb(f"Xm{i}", [128, 8, 128], BF16) for i in range(2)]
        Nm = [sb(f"Nm{i}", [128, 8, 128], BF16) for i in range(2)]
        Lm = [sb(f"Lm{i}", [128, 8, 128], BF16) for i in range(2)]
        TTs = [sb(f"TT{i}", [128, 8, 128], BF16) for i in range(2)]
        RHSs = [sb(f"RHSb{i}", [128, 512], BF16) for i in range(2)]
        Ubs = [sb(f"Ub{i}", [128, 512], BF16) for i in range(2)]
        S32 = sb("S32", [128, 4, 64], F32)
        Sbf = sb("Sbf", [128, 4, 64], BF16)
        P.op("dve", lambda e: e.memset(S32[:], 0.0), [], ["S32"])
        y32 = sb("cy32", [128, 512], F32)
        sq = sb("csq", [128, 512], F32)
        ynb = sb("cynb", [128, 512], BF16)
        yaff = sb("cyaff", [128, 4, 128], F32)
        t1b = sb("ct1", [128, 4, 128], F32)
        stt_ = sb("cstat", [128, 64], F32)

        def gen_AB(blk):
            tok0 = blk * NB
            hk = f"hT.{tok0 // 512}"
            for i0 in range(0, 15, 2):
                b = BK.get()
                bank, bk = BK.f32(b), BK.key(b)
                idxs = [i for i in (i0, i0 + 1) if i < 15]
                for j, idx in enumerate(idxs):
                    c0 = idx * 128 if idx < 14 else 1696
                    for kc in range(KC):
                        kb.mm(bank[:, j * NB:(j + 1) * NB], Wk[:, kc, c0:c0 + 128], hT[:, kc, tok0:tok0 + NB],
                              kc == 0, kc == KC - 1, [hk, "Wk"] + (["hTu"] if kc >= 4 else []), [bk])
                yield
                for j, idx in enumerate(idxs):
                    kb.cp("act", pT[:, idx, 1:NB + 1], bank[:, j * NB:(j + 1) * NB], [bk], [f"pT.{idx}"])
                BK.put(b)
                yield
            allp = [f"pT.{i}" for i in range(15)]
            kb.cp("dve", carry[:], pT[:, :, NB], allp, ["carry"])
            yield
            for idx in range(15):
                tmp, ktm = sh_p[idx % 2], f"shT.{idx % 2}"
                kb.act(tmp[:], pT[:, idx, 0:NB], AF.Copy, [f"pT.{idx}", "cols"], [ktm], scale=cols[:, 40 + idx:41 + idx])
                yield
                if 8 <= idx < 12:
                    kb.stt(vbf_p[blk % 2][:, idx - 8, :], pT[:, idx, 1:NB + 1], dc[:, idx:idx + 1], tmp[:], ALU.mult, ALU.add,
                           [f"pT.{idx}", "dc", ktm, "carry"], [f"vbf{blk % 2}.{idx - 8}"])
                else:
                    kb.stt(pT[:, idx, 1:NB + 1], pT[:, idx, 1:NB + 1], dc[:, idx:idx + 1], tmp[:], ALU.mult, ALU.add,
                           [f"pT.{idx}", "dc", ktm, "carry"], [f"pT.{idx}"])
                yield
            kb.cp("dve", pT[:, :, 0], carry[:], ["carry"], allp)
            yield

        ps = lambda idx: pT[:, idx, 1:NB + 1]

        def gen_lora(blk):
            kb.act(twa[0:64, :], pT[0:64, 12, 1:NB + 1], AF.Tanh, ["pT.12"], ["twa"])
            kb.cp("dve", twa[64:128, :], pT[64:128, 12, 1:NB + 1], ["pT.12"], ["twa"])
            yield
            th, kth = sh_p[0], "shT.0"
            kb.act(th[:], ps(13), AF.Tanh, ["pT.13"], [kth], scale=0.5)
            th2, kth2 = sh_p[1], "shT.1"
            kb.act(th2[:], ps(14), AF.Tanh, ["pT.14"], [kth2], scale=0.5)
            yield
            kb.ts("dve", sg1[:], th[:], 0.5, 0.5, ALU.mult, ALU.add, [kth], ["sg1"])
            kb.ts("pool", sg2[:], th2[:], 0.5, 0.5, ALU.mult, ALU.add, [kth2], ["sg2"])
            yield

        def gen_ct(ch, blk, ct):
            Tb = T_p[ch]
            tk = lambda i: f"T{ch}.{i}"
            cs = slice(ct * 128, (ct + 1) * 128)
            r_, k_, v_ = ps(ct), ps(4 + ct), ps(8 + ct)
            kr, kk_, kv = f"pT.{ct}", f"pT.{4 + ct}", f"pT.{8 + ct}"
            bz, bg = BK.get(), BK.get()
            bankZ, kz = BK.f32(bz), BK.key(bz)
            bankG, kg = BK.f32(bg), BK.key(bg)
            kb.mm(bankZ[:, 0:NB], wupz[:, cs], twa[:], True, True, ["wupb", "twa"], [kz])
            kb.mm(bankZ[:, NB:2 * NB], aupz[:, cs], twa[:], True, True, ["wupb", "twa"], [kz])
            kb.mm(bankG[:, 0:NB], gup1b[:, cs], sg1[:], True, False, ["gupb", "sg1"], [kg])
            kb.mm(bankG[:, 0:NB], gup2z[:, cs], sg2[:], False, True, ["gupb", "sg2"], [kg])
            yield
            thw, tha = Tb[0], Tb[1]
            kb.act(thw[:], bankZ[:, 0:NB], AF.Tanh, [kz, "dc"], [tk(0)], scale=0.5, bias=dc[:, 15 + ct:16 + ct])
            kb.act(tha[:], bankZ[:, NB:2 * NB], AF.Tanh, [kz, "dc"], [tk(1)], scale=0.5, bias=dc[:, 19 + ct:20 + ct])
            BK.put(bz)
            kk = Tb[4]
            kb.ts("pool", kk[:], k_, cols[:, 63 + ct:64 + ct], 0.0, ALU.mult, ALU.add, [kk_, "cols"], [tk(4)])
            yield
            kb.cp("act", gT[:, ct, :], bankG[:, 0:NB], [kg], [f"gT.{ct}"])
            BK.put(bg)
            kk2, k_kk2 = kk2_p[ch], f"kk2.{ch}"
            kb.act(kk2[:], kk[:], AF.Square, [tk(4)], [k_kk2])
            ld = Tb[0]
            kb.ts("dve", ld[:], thw[:], -CL, -CL, ALU.mult, ALU.add, [tk(0)], [tk(0)])
            yield
            bn_ = BK.get()
            bankN, kn = BK.f32(bn_), BK.key(bn_)
            kb.mm(bankN[:, 0:NB], bonesb[:], kk2[:], True, True, ["bonesb", k_kk2], [kn])
            cum = Tb[2]
            for c in range(2):
                ld_c, cum_c = ld[:, c * 128:(c + 1) * 128], cum[:, c * 128:(c + 1) * 128]
                P.op("dve", lambda e, cum_c=cum_c, ld_c=ld_c: e.tensor_tensor_scan(out=cum_c, data0=onesf, data1=ld_c, initial=0.0, op0=ALU.mult, op1=ALU.add),
                     [tk(0), "cqf"], [tk(2)])
            an = Tb[1]
            kb.ts("pool", an[:], tha[:], -0.5, -0.5, ALU.mult, ALU.add, [tk(1)], [tk(1)])
            yield
            sn = Tb[5]
            kb.act(sn[:], bankN[:, 0:NB], AF.Sqrt, [kn], [tk(5)])
            BK.put(bn_)
            cumex = Tb[3]
            kb.tt("pool", cumex[:], cum[:], ld[:], ALU.subtract, [tk(2), tk(0)], [tk(3)])
            yield
            ep, en, ex = Tb[6], Tb[7], Tb[8]
            kb.act(ep[:], cum[:], AF.Exp, [tk(2)], [tk(6)])
            kb.act(en[:], cum[:], AF.Exp, [tk(2)], [tk(7)], scale=-1.0)
            kb.ts("dve", sn[:], sn[:], 1e-12, None, ALU.max, None, [tk(5)], [tk(5)])
            tmp2 = Tb[9]
            kb.ts("pool", tmp2[:], an[:], dc[:, 23 + ct:24 + ct], dc[:, 27 + ct:28 + ct], ALU.mult, ALU.add, [tk(1), "dc"], [tk(9)])
            yield
            kb.act(ex[:], cumex[:], AF.Exp, [tk(3)], [tk(8)])
            P.op("dve", lambda e: e.reciprocal(out=sn[:], in_=sn[:]), [tk(5)], [tk(5)])
            k2 = Tb[9]
            kb.tt("pool", k2[:], k_, tmp2[:], ALU.mult, [kk_, tk(9)], [tk(9)])
            yield
            kkn = Tb[4]
            kb.tt("dve", kkn[:], kk[:], sn[:], ALU.mult, [tk(4), tk(5)], [tk(4)])
            c3 = lambda a: a.rearrange("p (c n) -> p c n", c=2)
            kb.cp("pool", PCt[:, ct, :], c3(ep[:])[:, :, 127], [tk(6)], [f"PCt.{ct}"])
            yield
            bn = Tb[5]
            kb.tt("pool", bn[:], kkn[:], an[:], ALU.mult, [tk(4), tk(1), tk(5)], [tk(5)])
            kb.tt("dve", KT[:, ct, :], k2[:], en[:], ALU.mult, [tk(9), tk(7)], [f"KT.{ct}"])
            yield
            for hh in range(2):
                pp = slice(64 * hh, 64 * hh + 64)
                kb.tt("dve", ARt[pp, ct, :, hh, 1, :], c3(r_)[pp], c3(ep[:])[pp], ALU.mult, [kr, tk(6)], [f"ARt.{ct}"])
                kb.tt("dve" if hh == 0 else "pool", ARt[pp, ct, :, hh, 0, :], c3(kkn[:])[pp], c3(ex[:])[pp], ALU.mult, [tk(4), tk(8)], [f"ARt.{ct}"])
                yield
            kb.tt("dve", BnT[:, ct, :], bn[:], en[:], ALU.mult, [tk(5), tk(7)], [f"BnT.{ct}"])
            kb.tt("pool", rkb[:, ct, :], r_, k2[:], ALU.mult, [kr, tk(9)], [f"rkb.{ct}"])
            yield

        def gen_tok(blk):
            for c in range(2):
                cs = slice(c * 128, (c + 1) * 128)
                for src, dst, nm, sk in ((KT, Ktok, "KT", "KT"), (BnT, Btok, "BnT", "BnT"), (vbf_p[blk % 2], Vtok, "vbf", f"vbf{blk % 2}")):
                    b = BK.get()
                    bankT, kt = BK.bf(b), BK.key(b)
                    for ct in range(4):
                        kb.tr(bankT[:, ct * 128:(ct + 1) * 128], src[:, ct, cs], [f"{sk}.{ct}"], [kt])
                    yield
                    kb.cpalt(dst[:, c, :], bankT[:, 0:512], [kt], [f"{nm}tok.{c}"])
                    BK.put(b)
                    yield

        def gen_F1(blk, c):
            cs = slice(c * 128, (c + 1) * 128)
            MT = MTs[c]
            for h in range(8):
                ct = h // 2
                b = BK.get()
                bankM, km = BK.f32(b), BK.key(b)
                rhsAR = ARt[:, ct, c, h % 2, :, :].rearrange("p a b -> p (a b)")
                kb.mm(bankM[:, 0:256], BnT[:, ct, cs], rhsAR, True, True, [f"BnT.{ct}", f"ARt.{ct}"], [km])
                kb.mm(bankM[:, 256:512], KT[:, ct, cs], rhsAR, True, True, [f"KT.{ct}", f"ARt.{ct}"], [km])
                yield
                if h % 2 == 0:
                    kb.tt("dve", MT[:, h, :], bankM, mask4, ALU.mult, [km, "cqf"], [f"MT{c}.{h}"])
                    BK.put(b)
                else:
                    mr, kmr = mraw_p[(h // 2) % 2], f"mraw.{(h // 2) % 2}"
                    kb.cp("act", mr[:], bankM, [km], [kmr])
                    BK.put(b)
                    yield
                    kb.tt("pool", MT[:, h, :], mr[:], mask4b[:], ALU.mult, [kmr, "mask4b"], [f"MT{c}.{h}"])
                yield
            for g4 in range(2):
                b = BK.get()
                bankL, kl = BK.f32(b), BK.key(b)
                for hh in range(4):
                    h = 4 * g4 + hh
                    kb.mm(bankL[:, hh * 128:(hh + 1) * 128], ARt[:, h // 2, c, h % 2, 0, :], BnT[:, h // 2, cs], True, True,
                          [f"ARt.{h // 2}", f"BnT.{h // 2}"], [kl])
                yield
                kb.tt("dve", Lm[0][:, 4 * g4:4 * g4 + 4, :], bankL.rearrange("p (c n) -> p c n", c=4),
                      maskLs.unsqueeze(1).to_broadcast([128, 4, 128]), ALU.mult, [kl, "cqf"], [f"Lm0.{g4}"])
                BK.put(b)
                hs = slice(4 * g4, 4 * g4 + 4)
                mtk = [f"MT{c}.{h}" for h in range(4 * g4, 4 * g4 + 4)]
                kb.cp("act", Nm[0][:, hs, :], MT[:, hs, 0:128], mtk, [f"Nm0.{g4}"])
                kb.tt("pool", Xm[0][:, hs, :], MT[:, hs, 0:128], identf.unsqueeze(1).to_broadcast([128, 4, 128]), ALU.add,
                      mtk + ["cqf"], [f"Xm0.{g4}"])
                yield

        def gen_inv_group(c, g4):
            hs = slice(4 * g4, 4 * g4 + 4)
            v4 = lambda bank: bank.rearrange("p (c n) -> p c n", c=4)
            cur = 0
            for lvl in range(7):
                nxt = cur ^ 1
                kN, kL, kX = f"Nm{cur}.{g4}", f"Lm{cur}.{g4}", f"Xm{cur}.{g4}"
                last = lvl == 6
                Xdst, kXd = (TTs[c], f"TT{c}.{g4}") if last else (Xm[nxt], f"Xm{nxt}.{g4}")
                bx = ba = bb = None
                if lvl >= 1:
                    bx = BK.get()
                    bankX, kbx = BK.f32(bx), BK.key(bx)
                    for hh in range(4):
                        h = 4 * g4 + hh
                        kb.mm(bankX[:, hh * 128:(hh + 1) * 128], Lm[cur][:, h, :], Xm[cur][:, h, :], True, True, [kL, kX], [kbx])
                if not last:
                    ba, bb = BK.get(), BK.get()
                    bankA, kba = BK.f32(ba), BK.key(ba)
                    bankB, kbb = BK.f32(bb), BK.key(bb)
                    for hh in range(4):
                        h = 4 * g4 + hh
                        kb.mm(bankA[:, hh * 128:(hh + 1) * 128], Lm[cur][:, h, :], Nm[cur][:, h, :], True, True, [kL, kN], [kba])
                    for hh in range(4):
                        h = 4 * g4 + hh
                        kb.mm(bankB[:, hh * 128:(hh + 1) * 128], Nm[cur][:, h, :], Lm[cur][:, h, :], True, True, [kL, kN], [kbb])
                yield
                if lvl >= 1:
                    kb.tt("dve", Xdst[:, hs, :], v4(bankX), Xm[cur][:, hs, :], ALU.add, [kbx, kX], [kXd])
                    BK.put(bx)
                else:
                    kb.cp("pool", Xdst[:, hs, :], Xm[cur][:, hs, :], [kX], [kXd])
                if not last:
                    kb.cp("act", Nm[nxt][:, hs, :], v4(bankA), [kba], [f"Nm{nxt}.{g4}"])
                    BK.put(ba)
                    if g4 == 0:
                        kb.cp("act", Lm[nxt][:, hs, :], v4(bankB), [kbb], [f"Lm{nxt}.{g4}"])
                    else:
                        kb.cp("dve", Lm[nxt][:, hs, :], v4(bankB), [kbb], [f"Lm{nxt}.{g4}"])
                    BK.put(bb)
                yield
                cur = nxt

        def gen_inv(blk, c):
            gs = [gen_inv_group(c, 0), gen_inv_group(c, 1)]
            while gs:
                for g in list(gs):
                    try:
                        next(g)
                    except StopIteration:
                        gs.remove(g)
                yield

        def gen_seqpost(blk, c):
            n_ch = blk * 2 + c
            cs = slice(c * 128, (c + 1) * 128)
            tok = slice(blk * NB + c * 128, blk * NB + (c + 1) * 128)
            MT, TT, RHSb, Ub = MTs[c], TTs[c], RHSs[c], Ubs[c]
            kTT = [f"TT{c}.0", f"TT{c}.1"]
            kRH, kUb = f"RHSb{c}", f"Ub{c}"
            br = BK.get()
            bankR, kr_ = BK.f32(br), BK.key(br)
            for h in range(8):
                ct = h // 2
                o = bankR[:, 64 * h:64 * h + 64]
                if n_ch > 0:
                    kb.mm(o, ARt[:, ct, c, h % 2, 0, :], Sbf[:, ct, :], True, False, [f"ARt.{ct}", "Sbf"], [kr_])
                kb.mm(o, MT[:, h, 256:384], Vtok[:, c, 64 * h:64 * h + 64], n_ch == 0, True, [f"MT{c}.{h}", f"vbftok.{c}"], [kr_])
            yield
            kb.cp("act", RHSb[:], bankR, [kr_], [kRH])
            BK.put(br)
            yield
            bu = BK.get()
            bankU, ku = BK.f32(bu), BK.key(bu)
            for h in range(8):
                kb.mm(bankU[:, 64 * h:64 * h + 64], TT[:, h, :], RHSb[:, 64 * h:64 * h + 64], True, True, kTT + [kRH], [ku])
            yield
            kb.cp("dve", Ub[:], bankU, [ku], [kUb])
            BK.put(bu)
            yield
            if n_ch < NT - 1:
                bs_ = BK.get()
                bankS, ks_ = BK.f32(bs_), BK.key(bs_)
                for ct in range(4):
                    o = bankS[:, ct * 128:(ct + 1) * 128]
                    kb.mm(o, Btok[:, c, ct * 128:(ct + 1) * 128], Ub[:, ct * 128:(ct + 1) * 128], True, False, [f"BnTtok.{c}", kUb], [ks_])
                    kb.mm(o, Ktok[:, c, ct * 128:(ct + 1) * 128], Vtok[:, c, ct * 128:(ct + 1) * 128], False, True, [f"KTtok.{c}", f"vbftok.{c}"], [ks_])
            by = BK.get()
            bankY, ky = BK.f32(by), BK.key(by)
            for h in range(8):
                ct = h // 2
                o = bankY[:, 64 * h:64 * h + 64]
                if n_ch > 0:
                    kb.mm(o, ARt[:, ct, c, h % 2, 1, :], Sbf[:, ct, :], True, False, [f"ARt.{ct}", "Sbf"], [ky])
                kb.mm(o, MT[:, h, 128:256], Ub[:, 64 * h:64 * h + 64], n_ch == 0, False, [f"MT{c}.{h}", kUb], [ky])
                kb.mm(o, MT[:, h, 384:512], Vtok[:, c, 64 * h:64 * h + 64], False, True, [f"MT{c}.{h}", f"vbftok.{c}"], [ky])
            yield
            if n_ch < NT - 1:
                for hh in range(2):
                    b0 = 64 * hh
                    kb.tt("dve", S32[b0:b0 + 64, :, :], bankS[b0:b0 + 64, :].rearrange("p (c n) -> p c n", c=4)[:, :, b0:b0 + 64],
                          S32[b0:b0 + 64, :, :], ALU.add, [ks_, "S32"], ["S32"])
                BK.put(bs_)
                kb.tt("dve", S32[:], S32[:], PCt[:, :, c].unsqueeze(2).to_broadcast([128, 4, 64]), ALU.mult,
                      ["S32"] + [f"PCt.{ct}" for ct in range(4)], ["S32"])
                kb.cp("act", Sbf[:], S32[:], ["S32"], ["Sbf"])
                yield
            k1, k2, ks, k3 = "cy32", "csq", "cstat", "cynb"
            kb.cp("act", y32[:], bankY, [ky], [k1])
            BK.put(by)
            yield
            v3 = lambda a: a.rearrange("p (h d) -> p h d", d=64)
            bc = lambda a: a.unsqueeze(2).to_broadcast([128, 8, 64])
            kb.red(stt_[:, 0:8], v3(y32[:]), ALU.add, [k1], [ks])
            kb.act(sq[:], y32[:], AF.Square, [k1], [k2])
            yield
            kb.red(stt_[:, 8:16], v3(sq[:]), ALU.add, [k2], [ks])
            yield
            kb.ts("dve", stt_[:, 16:24], stt_[:, 0:8], 1.0 / 64, None, ALU.mult, None, [ks], [ks])
            yield
            kb.tt("dve", stt_[:, 24:32], stt_[:, 16:24], stt_[:, 16:24], ALU.mult, [ks], [ks])
            kb.ts("dve", stt_[:, 32:40], stt_[:, 8:16], 1.0 / 64, 64e-5, ALU.mult, ALU.add, [ks], [ks])
            yield
            kb.tt("dve", stt_[:, 40:48], stt_[:, 32:40], stt_[:, 24:32], ALU.subtract, [ks], [ks])
            yield
            kb.tt("pool", stt_[:, 48:56], stt_[:, 40:48], mhalf[:, 0:8], ALU.pow, [ks, "mhalf"], [ks])
            yield
            kb.stt(stt_[:, 56:64], stt_[:, 16:24], -1.0, stt_[:, 48:56], ALU.mult, ALU.mult, [ks], [ks])
            kb.tt("pool", v3(y32[:]), v3(y32[:]), bc(stt_[:, 48:56]), ALU.mult, [k1, ks], [k1])
            yield
            kb.tt("dve", v3(ynb[:]), v3(y32[:]), bc(stt_[:, 56:64]), ALU.add, [k1, ks], [k3])
            yield
            b2 = BK.get()
            bankT, kt = BK.bf(b2), BK.key(b2)
            for ct in range(4):
                kb.tr(bankT[:, ct * 128:(ct + 1) * 128], ynb[:, ct * 128:(ct + 1) * 128], [k3], [kt])
            bbo = BK.get()
            bankBo, kbo = BK.f32(bbo), BK.key(bbo)
            for ct in range(4):
                kb.mm(bankBo[:, ct * 128:(ct + 1) * 128], RKblk[:, ct, :], rkb[:, ct, cs], True, True, ["RKblk", f"rkb.{ct}"], [kbo])
            yield
            for ct in range(4):
                kb.act(yaff[:, ct, :], bankT[:, ct * 128:(ct + 1) * 128], AF.Identity, [kt, "cols"], ["cyaff"],
                       scale=cols[:, 75 + ct:76 + ct], bias=cols[:, 79 + ct:80 + ct])
            kb.tt("dve", t1b[:], bankBo.rearrange("p (c n) -> p c n", c=4), vbf_p[blk % 2][:, :, cs], ALU.mult,
                  [kbo] + [f"vbf{blk % 2}.{ct}" for ct in range(4)], ["ct1"])
            BK.put(b2)
            BK.put(bbo)
            yield
            kb.tt("pool", t1b[:], t1b[:], yaff[:], ALU.add, ["ct1", "cyaff"], ["ct1"])
            yield
            kb.tt("dve", hT[:, 0:4, tok], t1b[:], gT[:, :, cs], ALU.mult, ["ct1"] + [f"gT.{ct}" for ct in range(4)],
                  [f"yT.{n_ch}.1", f"hT.{n_ch // 4}"])
            yield

        def gen_CD(blk):
            yield from gen_lora(blk)
            for pair in range(2):
                gs = [gen_ct(0, blk, 2 * pair), gen_ct(1, blk, 2 * pair + 1)]
                while gs:
                    for g in list(gs):
                        try:
                            next(g)
                        except StopIteration:
                            gs.remove(g)
                    yield
            yield from gen_tok(blk)

        def run(g):
            for _ in g:
                pass

        run(gen_AB(0))
        run(gen_CD(0))
        for blk in range(NBLK):
            run(gen_F1(blk, 0))
            interleave([gen_inv(blk, 0), gen_F1(blk, 1)])
            interleave([gen_seqpost(blk, 0), gen_inv(blk, 1)])
            if blk + 1 < NBLK:
                interleave([gen_seqpost(blk, 1), gen_AB(blk + 1)])
                if blk + 1 == NBLK - 1 and "D" in L["stages"]:
                    wv = lambda w: w.rearrange("(kc p) n -> p kc n", p=128)
                    Wout = hT[:, 4:8, :].rearrange("p a (b n) -> p (a b) n", n=1024)
                    kb.dma("pool", Wout, wv(L["w_out_d"]), [], ["Wout", "hTu"])
                    Wkv = WA[:, 0:16384].rearrange("p (k n) -> p k n", k=KC)
                    for hf in range(2):
                        kb.dma("pool", Wkv[:, :, hf * 1024:(hf + 1) * 1024], wv(L["wkv_d"])[:, :, hf * 1024:(hf + 1) * 1024], [], ["WA.0", "WA.1", "Wk"])
                run(gen_CD(blk + 1))
            else:
                run(gen_seqpost(blk, 1))
        P.barrier()
        P.flush()


def stage_D(nc, P, kb, L):
    cols, cqf, hT, yT, WA, mhalf, onesb, ss, BK = (L[k] for k in ("cols", "cqf", "hT", "yT", "WA", "mhalf", "onesb", "ss", "L_BK"))
    x_d, mem_d, gfin_d, out_d = L["x_d"], L["mem_d"], L["gfin_d"], L["out_d"]
    w_out_d, wq_d, wkv_d, wo_d, wupm_d, wdn_d = (L[k] for k in ("w_out_d", "wq_d", "wkv_d", "wo_d", "wupm_d", "wdn_d"))
    wview = lambda w: w.rearrange("(kc p) n -> p kc n", p=128)

    def run(g):
        for _ in g:
            pass

    with contextlib.ExitStack() as sd:
        sb = lambda n, s, d, st=sd: kb.sb(n, s, d, st=st)
        xres = sb("xres", [128, NT, D], F32)
        gfin = sb("gfin", [128, D], F32)
        kTm = sb("kTm", [128, KC, 256], BF16)
        vmem = sb("vmem", [128, 2, D], BF16)
        kmx = sb("kmx", [128, 4], F32)
        hb_p = [sb(f"dhb{i}", [128, D], BF16) for i in range(4)]
        sqj = sb("dsqj", [128, D], BF16)
        Wkv = WA[:, 0:16384].rearrange("p (k n) -> p k n", k=KC)
        W0 = WA[:, 0:8192].rearrange("p (k n) -> p k n", k=KC)
        W1 = WA[:, 8192:16384].rearrange("p (k n) -> p k n", k=KC)
        Wout = hT[:, 4:8, :].rearrange("p a (b n) -> p (a b) n", n=1024)
        xv = x_d.rearrange("(t p) d -> p t d", p=128)
        kb.dma("sp", xres[:, 0:4, :], xv[:, 0:4, :], [], [f"xres.{t}" for t in range(0, 4)])
        if "C" not in L["stages"]:
            kb.dma("pool", Wout, wview(w_out_d), [], ["Wout"])
            for hf in range(2):
                kb.dma("pool", Wkv[:, :, hf * 1024:(hf + 1) * 1024], wview(wkv_d)[:, :, hf * 1024:(hf + 1) * 1024], [], ["WA.0", "WA.1"])
        kb.dma("sp", gfin[:], gfin_d.partition_broadcast(128), [], ["gfin"])

        def gen_norm(tiles, src, gcol0, dst_fn, dkey, sscol0):
            for tl, t in enumerate(tiles):
                xa, xk = src(t)
                c = sscol0 + t
                kb.act(sqj[:], xa, AF.Square, [xk], ["sqj", f"ss{c}"], accum=ss[:, c:c + 1])
                yield
                kb.ts("dve", ss[:, c:c + 1], ss[:, c:c + 1], 1.0 / D, 1e-6, ALU.mult, ALU.add, [f"ss{c}"], [f"ss{c}"])
                yield
                kb.tt("pool", ss[:, c:c + 1], ss[:, c:c + 1], mhalf[:, 0:1], ALU.pow, [f"ss{c}", "mhalf"], [f"ss{c}"])
                yield
                kb.act(hb_p[tl][:], xa, AF.Copy, [xk, f"ss{c}"], [f"dhb.{tl}"], scale=ss[:, c:c + 1])
                yield
            n = len(tiles) * 128
            for kc in range(KC):
                b = BK.get()
                bankT, bk = BK.bf(b), BK.key(b)
                for tl in range(len(tiles)):
                    kb.tr(bankT[:, tl * 128:(tl + 1) * 128], hb_p[tl][:, kc * 128:(kc + 1) * 128], [f"dhb.{tl}"], [bk])
                yield
                o = dst_fn(kc)
                if kc % 2:
                    kb.act(o, bankT[:, 0:n], AF.Copy, [bk, "cols"], [dkey], scale=cols[:, gcol0 + kc:gcol0 + kc + 1])
                else:
                    kb.ts("dve", o, bankT[:, 0:n], cols[:, gcol0 + kc:gcol0 + kc + 1], None, ALU.mult, None, [bk, "cols"], [dkey])
                BK.put(b)
                yield

        with contextlib.ExitStack() as s0:
            memx = [sb(f"memx{i}", [128, D], F32, st=s0) for i in range(2)]
            memT = sb("memT", [128, KC, 256], BF16, st=s0)
            k2m = sb("k2m", [128, KC, 256], BF16, st=s0)
            for i in range(2):
                kb.dma("sp", memx[i][:], mem_d[i * 128:(i + 1) * 128, :], [], [f"memx.{i}"])
            for i in range(1, 4):
                kb.dma("sp", xres[:, 4 * i:4 * i + 4, :], xv[:, 4 * i:4 * i + 4, :], [], [f"xres.{t}" for t in range(4 * i, 4 * i + 4)])

            def gen_memkv():
                yield from gen_norm([0, 1], lambda t: (memx[t][:], f"memx.{t}"), 16, lambda kc: memT[:, kc, :], "memT", 16)
                for ft in range(KC):
                    b = BK.get()
                    bank, bk = BK.f32(b), BK.key(b)
                    for kc in range(KC):
                        kb.mm(bank[:, 0:256], Wkv[:, kc, ft * 128:(ft + 1) * 128], memT[:, kc, :], kc == 0, kc == KC - 1, ["WA.0", "memT"], [bk])
                    yield
                    kb.cpalt(kTm[:, ft, :], bank[:, 0:256], [bk], ["kTm"])
                    BK.put(b)
                    yield
                for mt in range(2):
                    for hf in range(2):
                        b = BK.get()
                        bank, bk = BK.f32(b), BK.key(b)
                        for kc in range(KC):
                            kb.mm(bank, memT[:, kc, mt * 128:(mt + 1) * 128], Wkv[:, kc, 1024 + hf * 512:1024 + (hf + 1) * 512],
                                  kc == 0, kc == KC - 1, ["WA.1", "memT"], [bk])
                        yield
                        kb.cpalt(vmem[:, mt, hf * 512:(hf + 1) * 512], bank, [bk], ["vmem"])
                        BK.put(b)
                        yield
                kb.act(k2m[:], kTm[:], AF.Square, ["kTm"], ["k2m"])
                yield
                for h in range(4):
                    b = BK.get()
                    bank, bk = BK.f32(b), BK.key(b)
                    kb.mm(bank[:, 0:256], onesb[:], k2m[:, 2 * h, :], True, False, ["onesb", "k2m"], [bk])
                    kb.mm(bank[:, 0:256], onesb[:], k2m[:, 2 * h + 1, :], False, True, ["onesb", "k2m"], [bk])
                    yield
                    kb.red(kmx[:, h:h + 1], bank[:, 0:256], ALU.max, [bk], ["kmx"])
                    BK.put(b)
                    yield

            def gen_wout():
                for t in range(NT):
                    tok = slice(t * 128, (t + 1) * 128)
                    for hf in range(2):
                        b = BK.get()
                        bank, bk = BK.f32(b), BK.key(b)
                        for kc in range(KC):
                            ysrc = yT[:, kc, tok] if kc < 4 else hT[:, kc - 4, tok]
                            kb.mm(bank, ysrc, Wout[:, kc, hf * 512:(hf + 1) * 512], kc == 0, kc == KC - 1,
                                  [f"yT.{t}.0", f"yT.{t}.1", "Wout"], [bk])
                        yield
                        xs = xres[:, t, hf * 512:(hf + 1) * 512]
                        kb.tt("dve", xs, bank, xs, ALU.add, [bk, f"xres.{t}"], [f"xres.{t}"])
                        BK.put(b)
                        yield

            interleave([gen_memkv(), gen_wout()])
            kb.dma("pool", W1, wview(wq_d), [], ["WA.0", "WA.1"])
            kb.dma("pool", W0, wview(wo_d), [], ["WA.0", "WA.1"])
            P.barrier()
            P.flush()

        srcx = lambda t: (xres[:, t, :], f"xres.{t}")
        norm_x = lambda blk: gen_norm(list(range(4 * blk, 4 * blk + 4)), srcx, 8, lambda kc: hT[:, kc, blk * 512:(blk + 1) * 512], f"hT.{blk}", 0)
        norm_m = lambda blk: gen_norm(list(range(4 * blk, 4 * blk + 4)), srcx, 24, lambda kc: hT[:, kc, blk * 512:(blk + 1) * 512], f"hT.{blk}", 16)
        with contextlib.ExitStack() as s2:
            qTb = yT[:, 0:2, :].rearrange("p a (b n) -> p (a b) n", n=512)
            PT = yT[:, 2:4, :].rearrange("p a (b n) -> p (a b) n", n=512)
            oTb_t = sb("oTb", [128, KC * 512], BF16, st=s2)
            oTb = oTb_t[:].rearrange("p (k n) -> p k n", k=KC)
            q2b_p = [sb(f"q2b{i}", [128, 2, 512], BF16, st=s2) for i in range(4)]
            nb_p = [sb(f"nb{i}", [128, 2], F32, st=s2) for i in range(4)]
            rc_p = [sb(f"rc{i}", [128, 512], F32, st=s2) for i in range(4)]

            def gen_head(sl, h):
                qk = [f"qTb.{2 * h}", f"qTb.{2 * h + 1}"]
                q2b, kq2 = q2b_p[sl], f"q2b.{sl}"
                kb.act(q2b[:], qTb[:, 2 * h:2 * h + 2, :], AF.Square, qk, [kq2])
                yield
                b = BK.get()
                bankQ, kq = BK.f32(b), BK.key(b)
                kb.mm(bankQ, onesb[:], q2b[:, 0, :], True, False, ["onesb", kq2], [kq])
                kb.mm(bankQ, onesb[:], q2b[:, 1, :], False, True, ["onesb", kq2], [kq])
                yield
                nb, kbs = nb_p[sl], f"nb.{sl}"
                kb.red(nb[:, 0:1], bankQ, ALU.max, [kq], [kbs])
                BK.put(b)
                yield
                kb.ts("dve", nb[:, 1:2], nb[:, 0:1], kmx[:, h:h + 1], -1.0 / 32, ALU.add, ALU.mult, [kbs, "kmx"], [kbs])
                yield
                for mt in range(2):
                    b = BK.get()
                    bankS, ksb = BK.f32(b), BK.key(b)
                    ms = slice(mt * 128, (mt + 1) * 128)
                    kb.mm(bankS, kTm[:, 2 * h, ms], qTb[:, 2 * h, :], True, False, ["kTm", qk[0]], [ksb])
                    kb.mm(bankS, kTm[:, 2 * h + 1, ms], qTb[:, 2 * h + 1, :], False, True, ["kTm", qk[1]], [ksb])
                    yield
                    kb.act(PT[:, 2 * h + mt, :], bankS, AF.Exp, [ksb, kbs], [f"PT.{2 * h + mt}"], scale=1.0 / 16, bias=nb[:, 1:2])
                    BK.put(b)
                    yield
                pk = [f"PT.{2 * h}", f"PT.{2 * h + 1}"]
                b = BK.get()
                bankRs, krs = BK.f32(b), BK.key(b)
                kb.mm(bankRs, onesb[:], PT[:, 2 * h, :], True, False, ["onesb", pk[0]], [krs])
                kb.mm(bankRs, onesb[:], PT[:, 2 * h + 1, :], False, True, ["onesb", pk[1]], [krs])
                yield
                rc, krc = rc_p[sl], f"rc.{sl}"
                P.op("dve", lambda e: e.reciprocal(out=rc[:], in_=bankRs), [krs], [krc])
                BK.put(b)
                yield
                for dt_ in range(2):
                    ft = 2 * h + dt_
                    b = BK.get()
                    bankO, kob = BK.f32(b), BK.key(b)
                    kb.mm(bankO, vmem[:, 0, ft * 128:(ft + 1) * 128], PT[:, 2 * h, :], True, False, ["vmem", pk[0]], [kob])
                    kb.mm(bankO, vmem[:, 1, ft * 128:(ft + 1) * 128], PT[:, 2 * h + 1, :], False, True, ["vmem", pk[1]], [kob])
                    yield
                    kb.tt("dve", oTb[:, ft, :], bankO, rc[:], ALU.mult, [kob, krc], [f"oTb.{ft}"])
                    BK.put(b)
                    yield

            def gen_attn(blk):
                bs = slice(blk * 512, (blk + 1) * 512)
                for ft in range(KC):
                    b = BK.get()
                    bank, bk = BK.f32(b), BK.key(b)
                    for kc in range(KC):
                        kb.mm(bank, W1[:, kc, ft * 128:(ft + 1) * 128], hT[:, kc, bs], kc == 0, kc == KC - 1, ["WA.1", f"hT.{blk}"], [bk])
                    yield
                    kb.cpalt(qTb[:, ft, :], bank, [bk], [f"qTb.{ft}"])
                    BK.put(b)
                    yield
                if blk == 3:
                    mlp_load(0)
                for pair in range(1):
                    gs = [gen_head(h, h) for h in range(4)]
                    while gs:
                        for g in list(gs):
                            try:
                                next(g)
                            except StopIteration:
                                gs.remove(g)
                        yield
                for tl in range(4):
                    t = blk * 4 + tl
                    for hf in range(2):
                        b = BK.get()
                        bank, bk = BK.f32(b), BK.key(b)
                        for kc in range(KC):
                            kb.mm(bank, oTb[:, kc, tl * 128:(tl + 1) * 128], W0[:, kc, hf * 512:(hf + 1) * 512], kc == 0, kc == KC - 1,
                                  [f"oTb.{kc}", "WA.0"], [bk])
                        yield
                        xs = xres[:, t, hf * 512:(hf + 1) * 512]
                        kb.tt("dve", xs, bank, xs, ALU.add, [bk, f"xres.{t}"], [f"xres.{t}"])
                        BK.put(b)
                        yield

            def norm_worker(blk):
                if blk + 1 < 4:
                    yield from norm_x(blk + 1)
                if blk - 1 >= 0:
                    yield from norm_m(blk - 1)

            s3 = s2
            aT_all = sb("aT_all", [128, 8, 512], BF16, st=s3)[:]
            rl_p = [sb(f"rl{i}", [128, 512], BF16, st=s3) for i in range(2)]
            o_p = [oTb_t[:, 2048 * i:2048 * (i + 1)].bitcast(F32) for i in range(2)]
            upv = wupm_d.rearrange("(kc p) n -> p kc n", p=128)
            dnv = wdn_d.rearrange("(f p) n -> p f n", p=128)
            ov = out_d.rearrange("(t p) d -> p t d", p=128)

            def mlp_w(j):
                s = (j + 1) % 2
                Wu = WA[:, s * 8192:s * 8192 + 4096].rearrange("p (k n) -> p k n", k=KC)
                Wd = WA[:, s * 8192 + 4096:(s + 1) * 8192].rearrange("p (f n) -> p f n", f=4)
                return s, Wu, Wd

            def mlp_load(j):
                s, Wu, Wd = mlp_w(j)
                kb.dma("pool", Wu, upv[:, :, j * 512:(j + 1) * 512], [], [f"WA.{s}"])
                kb.dma("pool", Wd, dnv[:, 4 * j:4 * j + 4, :], [], [f"WA.{s}"])

            run(norm_x(0))
            for blk in range(4):
                interleave([gen_attn(blk), norm_worker(blk)])
            run(norm_m(3))

            NJ = 8
            ai = 0
            ri = 0
            for j in range(NJ):
                s, Wu, Wd = mlp_w(j)
                if j > 0:
                    mlp_load(j)
                for blk in range(4):
                    bs = slice(blk * 512, (blk + 1) * 512)
                    ab = (ai % 2) * 4
                    ai += 1
                    for fft in range(4):
                        b = BK.get()
                        bank, bk = BK.f32(b), BK.key(b)
                        for kc in range(KC):
                            kb.mm(bank, Wu[:, kc, fft * 128:(fft + 1) * 128], hT[:, kc, bs], kc == 0, kc == KC - 1, [f"WA.{s}", f"hT.{blk}"], [bk])
                        rl, krl = rl_p[ri % 2], f"rl.{ri % 2}"
                        ri += 1
                        kb.act(rl[:], bank, AF.Relu, [bk], [krl])
                        BK.put(b)
                        kb.tt("dve", aT_all[:, ab + fft, :], rl[:], rl[:], ALU.mult, [krl], [f"aT.{ab + fft}"])
                    for tl in range(4):
                        t = blk * 4 + tl
                        for hf in range(2):
                            b = BK.get()
                            bank, bk = BK.f32(b), BK.key(b)
                            for fft in range(4):
                                kb.mm(bank, aT_all[:, ab + fft, tl * 128:(tl + 1) * 128], Wd[:, fft, hf * 512:(hf + 1) * 512], fft == 0, fft == 3,
                                      [f"aT.{ab + fft}", f"WA.{s}"], [bk])
                            xs = xres[:, t, hf * 512:(hf + 1) * 512]
                            kb.tt("dve", xs, bank, xs, ALU.add, [bk, f"xres.{t}"], [f"xres.{t}"])
                            BK.put(b)
                        if j == NJ - 1:
                            c = 32 + t
                            kb.act(sqj[:], xres[:, t, :], AF.Square, [f"xres.{t}"], ["sqj", f"ss{c}"], accum=ss[:, c:c + 1])
                            kb.ts("dve", ss[:, c:c + 1], ss[:, c:c + 1], 1.0 / D, 1e-6, ALU.mult, ALU.add, [f"ss{c}"], [f"ss{c}"])
                            kb.tt("pool", ss[:, c:c + 1], ss[:, c:c + 1], mhalf[:, 0:1], ALU.pow, [f"ss{c}", "mhalf"], [f"ss{c}"])
                            ot, ko = o_p[t % 2], f"ot.{t % 2}"
                            kb.stt(ot, xres[:, t, :], ss[:, c:c + 1], gfin[:], ALU.mult, ALU.mult, [f"xres.{t}", f"ss{c}", "gfin"],
                                   [ko] + [f"oTb.{k}" for k in range(KC)])
                            kb.dma("sp", ov[:, t, :], ot, [ko], [f"out.{t}"])
            P.barrier()
            P.flush()


def _consts():
    cq = np.zeros((128, NCQ), np.float32)
    i = np.arange(128)
    cq[:, 0:128] = np.eye(128, dtype=np.float32)
    strict = (i[:, None] < i[None, :]).astype(np.float32)
    incl = (i[:, None] <= i[None, :]).astype(np.float32)
    cq[:, 128:256] = strict
    cq[:, 256:384] = incl
    cq[:, 384:512] = strict
    cq[:, 512:640] = incl
    cq[:, 640:768] = (i[:, None] > i[None, :]).astype(np.float32)
    cq[:, 768:896] = ((i[:, None] // 64) == (i[None, :] // 64)).astype(np.float32)
    cq[:, 896:1024] = 1.0
    gam = 1.0 - 2.0 ** (-5.0 - np.arange(8, dtype=np.float64))
    xi = gam[None, :] ** (i[:, None] + 1.0)
    kz = 0.125 * gam[None, :] ** (-(i[:, None] + 1.0))
    gC = np.zeros((128, 4))
    for ct in range(4):
        for p in range(128):
            gC[p, ct] = gam[2 * ct + p // 64] ** 128.0
    invf = (10000.0 ** (-np.arange(32, dtype=np.float32) / 32.0)).astype(np.float32)
    return cq, xi.astype(np.float32), kz.astype(np.float32), gC.astype(np.float32), np.broadcast_to(invf[None], (128, 32))


def _col(v):
    v = np.asarray(v, np.float32).reshape(-1)
    return v.reshape(-1, 128).T


def make_inputs(inp):
    cq, xi, kz, gC, invf = _consts()
    cols = np.zeros((128, NCOLS), np.float32)
    cols[:, 0:8] = _col(inp["norm_mix"][0])
    cols[:, 8:16] = _col(inp["norm_xattn"][0])
    cols[:, 16:24] = _col(inp["norm_mem"][0])
    cols[:, 24:32] = _col(inp["norm_mlp"][0])
    cols[:, 32:36] = _col(inp["ret_gn_w"][0])
    cols[:, 36:40] = _col(inp["ret_gn_b"][0])
    mu = np.asarray(inp["rwkv_mu"][0], np.float32)
    cols[:, 40:54] = _col(mu[:1792])
    cols[:, 54] = mu[1696:1824]
    cols[:, 55:59] = _col(inp["rwkv_w0"][0])
    cols[:, 59:63] = _col(inp["rwkv_a0"][0])
    cols[:, 63:67] = _col(inp["rwkv_k_k"][0])
    cols[:, 67:71] = _col(inp["rwkv_k_a"][0])
    cols[:, 71:75] = _col(inp["rwkv_r_k"][0])
    cols[:, 75:79] = _col(inp["rwkv_gn_w"][0])
    cols[:, 79:83] = _col(inp["rwkv_gn_b"][0])
    cols[:, 83:91] = xi
    cols[:, 91:99] = kz
    cols[:, 99:103] = gC
    cols[:, 103:135] = invf
    shared = {
        "cols": cols, "cq": cq,
        "gfin": np.ascontiguousarray(inp["norm_final"], np.float32),
        "w_in": np.ascontiguousarray(inp["w_in"][0]),
        "w_up": np.ascontiguousarray(inp["rwkv_w_up"][0]),
        "a_up": np.ascontiguousarray(inp["rwkv_a_up"][0]),
        "g_up": np.ascontiguousarray(inp["rwkv_g_up"][0]),
        "w_out": np.ascontiguousarray(inp["w_out"][0]),
        "wq": np.ascontiguousarray(inp["xattn_w_q"][0]),
        "wkv": np.ascontiguousarray(inp["xattn_w_kv"][0]),
        "wo": np.ascontiguousarray(inp["xattn_w_o"][0]),
        "mlp_up": np.ascontiguousarray(inp["mlp_w_up"][0]),
        "mlp_down": np.ascontiguousarray(inp["mlp_w_down"][0]),
    }
    maps = []
    for b in range(8):
        m = dict(shared)
        m["x"] = np.ascontiguousarray(inp["x"][b], np.float32)
        m["mem"] = np.ascontiguousarray(inp["mem"][b], np.float32)
        m["pos"] = np.ascontiguousarray(np.asarray(inp["positions"][b], np.int32).reshape(NT, 128).T)
        maps.append(m)
    return maps


_NC_CACHE = {}


def kernel(**inputs):
    inp = {k: np.asarray(v) for k, v in inputs.items()}
    maps = make_inputs(inp)
    if "nc" not in _NC_CACHE:
        _NC_CACHE["nc"] = build()
    res = run_bass_kernel_spmd(_NC_CACHE["nc"], maps, core_ids=list(range(8)))
    return np.stack([np.asarray(r["out"], np.float32) for r in res.results], axis=0)
```

```python
import contextlib
import math
import numpy as np
import concourse.bass as bass
import concourse.mybir as mybir
from concourse.bass_utils import run_bass_kernel_spmd

F32 = mybir.dt.float32
BF16 = mybir.dt.bfloat16
I32 = mybir.dt.int32
AF = mybir.ActivationFunctionType
ALU = mybir.AluOpType
AX = mybir.AxisListType

T = 2048
D = 1024
NT = 16
KC = 8
NCOLS = 135
NCQ = 1024
CUT = 99
COMPUTE = ("pe", "act", "dve", "pool")
ISSUERS = ("pe", "act", "dve", "pool", "sp")


class Prog:
    def __init__(self, nc, st, n_dma=12, self_sync=True):
        self.nc = nc
        self.n_dma = n_dma
        self.self_sync = self_sync
        self.chans = list(COMPUTE) + [f"d{i}" for i in range(n_dma)]
        self.sems = {c: st.enter_context(nc.semaphore(f"s_{c}")) for c in self.chans}
        self.streams = {e: [] for e in ISSUERS}
        self.count = {c: 0 for c in self.chans}
        self.clock = {e: {} for e in ISSUERS}
        self.snap = {}
        self.wr = {}
        self.rd = {}
        self.rr = 0
        self.nops = 0
        self.nwaits = 0

    @staticmethod
    def _need(needs, d):
        for c, n in d.items():
            if needs.get(c, 0) < n:
                needs[c] = n

    def op(self, eng, fn, reads=(), writes=(), dma=False):
        needs = {}
        for k in reads:
            self._need(needs, self.wr.get(k, {}))
        for k in writes:
            self._need(needs, self.wr.get(k, {}))
            self._need(needs, self.rd.get(k, {}))
        if dma:
            chan = f"d{self.rr % self.n_dma}"
            self.rr += 1
            if self.count[chan]:
                self._need(needs, {chan: self.count[chan]})
        else:
            chan = eng
        clk = self.clock[eng]
        waits = []
        for c, n in needs.items():
            if c == eng and (eng == "pe" or not self.self_sync):
                continue
            if clk.get(c, 0) < n:
                waits.append((c, n))
                for c2, n2 in self.snap[(c, n)].items():
                    if clk.get(c2, 0) < n2:
                        clk[c2] = n2
        self.nwaits += len(waits)
        self.nops += 1
        n_new = self.count[chan] + 1
        self.count[chan] = n_new
        s = dict(clk)
        s[chan] = n_new
        self.snap[(chan, n_new)] = s
        if eng == "pe" and chan == "pe":
            clk["pe"] = n_new
        wset = set(writes)
        for k in wset:
            self.wr[k] = {chan: n_new}
            self.rd[k] = {}
        for k in reads:
            if k in wset:
                continue
            self.rd.setdefault(k, {})[chan] = n_new
        self.streams[eng].append((waits, fn, chan))

    def barrier(self):
        allk = {c: n for c, n in self.count.items() if n}
        for e in ISSUERS:
            clk = self.clock[e]
            waits = []
            for c, n in allk.items():
                if c == e and e == "pe":
                    continue
                if clk.get(c, 0) < n:
                    waits.append((c, n))
            if waits:
                self.streams[e].append((waits, None, None))
        for e in ISSUERS:
            for c, n in allk.items():
                if self.clock[e].get(c, 0) < n:
                    self.clock[e][c] = n

    def flush(self):
        nc = self.nc
        streams = self.streams
        if not any(streams[e] for e in ISSUERS):
            return
        self.streams = {e: [] for e in ISSUERS}
        sems = self.sems

        def val(c, n):
            return n * 16 if c.startswith("d") else n

        def run(engname):
            def body(e):
                for waits, fn, chan in streams[engname]:
                    for c, n in waits:
                        e.wait_ge(sems[c], val(c, n))
                    if fn is not None:
                        fn(e).then_inc(sems[chan], 16 if chan.startswith("d") else 1)
            return body

        with nc.Block() as block:
            block.tensor(run("pe"))
            block.scalar(run("act"))
            block.vector(run("dve"))
            block.gpsimd(run("pool"))
            block.sync(run("sp"))


class Ring:
    def __init__(self, kb, name, n, shape, dtype, st=None, psum=False):
        st = st or kb.st
        alloc = kb.nc.psum_tensor if psum else kb.nc.sbuf_tensor
        self.name = name
        self.t = [st.enter_context(alloc(f"rg_{name}{i}", shape, dtype)) for i in range(n)]
        self.i = 0

    def next(self):
        i = self.i % len(self.t)
        self.i += 1
        return self.t[i], f"{self.name}.{i}"


class ViewRing:
    def __init__(self, aps, keys):
        self.t, self.k, self.i = aps, keys, 0

    def next(self):
        i = self.i % len(self.t)
        self.i += 1
        return self.t[i], self.k[i]


class Banks:
    def __init__(self, pp):
        import collections
        self.pp = pp
        self.free = collections.deque(range(8))

    def get(self):
        return self.free.popleft()

    def put(self, i):
        self.free.append(i)

    def f32(self, i):
        return self.pp[:, i, :]

    def bf(self, i):
        return self.pp[:, i, :].bitcast(BF16)

    @staticmethod
    def key(i):
        return f"ps{i}"


def interleave(gens):
    gens = list(gens)
    while gens:
        for g in list(gens):
            try:
                next(g)
            except StopIteration:
                gens.remove(g)


class KB:
    def __init__(self, nc, P, st):
        self.nc, self.P, self.st = nc, P, st
        self.flip = 0

    def sb(self, name, shape, dtype, st=None):
        return (st or self.st).enter_context(self.nc.sbuf_tensor("sb_" + name, shape, dtype))

    def mm(self, out, lhsT, rhs, start, stop, r, w):
        self.P.op("pe", lambda e: e.matmul(out, lhsT=lhsT, rhs=rhs, start=start, stop=stop), r, w)

    def tr(self, out, in_, r, w):
        idb = self.identb[:]
        self.P.op("pe", lambda e: e.transpose(out=out, in_=in_, identity=idb), list(r) + ["identb"], w)

    def act(self, out, in_, func, r, w, scale=1.0, bias=0.0, accum=None):
        kw = {}
        if accum is not None:
            kw["accum_out"] = accum
        self.P.op("act", lambda e: e.activation(out=out, in_=in_, func=func, bias=bias, scale=scale, **kw), r, w)

    def tt(self, eng, out, in0, in1, op, r, w):
        self.P.op(eng, lambda e: e.tensor_tensor(out=out, in0=in0, in1=in1, op=op), r, w)

    def ts(self, eng, out, in0, s1, s2, op0, op1, r, w):
        if s2 is None:
            self.P.op(eng, lambda e: e.tensor_scalar(out=out, in0=in0, scalar1=s1, scalar2=None, op0=op0), r, w)
        else:
            self.P.op(eng, lambda e: e.tensor_scalar(out=out, in0=in0, scalar1=s1, scalar2=s2, op0=op0, op1=op1), r, w)

    def stt(self, out, in0, sc, in1, op0, op1, r, w):
        self.P.op("dve", lambda e: e.scalar_tensor_tensor(out=out, in0=in0, scalar=sc, in1=in1, op0=op0, op1=op1), r, w)

    def cp(self, eng, out, in_, r, w):
        if eng == "act":
            self.P.op("act", lambda e: e.activation(out=out, in_=in_, func=AF.Copy), r, w)
        else:
            self.P.op(eng, lambda e: e.tensor_copy(out=out, in_=in_), r, w)

    def cpalt(self, out, in_, r, w):
        self.flip ^= 1
        self.cp("act" if self.flip else "dve", out, in_, r, w)

    def red(self, out, in_, op, r, w):
        self.P.op("dve", lambda e: e.tensor_reduce(out=out, in_=in_, axis=AX.X, op=op), r, w)

    def dma(self, eng, out, in_, r, w):
        self.P.op(eng, lambda e: e.dma_start(out=out, in_=in_), r, w, dma=True)


def build(stages="ABCD", dbg=False):
    nc = bass.Bass("TRN2", target_bir_lowering=False)

    def dram(n, s, d, kind="ExternalInput"):
        return nc.dram_tensor(n, s, d, kind=kind).ap()

    x_d = dram("x", [T, D], F32)
    mem_d = dram("mem", [256, D], F32)
    pos_d = dram("pos", [128, NT], I32)
    cols_d = dram("cols", [128, NCOLS], F32)
    cq_d = dram("cq", [128, NCQ], F32)
    gfin_d = dram("gfin", [D], F32)
    w_in_d = dram("w_in", [D, 3872], F32)
    wup_d = dram("w_up", [64, 512], F32)
    aup_d = dram("a_up", [64, 512], F32)
    gup_d = dram("g_up", [160, 512], F32)
    w_out_d = dram("w_out", [D, D], F32)
    wq_d = dram("wq", [D, D], F32)
    wkv_d = dram("wkv", [D, 2 * D], F32)
    wo_d = dram("wo", [D, D], F32)
    wupm_d = dram("mlp_up", [D, 4 * D], F32)
    wdn_d = dram("mlp_down", [4 * D, D], F32)
    out_d = dram("out", [T, D], F32, kind="ExternalOutput")
    if dbg:
        dbg_yT = dram("dbg_yT", [128, KC, T], BF16, kind="ExternalOutput")
        dbg_hT = dram("dbg_hT", [128, KC, T], BF16, kind="ExternalOutput")

    def wview(w):
        return w.rearrange("(kc p) n -> p kc n", p=128)

    with contextlib.ExitStack() as st:
        P = Prog(nc, st)
        kb = KB(nc, P, st)
        cols = kb.sb("cols", [128, NCOLS], F32)
        dc = kb.sb("dcols", [128, 40], F32)
        cqf = kb.sb("cqf", [128, NCQ], F32)
        identb = kb.sb("identb", [128, 128], BF16)
        bonesb = kb.sb("bonesb", [128, 128], BF16)
        onesb = kb.sb("onesb", [128, 128], BF16)
        mhalf = kb.sb("mhalf", [128, 256], F32)
        hT = kb.sb("hT", [128, KC, T], BF16)
        yT = kb.sb("yT", [128, 4, T], BF16)
        WA = kb.sb("WA", [128, 16384], BF16)
        ss = kb.sb("ss", [128, 3 * NT], F32)
        kb.identb = identb
        pp = st.enter_context(nc.psum_tensor("pp", [128, 8, 512], F32))
        L_BK = Banks(pp)
        psA = ViewRing([pp[:, i, :] for i in range(6)], [f"ps{i}" for i in range(6)])
        psT = ViewRing([pp[:, i, :].bitcast(BF16) for i in (6, 7)], ["ps6", "ps7"])
        mask4 = cqf[:, 128:640]
        maskI = cqf[:, 256:384]
        maskLs = cqf[:, 640:768]
        identf = cqf[:, 0:128]

        kb.dma("sp", cols[:], cols_d, [], ["cols"])
        kb.dma("sp", cqf[:], cq_d, [], ["cqf"])
        kb.dma("pool", identb[:], cq_d[:, 0:128], [], ["identb"])
        kb.dma("pool", bonesb[:], cq_d[:, 768:896], [], ["bonesb"])
        kb.dma("pool", onesb[:], cq_d[:, 896:1024], [], ["onesb"])
        P.op("dve", lambda e: e.memset(mhalf[:], -0.5), [], ["mhalf"])
        kb.ts("dve", dc[:, 0:15], cols[:, 40:55], -1.0, 1.0, ALU.mult, ALU.add, ["cols"], ["dc"])
        kb.ts("dve", dc[:, 15:19], cols[:, 55:59], 0.5, None, ALU.mult, None, ["cols"], ["dc"])
        kb.ts("dve", dc[:, 19:23], cols[:, 59:63], 0.5, None, ALU.mult, None, ["cols"], ["dc"])
        kb.ts("dve", dc[:, 23:27], cols[:, 67:71], -1.0, None, ALU.mult, None, ["cols"], ["dc"])
        kb.ts("dve", dc[:, 27:31], cols[:, 67:71], -1.0, 1.0, ALU.mult, ALU.add, ["cols"], ["dc"])

        def norm_T(src, nblk, tpb, gcol0, dst, dstkey, sscol0, hb, sqj):
            for blk in range(nblk):
                hbs = []
                for tl in range(tpb):
                    t = blk * tpb + tl
                    xa, xk = src(t)
                    c = sscol0 + t
                    kb.act(sqj[:], xa, AF.Square, [xk], ["sqj", f"ss{c}"], accum=ss[:, c:c + 1])
                    kb.ts("dve", ss[:, c:c + 1], ss[:, c:c + 1], 1.0 / D, 1e-6, ALU.mult, ALU.add, [f"ss{c}"], [f"ss{c}"])
                    kb.tt("pool", ss[:, c:c + 1], ss[:, c:c + 1], mhalf[:, 0:1], ALU.pow, [f"ss{c}", "mhalf"], [f"ss{c}"])
                    h, hk = hb.next()
                    kb.act(h[:], xa, AF.Copy, [xk, f"ss{c}"], [hk], scale=ss[:, c:c + 1])
                    hbs.append((h, hk))
                for kc in range(KC):
                    bank, bk = psT.next()
                    for tl, (h, hk) in enumerate(hbs):
                        kb.tr(bank[:, tl * 128:(tl + 1) * 128], h[:, kc * 128:(kc + 1) * 128], [hk], [bk])
                    n = tpb * 128
                    o = dst[:, kc, blk * n:(blk + 1) * n]
                    kb.flip ^= 1
                    if kb.flip:
                        kb.act(o, bank[:, 0:n], AF.Copy, [bk, "cols"], [dstkey(blk)], scale=cols[:, gcol0 + kc:gcol0 + kc + 1])
                    else:
                        kb.ts("dve", o, bank[:, 0:n], cols[:, gcol0 + kc:gcol0 + kc + 1], None, ALU.mult, None, [bk, "cols"], [dstkey(blk)])

        def load_w(dst, src, key, eng="pool"):
            kb.dma(eng, dst, src, [], [key])

        Wv8 = lambda ncol: WA[:, 0:KC * ncol].rearrange("p (k n) -> p k n", k=KC)

        Wr = Wv8(2048)
        BK = L_BK

        def acquire(n=1):
            spins = 0
            while len(BK.free) < n:
                spins += 1
                assert spins < 100000, "PSUM bank pool deadlock"
                yield
            return [BK.get() for _ in range(n)]

        def rr(gens):
            gens = list(gens)
            while gens:
                for g in list(gens):
                    try:
                        next(g)
                    except StopIteration:
                        gens.remove(g)
                yield

        def run(g):
            for _ in g:
                pass

        if "B" not in stages:
            with contextlib.ExitStack() as sa:
                xring = Ring(kb, "xr", 3, [128, D], F32, st=sa)
                hb = Ring(kb, "hb", 8, [128, D], BF16, st=sa)
                sqj = kb.sb("sqj", [128, D], BF16, st=sa)

                def srcx(t):
                    xa, xk = xring.next()
                    kb.dma("sp", xa[:], x_d[t * 128:(t + 1) * 128, :], [], [xk])
                    return xa[:], xk
                norm_T(srcx, 4, 4, 0, hT, lambda b: f"hT.{b}", 0, hb, sqj)
                P.barrier()
                P.flush()
            P.op("dve", lambda e: e.memset(yT[:, 0:4, :], 0.0), [], [f"yT.{t}.0" for t in range(NT)])
        else:
            for gi in range(4):
                load_w(Wr[:, :, gi * 512:(gi + 1) * 512], wview(w_in_d)[:, :, gi * 512:(gi + 1) * 512], f"W.{gi}")
            with contextlib.ExitStack() as sbk:
                cos_t = kb.sb("cos_t", [128, NT, 32], F32, st=sbk)
                sin_t = kb.sb("sin_t", [128, NT, 32], F32, st=sbk)
                srope = sbk
                if True:
                    posi = kb.sb("posi", [128, NT], I32, st=srope)
                    posf = kb.sb("posf", [128, NT], F32, st=srope)
                    ang = kb.sb("ang", [128, NT, 32], F32, st=srope)
                    ra = kb.sb("ra", [128, NT, 32], F32, st=srope)
                    rb_ = kb.sb("rb_", [128, NT, 32], F32, st=srope)
                    ri = kb.sb("ri", [128, NT, 32], I32, st=srope)
                    kb.dma("sp", posi[:], pos_d, [], ["posi"])
                    kb.cp("dve", posf[:], posi[:], ["posi"], ["posf"])
                    kb.tt("dve", ang[:], posf[:].unsqueeze(2).to_broadcast([128, NT, 32]),
                          cols[:, 103:135].unsqueeze(1).to_broadcast([128, NT, 32]), ALU.mult, ["posf", "cols"], ["ang"])
                    C1 = 6.28125
                    C2 = 2.0 * math.pi - C1
                    for tab, shift, nm in ((sin_t, 0.0, "sin"), (cos_t, 0.5 * math.pi, "cos")):
                        kb.ts("dve", ra[:], ang[:], shift, 1.0 / (2.0 * math.pi), ALU.add, ALU.mult, ["ang"], ["ra"])
                        kb.cp("dve", ri[:], ra[:], ["ra"], ["ri"])
                        kb.cp("dve", rb_[:], ri[:], ["ri"], ["rb"])
                        kb.ts("dve", ra[:], ang[:], shift, None, ALU.add, None, ["ang"], ["ra"])
                        kb.stt(ra[:], rb_[:], -C1, ra[:], ALU.mult, ALU.add, ["rb", "ra"], ["ra"])
                        kb.stt(ra[:], rb_[:], -C2, ra[:], ALU.mult, ALU.add, ["rb", "ra"], ["ra"])
                        kb.ts("dve", rb_[:], ra[:], math.pi, -2.0 * math.pi, ALU.is_gt, ALU.mult, ["ra"], ["rb"])
                        kb.tt("dve", ra[:], ra[:], rb_[:], ALU.add, ["ra", "rb"], ["ra"])
                        kb.ts("dve", rb_[:], ra[:], -math.pi, 2.0 * math.pi, ALU.is_lt, ALU.mult, ["ra"], ["rb"])
                        kb.tt("dve", ra[:], ra[:], rb_[:], ALU.add, ["ra", "rb"], ["ra"])
                        kb.act(tab[:], ra[:], AF.Sin, ["ra"], [nm])

                sb_ = lambda n, shp, d: kb.sb(n, shp, d, st=sbk)
                W = 2
                xr_p = [sb_(f"xr{i}", [128, D], F32) for i in range(2)]
                hb_p = [sb_(f"hb{i}", [128, D], BF16) for i in range(4)]
                sqj = sb_("sqj", [128, D], BF16)
                qT_p = [sb_(f"qT{i}", [128, 4, 2, 512], BF16) for i in range(2)]
                for i in range(2):
                    P.op("dve", lambda e, i=i: e.memset(qT_p[i][:], 0.0), [], [f"qT{i}.{j}" for j in range(4)])
                kT_p = [sb_(f"kT{i}", [128, 4, 512], BF16) for i in range(2)]
                ktok_p = [sb_(f"ktok{i}", [128, 4, 512], BF16) for i in range(2)]
                vtok_p = [sb_(f"vtok{i}", [128, 4, 512], BF16) for i in range(2)]
                gateT_p = [sb_(f"gateT{i}", [128, 4, 512], BF16) for i in range(2)]
                xs_p = [sb_(f"xs{i}", [128, 512], F32) for i in range(W)]
                A_p = [sb_(f"rA{i}", [128, 512], F32) for i in range(W)]
                B_p = [sb_(f"rB{i}", [128, 512], F32) for i in range(W)]
                qtok_p = [sb_(f"qtok{i}", [128, 512], BF16) for i in range(W)]
                th_p = [sb_(f"thr{i}", [128, 512], F32) for i in range(2)]
                sT_p = [sb_(f"sT{i}", [128, 8, 128], BF16) for i in range(W)]
                y32_p = [sb_(f"y32{i}", [128, 512], F32) for i in range(W)]
                sq_p = [sb_(f"sqr{i}", [128, 512], F32) for i in range(W)]
                ynb_p = [sb_(f"ynb{i}", [128, 512], BF16) for i in range(W)]
                yaff_p = [sb_(f"yaff{i}", [128, 2, 128], F32) for i in range(W)]
                stat_p = [sb_(f"stat{i}", [128, 64], F32) for i in range(W)]
                R32 = sb_("R32", [128, 4, 64], F32)
                Rbf = sb_("Rbf", [128, 5, 4, 64], BF16)
                P.op("dve", lambda e: e.memset(R32[:], 0.0), [], ["R32"])

                def gen_normA(blk):
                    for tl in range(4):
                        t = blk * 4 + tl
                        xa, xk = xr_p[t % 2], f"xr.{t % 2}"
                        kb.dma("sp", xa[:], x_d[t * 128:(t + 1) * 128, :], [], [xk])
                        kb.act(sqj[:], xa[:], AF.Square, [xk], ["sqj", f"ss{t}"], accum=ss[:, t:t + 1])
                        yield
                        kb.ts("dve", ss[:, t:t + 1], ss[:, t:t + 1], 1.0 / D, 1e-6, ALU.mult, ALU.add, [f"ss{t}"], [f"ss{t}"])
                        yield
                        kb.tt("pool", ss[:, t:t + 1], ss[:, t:t + 1], mhalf[:, 0:1], ALU.pow, [f"ss{t}", "mhalf"], [f"ss{t}"])
                        yield
                        kb.act(hb_p[tl][:], xa[:], AF.Copy, [xk, f"ss{t}"], [f"hb.{tl}"], scale=ss[:, t:t + 1])
                        yield
                    for kc in range(KC):
                        (b,) = yield from acquire(1)
                        bankT, bk = BK.bf(b), BK.key(b)
                        for tl in range(4):
                            kb.tr(bankT[:, tl * 128:(tl + 1) * 128], hb_p[tl][:, kc * 128:(kc + 1) * 128], [f"hb.{tl}"], [bk])
                        yield
                        o = hT[:, kc, blk * 512:(blk + 1) * 512]
                        if kc % 2:
                            kb.act(o, bankT[:, 0:512], AF.Copy, [bk, "cols"], [f"hT.{blk}"], scale=cols[:, kc:kc + 1])
                        else:
                            kb.ts("dve", o, bankT[:, 0:512], cols[:, kc:kc + 1], None, ALU.mult, None, [bk, "cols"], [f"hT.{blk}"])
                        BK.put(b)
                        yield

                def post_gen(slot, b, eps, wcol0, bcol0, finish):
                    bank, bk = BK.f32(b), BK.key(b)
                    y32, k1 = y32_p[slot], f"y32.{slot}"
                    kb.cp("act", y32[:], bank, [bk], [k1])
                    BK.put(b)
                    yield
                    stt_, ks = stat_p[slot], f"stat.{slot}"
                    v3 = lambda a: a.rearrange("p (h d) -> p h d", d=64)
                    bc = lambda a: a.unsqueeze(2).to_broadcast([128, 8, 64])
                    kb.red(stt_[:, 0:8], v3(y32[:]), ALU.add, [k1], [ks])
                    sq, k2 = sq_p[slot], f"sq.{slot}"
                    kb.act(sq[:], y32[:], AF.Square, [k1], [k2])
                    yield
                    kb.red(stt_[:, 8:16], v3(sq[:]), ALU.add, [k2], [ks])
                    yield
                    kb.ts("dve", stt_[:, 16:24], stt_[:, 0:8], 1.0 / 64, None, ALU.mult, None, [ks], [ks])
                    yield
                    kb.tt("dve", stt_[:, 24:32], stt_[:, 16:24], stt_[:, 16:24], ALU.mult, [ks], [ks])
                    kb.ts("dve", stt_[:, 32:40], stt_[:, 8:16], 1.0 / 64, eps, ALU.mult, ALU.add, [ks], [ks])
                    yield
                    kb.tt("dve", stt_[:, 40:48], stt_[:, 32:40], stt_[:, 24:32], ALU.subtract, [ks], [ks])
                    yield
                    kb.tt("pool", stt_[:, 48:56], stt_[:, 40:48], mhalf[:, 0:8], ALU.pow, [ks, "mhalf"], [ks])
                    yield
                    kb.stt(stt_[:, 56:64], stt_[:, 16:24], -1.0, stt_[:, 48:56], ALU.mult, ALU.mult, [ks], [ks])
                    kb.tt("pool", v3(y32[:]), v3(y32[:]), bc(stt_[:, 48:56]), ALU.mult, [k1, ks], [k1])
                    yield
                    ynb, k3 = ynb_p[slot], f"ynb.{slot}"
                    kb.tt("dve", v3(ynb[:]), v3(y32[:]), bc(stt_[:, 56:64]), ALU.add, [k1, ks], [k3])
                    yield
                    (b2,) = yield from acquire(1)
                    bankT, kt = BK.bf(b2), BK.key(b2)
                    for ct in range(4):
                        kb.tr(bankT[:, ct * 128:(ct + 1) * 128], ynb[:, ct * 128:(ct + 1) * 128], [k3], [kt])
                    yield
                    for ct in range(4):
                        ya, k4 = yaff_p[slot][:, ct % 2, :], f"yaff.{slot}.{ct % 2}"
                        kb.act(ya, bankT[:, ct * 128:(ct + 1) * 128], AF.Identity, [kt, "cols"], [k4],
                               scale=cols[:, wcol0 + ct:wcol0 + ct + 1], bias=cols[:, bcol0 + ct:bcol0 + ct + 1])
                        finish(ct, ya, k4)
                        if ct == 3:
                            BK.put(b2)
                        yield

                def qk_chain(slot, blk, tl, wi):
                    par = blk % 2
                    t = blk * 4 + tl
                    tok = slice(t * 128, (t + 1) * 128)
                    hk = f"hT.{blk}"
                    tabcol = 83 if wi == 0 else 91
                    (b,) = yield from acquire(1)
                    bank, bk = BK.f32(b), BK.key(b)
                    for kc in range(KC):
                        kb.mm(bank, hT[:, kc, tok], Wr[:, kc, wi * 512:(wi + 1) * 512], kc == 0, kc == KC - 1, [hk, f"W.{wi}"], [bk])
                    yield
                    xs, kx = xs_p[slot], f"xs.{slot}"
                    kb.cp("act", xs[:], bank, [bk], [kx])
                    BK.put(b)
                    yield
                    A, ka = A_p[slot], f"rA.{slot}"
                    B, kbk = B_p[slot], f"rB.{slot}"
                    v4 = lambda a: a.rearrange("p (h two f) -> p h two f", two=2, f=32)
                    cs4 = cos_t[:, t, :].unsqueeze(1).unsqueeze(1).to_broadcast([128, 8, 2, 32])
                    sn3 = sin_t[:, t, :].unsqueeze(1).to_broadcast([128, 8, 32])
                    kb.tt("dve", v4(A[:]), v4(xs[:]), cs4, ALU.mult, [kx, "cos"], [ka])
                    kb.tt("pool", v4(B[:])[:, :, 0, :], v4(xs[:])[:, :, 1, :], sn3, ALU.mult, [kx, "sin"], [kbk])
                    kb.tt("pool", v4(B[:])[:, :, 1, :], v4(xs[:])[:, :, 0, :], sn3, ALU.mult, [kx, "sin"], [kbk])
                    yield
                    kb.tt("dve", v4(A[:])[:, :, 0, :], v4(A[:])[:, :, 0, :], v4(B[:])[:, :, 0, :], ALU.subtract, [ka, kbk], [ka])
                    kb.tt("dve", v4(A[:])[:, :, 1, :], v4(A[:])[:, :, 1, :], v4(B[:])[:, :, 1, :], ALU.add, [ka, kbk], [ka])
                    yield
                    scl = cols[:, tabcol:tabcol + 8].unsqueeze(2).to_broadcast([128, 8, 64])
                    A3 = A[:].rearrange("p (h d) -> p h d", d=64)
                    if wi == 0:
                        qtv, kq = qtok_p[slot][:], f"qtok.{slot}"
                    else:
                        qtv, kq = ktok_p[par][:, tl, :], f"ktok{par}.{tl}"
                    kb.tt("dve", qtv.rearrange("p (h d) -> p h d", d=64), A3, scl, ALU.mult, [ka, "cols"], [kq])
                    yield
                    (b2,) = yield from acquire(1)
                    bankT, kt = BK.bf(b2), BK.key(b2)
                    for ct in range(4):
                        kb.tr(bankT[:, ct * 128:(ct + 1) * 128], qtv[:, ct * 128:(ct + 1) * 128], [kq], [kt])
                    yield
                    bv = bankT[:, 0:512].rearrange("p (c n) -> p c n", c=4)
                    if wi == 0:
                        kb.cp("act", qT_p[par][0:64, :, 0, tl * 128:(tl + 1) * 128], bv[0:64], [kt], [f"qT{par}.{tl}"])
                        kb.cp("dve", qT_p[par][64:128, :, 1, tl * 128:(tl + 1) * 128], bv[64:128], [kt], [f"qT{par}.{tl}"])
                    else:
                        kb.cpalt(kT_p[par][:, :, tl * 128:(tl + 1) * 128], bv, [kt], [f"kT{par}.{tl}"])
                    BK.put(b2)
                    yield

                def v_chain(blk, tl):
                    par = blk % 2
                    t = blk * 4 + tl
                    tok = slice(t * 128, (t + 1) * 128)
                    (b,) = yield from acquire(1)
                    bank, bk = BK.f32(b), BK.key(b)
                    for kc in range(KC):
                        kb.mm(bank, hT[:, kc, tok], Wr[:, kc, 1024:1536], kc == 0, kc == KC - 1, [f"hT.{blk}", "W.2"], [bk])
                    yield
                    kb.cpalt(vtok_p[par][:, tl, :], bank, [bk], [f"vtok{par}.{tl}"])
                    BK.put(b)
                    yield

                def g_chain(blk, ct):
                    par = blk % 2
                    (b,) = yield from acquire(1)
                    bank, bk = BK.f32(b), BK.key(b)
                    for kc in range(KC):
                        kb.mm(bank, Wr[:, kc, 1536 + ct * 128:1536 + (ct + 1) * 128], hT[:, kc, blk * 512:(blk + 1) * 512],
                              kc == 0, kc == KC - 1, [f"hT.{blk}", "W.3"], [bk])
                    yield
                    th, kth = th_p[ct % 2], f"thr.{ct % 2}"
                    kb.act(th[:], bank, AF.Tanh, [bk], [kth], scale=0.5)
                    yield
                    kb.ts("pool", th[:], th[:], 0.5, 0.5, ALU.mult, ALU.add, [kth], [kth])
                    yield
                    kb.tt("dve", gateT_p[par][:, ct, :], th[:], bank, ALU.mult, [kth, bk], [f"gateT{par}.{ct}"])
                    BK.put(b)
                    yield

                def gen_proj(blk):
                    for k in range(4):
                        yield from rr([qk_chain((2 * k) % W, blk, k, 0), qk_chain((2 * k + 1) % W, blk, k, 1), v_chain(blk, k), g_chain(blk, k)])

                def chunk_chain(slot, blk, tl):
                    par = blk % 2
                    qT, kT_, vtok, gateT = qT_p[par], kT_p[par], vtok_p[par], gateT_p[par]
                    t = blk * 4 + tl
                    tok = slice(t * 128, (t + 1) * 128)
                    tl_ = slice(tl * 128, (tl + 1) * 128)
                    sT, ksT = sT_p[slot], f"sT.{slot}"
                    bs = yield from acquire(2)
                    for g4 in range(2):
                        bank, bk = BK.f32(bs[g4]), BK.key(bs[g4])
                        for hh in range(4):
                            h = 4 * g4 + hh
                            kb.mm(bank[:, hh * 128:(hh + 1) * 128], kT_[:, h // 2, tl_], qT[:, h // 2, h % 2, tl_], True, True,
                                  [f"kT{par}.{tl}", f"qT{par}.{tl}"], [bk])
                    yield
                    for g4 in range(2):
                        bank, bk = BK.f32(bs[g4]), BK.key(bs[g4])
                        kb.tt("dve", sT[:, 4 * g4:4 * g4 + 4, :], bank.rearrange("p (c n) -> p c n", c=4),
                              maskI.unsqueeze(1).to_broadcast([128, 4, 128]), ALU.mult, [bk, "cqf"], [ksT])
                        BK.put(bs[g4])
                        yield
                    (bo,) = yield from acquire(1)
                    bankO, ko = BK.f32(bo), BK.key(bo)
                    for h in range(8):
                        kb.mm(bankO[:, 64 * h:64 * h + 64], sT[:, h, :], vtok[:, tl, 64 * h:64 * h + 64], True, t == 0,
                              [ksT, f"vtok{par}.{tl}"], [ko])
                        if t > 0:
                            kb.mm(bankO[:, 64 * h:64 * h + 64], qT[:, h // 2, h % 2, tl_], Rbf[:, t % 5, h // 2, :], False, True,
                                  [f"qT{par}.{tl}", f"Rbf.{t % 5}"], [ko])
                    yield

                    def fin(ct, ya, k4):
                        kb.tt("dve", yT[:, ct, tok], ya, gateT[:, ct, tl_], ALU.mult, [k4, f"gateT{par}.{ct}"], [f"yT.{t}.0"])
                    yield from post_gen(slot, bo, 1e-5, 32, 36, fin)

                def gen_chunks(blk):
                    par = blk % 2
                    ktok, vtok = ktok_p[par], vtok_p[par]
                    for tl in range(4):
                        t = blk * 4 + tl
                        if t == NT - 1:
                            continue
                        (b,) = yield from acquire(1)
                        bankK, kk_ = BK.f32(b), BK.key(b)
                        for ct in range(4):
                            kb.mm(bankK[:, ct * 128:(ct + 1) * 128], ktok[:, tl, ct * 128:(ct + 1) * 128], vtok[:, tl, ct * 128:(ct + 1) * 128],
                                  True, True, [f"ktok{par}.{tl}", f"vtok{par}.{tl}"], [kk_])
                        yield
                        for hh in range(2):
                            b0 = 64 * hh
                            kb.tt("dve", R32[b0:b0 + 64, :, :], bankK[b0:b0 + 64, :].rearrange("p (c n) -> p c n", c=4)[:, :, b0:b0 + 64],
                                  R32[b0:b0 + 64, :, :], ALU.add, [kk_, "R32"], ["R32"])
                        BK.put(b)
                        yield
                        kb.tt("dve", R32[:], R32[:], cols[:, 99:103].unsqueeze(2).to_broadcast([128, 4, 64]), ALU.mult, ["R32", "cols"], ["R32"])
                        yield
                        kb.cp("act", Rbf[:, (t + 1) % 5, :, :], R32[:], ["R32"], [f"Rbf.{(t + 1) % 5}"])
                        yield
                    yield from rr([chunk_chain(0, blk, 0), chunk_chain(1, blk, 1)])
                    yield from rr([chunk_chain(0, blk, 2), chunk_chain(1, blk, 3)])

                run(gen_normA(0))
                interleave([gen_proj(0), gen_normA(1)])
                for blk in range(4):
                    if blk == 3 and "C" in stages:
                        Wk = Wv8(1824)
                        kb.dma("pool", Wk[:, :, :], wview(w_in_d)[:, :, 2048:3872], [], ["Wk", "W.0", "W.1", "W.2", "W.3"])
                    gs = [gen_chunks(blk)]
                    if blk + 1 < 4:
                        gs.append(gen_proj(blk + 1))
                    if blk + 2 < 4:
                        gs.append(gen_normA(blk + 2))
                    interleave(gs)
                if dbg:
                    kb.dma("sp", dbg_hT, hT[:], [f"hT.{b}" for b in range(4)], [])
                P.barrier()
                P.flush()

        if "C" in stages:
            stage_C(nc, P, kb, locals())
        else:
            P.op("dve", lambda e: e.memset(hT[:, 0:4, :], 0.0), [f"hT.{b}" for b in range(4)], [f"yT.{t}.1" for t in range(NT)])

        if dbg:
            kb.dma("sp", dbg_yT[:, 0:4, :], yT[:], [f"yT.{t}.0" for t in range(NT)], [])
            kb.dma("sp", dbg_yT[:, 4:8, :], hT[:, 0:4, :], [f"yT.{t}.1" for t in range(NT)], [])

        if "D" in stages:
            stage_D(nc, P, kb, locals())
        P.barrier()
        P.flush()
    return nc


def stage_C(nc, P, kb, L):
    cols, dc, cqf, hT, WA, mhalf, bonesb, BK = (L[k] for k in ("cols", "dc", "cqf", "hT", "WA", "mhalf", "bonesb", "L_BK"))
    wup_d, aup_d, gup_d, w_in_d = L["wup_d"], L["aup_d"], L["gup_d"], L["w_in_d"]
    mask4, maskLs, identf = L["mask4"], L["maskLs"], L["identf"]
    onesf = cqf[:, 896:1024]
    Wk = WA[:, 0:KC * 1824].rearrange("p (k n) -> p k n", k=KC)
    CL = 0.5 * math.exp(-0.5)
    NB = 256
    NBLK = T // NB
    with contextlib.ExitStack() as sc:
        if "B" not in L["stages"]:
            kb.dma("pool", Wk[:, :, :], w_in_d.rearrange("(kc p) n -> p kc n", p=128)[:, :, 2048:3872], [], ["Wk"])
        sb = lambda n, s, d: kb.sb(n, s, d, st=sc)
        wupz = sb("wupz", [128, 512], BF16)
        aupz = sb("aupz", [128, 512], BF16)
        gup1b = sb("gup1b", [128, 512], BF16)
        gup2z = sb("gup2z", [128, 512], BF16)
        for tz in (wupz, aupz, gup2z):
            P.op("dve", lambda e, tz=tz: e.memset(tz[:], 0.0), [], ["wupb", "gupb"])
        kb.dma("pool", wupz[0:64, :], wup_d, [], ["wupb"])
        kb.dma("pool", aupz[64:128, :], aup_d, [], ["wupb"])
        kb.dma("pool", gup1b[:], gup_d[0:128, :], [], ["gupb"])
        kb.dma("pool", gup2z[96:128, :], gup_d[128:160, :], [], ["gupb"])
        RKblk = sb("RKblk", [128, 4, 128], BF16)
        for ct in range(4):
            kb.ts("dve", RKblk[:, ct, :], cqf[:, 768:896], cols[:, 71 + ct:72 + ct], None, ALU.mult, None, ["cqf", "cols"], ["RKblk"])
        pT = sb("pT", [128, 15, NB + 1], F32)
        P.op("dve", lambda e: e.memset(pT[:], 0.0), [], [f"pT.{i}" for i in range(15)])
        carry = sb("carry", [128, 15], F32)
        NTMP = 10
        T_p = [[sb(f"T{c}_{i}", [128, NB], F32) for i in range(NTMP)] for c in range(2)]
        sh_p = [sb(f"shT{i}", [128, NB], F32) for i in range(2)]
        kk2_p = [sb(f"kk2{i}", [128, NB], BF16) for i in range(2)]
        twa = sb("twa", [128, NB], BF16)
        sg1 = sb("sg1", [128, NB], BF16)
        sg2 = sb("sg2", [128, NB], BF16)
        ARt = sb("ARt", [128, 4, 2, 2, 2, 128], BF16)
        P.op("dve", lambda e: e.memset(ARt[:], 0.0), [], [f"ARt.{i}" for i in range(4)])
        KT = sb("KT", [128, 4, NB], BF16)
        BnT = sb("BnT", [128, 4, NB], BF16)
        rkb = sb("rkb", [128, 4, NB], BF16)
        vbf_p = [sb(f"vbf{i}", [128, 4, NB], BF16) for i in range(2)]
        gT = sb("gT", [128, 4, NB], BF16)
        Ktok = sb("Ktok", [128, 2, 512], BF16)
        Btok = sb("Btok", [128, 2, 512], BF16)
        Vtok = sb("Vtok", [128, 2, 512], BF16)
        PCt = sb("PCt", [128, 4, 2], F32)
        MTs = [sb(f"MT{i}", [128, 8, 512], BF16) for i in range(2)]
        mraw_p = [sb(f"mraw{i}", [128, 512], BF16) for i in range(2)]
        mask4b = sb("mask4b", [128, 512], BF16)
        kb.dma("pool", mask4b[:], L["cq_d"][:, 128:640], [], ["mask4b"])
        Xm = [sb(f"Xm{i}", [128, 8, 128], BF16) for i in range(2)]
        Nm = [sb(f"Nm{i}", [128, 8, 128], BF16) for i in range(2)]
        Lm = [sb(f"Lm{i}", [128, 8, 128], BF16) for i in range(2)]
        TTs = [sb(f"TT{i}", [128, 8, 128], BF16) for i in range(2)]
        RHSs = [sb(f"RHSb{i}", [128, 512], BF16) for i in range(2)]
        Ubs = [sb(f"Ub{i}", [128, 512], BF16) for i in range(2)]
        S32 = sb("S32", [128, 4, 64], F32)
        Sbf = sb("Sbf", [128, 4, 64], BF16)
        P.op("dve", lambda e: e.memset(S32[:], 0.0), [], ["S32"])
        y32 = sb("cy32", [128, 512], F32)
        sq = sb("csq", [128, 512], F32)
        ynb = sb("cynb", [128, 512], BF16)
        yaff = sb("cyaff", [128, 4, 128], F32)
        t1b = sb("ct1", [128, 4, 128], F32)
        stt_ = sb("cstat", [128, 64], F32)

        def gen_AB(blk):
            tok0 = blk * NB
            hk = f"hT.{tok0 // 512}"
            for i0 in range(0, 15, 2):
                b = BK.get()
                bank, bk = BK.f32(b), BK.key(b)
                idxs = [i for i in (i0, i0 + 1) if i < 15]
                for j, idx in enumerate(idxs):
                    c0 = idx * 128 if idx < 14 else 1696
                    for kc in range(KC):
                        kb.mm(bank[:, j * NB:(j + 1) * NB], Wk[:, kc, c0:c0 + 128], hT[:, kc, tok0:tok0 + NB],
                              kc == 0, kc == KC - 1, [hk, "Wk"] + (["hTu"] if kc >= 4 else []), [bk])
                yield
                for j, idx in enumerate(idxs):
                    kb.cp("act", pT[:, idx, 1:NB + 1], bank[:, j * NB:(j + 1) * NB], [bk], [f"pT.{idx}"])
                BK.put(b)
                yield
            allp = [f"pT.{i}" for i in range(15)]
            kb.cp("dve", carry[:], pT[:, :, NB], allp, ["carry"])
            yield
            for idx in range(15):
                tmp, ktm = sh_p[idx % 2], f"shT.{idx % 2}"
                kb.act(tmp[:], pT[:, idx, 0:NB], AF.Copy, [f"pT.{idx}", "cols"], [ktm], scale=cols[:, 40 + idx:41 + idx])
                yield
                if 8 <= idx < 12:
                    kb.stt(vbf_p[blk % 2][:, idx - 8, :], pT[:, idx, 1:NB + 1], dc[:, idx:idx + 1], tmp[:], ALU.mult, ALU.add,
                           [f"pT.{idx}", "dc", ktm, "carry"], [f"vbf{blk % 2}.{idx - 8}"])
                else:
                    kb.stt(pT[:, idx, 1:NB + 1], pT[:, idx, 1:NB + 1], dc[:, idx:idx + 1], tmp[:], ALU.mult, ALU.add,
                           [f"pT.{idx}", "dc", ktm, "carry"], [f"pT.{idx}"])
                yield
            kb.cp("dve", pT[:, :, 0], carry[:], ["carry"], allp)
            yield

        ps = lambda idx: pT[:, idx, 1:NB + 1]

        def gen_lora(blk):
            kb.act(twa[0:64, :], pT[0:64, 12, 1:NB + 1], AF.Tanh, ["pT.12"], ["twa"])
            kb.cp("dve", twa[64:128, :], pT[64:128, 12, 1:NB + 1], ["pT.12"], ["twa"])
            yield
            th, kth = sh_p[0], "shT.0"
            kb.act(th[:], ps(13), AF.Tanh, ["pT.13"], [kth], scale=0.5)
            th2, kth2 = sh_p[1], "shT.1"
            kb.act(th2[:], ps(14), AF.Tanh, ["pT.14"], [kth2], scale=0.5)
            yield
            kb.ts("dve", sg1[:], th[:], 0.5, 0.5, ALU.mult, ALU.add, [kth], ["sg1"])
            kb.ts("pool", sg2[:], th2[:], 0.5, 0.5, ALU.mult, ALU.add, [kth2], ["sg2"])
            yield

        def gen_ct(ch, blk, ct):
            Tb = T_p[ch]
            tk = lambda i: f"T{ch}.{i}"
            cs = slice(ct * 128, (ct + 1) * 128)
            r_, k_, v_ = ps(ct), ps(4 + ct), ps(8 + ct)
            kr, kk_, kv = f"pT.{ct}", f"pT.{4 + ct}", f"pT.{8 + ct}"
            bz, bg = BK.get(), BK.get()
            bankZ, kz = BK.f32(bz), BK.key(bz)
            bankG, kg = BK.f32(bg), BK.key(bg)
            kb.mm(bankZ[:, 0:NB], wupz[:, cs], twa[:], True, True, ["wupb", "twa"], [kz])
            kb.mm(bankZ[:, NB:2 * NB], aupz[:, cs], twa[:], True, True, ["wupb", "twa"], [kz])
            kb.mm(bankG[:, 0:NB], gup1b[:, cs], sg1[:], True, False, ["gupb", "sg1"], [kg])
            kb.mm(bankG[:, 0:NB], gup2z[:, cs], sg2[:], False, True, ["gupb", "sg2"], [kg])
            yield
            thw, tha = Tb[0], Tb[1]
            kb.act(thw[:], bankZ[:, 0:NB], AF.Tanh, [kz, "dc"], [tk(0)], scale=0.5, bias=dc[:, 15 + ct:16 + ct])
            kb.act(tha[:], bankZ[:, NB:2 * NB], AF.Tanh, [kz, "dc"], [tk(1)], scale=0.5, bias=dc[:, 19 + ct:20 + ct])
            BK.put(bz)
            kk = Tb[4]
            kb.ts("pool", kk[:], k_, cols[:, 63 + ct:64 + ct], 0.0, ALU.mult, ALU.add, [kk_, "cols"], [tk(4)])
            yield
            kb.cp("act", gT[:, ct, :], bankG[:, 0:NB], [kg], [f"gT.{ct}"])
            BK.put(bg)
            kk2, k_kk2 = kk2_p[ch], f"kk2.{ch}"
            kb.act(kk2[:], kk[:], AF.Square, [tk(4)], [k_kk2])
            ld = Tb[0]
            kb.ts("dve", ld[:], thw[:], -CL, -CL, ALU.mult, ALU.add, [tk(0)], [tk(0)])
            yield
            bn_ = BK.get()
            bankN, kn = BK.f32(bn_), BK.key(bn_)
            kb.mm(bankN[:, 0:NB], bonesb[:], kk2[:], True, True, ["bonesb", k_kk2], [kn])
            cum = Tb[2]
            for c in range(2):
                ld_c, cum_c = ld[:, c * 128:(c + 1) * 128], cum[:, c * 128:(c + 1) * 128]
                P.op("dve", lambda e, cum_c=cum_c, ld_c=ld_c: e.tensor_tensor_scan(out=cum_c, data0=onesf, data1=ld_c, initial=0.0, op0=ALU.mult, op1=ALU.add),
                     [tk(0), "cqf"], [tk(2)])
            an = Tb[1]
            kb.ts("pool", an[:], tha[:], -0.5, -0.5, ALU.mult, ALU.add, [tk(1)], [tk(1)])
            yield
            sn = Tb[5]
            kb.act(sn[:], bankN[:, 0:NB], AF.Sqrt, [kn], [tk(5)])
            BK.put(bn_)
            cumex = Tb[3]
            kb.tt("pool", cumex[:], cum[:], ld[:], ALU.subtract, [tk(2), tk(0)], [tk(3)])
            yield
            ep, en, ex = Tb[6], Tb[7], Tb[8]
            kb.act(ep[:], cum[:], AF.Exp, [tk(2)], [tk(6)])
            kb.act(en[:], cum[:], AF.Exp, [tk(2)], [tk(7)], scale=-1.0)
            kb.ts("dve", sn[:], sn[:], 1e-12, None, ALU.max, None, [tk(5)], [tk(5)])
            tmp2 = Tb[9]
            kb.ts("pool", tmp2[:], an[:], dc[:, 23 + ct:24 + ct], dc[:, 27 + ct:28 + ct], ALU.mult, ALU.add, [tk(1), "dc"], [tk(9)])
            yield
            kb.act(ex[:], cumex[:], AF.Exp, [tk(3)], [tk(8)])
            P.op("dve", lambda e: e.reciprocal(out=sn[:], in_=sn[:]), [tk(5)], [tk(5)])
            k2 = Tb[9]
            kb.tt("pool", k2[:], k_, tmp2[:], ALU.mult, [kk_, tk(9)], [tk(9)])
            yield
            kkn = Tb[4]
            kb.tt("dve", kkn[:], kk[:], sn[:], ALU.mult, [tk(4), tk(5)], [tk(4)])
            c3 = lambda a: a.rearrange("p (c n) -> p c n", c=2)
            kb.cp("pool", PCt[:, ct, :], c3(ep[:])[:, :, 127], [tk(6)], [f"PCt.{ct}"])
            yield
            bn = Tb[5]
            kb.tt("pool", bn[:], kkn[:], an[:], ALU.mult, [tk(4), tk(1), tk(5)], [tk(5)])
            kb.tt("dve", KT[:, ct, :], k2[:], en[:], ALU.mult, [tk(9), tk(7)], [f"KT.{ct}"])
            yield
            for hh in range(2):
                pp = slice(64 * hh, 64 * hh + 64)
                kb.tt("dve", ARt[pp, ct, :, hh, 1, :], c3(r_)[pp], c3(ep[:])[pp], ALU.mult, [kr, tk(6)], [f"ARt.{ct}"])
                kb.tt("dve" if hh == 0 else "pool", ARt[pp, ct, :, hh, 0, :], c3(kkn[:])[pp], c3(ex[:])[pp], ALU.mult, [tk(4), tk(8)], [f"ARt.{ct}"])
                yield
            kb.tt("dve", BnT[:, ct, :], bn[:], en[:], ALU.mult, [tk(5), tk(7)], [f"BnT.{ct}"])
            kb.tt("pool", rkb[:, ct, :], r_, k2[:], ALU.mult, [kr, tk(9)], [f"rkb.{ct}"])
            yield

        def gen_tok(blk):
            for c in range(2):
                cs = slice(c * 128, (c + 1) * 128)
                for src, dst, nm, sk in ((KT, Ktok, "KT", "KT"), (BnT, Btok, "BnT", "BnT"), (vbf_p[blk % 2], Vtok, "vbf", f"vbf{blk % 2}")):
                    b = BK.get()
                    bankT, kt = BK.bf(b), BK.key(b)
                    for ct in range(4):
                        kb.tr(bankT[:, ct * 128:(ct + 1) * 128], src[:, ct, cs], [f"{sk}.{ct}"], [kt])
                    yield
                    kb.cpalt(dst[:, c, :], bankT[:, 0:512], [kt], [f"{nm}tok.{c}"])
                    BK.put(b)
                    yield

        def gen_F1(blk, c):
            cs = slice(c * 128, (c + 1) * 128)
            MT = MTs[c]
            for h in range(8):
                ct = h // 2
                b = BK.get()
                bankM, km = BK.f32(b), BK.key(b)
                rhsAR = ARt[:, ct, c, h % 2, :, :].rearrange("p a b -> p (a b)")
                kb.mm(bankM[:, 0:256], BnT[:, ct, cs], rhsAR, True, True, [f"BnT.{ct}", f"ARt.{ct}"], [km])
                kb.mm(bankM[:, 256:512], KT[:, ct, cs], rhsAR, True, True, [f"KT.{ct}", f"ARt.{ct}"], [km])
                yield
                if h % 2 == 0:
                    kb.tt("dve", MT[:, h, :], bankM, mask4, ALU.mult, [km, "cqf"], [f"MT{c}.{h}"])
                    BK.put(b)
                else:
                    mr, kmr = mraw_p[(h // 2) % 2], f"mraw.{(h // 2) % 2}"
                    kb.cp("act", mr[:], bankM, [km], [kmr])
                    BK.put(b)
                    yield
                    kb.tt("pool", MT[:, h, :], mr[:], mask4b[:], ALU.mult, [kmr, "mask4b"], [f"MT{c}.{h}"])
                yield
            for g4 in range(2):
                b = BK.get()
                bankL, kl = BK.f32(b), BK.key(b)
                for hh in range(4):
                    h = 4 * g4 + hh
                    kb.mm(bankL[:, hh * 128:(hh + 1) * 128], ARt[:, h // 2, c, h % 2, 0, :], BnT[:, h // 2, cs], True, True,
                          [f"ARt.{h // 2}", f"BnT.{h // 2}"], [kl])
                yield
                kb.tt("dve", Lm[0][:, 4 * g4:4 * g4 + 4, :], bankL.rearrange("p (c n) -> p c n", c=4),
                      maskLs.unsqueeze(1).to_broadcast([128, 4, 128]), ALU.mult, [kl, "cqf"], [f"Lm0.{g4}"])
                BK.put(b)
                hs = slice(4 * g4, 4 * g4 + 4)
                mtk = [f"MT{c}.{h}" for h in range(4 * g4, 4 * g4 + 4)]
                kb.cp("act", Nm[0][:, hs, :], MT[:, hs, 0:128], mtk, [f"Nm0.{g4}"])
                kb.tt("pool", Xm[0][:, hs, :], MT[:, hs, 0:128], identf.unsqueeze(1).to_broadcast([128, 4, 128]), ALU.add,
                      mtk + ["cqf"], [f"Xm0.{g4}"])
                yield

        def gen_inv_group(c, g4):
            hs = slice(4 * g4, 4 * g4 + 4)
            v4 = lambda bank: bank.rearrange("p (c n) -> p c n", c=4)
            cur = 0
            for lvl in range(7):
                nxt = cur ^ 1
                kN, kL, kX = f"Nm{cur}.{g4}", f"Lm{cur}.{g4}", f"Xm{cur}.{g4}"
                last = lvl == 6
                Xdst, kXd = (TTs[c], f"TT{c}.{g4}") if last else (Xm[nxt], f"Xm{nxt}.{g4}")
                bx = ba = bb = None
                if lvl >= 1:
                    bx = BK.get()
                    bankX, kbx = BK.f32(bx), BK.key(bx)
                    for hh in range(4):
                        h = 4 * g4 + hh
                        kb.mm(bankX[:, hh * 128:(hh + 1) * 128], Lm[cur][:, h, :], Xm[cur][:, h, :], True, True, [kL, kX], [kbx])
                if not last:
                    ba, bb = BK.get(), BK.get()
                    bankA, kba = BK.f32(ba), BK.key(ba)
                    bankB, kbb = BK.f32(bb), BK.key(bb)
                    for hh in range(4):
                        h = 4 * g4 + hh
                        kb.mm(bankA[:, hh * 128:(hh + 1) * 128], Lm[cur][:, h, :], Nm[cur][:, h, :], True, True, [kL, kN], [kba])
                    for hh in range(4):
                        h = 4 * g4 + hh
                        kb.mm(bankB[:, hh * 128:(hh + 1) * 128], Nm[cur][:, h, :], Lm[cur][:, h, :], True, True, [kL, kN], [kbb])
                yield
                if lvl >= 1:
                    kb.tt("dve", Xdst[:, hs, :], v4(bankX), Xm[cur][:, hs, :], ALU.add, [kbx, kX], [kXd])
                    BK.put(bx)
                else:
                    kb.cp("pool", Xdst[:, hs, :], Xm[cur][:, hs, :], [kX], [kXd])
                if not last:
                    kb.cp("act", Nm[nxt][:, hs, :], v4(bankA), [kba], [f"Nm{nxt}.{g4}"])
                    BK.put(ba)
                    if g4 == 0:
                        kb.cp("act", Lm[nxt][:, hs, :], v4(bankB), [kbb], [f"Lm{nxt}.{g4}"])
                    else:
                        kb.cp("dve", Lm[nxt][:, hs, :], v4(bankB), [kbb], [f"Lm{nxt}.{g4}"])
                    BK.put(bb)
                yield
                cur = nxt

        def gen_inv(blk, c):
            gs = [gen_inv_group(c, 0), gen_inv_group(c, 1)]
            while gs:
                for g in list(gs):
                    try:
                        next(g)
                    except StopIteration:
                        gs.remove(g)
                yield

        def gen_seqpost(blk, c):
            n_ch = blk * 2 + c
            cs = slice(c * 128, (c + 1) * 128)
            tok = slice(blk * NB + c * 128, blk * NB + (c + 1) * 128)
            MT, TT, RHSb, Ub = MTs[c], TTs[c], RHSs[c], Ubs[c]
            kTT = [f"TT{c}.0", f"TT{c}.1"]
            kRH, kUb = f"RHSb{c}", f"Ub{c}"
            br = BK.get()
            bankR, kr_ = BK.f32(br), BK.key(br)
            for h in range(8):
                ct = h // 2
                o = bankR[:, 64 * h:64 * h + 64]
                if n_ch > 0:
                    kb.mm(o, ARt[:, ct, c, h % 2, 0, :], Sbf[:, ct, :], True, False, [f"ARt.{ct}", "Sbf"], [kr_])
                kb.mm(o, MT[:, h, 256:384], Vtok[:, c, 64 * h:64 * h + 64], n_ch == 0, True, [f"MT{c}.{h}", f"vbftok.{c}"], [kr_])
            yield
            kb.cp("act", RHSb[:], bankR, [kr_], [kRH])
            BK.put(br)
            yield
            bu = BK.get()
            bankU, ku = BK.f32(bu), BK.key(bu)
            for h in range(8):
                kb.mm(bankU[:, 64 * h:64 * h + 64], TT[:, h, :], RHSb[:, 64 * h:64 * h + 64], True, True, kTT + [kRH], [ku])
            yield
            kb.cp("dve", Ub[:], bankU, [ku], [kUb])
            BK.put(bu)
            yield
            if n_ch < NT - 1:
                bs_ = BK.get()
                bankS, ks_ = BK.f32(bs_), BK.key(bs_)
                for ct in range(4):
                    o = bankS[:, ct * 128:(ct + 1) * 128]
                    kb.mm(o, Btok[:, c, ct * 128:(ct + 1) * 128], Ub[:, ct * 128:(ct + 1) * 128], True, False, [f"BnTtok.{c}", kUb], [ks_])
                    kb.mm(o, Ktok[:, c, ct * 128:(ct + 1) * 128], Vtok[:, c, ct * 128:(ct + 1) * 128], False, True, [f"KTtok.{c}", f"vbftok.{c}"], [ks_])
            by = BK.get()
            bankY, ky = BK.f32(by), BK.key(by)
            for h in range(8):
                ct = h // 2
                o = bankY[:, 64 * h:64 * h + 64]
                if n_ch > 0:
                    kb.mm(o, ARt[:, ct, c, h % 2, 1, :], Sbf[:, ct, :], True, False, [f"ARt.{ct}", "Sbf"], [ky])
                kb.mm(o, MT[:, h, 128:256], Ub[:, 64 * h:64 * h + 64], n_ch == 0, False, [f"MT{c}.{h}", kUb], [ky])
                kb.mm(o, MT[:, h, 384:512], Vtok[:, c, 64 * h:64 * h + 64], False, True, [f"MT{c}.{h}", f"vbftok.{c}"], [ky])
            yield
            if n_ch < NT - 1:
                for hh in range(2):
                    b0 = 64 * hh
                    kb.tt("dve", S32[b0:b0 + 64, :, :], bankS[b0:b0 + 64, :].rearrange("p (c n) -> p c n", c=4)[:, :, b0:b0 + 64],
                          S32[b0:b0 + 64, :, :], ALU.add, [ks_, "S32"], ["S32"])
                BK.put(bs_)
                kb.tt("dve", S32[:], S32[:], PCt[:, :, c].unsqueeze(2).to_broadcast([128, 4, 64]), ALU.mult,
                      ["S32"] + [f"PCt.{ct}" for ct in range(4)], ["S32"])
                kb.cp("act", Sbf[:], S32[:], ["S32"], ["Sbf"])
                yield
            k1, k2, ks, k3 = "cy32", "csq", "cstat", "cynb"
            kb.cp("act", y32[:], bankY, [ky], [k1])
            BK.put(by)
            yield
            v3 = lambda a: a.rearrange("p (h d) -> p h d", d=64)
            bc = lambda a: a.unsqueeze(2).to_broadcast([128, 8, 64])
            kb.red(stt_[:, 0:8], v3(y32[:]), ALU.add, [k1], [ks])
            kb.act(sq[:], y32[:], AF.Square, [k1], [k2])
            yield
            kb.red(stt_[:, 8:16], v3(sq[:]), ALU.add, [k2], [ks])
            yield
            kb.ts("dve", stt_[:, 16:24], stt_[:, 0:8], 1.0 / 64, None, ALU.mult, None, [ks], [ks])
            yield
            kb.tt("dve", stt_[:, 24:32], stt_[:, 16:24], stt_[:, 16:24], ALU.mult, [ks], [ks])
            kb.ts("dve", stt_[:, 32:40], stt_[:, 8:16], 1.0 / 64, 64e-5, ALU.mult, ALU.add, [ks], [ks])
            yield
            kb.tt("dve", stt_[:, 40:48], stt_[:, 32:40], stt_[:, 24:32], ALU.subtract, [ks], [ks])
            yield
            kb.tt("pool", stt_[:, 48:56], stt_[:, 40:48], mhalf[:, 0:8], ALU.pow, [ks, "mhalf"], [ks])
            yield
            kb.stt(stt_[:, 56:64], stt_[:, 16:24], -1.0, stt_[:, 48:56], ALU.mult, ALU.mult, [ks], [ks])
            kb.tt("pool", v3(y32[:]), v3(y32[:]), bc(stt_[:, 48:56]), ALU.mult, [k1, ks], [k1])
            yield
            kb.tt("dve", v3(ynb[:]), v3(y32[:]), bc(stt_[:, 56:64]), ALU.add, [k1, ks], [k3])
            yield
            b2 = BK.get()
            bankT, kt = BK.bf(b2), BK.key(b2)
            for ct in range(4):
                kb.tr(bankT[:, ct * 128:(ct + 1) * 128], ynb[:, ct * 128:(ct + 1) * 128], [k3], [kt])
            bbo = BK.get()
            bankBo, kbo = BK.f32(bbo), BK.key(bbo)
            for ct in range(4):
                kb.mm(bankBo[:, ct * 128:(ct + 1) * 128], RKblk[:, ct, :], rkb[:, ct, cs], True, True, ["RKblk", f"rkb.{ct}"], [kbo])
            yield
            for ct in range(4):
                kb.act(yaff[:, ct, :], bankT[:, ct * 128:(ct + 1) * 128], AF.Identity, [kt, "cols"], ["cyaff"],
                       scale=cols[:, 75 + ct:76 + ct], bias=cols[:, 79 + ct:80 + ct])
            kb.tt("dve", t1b[:], bankBo.rearrange("p (c n) -> p c n", c=4), vbf_p[blk % 2][:, :, cs], ALU.mult,
                  [kbo] + [f"vbf{blk % 2}.{ct}" for ct in range(4)], ["ct1"])
            BK.put(b2)
            BK.put(bbo)
            yield
            kb.tt("pool", t1b[:], t1b[:], yaff[:], ALU.add, ["ct1", "cyaff"], ["ct1"])
            yield
            kb.tt("dve", hT[:, 0:4, tok], t1b[:], gT[:, :, cs], ALU.mult, ["ct1"] + [f"gT.{ct}" for ct in range(4)],
                  [f"yT.{n_ch}.1", f"hT.{n_ch // 4}"])
            yield

        def gen_CD(blk):
            yield from gen_lora(blk)
            for pair in range(2):
                gs = [gen_ct(0, blk, 2 * pair), gen_ct(1, blk, 2 * pair + 1)]
                while gs:
                    for g in list(gs):
                        try:
                            next(g)
                        except StopIteration:
                            gs.remove(g)
                    yield
            yield from gen_tok(blk)

        def run(g):
            for _ in g:
                pass

        run(gen_AB(0))
        run(gen_CD(0))
        for blk in range(NBLK):
            run(gen_F1(blk, 0))
            interleave([gen_inv(blk, 0), gen_F1(blk, 1)])
            interleave([gen_seqpost(blk, 0), gen_inv(blk, 1)])
            if blk + 1 < NBLK:
                interleave([gen_seqpost(blk, 1), gen_AB(blk + 1)])
                if blk + 1 == NBLK - 1 and "D" in L["stages"]:
                    wv = lambda w: w.rearrange("(kc p) n -> p kc n", p=128)
                    Wout = WA[:, 0:8192].rearrange("p (k n) -> p k n", k=KC)
                    WkvK = WA[:, 8192:16384].rearrange("p (k n) -> p k n", k=KC)
                    WkvV = hT[:, 4:8, :].rearrange("p a (b n) -> p (a b) n", n=1024)
                    kb.dma("pool", Wout, wv(L["w_out_d"]), [], ["WA.0", "Wk"])
                    kb.dma("pool", WkvK, wv(L["wkv_d"])[:, :, 0:1024], [], ["WA.1", "Wk"])
                    kb.dma("pool", WkvV, wv(L["wkv_d"])[:, :, 1024:2048], [], ["WkvV", "hTu"])
                run(gen_CD(blk + 1))
            else:
                run(gen_seqpost(blk, 1))
        P.barrier()
        P.flush()


def stage_D(nc, P, kb, L):
    cols, cqf, hT, yT, WA, mhalf, onesb, ss, BK = (L[k] for k in ("cols", "cqf", "hT", "yT", "WA", "mhalf", "onesb", "ss", "L_BK"))
    x_d, mem_d, gfin_d, out_d = L["x_d"], L["mem_d"], L["gfin_d"], L["out_d"]
    w_out_d, wq_d, wkv_d, wo_d, wupm_d, wdn_d = (L[k] for k in ("w_out_d", "wq_d", "wkv_d", "wo_d", "wupm_d", "wdn_d"))
    wview = lambda w: w.rearrange("(kc p) n -> p kc n", p=128)

    def run(g):
        for _ in g:
            pass

    with contextlib.ExitStack() as sd:
        sb = lambda n, s, d, st=sd: kb.sb(n, s, d, st=st)
        xres = sb("xres", [128, NT, D], F32)
        gfin = sb("gfin", [128, D], F32)
        kTm = sb("kTm", [128, KC, 256], BF16)
        vmem = sb("vmem", [128, 2, D], BF16)
        kmx = sb("kmx", [128, 4], F32)
        hb_p = [sb(f"dhb{i}", [128, D], BF16) for i in range(4)]
        sqj = sb("dsqj", [128, D], BF16)
        W0 = WA[:, 0:8192].rearrange("p (k n) -> p k n", k=KC)
        W1 = WA[:, 8192:16384].rearrange("p (k n) -> p k n", k=KC)
        Wout, WkvK = W0, W1
        WkvV = hT[:, 4:8, :].rearrange("p a (b n) -> p (a b) n", n=1024)
        xv = x_d.rearrange("(t p) d -> p t d", p=128)
        kb.dma("sp", xres[:, 0:4, :], xv[:, 0:4, :], [], [f"xres.{t}" for t in range(0, 4)])
        if "C" not in L["stages"]:
            kb.dma("pool", Wout, wview(w_out_d), [], ["WA.0"])
            kb.dma("pool", WkvK, wview(wkv_d)[:, :, 0:1024], [], ["WA.1"])
            kb.dma("pool", WkvV, wview(wkv_d)[:, :, 1024:2048], [], ["WkvV"])
        kb.dma("sp", gfin[:], gfin_d.partition_broadcast(128), [], ["gfin"])

        def gen_norm(tiles, src, gcol0, dst_fn, dkey, sscol0):
            for tl, t in enumerate(tiles):
                xa, xk = src(t)
                c = sscol0 + t
                kb.act(sqj[:], xa, AF.Square, [xk], ["sqj", f"ss{c}"], accum=ss[:, c:c + 1])
                yield
                kb.ts("dve", ss[:, c:c + 1], ss[:, c:c + 1], 1.0 / D, 1e-6, ALU.mult, ALU.add, [f"ss{c}"], [f"ss{c}"])
                yield
                kb.tt("pool", ss[:, c:c + 1], ss[:, c:c + 1], mhalf[:, 0:1], ALU.pow, [f"ss{c}", "mhalf"], [f"ss{c}"])
                yield
                kb.act(hb_p[tl][:], xa, AF.Copy, [xk, f"ss{c}"], [f"dhb.{tl}"], scale=ss[:, c:c + 1])
                yield
            n = len(tiles) * 128
            for kc in range(KC):
                b = BK.get()
                bankT, bk = BK.bf(b), BK.key(b)
                for tl in range(len(tiles)):
                    kb.tr(bankT[:, tl * 128:(tl + 1) * 128], hb_p[tl][:, kc * 128:(kc + 1) * 128], [f"dhb.{tl}"], [bk])
                yield
                o = dst_fn(kc)
                dk = dkey if isinstance(dkey, list) else [dkey]
                if kc % 2:
                    kb.act(o, bankT[:, 0:n], AF.Copy, [bk, "cols"], dk, scale=cols[:, gcol0 + kc:gcol0 + kc + 1])
                else:
                    kb.ts("dve", o, bankT[:, 0:n], cols[:, gcol0 + kc:gcol0 + kc + 1], None, ALU.mult, None, [bk, "cols"], dk)
                BK.put(b)
                yield

        with contextlib.ExitStack() as s0:
            memx = [sb(f"memx{i}", [128, D], F32, st=s0) for i in range(2)]
            memT = sb("memT", [128, KC, 256], BF16, st=s0)
            k2m = sb("k2m", [128, KC, 256], BF16, st=s0)
            for i in range(2):
                kb.dma("sp", memx[i][:], mem_d[i * 128:(i + 1) * 128, :], [], [f"memx.{i}"])
            for i in range(1, 4):
                kb.dma("sp", xres[:, 4 * i:4 * i + 4, :], xv[:, 4 * i:4 * i + 4, :], [], [f"xres.{t}" for t in range(4 * i, 4 * i + 4)])

            def gen_memkv():
                yield from gen_norm([0, 1], lambda t: (memx[t][:], f"memx.{t}"), 16, lambda kc: memT[:, kc, :], "memT", 16)
                for ft in range(KC):
                    b = BK.get()
                    bank, bk = BK.f32(b), BK.key(b)
                    for kc in range(KC):
                        kb.mm(bank[:, 0:256], WkvK[:, kc, ft * 128:(ft + 1) * 128], memT[:, kc, :], kc == 0, kc == KC - 1, ["WA.1", "memT"], [bk])
                    yield
                    kb.cpalt(kTm[:, ft, :], bank[:, 0:256], [bk], ["kTm"])
                    BK.put(b)
                    yield
                for mt in range(2):
                    for hf in range(2):
                        b = BK.get()
                        bank, bk = BK.f32(b), BK.key(b)
                        for kc in range(KC):
                            kb.mm(bank, memT[:, kc, mt * 128:(mt + 1) * 128], WkvV[:, kc, hf * 512:(hf + 1) * 512],
                                  kc == 0, kc == KC - 1, ["WkvV", "memT"], [bk])
                        yield
                        kb.cpalt(vmem[:, mt, hf * 512:(hf + 1) * 512], bank, [bk], ["vmem"])
                        BK.put(b)
                        yield
                kb.act(k2m[:], kTm[:], AF.Square, ["kTm"], ["k2m"])
                yield
                for h in range(4):
                    b = BK.get()
                    bank, bk = BK.f32(b), BK.key(b)
                    kb.mm(bank[:, 0:256], onesb[:], k2m[:, 2 * h, :], True, False, ["onesb", "k2m"], [bk])
                    kb.mm(bank[:, 0:256], onesb[:], k2m[:, 2 * h + 1, :], False, True, ["onesb", "k2m"], [bk])
                    yield
                    kb.red(kmx[:, h:h + 1], bank[:, 0:256], ALU.max, [bk], ["kmx"])
                    BK.put(b)
                    yield

            def gen_wout():
                for t in range(NT):
                    tok = slice(t * 128, (t + 1) * 128)
                    for hf in range(2):
                        b = BK.get()
                        bank, bk = BK.f32(b), BK.key(b)
                        for kc in range(KC):
                            ysrc = yT[:, kc, tok] if kc < 4 else hT[:, kc - 4, tok]
                            kb.mm(bank, ysrc, Wout[:, kc, hf * 512:(hf + 1) * 512], kc == 0, kc == KC - 1,
                                  [f"yT.{t}.0", f"yT.{t}.1", "WA.0", f"hT.{t // 4}"], [bk])
                        yield
                        xs = xres[:, t, hf * 512:(hf + 1) * 512]
                        kb.tt("dve", xs, bank, xs, ALU.add, [bk, f"xres.{t}"], [f"xres.{t}"])
                        BK.put(b)
                        yield

            interleave([gen_memkv(), gen_wout()])
            kb.dma("pool", W1, wview(wq_d), [], ["WA.1"])
            kb.dma("pool", W0, wview(wo_d), [], ["WA.0"])
            srcx = lambda t: (xres[:, t, :], f"xres.{t}")
            run(gen_norm([0, 1, 2, 3], srcx, 8, lambda kc: hT[:, kc, 0:512], ["hT.0", "WkvV"], 0))
            P.barrier()
            P.flush()

        srcx = lambda t: (xres[:, t, :], f"xres.{t}")
        norm_x = lambda blk: gen_norm(list(range(4 * blk, 4 * blk + 4)), srcx, 8, lambda kc: hT[:, kc, blk * 512:(blk + 1) * 512], f"hT.{blk}", 0)
        norm_m = lambda blk: gen_norm(list(range(4 * blk, 4 * blk + 4)), srcx, 24, lambda kc: hT[:, kc, blk * 512:(blk + 1) * 512], f"hT.{blk}", 16)
        with contextlib.ExitStack() as s2:
            qTb = yT[:, 0:2, :].rearrange("p a (b n) -> p (a b) n", n=512)
            PT = yT[:, 2:4, :].rearrange("p a (b n) -> p (a b) n", n=512)
            oTb_t = sb("oTb", [128, KC * 512], BF16, st=s2)
            oTb = oTb_t[:].rearrange("p (k n) -> p k n", k=KC)
            q2b_p = [sb(f"q2b{i}", [128, 2, 512], BF16, st=s2) for i in range(4)]
            nb_p = [sb(f"nb{i}", [128, 2], F32, st=s2) for i in range(4)]
            rc_p = [sb(f"rc{i}", [128, 512], F32, st=s2) for i in range(4)]

            def gen_head(sl, h):
                qk = [f"qTb.{2 * h}", f"qTb.{2 * h + 1}"]
                q2b, kq2 = q2b_p[sl], f"q2b.{sl}"
                kb.act(q2b[:], qTb[:, 2 * h:2 * h + 2, :], AF.Square, qk, [kq2])
                yield
                b = BK.get()
                bankQ, kq = BK.f32(b), BK.key(b)
                kb.mm(bankQ, onesb[:], q2b[:, 0, :], True, False, ["onesb", kq2], [kq])
                kb.mm(bankQ, onesb[:], q2b[:, 1, :], False, True, ["onesb", kq2], [kq])
                yield
                nb, kbs = nb_p[sl], f"nb.{sl}"
                kb.red(nb[:, 0:1], bankQ, ALU.max, [kq], [kbs])
                BK.put(b)
                yield
                kb.ts("dve", nb[:, 1:2], nb[:, 0:1], kmx[:, h:h + 1], -1.0 / 32, ALU.add, ALU.mult, [kbs, "kmx"], [kbs])
                yield
                for mt in range(2):
                    b = BK.get()
                    bankS, ksb = BK.f32(b), BK.key(b)
                    ms = slice(mt * 128, (mt + 1) * 128)
                    kb.mm(bankS, kTm[:, 2 * h, ms], qTb[:, 2 * h, :], True, False, ["kTm", qk[0]], [ksb])
                    kb.mm(bankS, kTm[:, 2 * h + 1, ms], qTb[:, 2 * h + 1, :], False, True, ["kTm", qk[1]], [ksb])
                    yield
                    kb.act(PT[:, 2 * h + mt, :], bankS, AF.Exp, [ksb, kbs], [f"PT.{2 * h + mt}"], scale=1.0 / 16, bias=nb[:, 1:2])
                    BK.put(b)
                    yield
                pk = [f"PT.{2 * h}", f"PT.{2 * h + 1}"]
                b = BK.get()
                bankRs, krs = BK.f32(b), BK.key(b)
                kb.mm(bankRs, onesb[:], PT[:, 2 * h, :], True, False, ["onesb", pk[0]], [krs])
                kb.mm(bankRs, onesb[:], PT[:, 2 * h + 1, :], False, True, ["onesb", pk[1]], [krs])
                yield
                rc, krc = rc_p[sl], f"rc.{sl}"
                P.op("dve", lambda e: e.reciprocal(out=rc[:], in_=bankRs), [krs], [krc])
                BK.put(b)
                yield
                for dt_ in range(2):
                    ft = 2 * h + dt_
                    b = BK.get()
                    bankO, kob = BK.f32(b), BK.key(b)
                    kb.mm(bankO, vmem[:, 0, ft * 128:(ft + 1) * 128], PT[:, 2 * h, :], True, False, ["vmem", pk[0]], [kob])
                    kb.mm(bankO, vmem[:, 1, ft * 128:(ft + 1) * 128], PT[:, 2 * h + 1, :], False, True, ["vmem", pk[1]], [kob])
                    yield
                    kb.tt("dve", oTb[:, ft, :], bankO, rc[:], ALU.mult, [kob, krc], [f"oTb.{ft}"])
                    BK.put(b)
                    yield

            def gen_attn(blk):
                bs = slice(blk * 512, (blk + 1) * 512)
                for ft in range(KC):
                    b = BK.get()
                    bank, bk = BK.f32(b), BK.key(b)
                    for kc in range(KC):
                        kb.mm(bank, W1[:, kc, ft * 128:(ft + 1) * 128], hT[:, kc, bs], kc == 0, kc == KC - 1, ["WA.1", f"hT.{blk}"], [bk])
                    yield
                    kb.cpalt(qTb[:, ft, :], bank, [bk], [f"qTb.{ft}"])
                    BK.put(b)
                    yield
                if blk == 3:
                    mlp_load(0)
                for pair in range(1):
                    gs = [gen_head(h, h) for h in range(4)]
                    while gs:
                        for g in list(gs):
                            try:
                                next(g)
                            except StopIteration:
                                gs.remove(g)
                        yield
                for tl in range(4):
                    t = blk * 4 + tl
                    for hf in range(2):
                        b = BK.get()
                        bank, bk = BK.f32(b), BK.key(b)
                        for kc in range(KC):
                            kb.mm(bank, oTb[:, kc, tl * 128:(tl + 1) * 128], W0[:, kc, hf * 512:(hf + 1) * 512], kc == 0, kc == KC - 1,
                                  [f"oTb.{kc}", "WA.0"], [bk])
                        yield
                        xs = xres[:, t, hf * 512:(hf + 1) * 512]
                        kb.tt("dve", xs, bank, xs, ALU.add, [bk, f"xres.{t}"], [f"xres.{t}"])
                        BK.put(b)
                        yield

            def norm_worker(blk):
                if blk + 1 < 4:
                    yield from norm_x(blk + 1)
                if blk - 1 >= 0:
                    yield from norm_m(blk - 1)

            s3 = s2
            aT_all = sb("aT_all", [128, 8, 512], BF16, st=s3)[:]
            rl_p = [sb(f"rl{i}", [128, 512], BF16, st=s3) for i in range(2)]
            o_p = [oTb_t[:, 2048 * i:2048 * (i + 1)].bitcast(F32) for i in range(2)]
            upv = wupm_d.rearrange("(kc p) n -> p kc n", p=128)
            dnv = wdn_d.rearrange("(f p) n -> p f n", p=128)
            ov = out_d.rearrange("(t p) d -> p t d", p=128)

            def mlp_w(j):
                s = (j + 1) % 2
                Wu = WA[:, s * 8192:s * 8192 + 4096].rearrange("p (k n) -> p k n", k=KC)
                Wd = WA[:, s * 8192 + 4096:(s + 1) * 8192].rearrange("p (f n) -> p f n", f=4)
                return s, Wu, Wd

            def mlp_load(j):
                s, Wu, Wd = mlp_w(j)
                kb.dma("pool", Wu, upv[:, :, j * 512:(j + 1) * 512], [], [f"WA.{s}"])
                kb.dma("pool", Wd, dnv[:, 4 * j:4 * j + 4, :], [], [f"WA.{s}"])

            for blk in range(4):
                interleave([gen_attn(blk), norm_worker(blk)])
            run(norm_m(3))

            NJ = 8
            ai = 0
            ri = 0
            for j in range(NJ):
                s, Wu, Wd = mlp_w(j)
                if j > 0:
                    mlp_load(j)
                for blk in range(4):
                    bs = slice(blk * 512, (blk + 1) * 512)
                    ab = (ai % 2) * 4
                    ai += 1
                    for fft in range(4):
                        b = BK.get()
                        bank, bk = BK.f32(b), BK.key(b)
                        for kc in range(KC):
                            kb.mm(bank, Wu[:, kc, fft * 128:(fft + 1) * 128], hT[:, kc, bs], kc == 0, kc == KC - 1, [f"WA.{s}", f"hT.{blk}"], [bk])
                        rl, krl = rl_p[ri % 2], f"rl.{ri % 2}"
                        ri += 1
                        kb.act(rl[:], bank, AF.Relu, [bk], [krl])
                        BK.put(b)
                        kb.tt("dve", aT_all[:, ab + fft, :], rl[:], rl[:], ALU.mult, [krl], [f"aT.{ab + fft}"])
                    for tl in range(4):
                        t = blk * 4 + tl
                        for hf in range(2):
                            b = BK.get()
                            bank, bk = BK.f32(b), BK.key(b)
                            for fft in range(4):
                                kb.mm(bank, aT_all[:, ab + fft, tl * 128:(tl + 1) * 128], Wd[:, fft, hf * 512:(hf + 1) * 512], fft == 0, fft == 3,
                                      [f"aT.{ab + fft}", f"WA.{s}"], [bk])
                            xs = xres[:, t, hf * 512:(hf + 1) * 512]
                            kb.tt("dve", xs, bank, xs, ALU.add, [bk, f"xres.{t}"], [f"xres.{t}"])
                            BK.put(b)
                        if j == NJ - 1:
                            c = 32 + t
                            kb.act(sqj[:], xres[:, t, :], AF.Square, [f"xres.{t}"], ["sqj", f"ss{c}"], accum=ss[:, c:c + 1])
                            kb.ts("dve", ss[:, c:c + 1], ss[:, c:c + 1], 1.0 / D, 1e-6, ALU.mult, ALU.add, [f"ss{c}"], [f"ss{c}"])
                            kb.tt("pool", ss[:, c:c + 1], ss[:, c:c + 1], mhalf[:, 0:1], ALU.pow, [f"ss{c}", "mhalf"], [f"ss{c}"])
                            ot, ko = o_p[t % 2], f"ot.{t % 2}"
                            kb.stt(ot, xres[:, t, :], ss[:, c:c + 1], gfin[:], ALU.mult, ALU.mult, [f"xres.{t}", f"ss{c}", "gfin"],
                                   [ko] + [f"oTb.{k}" for k in range(KC)])
                            kb.dma("sp", ov[:, t, :], ot, [ko], [f"out.{t}"])
            P.barrier()
            P.flush()


def _consts():
    cq = np.zeros((128, NCQ), np.float32)
    i = np.arange(128)
    cq[:, 0:128] = np.eye(128, dtype=np.float32)
    strict = (i[:, None] < i[None, :]).astype(np.float32)
    incl = (i[:, None] <= i[None, :]).astype(np.float32)
    cq[:, 128:256] = strict
    cq[:, 256:384] = incl
    cq[:, 384:512] = strict
    cq[:, 512:640] = incl
    cq[:, 640:768] = (i[:, None] > i[None, :]).astype(np.float32)
    cq[:, 768:896] = ((i[:, None] // 64) == (i[None, :] // 64)).astype(np.float32)
    cq[:, 896:1024] = 1.0
    gam = 1.0 - 2.0 ** (-5.0 - np.arange(8, dtype=np.float64))
    xi = gam[None, :] ** (i[:, None] + 1.0)
    kz = 0.125 * gam[None, :] ** (-(i[:, None] + 1.0))
    gC = np.zeros((128, 4))
    for ct in range(4):
        for p in range(128):
            gC[p, ct] = gam[2 * ct + p // 64] ** 128.0
    invf = (10000.0 ** (-np.arange(32, dtype=np.float32) / 32.0)).astype(np.float32)
    return cq, xi.astype(np.float32), kz.astype(np.float32), gC.astype(np.float32), np.broadcast_to(invf[None], (128, 32))


def _col(v):
    v = np.asarray(v, np.float32).reshape(-1)
    return v.reshape(-1, 128).T


def make_inputs(inp):
    cq, xi, kz, gC, invf = _consts()
    cols = np.zeros((128, NCOLS), np.float32)
    cols[:, 0:8] = _col(inp["norm_mix"][0])
    cols[:, 8:16] = _col(inp["norm_xattn"][0])
    cols[:, 16:24] = _col(inp["norm_mem"][0])
    cols[:, 24:32] = _col(inp["norm_mlp"][0])
    cols[:, 32:36] = _col(inp["ret_gn_w"][0])
    cols[:, 36:40] = _col(inp["ret_gn_b"][0])
    mu = np.asarray(inp["rwkv_mu"][0], np.float32)
    cols[:, 40:54] = _col(mu[:1792])
    cols[:, 54] = mu[1696:1824]
    cols[:, 55:59] = _col(inp["rwkv_w0"][0])
    cols[:, 59:63] = _col(inp["rwkv_a0"][0])
    cols[:, 63:67] = _col(inp["rwkv_k_k"][0])
    cols[:, 67:71] = _col(inp["rwkv_k_a"][0])
    cols[:, 71:75] = _col(inp["rwkv_r_k"][0])
    cols[:, 75:79] = _col(inp["rwkv_gn_w"][0])
    cols[:, 79:83] = _col(inp["rwkv_gn_b"][0])
    cols[:, 83:91] = xi
    cols[:, 91:99] = kz
    cols[:, 99:103] = gC
    cols[:, 103:135] = invf
    shared = {
        "cols": cols, "cq": cq,
        "gfin": np.ascontiguousarray(inp["norm_final"], np.float32),
        "w_in": np.ascontiguousarray(inp["w_in"][0]),
        "w_up": np.ascontiguousarray(inp["rwkv_w_up"][0]),
        "a_up": np.ascontiguousarray(inp["rwkv_a_up"][0]),
        "g_up": np.ascontiguousarray(inp["rwkv_g_up"][0]),
        "w_out": np.ascontiguousarray(inp["w_out"][0]),
        "wq": np.ascontiguousarray(inp["xattn_w_q"][0]),
        "wkv": np.ascontiguousarray(inp["xattn_w_kv"][0]),
        "wo": np.ascontiguousarray(inp["xattn_w_o"][0]),
        "mlp_up": np.ascontiguousarray(inp["mlp_w_up"][0]),
        "mlp_down": np.ascontiguousarray(inp["mlp_w_down"][0]),
    }
    maps = []
    for b in range(8):
        m = dict(shared)
        m["x"] = np.ascontiguousarray(inp["x"][b], np.float32)
        m["mem"] = np.ascontiguousarray(inp["mem"][b], np.float32)
        m["pos"] = np.ascontiguousarray(np.asarray(inp["positions"][b], np.int32).reshape(NT, 128).T)
        maps.append(m)
    return maps


_NC_CACHE = {}


def kernel(**inputs):
    inp = {k: np.asarray(v) for k, v in inputs.items()}
    maps = make_inputs(inp)
    if "nc" not in _NC_CACHE:
        _NC_CACHE["nc"] = build()
    res = run_bass_kernel_spmd(_NC_CACHE["nc"], maps, core_ids=list(range(8)))
    return np.stack([np.asarray(r["out"], np.float32) for r in res.results], axis=0)
```

```python
import contextlib
import math
import numpy as np
import concourse.bass as bass
import concourse.mybir as mybir
from concourse.bass_utils import run_bass_kernel_spmd

F32 = mybir.dt.float32
BF16 = mybir.dt.bfloat16
I32 = mybir.dt.int32
AF = mybir.ActivationFunctionType
ALU = mybir.AluOpType
AX = mybir.AxisListType

T = 2048
D = 1024
NT = 16
KC = 8
NCOLS = 135
NCQ = 1024
CUT = 99
COMPUTE = ("pe", "act", "dve", "pool")
ISSUERS = ("pe", "act", "dve", "pool", "sp")


class Prog:
    def __init__(self, nc, st, n_dma=12, self_sync=True):
        self.nc = nc
        self.n_dma = n_dma
        self.self_sync = self_sync
        self.chans = list(COMPUTE) + [f"d{i}" for i in range(n_dma)]
        self.sems = {c: st.enter_context(nc.semaphore(f"s_{c}")) for c in self.chans}
        self.streams = {e: [] for e in ISSUERS}
        self.count = {c: 0 for c in self.chans}
        self.clock = {e: {} for e in ISSUERS}
        self.snap = {}
        self.wr = {}
        self.rd = {}
        self.rr = 0
        self.nops = 0
        self.nwaits = 0

    @staticmethod
    def _need(needs, d):
        for c, n in d.items():
            if needs.get(c, 0) < n:
                needs[c] = n

    def op(self, eng, fn, reads=(), writes=(), dma=False):
        needs = {}
        for k in reads:
            self._need(needs, self.wr.get(k, {}))
        for k in writes:
            self._need(needs, self.wr.get(k, {}))
            self._need(needs, self.rd.get(k, {}))
        if dma:
            chan = f"d{self.rr % self.n_dma}"
            self.rr += 1
            if self.count[chan]:
                self._need(needs, {chan: self.count[chan]})
        else:
            chan = eng
        clk = self.clock[eng]
        waits = []
        for c, n in needs.items():
            if c == eng and (eng == "pe" or not self.self_sync):
                continue
            if clk.get(c, 0) < n:
                waits.append((c, n))
                for c2, n2 in self.snap[(c, n)].items():
                    if clk.get(c2, 0) < n2:
                        clk[c2] = n2
        self.nwaits += len(waits)
        self.nops += 1
        n_new = self.count[chan] + 1
        self.count[chan] = n_new
        s = dict(clk)
        s[chan] = n_new
        self.snap[(chan, n_new)] = s
        if eng == "pe" and chan == "pe":
            clk["pe"] = n_new
        wset = set(writes)
        for k in wset:
            self.wr[k] = {chan: n_new}
            self.rd[k] = {}
        for k in reads:
            if k in wset:
                continue
            self.rd.setdefault(k, {})[chan] = n_new
        self.streams[eng].append((waits, fn, chan))

    def barrier(self):
        allk = {c: n for c, n in self.count.items() if n}
        for e in ISSUERS:
            clk = self.clock[e]
            waits = []
            for c, n in allk.items():
                if c == e and e == "pe":
                    continue
                if clk.get(c, 0) < n:
                    waits.append((c, n))
            if waits:
                self.streams[e].append((waits, None, None))
        for e in ISSUERS:
            for c, n in allk.items():
                if self.clock[e].get(c, 0) < n:
                    self.clock[e][c] = n

    def flush(self):
        nc = self.nc
        streams = self.streams
        if not any(streams[e] for e in ISSUERS):
            return
        self.streams = {e: [] for e in ISSUERS}
        sems = self.sems

        def val(c, n):
            return n * 16 if c.startswith("d") else n

        def run(engname):
            def body(e):
                for waits, fn, chan in streams[engname]:
                    for c, n in waits:
                        e.wait_ge(sems[c], val(c, n))
                    if fn is not None:
                        fn(e).then_inc(sems[chan], 16 if chan.startswith("d") else 1)
            return body

        with nc.Block() as block:
            block.tensor(run("pe"))
            block.scalar(run("act"))
            block.vector(run("dve"))
            block.gpsimd(run("pool"))
            block.sync(run("sp"))


class Ring:
    def __init__(self, kb, name, n, shape, dtype, st=None, psum=False):
        st = st or kb.st
        alloc = kb.nc.psum_tensor if psum else kb.nc.sbuf_tensor
        self.name = name
        self.t = [st.enter_context(alloc(f"rg_{name}{i}", shape, dtype)) for i in range(n)]
        self.i = 0

    def next(self):
        i = self.i % len(self.t)
        self.i += 1
        return self.t[i], f"{self.name}.{i}"


class ViewRing:
    def __init__(self, aps, keys):
        self.t, self.k, self.i = aps, keys, 0

    def next(self):
        i = self.i % len(self.t)
        self.i += 1
        return self.t[i], self.k[i]


class Banks:
    def __init__(self, pp):
        import collections
        self.pp = pp
        self.free = collections.deque(range(8))

    def get(self):
        return self.free.popleft()

    def put(self, i):
        self.free.append(i)

    def f32(self, i):
        return self.pp[:, i, :]

    def bf(self, i):
        return self.pp[:, i, :].bitcast(BF16)

    @staticmethod
    def key(i):
        return f"ps{i}"


def interleave(gens):
    gens = list(gens)
    while gens:
        for g in list(gens):
            try:
                next(g)
            except StopIteration:
                gens.remove(g)


class KB:
    def __init__(self, nc, P, st):
        self.nc, self.P, self.st = nc, P, st
        self.flip = 0

    def sb(self, name, shape, dtype, st=None):
        return (st or self.st).enter_context(self.nc.sbuf_tensor("sb_" + name, shape, dtype))

    def mm(self, out, lhsT, rhs, start, stop, r, w):
        self.P.op("pe", lambda e: e.matmul(out, lhsT=lhsT, rhs=rhs, start=start, stop=stop), r, w)

    def tr(self, out, in_, r, w):
        idb = self.identb[:]
        self.P.op("pe", lambda e: e.transpose(out=out, in_=in_, identity=idb), list(r) + ["identb"], w)

    def act(self, out, in_, func, r, w, scale=1.0, bias=0.0, accum=None):
        kw = {}
        if accum is not None:
            kw["accum_out"] = accum
        self.P.op("act", lambda e: e.activation(out=out, in_=in_, func=func, bias=bias, scale=scale, **kw), r, w)

    def tt(self, eng, out, in0, in1, op, r, w):
        self.P.op(eng, lambda e: e.tensor_tensor(out=out, in0=in0, in1=in1, op=op), r, w)

    def ts(self, eng, out, in0, s1, s2, op0, op1, r, w):
        if s2 is None:
            self.P.op(eng, lambda e: e.tensor_scalar(out=out, in0=in0, scalar1=s1, scalar2=None, op0=op0), r, w)
        else:
            self.P.op(eng, lambda e: e.tensor_scalar(out=out, in0=in0, scalar1=s1, scalar2=s2, op0=op0, op1=op1), r, w)

    def stt(self, out, in0, sc, in1, op0, op1, r, w):
        self.P.op("dve", lambda e: e.scalar_tensor_tensor(out=out, in0=in0, scalar=sc, in1=in1, op0=op0, op1=op1), r, w)

    def cp(self, eng, out, in_, r, w):
        if eng == "act":
            self.P.op("act", lambda e: e.activation(out=out, in_=in_, func=AF.Copy), r, w)
        else:
            self.P.op(eng, lambda e: e.tensor_copy(out=out, in_=in_), r, w)

    def cpalt(self, out, in_, r, w):
        self.flip ^= 1
        self.cp("act" if self.flip else "dve", out, in_, r, w)

    def red(self, out, in_, op, r, w):
        self.P.op("dve", lambda e: e.tensor_reduce(out=out, in_=in_, axis=AX.X, op=op), r, w)

    def dma(self, eng, out, in_, r, w):
        self.P.op(eng, lambda e: e.dma_start(out=out, in_=in_), r, w, dma=True)


def build(stages="ABCD", dbg=False):
    nc = bass.Bass("TRN2", target_bir_lowering=False)

    def dram(n, s, d, kind="ExternalInput"):
        return nc.dram_tensor(n, s, d, kind=kind).ap()

    x_d = dram("x", [T, D], F32)
    mem_d = dram("mem", [256, D], F32)
    pos_d = dram("pos", [128, NT], I32)
    cols_d = dram("cols", [128, NCOLS], F32)
    cq_d = dram("cq", [128, NCQ], F32)
    gfin_d = dram("gfin", [D], F32)
    w_in_d = dram("w_in", [D, 3872], F32)
    wup_d = dram("w_up", [64, 512], F32)
    aup_d = dram("a_up", [64, 512], F32)
    gup_d = dram("g_up", [160, 512], F32)
    w_out_d = dram("w_out", [D, D], F32)
    wq_d = dram("wq", [D, D], F32)
    wkv_d = dram("wkv", [D, 2 * D], F32)
    wo_d = dram("wo", [D, D], F32)
    wupm_d = dram("mlp_up", [D, 4 * D], F32)
    wdn_d = dram("mlp_down", [4 * D, D], F32)
    out_d = dram("out", [T, D], F32, kind="ExternalOutput")
    if dbg:
        dbg_yT = dram("dbg_yT", [128, KC, T], BF16, kind="ExternalOutput")
        dbg_hT = dram("dbg_hT", [128, KC, T], BF16, kind="ExternalOutput")

    def wview(w):
        return w.rearrange("(kc p) n -> p kc n", p=128)

    with contextlib.ExitStack() as st:
        P = Prog(nc, st)
        kb = KB(nc, P, st)
        cols = kb.sb("cols", [128, NCOLS], F32)
        dc = kb.sb("dcols", [128, 40], F32)
        cqf = kb.sb("cqf", [128, NCQ], F32)
        identb = kb.sb("identb", [128, 128], BF16)
        bonesb = kb.sb("bonesb", [128, 128], BF16)
        onesb = kb.sb("onesb", [128, 128], BF16)
        mhalf = kb.sb("mhalf", [128, 256], F32)
        hT = kb.sb("hT", [128, KC, T], BF16)
        yT = kb.sb("yT", [128, 4, T], BF16)
        WA = kb.sb("WA", [128, 16384], BF16)
        ss = kb.sb("ss", [128, 3 * NT], F32)
        kb.identb = identb
        pp = st.enter_context(nc.psum_tensor("pp", [128, 8, 512], F32))
        L_BK = Banks(pp)
        psA = ViewRing([pp[:, i, :] for i in range(6)], [f"ps{i}" for i in range(6)])
        psT = ViewRing([pp[:, i, :].bitcast(BF16) for i in (6, 7)], ["ps6", "ps7"])
        mask4 = cqf[:, 128:640]
        maskI = cqf[:, 256:384]
        maskLs = cqf[:, 640:768]
        identf = cqf[:, 0:128]

        kb.dma("sp", cols[:], cols_d, [], ["cols"])
        kb.dma("sp", cqf[:], cq_d, [], ["cqf"])
        kb.dma("pool", identb[:], cq_d[:, 0:128], [], ["identb"])
        kb.dma("pool", bonesb[:], cq_d[:, 768:896], [], ["bonesb"])
        kb.dma("pool", onesb[:], cq_d[:, 896:1024], [], ["onesb"])
        P.op("dve", lambda e: e.memset(mhalf[:], -0.5), [], ["mhalf"])
        kb.ts("dve", dc[:, 0:15], cols[:, 40:55], -1.0, 1.0, ALU.mult, ALU.add, ["cols"], ["dc"])
        kb.ts("dve", dc[:, 15:19], cols[:, 55:59], 0.5, None, ALU.mult, None, ["cols"], ["dc"])
        kb.ts("dve", dc[:, 19:23], cols[:, 59:63], 0.5, None, ALU.mult, None, ["cols"], ["dc"])
        kb.ts("dve", dc[:, 23:27], cols[:, 67:71], -1.0, None, ALU.mult, None, ["cols"], ["dc"])
        kb.ts("dve", dc[:, 27:31], cols[:, 67:71], -1.0, 1.0, ALU.mult, ALU.add, ["cols"], ["dc"])

        def norm_T(src, nblk, tpb, gcol0, dst, dstkey, sscol0, hb, sqj):
            for blk in range(nblk):
                hbs = []
                for tl in range(tpb):
                    t = blk * tpb + tl
                    xa, xk = src(t)
                    c = sscol0 + t
                    kb.act(sqj[:], xa, AF.Square, [xk], ["sqj", f"ss{c}"], accum=ss[:, c:c + 1])
                    kb.ts("dve", ss[:, c:c + 1], ss[:, c:c + 1], 1.0 / D, 1e-6, ALU.mult, ALU.add, [f"ss{c}"], [f"ss{c}"])
                    kb.tt("pool", ss[:, c:c + 1], ss[:, c:c + 1], mhalf[:, 0:1], ALU.pow, [f"ss{c}", "mhalf"], [f"ss{c}"])
                    h, hk = hb.next()
                    kb.act(h[:], xa, AF.Copy, [xk, f"ss{c}"], [hk], scale=ss[:, c:c + 1])
                    hbs.append((h, hk))
                for kc in range(KC):
                    bank, bk = psT.next()
                    for tl, (h, hk) in enumerate(hbs):
                        kb.tr(bank[:, tl * 128:(tl + 1) * 128], h[:, kc * 128:(kc + 1) * 128], [hk], [bk])
                    n = tpb * 128
                    o = dst[:, kc, blk * n:(blk + 1) * n]
                    kb.flip ^= 1
                    if kb.flip:
                        kb.act(o, bank[:, 0:n], AF.Copy, [bk, "cols"], [dstkey(blk)], scale=cols[:, gcol0 + kc:gcol0 + kc + 1])
                    else:
                        kb.ts("dve", o, bank[:, 0:n], cols[:, gcol0 + kc:gcol0 + kc + 1], None, ALU.mult, None, [bk, "cols"], [dstkey(blk)])

        def load_w(dst, src, key, eng="pool"):
            kb.dma(eng, dst, src, [], [key])

        Wv8 = lambda ncol: WA[:, 0:KC * ncol].rearrange("p (k n) -> p k n", k=KC)

        Wr = Wv8(2048)
        BK = L_BK

        def acquire(n=1):
            spins = 0
            while len(BK.free) < n:
                spins += 1
                assert spins < 100000, "PSUM bank pool deadlock"
                yield
            return [BK.get() for _ in range(n)]

        def rr(gens):
            gens = list(gens)
            while gens:
                for g in list(gens):
                    try:
                        next(g)
                    except StopIteration:
                        gens.remove(g)
                yield

        def run(g):
            for _ in g:
                pass

        if "B" not in stages:
            with contextlib.ExitStack() as sa:
                xring = Ring(kb, "xr", 3, [128, D], F32, st=sa)
                hb = Ring(kb, "hb", 8, [128, D], BF16, st=sa)
                sqj = kb.sb("sqj", [128, D], BF16, st=sa)

                def srcx(t):
                    xa, xk = xring.next()
                    kb.dma("sp", xa[:], x_d[t * 128:(t + 1) * 128, :], [], [xk])
                    return xa[:], xk
                norm_T(srcx, 4, 4, 0, hT, lambda b: f"hT.{b}", 0, hb, sqj)
                P.barrier()
                P.flush()
            P.op("dve", lambda e: e.memset(yT[:, 0:4, :], 0.0), [], [f"yT.{t}.0" for t in range(NT)])
        else:
            for gi in range(4):
                load_w(Wr[:, :, gi * 512:(gi + 1) * 512], wview(w_in_d)[:, :, gi * 512:(gi + 1) * 512], f"W.{gi}")
            with contextlib.ExitStack() as sbk:
                cos_t = kb.sb("cos_t", [128, NT, 32], F32, st=sbk)
                sin_t = kb.sb("sin_t", [128, NT, 32], F32, st=sbk)
                srope = sbk
                if True:
                    posi = kb.sb("posi", [128, NT], I32, st=srope)
                    posf = kb.sb("posf", [128, NT], F32, st=srope)
                    ang = kb.sb("ang", [128, NT, 32], F32, st=srope)
                    ra = kb.sb("ra", [128, NT, 32], F32, st=srope)
                    rb_ = kb.sb("rb_", [128, NT, 32], F32, st=srope)
                    ri = kb.sb("ri", [128, NT, 32], I32, st=srope)
                    kb.dma("sp", posi[:], pos_d, [], ["posi"])
                    kb.cp("dve", posf[:], posi[:], ["posi"], ["posf"])
                    kb.tt("dve", ang[:], posf[:].unsqueeze(2).to_broadcast([128, NT, 32]),
                          cols[:, 103:135].unsqueeze(1).to_broadcast([128, NT, 32]), ALU.mult, ["posf", "cols"], ["ang"])
                    C1 = 6.28125
                    C2 = 2.0 * math.pi - C1
                    for tab, shift, nm in ((sin_t, 0.0, "sin"), (cos_t, 0.5 * math.pi, "cos")):
                        kb.ts("dve", ra[:], ang[:], shift, 1.0 / (2.0 * math.pi), ALU.add, ALU.mult, ["ang"], ["ra"])
                        kb.cp("dve", ri[:], ra[:], ["ra"], ["ri"])
                        kb.cp("dve", rb_[:], ri[:], ["ri"], ["rb"])
                        kb.ts("dve", ra[:], ang[:], shift, None, ALU.add, None, ["ang"], ["ra"])
                        kb.stt(ra[:], rb_[:], -C1, ra[:], ALU.mult, ALU.add, ["rb", "ra"], ["ra"])
                        kb.stt(ra[:], rb_[:], -C2, ra[:], ALU.mult, ALU.add, ["rb", "ra"], ["ra"])
                        kb.ts("dve", rb_[:], ra[:], math.pi, -2.0 * math.pi, ALU.is_gt, ALU.mult, ["ra"], ["rb"])
                        kb.tt("dve", ra[:], ra[:], rb_[:], ALU.add, ["ra", "rb"], ["ra"])
                        kb.ts("dve", rb_[:], ra[:], -math.pi, 2.0 * math.pi, ALU.is_lt, ALU.mult, ["ra"], ["rb"])
                        kb.tt("dve", ra[:], ra[:], rb_[:], ALU.add, ["ra", "rb"], ["ra"])
                        kb.act(tab[:], ra[:], AF.Sin, ["ra"], [nm])

                sb_ = lambda n, shp, d: kb.sb(n, shp, d, st=sbk)
                W = 2
                xr_p = [sb_(f"xr{i}", [128, D], F32) for i in range(2)]
                hb_p = [sb_(f"hb{i}", [128, D], BF16) for i in range(4)]
                sqj = sb_("sqj", [128, D], BF16)
                qT_p = [sb_(f"qT{i}", [128, 4, 2, 512], BF16) for i in range(2)]
                for i in range(2):
                    P.op("dve", lambda e, i=i: e.memset(qT_p[i][:], 0.0), [], [f"qT{i}.{j}" for j in range(4)])
                kT_p = [sb_(f"kT{i}", [128, 4, 512], BF16) for i in range(2)]
                ktok_p = [sb_(f"ktok{i}", [128, 4, 512], BF16) for i in range(2)]
                vtok_p = [sb_(f"vtok{i}", [128, 4, 512], BF16) for i in range(2)]
                gateT_p = [sb_(f"gateT{i}", [128, 4, 512], BF16) for i in range(2)]
                xs_p = [sb_(f"xs{i}", [128, 512], F32) for i in range(W)]
                A_p = [sb_(f"rA{i}", [128, 512], F32) for i in range(W)]
                B_p = [sb_(f"rB{i}", [128, 512], F32) for i in range(W)]
                qtok_p = [sb_(f"qtok{i}", [128, 512], BF16) for i in range(W)]
                th_p = [sb_(f"thr{i}", [128, 512], F32) for i in range(2)]
                sT_p = [sb_(f"sT{i}", [128, 8, 128], BF16) for i in range(W)]
                y32_p = [sb_(f"y32{i}", [128, 512], F32) for i in range(W)]
                sq_p = [sb_(f"sqr{i}", [128, 512], F32) for i in range(W)]
                ynb_p = [sb_(f"ynb{i}", [128, 512], BF16) for i in range(W)]
                yaff_p = [sb_(f"yaff{i}", [128, 2, 128], F32) for i in range(W)]
                stat_p = [sb_(f"stat{i}", [128, 64], F32) for i in range(W)]
                R32 = sb_("R32", [128, 4, 64], F32)
                Rbf = sb_("Rbf", [128, 5, 4, 64], BF16)
                P.op("dve", lambda e: e.memset(R32[:], 0.0), [], ["R32"])

                def gen_normA(blk):
                    for tl in range(4):
                        t = blk * 4 + tl
                        xa, xk = xr_p[t % 2], f"xr.{t % 2}"
                        kb.dma("sp", xa[:], x_d[t * 128:(t + 1) * 128, :], [], [xk])
                        kb.act(sqj[:], xa[:], AF.Square, [xk], ["sqj", f"ss{t}"], accum=ss[:, t:t + 1])
                        yield
                        kb.ts("dve", ss[:, t:t + 1], ss[:, t:t + 1], 1.0 / D, 1e-6, ALU.mult, ALU.add, [f"ss{t}"], [f"ss{t}"])
                        yield
                        kb.tt("pool", ss[:, t:t + 1], ss[:, t:t + 1], mhalf[:, 0:1], ALU.pow, [f"ss{t}", "mhalf"], [f"ss{t}"])
                        yield
                        kb.act(hb_p[tl][:], xa[:], AF.Copy, [xk, f"ss{t}"], [f"hb.{tl}"], scale=ss[:, t:t + 1])
                        yield
                    for kc in range(KC):
                        (b,) = yield from acquire(1)
                        bankT, bk = BK.bf(b), BK.key(b)
                        for tl in range(4):
                            kb.tr(bankT[:, tl * 128:(tl + 1) * 128], hb_p[tl][:, kc * 128:(kc + 1) * 128], [f"hb.{tl}"], [bk])
                        yield
                        o = hT[:, kc, blk * 512:(blk + 1) * 512]
                        if kc % 2:
                            kb.act(o, bankT[:, 0:512], AF.Copy, [bk, "cols"], [f"hT.{blk}"], scale=cols[:, kc:kc + 1])
                        else:
                            kb.ts("dve", o, bankT[:, 0:512], cols[:, kc:kc + 1], None, ALU.mult, None, [bk, "cols"], [f"hT.{blk}"])
                        BK.put(b)
                        yield

                def post_gen(slot, b, eps, wcol0, bcol0, finish):
                    bank, bk = BK.f32(b), BK.key(b)
                    y32, k1 = y32_p[slot], f"y32.{slot}"
                    kb.cp("act", y32[:], bank, [bk], [k1])
                    BK.put(b)
                    yield
                    stt_, ks = stat_p[slot], f"stat.{slot}"
                    v3 = lambda a: a.rearrange("p (h d) -> p h d", d=64)
                    bc = lambda a: a.unsqueeze(2).to_broadcast([128, 8, 64])
                    kb.red(stt_[:, 0:8], v3(y32[:]), ALU.add, [k1], [ks])
                    sq, k2 = sq_p[slot], f"sq.{slot}"
                    kb.act(sq[:], y32[:], AF.Square, [k1], [k2])
                    yield
                    kb.red(stt_[:, 8:16], v3(sq[:]), ALU.add, [k2], [ks])
                    yield
                    kb.ts("dve", stt_[:, 16:24], stt_[:, 0:8], 1.0 / 64, None, ALU.mult, None, [ks], [ks])
                    yield
                    kb.tt("dve", stt_[:, 24:32], stt_[:, 16:24], stt_[:, 16:24], ALU.mult, [ks], [ks])
                    kb.ts("dve", stt_[:, 32:40], stt_[:, 8:16], 1.0 / 64, eps, ALU.mult, ALU.add, [ks], [ks])
                    yield
                    kb.tt("dve", stt_[:, 40:48], stt_[:, 32:40], stt_[:, 24:32], ALU.subtract, [ks], [ks])
                    yield
                    kb.tt("pool", stt_[:, 48:56], stt_[:, 40:48], mhalf[:, 0:8], ALU.pow, [ks, "mhalf"], [ks])
                    yield
                    kb.stt(stt_[:, 56:64], stt_[:, 16:24], -1.0, stt_[:, 48:56], ALU.mult, ALU.mult, [ks], [ks])
                    kb.tt("pool", v3(y32[:]), v3(y32[:]), bc(stt_[:, 48:56]), ALU.mult, [k1, ks], [k1])
                    yield
                    ynb, k3 = ynb_p[slot], f"ynb.{slot}"
                    kb.tt("dve", v3(ynb[:]), v3(y32[:]), bc(stt_[:, 56:64]), ALU.add, [k1, ks], [k3])
                    yield
                    (b2,) = yield from acquire(1)
                    bankT, kt = BK.bf(b2), BK.key(b2)
                    for ct in range(4):
                        kb.tr(bankT[:, ct * 128:(ct + 1) * 128], ynb[:, ct * 128:(ct + 1) * 128], [k3], [kt])
                    yield
                    for ct in range(4):
                        ya, k4 = yaff_p[slot][:, ct % 2, :], f"yaff.{slot}.{ct % 2}"
                        kb.act(ya, bankT[:, ct * 128:(ct + 1) * 128], AF.Identity, [kt, "cols"], [k4],
                               scale=cols[:, wcol0 + ct:wcol0 + ct + 1], bias=cols[:, bcol0 + ct:bcol0 + ct + 1])
                        finish(ct, ya, k4)
                        if ct == 3:
                            BK.put(b2)
                        yield

                def qk_chain(slot, blk, tl, wi):
                    par = blk % 2
                    t = blk * 4 + tl
                    tok = slice(t * 128, (t + 1) * 128)
                    hk = f"hT.{blk}"
                    tabcol = 83 if wi == 0 else 91
                    (b,) = yield from acquire(1)
                    bank, bk = BK.f32(b), BK.key(b)
                    for kc in range(KC):
                        kb.mm(bank, hT[:, kc, tok], Wr[:, kc, wi * 512:(wi + 1) * 512], kc == 0, kc == KC - 1, [hk, f"W.{wi}"], [bk])
                    yield
                    xs, kx = xs_p[slot], f"xs.{slot}"
                    kb.cp("act", xs[:], bank, [bk], [kx])
                    BK.put(b)
                    yield
                    A, ka = A_p[slot], f"rA.{slot}"
                    B, kbk = B_p[slot], f"rB.{slot}"
                    v4 = lambda a: a.rearrange("p (h two f) -> p h two f", two=2, f=32)
                    cs4 = cos_t[:, t, :].unsqueeze(1).unsqueeze(1).to_broadcast([128, 8, 2, 32])
                    sn3 = sin_t[:, t, :].unsqueeze(1).to_broadcast([128, 8, 32])
                    kb.tt("dve", v4(A[:]), v4(xs[:]), cs4, ALU.mult, [kx, "cos"], [ka])
                    kb.tt("pool", v4(B[:])[:, :, 0, :], v4(xs[:])[:, :, 1, :], sn3, ALU.mult, [kx, "sin"], [kbk])
                    kb.tt("pool", v4(B[:])[:, :, 1, :], v4(xs[:])[:, :, 0, :], sn3, ALU.mult, [kx, "sin"], [kbk])
                    yield
                    kb.tt("dve", v4(A[:])[:, :, 0, :], v4(A[:])[:, :, 0, :], v4(B[:])[:, :, 0, :], ALU.subtract, [ka, kbk], [ka])
                    kb.tt("dve", v4(A[:])[:, :, 1, :], v4(A[:])[:, :, 1, :], v4(B[:])[:, :, 1, :], ALU.add, [ka, kbk], [ka])
                    yield
                    scl = cols[:, tabcol:tabcol + 8].unsqueeze(2).to_broadcast([128, 8, 64])
                    A3 = A[:].rearrange("p (h d) -> p h d", d=64)
                    if wi == 0:
                        qtv, kq = qtok_p[slot][:], f"qtok.{slot}"
                    else:
                        qtv, kq = ktok_p[par][:, tl, :], f"ktok{par}.{tl}"
                    kb.tt("dve", qtv.rearrange("p (h d) -> p h d", d=64), A3, scl, ALU.mult, [ka, "cols"], [kq])
                    yield
                    (b2,) = yield from acquire(1)
                    bankT, kt = BK.bf(b2), BK.key(b2)
                    for ct in range(4):
                        kb.tr(bankT[:, ct * 128:(ct + 1) * 128], qtv[:, ct * 128:(ct + 1) * 128], [kq], [kt])
                    yield
                    bv = bankT[:, 0:512].rearrange("p (c n) -> p c n", c=4)
                    if wi == 0:
                        kb.cp("act", qT_p[par][0:64, :, 0, tl * 128:(tl + 1) * 128], bv[0:64], [kt], [f"qT{par}.{tl}"])
                        kb.cp("dve", qT_p[par][64:128, :, 1, tl * 128:(tl + 1) * 128], bv[64:128], [kt], [f"qT{par}.{tl}"])
                    else:
                        kb.cpalt(kT_p[par][:, :, tl * 128:(tl + 1) * 128], bv, [kt], [f"kT{par}.{tl}"])
                    BK.put(b2)
                    yield

                def v_chain(blk, tl):
                    par = blk % 2
                    t = blk * 4 + tl
                    tok = slice(t * 128, (t + 1) * 128)
                    (b,) = yield from acquire(1)
                    bank, bk = BK.f32(b), BK.key(b)
                    for kc in range(KC):
                        kb.mm(bank, hT[:, kc, tok], Wr[:, kc, 1024:1536], kc == 0, kc == KC - 1, [f"hT.{blk}", "W.2"], [bk])
                    yield
                    kb.cpalt(vtok_p[par][:, tl, :], bank, [bk], [f"vtok{par}.{tl}"])
                    BK.put(b)
                    yield

                def g_chain(blk, ct):
                    par = blk % 2
                    (b,) = yield from acquire(1)
                    bank, bk = BK.f32(b), BK.key(b)
                    for kc in range(KC):
                        kb.mm(bank, Wr[:, kc, 1536 + ct * 128:1536 + (ct + 1) * 128], hT[:, kc, blk * 512:(blk + 1) * 512],
                              kc == 0, kc == KC - 1, [f"hT.{blk}", "W.3"], [bk])
                    yield
                    th, kth = th_p[ct % 2], f"thr.{ct % 2}"
                    kb.act(th[:], bank, AF.Tanh, [bk], [kth], scale=0.5)
                    yield
                    kb.ts("pool", th[:], th[:], 0.5, 0.5, ALU.mult, ALU.add, [kth], [kth])
                    yield
                    kb.tt("dve", gateT_p[par][:, ct, :], th[:], bank, ALU.mult, [kth, bk], [f"gateT{par}.{ct}"])
                    BK.put(b)
                    yield

                def gen_proj(blk):
                    for k in range(4):
                        yield from rr([qk_chain((2 * k) % W, blk, k, 0), qk_chain((2 * k + 1) % W, blk, k, 1), v_chain(blk, k), g_chain(blk, k)])

                def chunk_chain(slot, blk, tl):
                    par = blk % 2
                    qT, kT_, vtok, gateT = qT_p[par], kT_p[par], vtok_p[par], gateT_p[par]
                    t = blk * 4 + tl
                    tok = slice(t * 128, (t + 1) * 128)
                    tl_ = slice(tl * 128, (tl + 1) * 128)
                    sT, ksT = sT_p[slot], f"sT.{slot}"
                    bs = yield from acquire(2)
                    for g4 in range(2):
                        bank, bk = BK.f32(bs[g4]), BK.key(bs[g4])
                        for hh in range(4):
                            h = 4 * g4 + hh
                            kb.mm(bank[:, hh * 128:(hh + 1) * 128], kT_[:, h // 2, tl_], qT[:, h // 2, h % 2, tl_], True, True,
                                  [f"kT{par}.{tl}", f"qT{par}.{tl}"], [bk])
                    yield
                    for g4 in range(2):
                        bank, bk = BK.f32(bs[g4]), BK.key(bs[g4])
                        kb.tt("dve", sT[:, 4 * g4:4 * g4 + 4, :], bank.rearrange("p (c n) -> p c n", c=4),
                              maskI.unsqueeze(1).to_broadcast([128, 4, 128]), ALU.mult, [bk, "cqf"], [ksT])
                        BK.put(bs[g4])
                        yield
                    (bo,) = yield from acquire(1)
                    bankO, ko = BK.f32(bo), BK.key(bo)
                    for h in range(8):
                        kb.mm(bankO[:, 64 * h:64 * h + 64], sT[:, h, :], vtok[:, tl, 64 * h:64 * h + 64], True, t == 0,
                              [ksT, f"vtok{par}.{tl}"], [ko])
                        if t > 0:
                            kb.mm(bankO[:, 64 * h:64 * h + 64], qT[:, h // 2, h % 2, tl_], Rbf[:, t % 5, h // 2, :], False, True,
                                  [f"qT{par}.{tl}", f"Rbf.{t % 5}"], [ko])
                    yield

                    def fin(ct, ya, k4):
                        kb.tt("dve", yT[:, ct, tok], ya, gateT[:, ct, tl_], ALU.mult, [k4, f"gateT{par}.{ct}"], [f"yT.{t}.0"])
                    yield from post_gen(slot, bo, 1e-5, 32, 36, fin)

                def gen_chunks(blk):
                    par = blk % 2
                    ktok, vtok = ktok_p[par], vtok_p[par]
                    for tl in range(4):
                        t = blk * 4 + tl
                        if t == NT - 1:
                            continue
                        (b,) = yield from acquire(1)
                        bankK, kk_ = BK.f32(b), BK.key(b)
                        for ct in range(4):
                            kb.mm(bankK[:, ct * 128:(ct + 1) * 128], ktok[:, tl, ct * 128:(ct + 1) * 128], vtok[:, tl, ct * 128:(ct + 1) * 128],
                                  True, True, [f"ktok{par}.{tl}", f"vtok{par}.{tl}"], [kk_])
                        yield
                        for hh in range(2):
                            b0 = 64 * hh
                            kb.tt("dve", R32[b0:b0 + 64, :, :], bankK[b0:b0 + 64, :].rearrange("p (c n) -> p c n", c=4)[:, :, b0:b0 + 64],
                                  R32[b0:b0 + 64, :, :], ALU.add, [kk_, "R32"], ["R32"])
                        BK.put(b)
                        yield
                        kb.tt("dve", R32[:], R32[:], cols[:, 99:103].unsqueeze(2).to_broadcast([128, 4, 64]), ALU.mult, ["R32", "cols"], ["R32"])
                        yield
                        kb.cp("act", Rbf[:, (t + 1) % 5, :, :], R32[:], ["R32"], [f"Rbf.{(t + 1) % 5}"])
                        yield
                    yield from rr([chunk_chain(0, blk, 0), chunk_chain(1, blk, 1)])
                    yield from rr([chunk_chain(0, blk, 2), chunk_chain(1, blk, 3)])

                run(gen_normA(0))
                interleave([gen_proj(0), gen_normA(1)])
                for blk in range(4):
                    if blk == 3 and "C" in stages:
                        Wk = Wv8(1824)
                        kb.dma("pool", Wk[:, :, :], wview(w_in_d)[:, :, 2048:3872], [], ["Wk", "W.0", "W.1", "W.2", "W.3"])
                    gs = [gen_chunks(blk)]
                    if blk + 1 < 4:
                        gs.append(gen_proj(blk + 1))
                    if blk + 2 < 4:
                        gs.append(gen_normA(blk + 2))
                    interleave(gs)
                if dbg:
                    kb.dma("sp", dbg_hT, hT[:], [f"hT.{b}" for b in range(4)], [])
                P.barrier()
                P.flush()

        if "C" in stages:
            stage_C(nc, P, kb, locals())
        else:
            P.op("dve", lambda e: e.memset(hT[:, 0:4, :], 0.0), [f"hT.{b}" for b in range(4)], [f"yT.{t}.1" for t in range(NT)])

        if dbg:
            kb.dma("sp", dbg_yT[:, 0:4, :], yT[:], [f"yT.{t}.0" for t in range(NT)], [])
            kb.dma("sp", dbg_yT[:, 4:8, :], hT[:, 0:4, :], [f"yT.{t}.1" for t in range(NT)], [])

        if "D" in stages:
            stage_D(nc, P, kb, locals())
        P.barrier()
        P.flush()
    return nc


def stage_C(nc, P, kb, L):
    cols, dc, cqf, hT, WA, mhalf, bonesb, BK = (L[k] for k in ("cols", "dc", "cqf", "hT", "WA", "mhalf", "bonesb", "L_BK"))
    wup_d, aup_d, gup_d, w_in_d = L["wup_d"], L["aup_d"], L["gup_d"], L["w_in_d"]
    mask4, maskLs, identf = L["mask4"], L["maskLs"], L["identf"]
    onesf = cqf[:, 896:1024]
    Wk = WA[:, 0:KC * 1824].rearrange("p (k n) -> p k n", k=KC)
    CL = 0.5 * math.exp(-0.5)
    NB = 256
    NBLK = T // NB
    with contextlib.ExitStack() as sc:
        if "B" not in L["stages"]:
            kb.dma("pool", Wk[:, :, :], w_in_d.rearrange("(kc p) n -> p kc n", p=128)[:, :, 2048:3872], [], ["Wk"])
        sb = lambda n, s, d: kb.sb(n, s, d, st=sc)
        wupz = sb("wupz", [128, 512], BF16)
        aupz = sb("aupz", [128, 512], BF16)
        gup1b = sb("gup1b", [128, 512], BF16)
        gup2z = sb("gup2z", [128, 512], BF16)
        for tz in (wupz, aupz, gup2z):
            P.op("dve", lambda e, tz=tz: e.memset(tz[:], 0.0), [], ["wupb", "gupb"])
        kb.dma("pool", wupz[0:64, :], wup_d, [], ["wupb"])
        kb.dma("pool", aupz[64:128, :], aup_d, [], ["wupb"])
        kb.dma("pool", gup1b[:], gup_d[0:128, :], [], ["gupb"])
        kb.dma("pool", gup2z[96:128, :], gup_d[128:160, :], [], ["gupb"])
        RKblk = sb("RKblk", [128, 4, 128], BF16)
        for ct in range(4):
            kb.ts("dve", RKblk[:, ct, :], cqf[:, 768:896], cols[:, 71 + ct:72 + ct], None, ALU.mult, None, ["cqf", "cols"], ["RKblk"])
        pT = sb("pT", [128, 15, NB + 1], F32)
        P.op("dve", lambda e: e.memset(pT[:], 0.0), [], [f"pT.{i}" for i in range(15)])
        carry = sb("carry", [128, 15], F32)
        NTMP = 10
        T_p = [[sb(f"T{c}_{i}", [128, NB], F32) for i in range(NTMP)] for c in range(2)]
        sh_p = [sb(f"shT{i}", [128, NB], F32) for i in range(2)]
        kk2_p = [sb(f"kk2{i}", [128, NB], BF16) for i in range(2)]
        twa = sb("twa", [128, NB], BF16)
        sg1 = sb("sg1", [128, NB], BF16)
        sg2 = sb("sg2", [128, NB], BF16)
        ARt = sb("ARt", [128, 4, 2, 2, 2, 128], BF16)
        P.op("dve", lambda e: e.memset(ARt[:], 0.0), [], [f"ARt.{i}" for i in range(4)])
        KT = sb("KT", [128, 4, NB], BF16)
        BnT = sb("BnT", [128, 4, NB], BF16)
        rkb = sb("rkb", [128, 4, NB], BF16)
        vbf_p = [sb(f"vbf{i}", [128, 4, NB], BF16) for i in range(2)]
        gT = sb("gT", [128, 4, NB], BF16)
        Ktok = sb("Ktok", [128, 2, 512], BF16)
        Btok = sb("Btok", [128, 2, 512], BF16)
        Vtok = sb("Vtok", [128, 2, 512], BF16)
        PCt = sb("PCt", [128, 4, 2], F32)
        MTs = [sb(f"MT{i}", [128, 8, 512], BF16) for i in range(2)]
        mraw_p = [sb(f"mraw{i}", [128, 512], BF16) for i in range(2)]
        mask4b = sb("mask4b", [128, 512], BF16)
        kb.dma("pool", mask4b[:], L["cq_d"][:, 128:640], [], ["mask4b"])
        Xm = [sb(f"Xm{i}", [128, 8, 128], BF16) for i in range(2)]
        Nm = [sb(f"Nm{i}", [128, 8, 128], BF16) for i in range(2)]
        Lm = [sb(f"Lm{i}", [128, 8, 128], BF16) for i in range(2)]
        TTs = [sb(f"TT{i}", [128, 8, 128], BF16) for i in range(2)]
        RHSs = [sb(f"RHSb{i}", [128, 512], BF16) for i in range(2)]
        Ubs = [sb(f"Ub{i}", [128, 512], BF16) for i in range(2)]
        S32 = sb("S32", [128, 4, 64], F32)
        Sbf = sb("Sbf", [128, 4, 64], BF16)
        P.op("dve", lambda e: e.memset(S32[:], 0.0), [], ["S32"])
        y32 = sb("cy32", [128, 512], F32)
        sq = sb("csq", [128, 512], F32)
        ynb = sb("cynb", [128, 512], BF16)
        yaff = sb("cyaff", [128, 4, 128], F32)
        t1b = sb("ct1", [128, 4, 128], F32)
        stt_ = sb("cstat", [128, 64], F32)

        def gen_AB(blk):
            tok0 = blk * NB
            hk = f"hT.{tok0 // 512}"
            for i0 in range(0, 15, 2):
                b = BK.get()
                bank, bk = BK.f32(b), BK.key(b)
                idxs = [i for i in (i0, i0 + 1) if i < 15]
                for j, idx in enumerate(idxs):
                    c0 = idx * 128 if idx < 14 else 1696
                    for kc in range(KC):
                        kb.mm(bank[:, j * NB:(j + 1) * NB], Wk[:, kc, c0:c0 + 128], hT[:, kc, tok0:tok0 + NB],
                              kc == 0, kc == KC - 1, [hk, "Wk"] + (["hTu"] if kc >= 4 else []), [bk])
                yield
                for j, idx in enumerate(idxs):
                    kb.cp("act", pT[:, idx, 1:NB + 1], bank[:, j * NB:(j + 1) * NB], [bk], [f"pT.{idx}"])
                BK.put(b)
                yield
            allp = [f"pT.{i}" for i in range(15)]
            kb.cp("dve", carry[:], pT[:, :, NB], allp, ["carry"])
            yield
            for idx in range(15):
                tmp, ktm = sh_p[idx % 2], f"shT.{idx % 2}"
                kb.act(tmp[:], pT[:, idx, 0:NB], AF.Copy, [f"pT.{idx}", "cols"], [ktm], scale=cols[:, 40 + idx:41 + idx])
                yield
                if 8 <= idx < 12:
                    kb.stt(vbf_p[blk % 2][:, idx - 8, :], pT[:, idx, 1:NB + 1], dc[:, idx:idx + 1], tmp[:], ALU.mult, ALU.add,
                           [f"pT.{idx}", "dc", ktm, "carry"], [f"vbf{blk % 2}.{idx - 8}"])
                else:
                    kb.stt(pT[:, idx, 1:NB + 1], pT[:, idx, 1:NB + 1], dc[:, idx:idx + 1], tmp[:], ALU.mult, ALU.add,
                           [f"pT.{idx}", "dc", ktm, "carry"], [f"pT.{idx}"])
                yield
            kb.cp("dve", pT[:, :, 0], carry[:], ["carry"], allp)
            yield

        ps = lambda idx: pT[:, idx, 1:NB + 1]

        def gen_lora(blk):
            kb.act(twa[0:64, :], pT[0:64, 12, 1:NB + 1], AF.Tanh, ["pT.12"], ["twa"])
            kb.cp("dve", twa[64:128, :], pT[64:128, 12, 1:NB + 1], ["pT.12"], ["twa"])
            yield
            th, kth = sh_p[0], "shT.0"
            kb.act(th[:], ps(13), AF.Tanh, ["pT.13"], [kth], scale=0.5)
            th2, kth2 = sh_p[1], "shT.1"
            kb.act(th2[:], ps(14), AF.Tanh, ["pT.14"], [kth2], scale=0.5)
            yield
            kb.ts("dve", sg1[:], th[:], 0.5, 0.5, ALU.mult, ALU.add, [kth], ["sg1"])
            kb.ts("pool", sg2[:], th2[:], 0.5, 0.5, ALU.mult, ALU.add, [kth2], ["sg2"])
            yield

        def gen_ct(ch, blk, ct):
            Tb = T_p[ch]
            tk = lambda i: f"T{ch}.{i}"
            cs = slice(ct * 128, (ct + 1) * 128)
            r_, k_, v_ = ps(ct), ps(4 + ct), ps(8 + ct)
            kr, kk_, kv = f"pT.{ct}", f"pT.{4 + ct}", f"pT.{8 + ct}"
            bz, bg = BK.get(), BK.get()
            bankZ, kz = BK.f32(bz), BK.key(bz)
            bankG, kg = BK.f32(bg), BK.key(bg)
            kb.mm(bankZ[:, 0:NB], wupz[:, cs], twa[:], True, True, ["wupb", "twa"], [kz])
            kb.mm(bankZ[:, NB:2 * NB], aupz[:, cs], twa[:], True, True, ["wupb", "twa"], [kz])
            kb.mm(bankG[:, 0:NB], gup1b[:, cs], sg1[:], True, False, ["gupb", "sg1"], [kg])
            kb.mm(bankG[:, 0:NB], gup2z[:, cs], sg2[:], False, True, ["gupb", "sg2"], [kg])
            yield
            thw, tha = Tb[0], Tb[1]
            kb.act(thw[:], bankZ[:, 0:NB], AF.Tanh, [kz, "dc"], [tk(0)], scale=0.5, bias=dc[:, 15 + ct:16 + ct])
            kb.act(tha[:], bankZ[:, NB:2 * NB], AF.Tanh, [kz, "dc"], [tk(1)], scale=0.5, bias=dc[:, 19 + ct:20 + ct])
            BK.put(bz)
            kk = Tb[4]
            kb.ts("pool", kk[:], k_, cols[:, 63 + ct:64 + ct], 0.0, ALU.mult, ALU.add, [kk_, "cols"], [tk(4)])
            yield
            kb.cp("act", gT[:, ct, :], bankG[:, 0:NB], [kg], [f"gT.{ct}"])
            BK.put(bg)
            kk2, k_kk2 = kk2_p[ch], f"kk2.{ch}"
            kb.act(kk2[:], kk[:], AF.Square, [tk(4)], [k_kk2])
            ld = Tb[0]
            kb.ts("dve", ld[:], thw[:], -CL, -CL, ALU.mult, ALU.add, [tk(0)], [tk(0)])
            yield
            bn_ = BK.get()
            bankN, kn = BK.f32(bn_), BK.key(bn_)
            kb.mm(bankN[:, 0:NB], bonesb[:], kk2[:], True, True, ["bonesb", k_kk2], [kn])
            cum = Tb[2]
            for c in range(2):
                ld_c, cum_c = ld[:, c * 128:(c + 1) * 128], cum[:, c * 128:(c + 1) * 128]
                P.op("dve", lambda e, cum_c=cum_c, ld_c=ld_c: e.tensor_tensor_scan(out=cum_c, data0=onesf, data1=ld_c, initial=0.0, op0=ALU.mult, op1=ALU.add),
                     [tk(0), "cqf"], [tk(2)])
            an = Tb[1]
            kb.ts("pool", an[:], tha[:], -0.5, -0.5, ALU.mult, ALU.add, [tk(1)], [tk(1)])
            yield
            sn = Tb[5]
            kb.act(sn[:], bankN[:, 0:NB], AF.Sqrt, [kn], [tk(5)])
            BK.put(bn_)
            cumex = Tb[3]
            kb.tt("pool", cumex[:], cum[:], ld[:], ALU.subtract, [tk(2), tk(0)], [tk(3)])
            yield
            ep, en, ex = Tb[6], Tb[7], Tb[8]
            kb.act(ep[:], cum[:], AF.Exp, [tk(2)], [tk(6)])
            kb.act(en[:], cum[:], AF.Exp, [tk(2)], [tk(7)], scale=-1.0)
            kb.ts("dve", sn[:], sn[:], 1e-12, None, ALU.max, None, [tk(5)], [tk(5)])
            tmp2 = Tb[9]
            kb.ts("pool", tmp2[:], an[:], dc[:, 23 + ct:24 + ct], dc[:, 27 + ct:28 + ct], ALU.mult, ALU.add, [tk(1), "dc"], [tk(9)])
            yield
            kb.act(ex[:], cumex[:], AF.Exp, [tk(3)], [tk(8)])
            P.op("dve", lambda e: e.reciprocal(out=sn[:], in_=sn[:]), [tk(5)], [tk(5)])
            k2 = Tb[9]
            kb.tt("pool", k2[:], k_, tmp2[:], ALU.mult, [kk_, tk(9)], [tk(9)])
            yield
            kkn = Tb[4]
            kb.tt("dve", kkn[:], kk[:], sn[:], ALU.mult, [tk(4), tk(5)], [tk(4)])
            c3 = lambda a: a.rearrange("p (c n) -> p c n", c=2)
            kb.cp("pool", PCt[:, ct, :], c3(ep[:])[:, :, 127], [tk(6)], [f"PCt.{ct}"])
            yield
            bn = Tb[5]
            kb.tt("pool", bn[:], kkn[:], an[:], ALU.mult, [tk(4), tk(1), tk(5)], [tk(5)])
            kb.tt("dve", KT[:, ct, :], k2[:], en[:], ALU.mult, [tk(9), tk(7)], [f"KT.{ct}"])
            yield
            for hh in range(2):
                pp = slice(64 * hh, 64 * hh + 64)
                kb.tt("dve", ARt[pp, ct, :, hh, 1, :], c3(r_)[pp], c3(ep[:])[pp], ALU.mult, [kr, tk(6)], [f"ARt.{ct}"])
                kb.tt("dve" if hh == 0 else "pool", ARt[pp, ct, :, hh, 0, :], c3(kkn[:])[pp], c3(ex[:])[pp], ALU.mult, [tk(4), tk(8)], [f"ARt.{ct}"])
                yield
            kb.tt("dve", BnT[:, ct, :], bn[:], en[:], ALU.mult, [tk(5), tk(7)], [f"BnT.{ct}"])
            kb.tt("pool", rkb[:, ct, :], r_, k2[:], ALU.mult, [kr, tk(9)], [f"rkb.{ct}"])
            yield

        def gen_tok(blk):
            for c in range(2):
                cs = slice(c * 128, (c + 1) * 128)
                for src, dst, nm, sk in ((KT, Ktok, "KT", "KT"), (BnT, Btok, "BnT", "BnT"), (vbf_p[blk % 2], Vtok, "vbf", f"vbf{blk % 2}")):
                    b = BK.get()
                    bankT, kt = BK.bf(b), BK.key(b)
                    for ct in range(4):
                        kb.tr(bankT[:, ct * 128:(ct + 1) * 128], src[:, ct, cs], [f"{sk}.{ct}"], [kt])
                    yield
                    kb.cpalt(dst[:, c, :], bankT[:, 0:512], [kt], [f"{nm}tok.{c}"])
                    BK.put(b)
                    yield

        def gen_F1(blk, c):
            cs = slice(c * 128, (c + 1) * 128)
            MT = MTs[c]
            for h in range(8):
                ct = h // 2
                b = BK.get()
                bankM, km = BK.f32(b), BK.key(b)
                rhsAR = ARt[:, ct, c, h % 2, :, :].rearrange("p a b -> p (a b)")
                kb.mm(bankM[:, 0:256], BnT[:, ct, cs], rhsAR, True, True, [f"BnT.{ct}", f"ARt.{ct}"], [km])
                kb.mm(bankM[:, 256:512], KT[:, ct, cs], rhsAR, True, True, [f"KT.{ct}", f"ARt.{ct}"], [km])
                yield
                if h % 2 == 0:
                    kb.tt("dve", MT[:, h, :], bankM, mask4, ALU.mult, [km, "cqf"], [f"MT{c}.{h}"])
                    BK.put(b)
                else:
                    mr, kmr = mraw_p[(h // 2) % 2], f"mraw.{(h // 2) % 2}"
                    kb.cp("act", mr[:], bankM, [km], [kmr])
                    BK.put(b)
                    yield
                    kb.tt("pool", MT[:, h, :], mr[:], mask4b[:], ALU.mult, [kmr, "mask4b"], [f"MT{c}.{h}"])
                yield
            for g4 in range(2):
                b = BK.get()
                bankL, kl = BK.f32(b), BK.key(b)
                for hh in range(4):
                    h = 4 * g4 + hh
                    kb.mm(bankL[:, hh * 128:(hh + 1) * 128], ARt[:, h // 2, c, h % 2, 0, :], BnT[:, h // 2, cs], True, True,
                          [f"ARt.{h // 2}", f"BnT.{h // 2}"], [kl])
                yield
                kb.tt("dve", Lm[0][:, 4 * g4:4 * g4 + 4, :], bankL.rearrange("p (c n) -> p c n", c=4),
                      maskLs.unsqueeze(1).to_broadcast([128, 4, 128]), ALU.mult, [kl, "cqf"], [f"Lm0.{g4}"])
                BK.put(b)
                hs = slice(4 * g4, 4 * g4 + 4)
                mtk = [f"MT{c}.{h}" for h in range(4 * g4, 4 * g4 + 4)]
                kb.cp("act", Nm[0][:, hs, :], MT[:, hs, 0:128], mtk, [f"Nm0.{g4}"])
                kb.tt("pool", Xm[0][:, hs, :], MT[:, hs, 0:128], identf.unsqueeze(1).to_broadcast([128, 4, 128]), ALU.add,
                      mtk + ["cqf"], [f"Xm0.{g4}"])
                yield

        def gen_inv_group(c, g4):
            hs = slice(4 * g4, 4 * g4 + 4)
            v4 = lambda bank: bank.rearrange("p (c n) -> p c n", c=4)
            cur = 0
            for lvl in range(7):
                nxt = cur ^ 1
                kN, kL, kX = f"Nm{cur}.{g4}", f"Lm{cur}.{g4}", f"Xm{cur}.{g4}"
                last = lvl == 6
                Xdst, kXd = (TTs[c], f"TT{c}.{g4}") if last else (Xm[nxt], f"Xm{nxt}.{g4}")
                bx = ba = bb = None
                if lvl >= 1:
                    bx = BK.get()
                    bankX, kbx = BK.f32(bx), BK.key(bx)
                    for hh in range(4):
                        h = 4 * g4 + hh
                        kb.mm(bankX[:, hh * 128:(hh + 1) * 128], Lm[cur][:, h, :], Xm[cur][:, h, :], True, True, [kL, kX], [kbx])
                if not last:
                    ba, bb = BK.get(), BK.get()
                    bankA, kba = BK.f32(ba), BK.key(ba)
                    bankB, kbb = BK.f32(bb), BK.key(bb)
                    for hh in range(4):
                        h = 4 * g4 + hh
                        kb.mm(bankA[:, hh * 128:(hh + 1) * 128], Lm[cur][:, h, :], Nm[cur][:, h, :], True, True, [kL, kN], [kba])
                    for hh in range(4):
                        h = 4 * g4 + hh
                        kb.mm(bankB[:, hh * 128:(hh + 1) * 128], Nm[cur][:, h, :], Lm[cur][:, h, :], True, True, [kL, kN], [kbb])
                yield
                if lvl >= 1:
                    kb.tt("dve", Xdst[:, hs, :], v4(bankX), Xm[cur][:, hs, :], ALU.add, [kbx, kX], [kXd])
                    BK.put(bx)
                else:
                    kb.cp("pool", Xdst[:, hs, :], Xm[cur][:, hs, :], [kX], [kXd])
                if not last:
                    kb.cp("act", Nm[nxt][:, hs, :], v4(bankA), [kba], [f"Nm{nxt}.{g4}"])
                    BK.put(ba)
                    if g4 == 0:
                        kb.cp("act", Lm[nxt][:, hs, :], v4(bankB), [kbb], [f"Lm{nxt}.{g4}"])
                    else:
                        kb.cp("dve", Lm[nxt][:, hs, :], v4(bankB), [kbb], [f"Lm{nxt}.{g4}"])
                    BK.put(bb)
                yield
                cur = nxt

        def gen_inv(blk, c):
            gs = [gen_inv_group(c, 0), gen_inv_group(c, 1)]
            while gs:
                for g in list(gs):
                    try:
                        next(g)
                    except StopIteration:
                        gs.remove(g)
                yield

        def gen_seqpost(blk, c):
            n_ch = blk * 2 + c
            cs = slice(c * 128, (c + 1) * 128)
            tok = slice(blk * NB + c * 128, blk * NB + (c + 1) * 128)
            MT, TT, RHSb, Ub = MTs[c], TTs[c], RHSs[c], Ubs[c]
            kTT = [f"TT{c}.0", f"TT{c}.1"]
            kRH, kUb = f"RHSb{c}", f"Ub{c}"
            br = BK.get()
            bankR, kr_ = BK.f32(br), BK.key(br)
            for h in range(8):
                ct = h // 2
                o = bankR[:, 64 * h:64 * h + 64]
                if n_ch > 0:
                    kb.mm(o, ARt[:, ct, c, h % 2, 0, :], Sbf[:, ct, :], True, False, [f"ARt.{ct}", "Sbf"], [kr_])
                kb.mm(o, MT[:, h, 256:384], Vtok[:, c, 64 * h:64 * h + 64], n_ch == 0, True, [f"MT{c}.{h}", f"vbftok.{c}"], [kr_])
            yield
            kb.cp("act", RHSb[:], bankR, [kr_], [kRH])
            BK.put(br)
            yield
            bu = BK.get()
            bankU, ku = BK.f32(bu), BK.key(bu)
            for h in range(8):
                kb.mm(bankU[:, 64 * h:64 * h + 64], TT[:, h, :], RHSb[:, 64 * h:64 * h + 64], True, True, kTT + [kRH], [ku])
            yield
            kb.cp("dve", Ub[:], bankU, [ku], [kUb])
            BK.put(bu)
            yield
            if n_ch < NT - 1:
                bs_ = BK.get()
                bankS, ks_ = BK.f32(bs_), BK.key(bs_)
                for ct in range(4):
                    o = bankS[:, ct * 128:(ct + 1) * 128]
                    kb.mm(o, Btok[:, c, ct * 128:(ct + 1) * 128], Ub[:, ct * 128:(ct + 1) * 128], True, False, [f"BnTtok.{c}", kUb], [ks_])
                    kb.mm(o, Ktok[:, c, ct * 128:(ct + 1) * 128], Vtok[:, c, ct * 128:(ct + 1) * 128], False, True, [f"KTtok.{c}", f"vbftok.{c}"], [ks_])
            by = BK.get()
            bankY, ky = BK.f32(by), BK.key(by)
            for h in range(8):
                ct = h // 2
                o = bankY[:, 64 * h:64 * h + 64]
                if n_ch > 0:
                    kb.mm(o, ARt[:, ct, c, h % 2, 1, :], Sbf[:, ct, :], True, False, [f"ARt.{ct}", "Sbf"], [ky])
                kb.mm(o, MT[:, h, 128:256], Ub[:, 64 * h:64 * h + 64], n_ch == 0, False, [f"MT{c}.{h}", kUb], [ky])
                kb.mm(o, MT[:, h, 384:512], Vtok[:, c, 64 * h:64 * h + 64], False, True, [f"MT{c}.{h}", f"vbftok.{c}"], [ky])
            yield
            if n_ch < NT - 1:
                for hh in range(2):
                    b0 = 64 * hh
                    kb.tt("dve", S32[b0:b0 + 64, :, :], bankS[b0:b0 + 64, :].rearrange("p (c n) -> p c n", c=4)[:, :, b0:b0 + 64],
                          S32[b0:b0 + 64, :, :], ALU.add, [ks_, "S32"], ["S32"])
                BK.put(bs_)
                kb.tt("dve", S32[:], S32[:], PCt[:, :, c].unsqueeze(2).to_broadcast([128, 4, 64]), ALU.mult,
                      ["S32"] + [f"PCt.{ct}" for ct in range(4)], ["S32"])
                kb.cp("act", Sbf[:], S32[:], ["S32"], ["Sbf"])
                yield
            k1, k2, ks, k3 = "cy32", "csq", "cstat", "cynb"
            kb.cp("act", y32[:], bankY, [ky], [k1])
            BK.put(by)
            yield
            v3 = lambda a: a.rearrange("p (h d) -> p h d", d=64)
            bc = lambda a: a.unsqueeze(2).to_broadcast([128, 8, 64])
            kb.red(stt_[:, 0:8], v3(y32[:]), ALU.add, [k1], [ks])
            kb.act(sq[:], y32[:], AF.Square, [k1], [k2])
            yield
            kb.red(stt_[:, 8:16], v3(sq[:]), ALU.add, [k2], [ks])
            yield
            kb.ts("dve", stt_[:, 16:24], stt_[:, 0:8], 1.0 / 64, None, ALU.mult, None, [ks], [ks])
            yield
            kb.tt("dve", stt_[:, 24:32], stt_[:, 16:24], stt_[:, 16:24], ALU.mult, [ks], [ks])
            kb.ts("dve", stt_[:, 32:40], stt_[:, 8:16], 1.0 / 64, 64e-5, ALU.mult, ALU.add, [ks], [ks])
            yield
            kb.tt("dve", stt_[:, 40:48], stt_[:, 32:40], stt_[:, 24:32], ALU.subtract, [ks], [ks])
            yield
            kb.tt("pool", stt_[:, 48:56], stt_[:, 40:48], mhalf[:, 0:8], ALU.pow, [ks, "mhalf"], [ks])
            yield
            kb.stt(stt_[:, 56:64], stt_[:, 16:24], -1.0, stt_[:, 48:56], ALU.mult, ALU.mult, [ks], [ks])
            kb.tt("pool", v3(y32[:]), v3(y32[:]), bc(stt_[:, 48:56]), ALU.mult, [k1, ks], [k1])
            yield
            kb.tt("dve", v3(ynb[:]), v3(y32[:]), bc(stt_[:, 56:64]), ALU.add, [k1, ks], [k3])
            yield
            b2 = BK.get()
            bankT, kt = BK.bf(b2), BK.key(b2)
            for ct in range(4):
                kb.tr(bankT[:, ct * 128:(ct + 1) * 128], ynb[:, ct * 128:(ct + 1) * 128], [k3], [kt])
            bbo = BK.get()
            bankBo, kbo = BK.f32(bbo), BK.key(bbo)
            for ct in range(4):
                kb.mm(bankBo[:, ct * 128:(ct + 1) * 128], RKblk[:, ct, :], rkb[:, ct, cs], True, True, ["RKblk", f"rkb.{ct}"], [kbo])
            yield
            for ct in range(4):
                kb.act(yaff[:, ct, :], bankT[:, ct * 128:(ct + 1) * 128], AF.Identity, [kt, "cols"], ["cyaff"],
                       scale=cols[:, 75 + ct:76 + ct], bias=cols[:, 79 + ct:80 + ct])
            kb.tt("dve", t1b[:], bankBo.rearrange("p (c n) -> p c n", c=4), vbf_p[blk % 2][:, :, cs], ALU.mult,
                  [kbo] + [f"vbf{blk % 2}.{ct}" for ct in range(4)], ["ct1"])
            BK.put(b2)
            BK.put(bbo)
            yield
            kb.tt("pool", t1b[:], t1b[:], yaff[:], ALU.add, ["ct1", "cyaff"], ["ct1"])
            yield
            kb.tt("dve", hT[:, 0:4, tok], t1b[:], gT[:, :, cs], ALU.mult, ["ct1"] + [f"gT.{ct}" for ct in range(4)],
                  [f"yT.{n_ch}.1", f"hT.{n_ch // 4}"])
            yield

        def gen_CD(blk):
            yield from gen_lora(blk)
            for pair in range(2):
                gs = [gen_ct(0, blk, 2 * pair), gen_ct(1, blk, 2 * pair + 1)]
                while gs:
                    for g in list(gs):
                        try:
                            next(g)
                        except StopIteration:
                            gs.remove(g)
                    yield
            yield from gen_tok(blk)

        def run(g):
            for _ in g:
                pass

        run(gen_AB(0))
        run(gen_CD(0))
        for blk in range(NBLK):
            run(gen_F1(blk, 0))
            interleave([gen_inv(blk, 0), gen_F1(blk, 1)])
            interleave([gen_seqpost(blk, 0), gen_inv(blk, 1)])
            if blk + 1 < NBLK:
                interleave([gen_seqpost(blk, 1), gen_AB(blk + 1)])
                if blk + 1 == NBLK - 1 and "D" in L["stages"]:
                    wv = lambda w: w.rearrange("(kc p) n -> p kc n", p=128)
                    Wout = WA[:, 0:8192].rearrange("p (k n) -> p k n", k=KC)
                    WkvK = WA[:, 8192:16384].rearrange("p (k n) -> p k n", k=KC)
                    WkvV = hT[:, 4:8, :].rearrange("p a (b n) -> p (a b) n", n=1024)
                    kb.dma("pool", Wout, wv(L["w_out_d"]), [], ["WA.0", "Wk"])
                    kb.dma("pool", WkvK, wv(L["wkv_d"])[:, :, 0:1024], [], ["WA.1", "Wk"])
                    kb.dma("pool", WkvV, wv(L["wkv_d"])[:, :, 1024:2048], [], ["WkvV", "hTu"])
                run(gen_CD(blk + 1))
            else:
                run(gen_seqpost(blk, 1))
        P.barrier()
        P.flush()


def stage_D(nc, P, kb, L):
    cols, cqf, hT, yT, WA, mhalf, onesb, ss, BK = (L[k] for k in ("cols", "cqf", "hT", "yT", "WA", "mhalf", "onesb", "ss", "L_BK"))
    x_d, mem_d, gfin_d, out_d = L["x_d"], L["mem_d"], L["gfin_d"], L["out_d"]
    w_out_d, wq_d, wkv_d, wo_d, wupm_d, wdn_d = (L[k] for k in ("w_out_d", "wq_d", "wkv_d", "wo_d", "wupm_d", "wdn_d"))
    wview = lambda w: w.rearrange("(kc p) n -> p kc n", p=128)

    def run(g):
        for _ in g:
            pass

    with contextlib.ExitStack() as sd:
        sb = lambda n, s, d, st=sd: kb.sb(n, s, d, st=st)
        xres = sb("xres", [128, NT, D], F32)
        gfin = sb("gfin", [128, D], F32)
        kTm = sb("kTm", [128, KC, 256], BF16)
        vmem = sb("vmem", [128, 2, D], BF16)
        kmx = sb("kmx", [128, 4], F32)
        hb_p = [sb(f"dhb{i}", [128, D], BF16) for i in range(4)]
        sqj = sb("dsqj", [128, D], BF16)
        W0 = WA[:, 0:8192].rearrange("p (k n) -> p k n", k=KC)
        W1 = WA[:, 8192:16384].rearrange("p (k n) -> p k n", k=KC)
        Wout, WkvK = W0, W1
        WkvV = hT[:, 4:8, :].rearrange("p a (b n) -> p (a b) n", n=1024)
        xv = x_d.rearrange("(t p) d -> p t d", p=128)
        kb.dma("sp", xres[:, 0:4, :], xv[:, 0:4, :], [], [f"xres.{t}" for t in range(0, 4)])
        if "C" not in L["stages"]:
            kb.dma("pool", Wout, wview(w_out_d), [], ["WA.0"])
            kb.dma("pool", WkvK, wview(wkv_d)[:, :, 0:1024], [], ["WA.1"])
            kb.dma("pool", WkvV, wview(wkv_d)[:, :, 1024:2048], [], ["WkvV"])
        kb.dma("sp", gfin[:], gfin_d.partition_broadcast(128), [], ["gfin"])

        def gen_norm(tiles, src, gcol0, dst_fn, dkey, sscol0):
            for tl, t in enumerate(tiles):
                xa, xk = src(t)
                c = sscol0 + t
                kb.act(sqj[:], xa, AF.Square, [xk], ["sqj", f"ss{c}"], accum=ss[:, c:c + 1])
                yield
                kb.ts("dve", ss[:, c:c + 1], ss[:, c:c + 1], 1.0 / D, 1e-6, ALU.mult, ALU.add, [f"ss{c}"], [f"ss{c}"])
                yield
                kb.tt("pool", ss[:, c:c + 1], ss[:, c:c + 1], mhalf[:, 0:1], ALU.pow, [f"ss{c}", "mhalf"], [f"ss{c}"])
                yield
                kb.act(hb_p[tl][:], xa, AF.Copy, [xk, f"ss{c}"], [f"dhb.{tl}"], scale=ss[:, c:c + 1])
                yield
            n = len(tiles) * 128
            for kc in range(KC):
                b = BK.get()
                bankT, bk = BK.bf(b), BK.key(b)
                for tl in range(len(tiles)):
                    kb.tr(bankT[:, tl * 128:(tl + 1) * 128], hb_p[tl][:, kc * 128:(kc + 1) * 128], [f"dhb.{tl}"], [bk])
                yield
                o = dst_fn(kc)
                dk = dkey if isinstance(dkey, list) else [dkey]
                if kc % 2:
                    kb.act(o, bankT[:, 0:n], AF.Copy, [bk, "cols"], dk, scale=cols[:, gcol0 + kc:gcol0 + kc + 1])
                else:
                    kb.ts("dve", o, bankT[:, 0:n], cols[:, gcol0 + kc:gcol0 + kc + 1], None, ALU.mult, None, [bk, "cols"], dk)
                BK.put(b)
                yield

        with contextlib.ExitStack() as s0:
            memx = [sb(f"memx{i}", [128, D], F32, st=s0) for i in range(2)]
            memT = sb("memT", [128, KC, 256], BF16, st=s0)
            k2m = sb("k2m", [128, KC, 256], BF16, st=s0)
            for i in range(2):
                kb.dma("sp", memx[i][:], mem_d[i * 128:(i + 1) * 128, :], [], [f"memx.{i}"])
            for i in range(1, 4):
                kb.dma("sp", xres[:, 4 * i:4 * i + 4, :], xv[:, 4 * i:4 * i + 4, :], [], [f"xres.{t}" for t in range(4 * i, 4 * i + 4)])

            def gen_memkv():
                yield from gen_norm([0, 1], lambda t: (memx[t][:], f"memx.{t}"), 16, lambda kc: memT[:, kc, :], "memT", 16)
                for ft in range(KC):
                    b = BK.get()
                    bank, bk = BK.f32(b), BK.key(b)
                    for kc in range(KC):
                        kb.mm(bank[:, 0:256], WkvK[:, kc, ft * 128:(ft + 1) * 128], memT[:, kc, :], kc == 0, kc == KC - 1, ["WA.1", "memT"], [bk])
                    yield
                    kb.cpalt(kTm[:, ft, :], bank[:, 0:256], [bk], ["kTm"])
                    BK.put(b)
                    yield
                for mt in range(2):
                    for hf in range(2):
                        b = BK.get()
                        bank, bk = BK.f32(b), BK.key(b)
                        for kc in range(KC):
                            kb.mm(bank, memT[:, kc, mt * 128:(mt + 1) * 128], WkvV[:, kc, hf * 512:(hf + 1) * 512],
                                  kc == 0, kc == KC - 1, ["WkvV", "memT"], [bk])
                        yield
                        kb.cpalt(vmem[:, mt, hf * 512:(hf + 1) * 512], bank, [bk], ["vmem"])
                        BK.put(b)
                        yield
                kb.act(k2m[:], kTm[:], AF.Square, ["kTm"], ["k2m"])
                yield
                for h in range(4):
                    b = BK.get()
                    bank, bk = BK.f32(b), BK.key(b)
                    kb.mm(bank[:, 0:256], onesb[:], k2m[:, 2 * h, :], True, False, ["onesb", "k2m"], [bk])
                    kb.mm(bank[:, 0:256], onesb[:], k2m[:, 2 * h + 1, :], False, True, ["onesb", "k2m"], [bk])
                    yield
                    kb.red(kmx[:, h:h + 1], bank[:, 0:256], ALU.max, [bk], ["kmx"])
                    BK.put(b)
                    yield

            def gen_wout():
                for t in range(NT):
                    tok = slice(t * 128, (t + 1) * 128)
                    for hf in range(2):
                        b = BK.get()
                        bank, bk = BK.f32(b), BK.key(b)
                        for kc in range(KC):
                            ysrc = yT[:, kc, tok] if kc < 4 else hT[:, kc - 4, tok]
                            kb.mm(bank, ysrc, Wout[:, kc, hf * 512:(hf + 1) * 512], kc == 0, kc == KC - 1,
                                  [f"yT.{t}.0", f"yT.{t}.1", "WA.0", f"hT.{t // 4}"], [bk])
                        yield
                        xs = xres[:, t, hf * 512:(hf + 1) * 512]
                        kb.tt("dve", xs, bank, xs, ALU.add, [bk, f"xres.{t}"], [f"xres.{t}"])
                        BK.put(b)
                        yield

            interleave([gen_memkv(), gen_wout()])
            kb.dma("pool", W1, wview(wq_d), [], ["WA.1"])
            kb.dma("pool", W0, wview(wo_d), [], ["WA.0"])
            srcx = lambda t: (xres[:, t, :], f"xres.{t}")
            run(gen_norm([0, 1, 2, 3], srcx, 8, lambda kc: hT[:, kc, 0:512], ["hT.0", "WkvV"], 0))
            P.barrier()
            P.flush()

        srcx = lambda t: (xres[:, t, :], f"xres.{t}")
        norm_x = lambda blk: gen_norm(list(range(4 * blk, 4 * blk + 4)), srcx, 8, lambda kc: hT[:, kc, blk * 512:(blk + 1) * 512], f"hT.{blk}", 0)
        norm_m = lambda blk: gen_norm(list(range(4 * blk, 4 * blk + 4)), srcx, 24, lambda kc: hT[:, kc, blk * 512:(blk + 1) * 512], f"hT.{blk}", 16)
        with contextlib.ExitStack() as s2:
            qTb = yT[:, 0:2, :].rearrange("p a (b n) -> p (a b) n", n=512)
            PT = yT[:, 2:4, :].rearrange("p a (b n) -> p (a b) n", n=512)
            oTb_t = sb("oTb", [128, KC * 512], BF16, st=s2)
            oTb = oTb_t[:].rearrange("p (k n) -> p k n", k=KC)
            q2b_p = [sb(f"q2b{i}", [128, 2, 512], BF16, st=s2) for i in range(4)]
            nb_p = [sb(f"nb{i}", [128, 2], F32, st=s2) for i in range(4)]
            rc_p = [sb(f"rc{i}", [128, 512], F32, st=s2) for i in range(4)]

            def gen_head(sl, h):
                qk = [f"qTb.{2 * h}", f"qTb.{2 * h + 1}"]
                q2b, kq2 = q2b_p[sl], f"q2b.{sl}"
                kb.act(q2b[:], qTb[:, 2 * h:2 * h + 2, :], AF.Square, qk, [kq2])
                yield
                b = BK.get()
                bankQ, kq = BK.f32(b), BK.key(b)
                kb.mm(bankQ, onesb[:], q2b[:, 0, :], True, False, ["onesb", kq2], [kq])
                kb.mm(bankQ, onesb[:], q2b[:, 1, :], False, True, ["onesb", kq2], [kq])
                yield
                nb, kbs = nb_p[sl], f"nb.{sl}"
                kb.red(nb[:, 0:1], bankQ, ALU.max, [kq], [kbs])
                BK.put(b)
                yield
                kb.ts("dve", nb[:, 1:2], nb[:, 0:1], kmx[:, h:h + 1], -1.0 / 32, ALU.add, ALU.mult, [kbs, "kmx"], [kbs])
                yield
                for mt in range(2):
                    b = BK.get()
                    bankS, ksb = BK.f32(b), BK.key(b)
                    ms = slice(mt * 128, (mt + 1) * 128)
                    kb.mm(bankS, kTm[:, 2 * h, ms], qTb[:, 2 * h, :], True, False, ["kTm", qk[0]], [ksb])
                    kb.mm(bankS, kTm[:, 2 * h + 1, ms], qTb[:, 2 * h + 1, :], False, True, ["kTm", qk[1]], [ksb])
                    yield
                    kb.act(PT[:, 2 * h + mt, :], bankS, AF.Exp, [ksb, kbs], [f"PT.{2 * h + mt}"], scale=1.0 / 16, bias=nb[:, 1:2])
                    BK.put(b)
                    yield
                pk = [f"PT.{2 * h}", f"PT.{2 * h + 1}"]
                b = BK.get()
                bankRs, krs = BK.f32(b), BK.key(b)
                kb.mm(bankRs, onesb[:], PT[:, 2 * h, :], True, False, ["onesb", pk[0]], [krs])
                kb.mm(bankRs, onesb[:], PT[:, 2 * h + 1, :], False, True, ["onesb", pk[1]], [krs])
                yield
                rc, krc = rc_p[sl], f"rc.{sl}"
                P.op("dve", lambda e: e.reciprocal(out=rc[:], in_=bankRs), [krs], [krc])
                BK.put(b)
                yield
                for dt_ in range(2):
                    ft = 2 * h + dt_
                    b = BK.get()
                    bankO, kob = BK.f32(b), BK.key(b)
                    kb.mm(bankO, vmem[:, 0, ft * 128:(ft + 1) * 128], PT[:, 2 * h, :], True, False, ["vmem", pk[0]], [kob])
                    kb.mm(bankO, vmem[:, 1, ft * 128:(ft + 1) * 128], PT[:, 2 * h + 1, :], False, True, ["vmem", pk[1]], [kob])
                    yield
                    kb.tt("dve", oTb[:, ft, :], bankO, rc[:], ALU.mult, [kob, krc], [f"oTb.{ft}"])
                    BK.put(b)
                    yield

            def gen_attn(blk):
                bs = slice(blk * 512, (blk + 1) * 512)
                for ft in range(KC):
                    b = BK.get()
                    bank, bk = BK.f32(b), BK.key(b)
                    for kc in range(KC):
                        kb.mm(bank, W1[:, kc, ft * 128:(ft + 1) * 128], hT[:, kc, bs], kc == 0, kc == KC - 1, ["WA.1", f"hT.{blk}"], [bk])
                    yield
                    kb.cpalt(qTb[:, ft, :], bank, [bk], [f"qTb.{ft}"])
                    BK.put(b)
                    yield
                if blk == 3:
                    mlp_load(0)
                for pair in range(1):
                    gs = [gen_head(h, h) for h in range(4)]
                    while gs:
                        for g in list(gs):
                            try:
                                next(g)
                            except StopIteration:
                                gs.remove(g)
                        yield
                for tl in range(4):
                    t = blk * 4 + tl
                    for hf in range(2):
                        b = BK.get()
                        bank, bk = BK.f32(b), BK.key(b)
                        for kc in range(KC):
                            kb.mm(bank, oTb[:, kc, tl * 128:(tl + 1) * 128], W0[:, kc, hf * 512:(hf + 1) * 512], kc == 0, kc == KC - 1,
                                  [f"oTb.{kc}", "WA.0"], [bk])
                        yield
                        xs = xres[:, t, hf * 512:(hf + 1) * 512]
                        kb.tt("dve", xs, bank, xs, ALU.add, [bk, f"xres.{t}"], [f"xres.{t}"])
                        BK.put(b)
                        yield

            def norm_worker(blk):
                if blk + 1 < 4:
                    yield from norm_x(blk + 1)
                if blk - 1 >= 0:
                    yield from norm_m(blk - 1)

            s3 = s2
            aT_all = sb("aT_all", [128, 8, 512], BF16, st=s3)[:]
            rl_p = [sb(f"rl{i}", [128, 512], BF16, st=s3) for i in range(2)]
            o_p = [oTb_t[:, 2048 * i:2048 * (i + 1)].bitcast(F32) for i in range(2)]
            upv = wupm_d.rearrange("(kc p) n -> p kc n", p=128)
            dnv = wdn_d.rearrange("(f p) n -> p f n", p=128)
            ov = out_d.rearrange("(t p) d -> p t d", p=128)

            def mlp_w(j):
                s = (j + 1) % 2
                Wu = WA[:, s * 8192:s * 8192 + 4096].rearrange("p (k n) -> p k n", k=KC)
                Wd = WA[:, s * 8192 + 4096:(s + 1) * 8192].rearrange("p (f n) -> p f n", f=4)
                return s, Wu, Wd

            def mlp_load(j):
                s, Wu, Wd = mlp_w(j)
                kb.dma("pool", Wu, upv[:, :, j * 512:(j + 1) * 512], [], [f"WA.{s}"])
                kb.dma("pool", Wd, dnv[:, 4 * j:4 * j + 4, :], [], [f"WA.{s}"])

            for blk in range(4):
                interleave([gen_attn(blk), norm_worker(blk)])
            run(norm_m(3))

            NJ = 8
            ai = 0
            ri = 0
            for j in range(NJ):
                s, Wu, Wd = mlp_w(j)
                if j > 0:
                    mlp_load(j)
                for blk in range(4):
                    bs = slice(blk * 512, (blk + 1) * 512)
                    ab = (ai % 2) * 4
                    ai += 1
                    for fft in range(4):
                        b = BK.get()
                        bank, bk = BK.f32(b), BK.key(b)
                        for kc in range(KC):
                            kb.mm(bank, Wu[:, kc, fft * 128:(fft + 1) * 128], hT[:, kc, bs], kc == 0, kc == KC - 1, [f"WA.{s}", f"hT.{blk}"], [bk])
                        rl, krl = rl_p[ri % 2], f"rl.{ri % 2}"
                        ri += 1
                        kb.act(rl[:], bank, AF.Relu, [bk], [krl])
                        BK.put(b)
                        kb.tt("dve", aT_all[:, ab + fft, :], rl[:], rl[:], ALU.mult, [krl], [f"aT.{ab + fft}"])
                    for tl in range(4):
                        t = blk * 4 + tl
                        for hf in range(2):
                            b = BK.get()
                            bank, bk = BK.f32(b), BK.key(b)
                            for fft in range(4):
                                kb.mm(bank, aT_all[:, ab + fft, tl * 128:(tl + 1) * 128], Wd[:, fft, hf * 512:(hf + 1) * 512], fft == 0, fft == 3,
                                      [f"aT.{ab + fft}", f"WA.{s}"], [bk])
                            xs = xres[:, t, hf * 512:(hf + 1) * 512]
                            kb.tt("dve", xs, bank, xs, ALU.add, [bk, f"xres.{t}"], [f"xres.{t}"])
                            BK.put(b)
                    if j == NJ - 1:
                        for fb in ([blk - 1] if blk > 0 else []) + ([3] if blk == 3 else []):
                            for t in range(4 * fb, 4 * fb + 4):
                                c = 32 + t
                                kb.act(sqj[:], xres[:, t, :], AF.Square, [f"xres.{t}"], ["sqj", f"ss{c}"], accum=ss[:, c:c + 1])
                                kb.ts("dve", ss[:, c:c + 1], ss[:, c:c + 1], 1.0 / D, 1e-6, ALU.mult, ALU.add, [f"ss{c}"], [f"ss{c}"])
                                kb.tt("pool", ss[:, c:c + 1], ss[:, c:c + 1], mhalf[:, 0:1], ALU.pow, [f"ss{c}", "mhalf"], [f"ss{c}"])
                                ot, ko = o_p[t % 2], f"ot.{t % 2}"
                                kb.stt(ot, xres[:, t, :], ss[:, c:c + 1], gfin[:], ALU.mult, ALU.mult, [f"xres.{t}", f"ss{c}", "gfin"],
                                       [ko] + [f"oTb.{k}" for k in range(KC)])
                                kb.dma("sp", ov[:, t, :], ot, [ko], [f"out.{t}"])
            P.barrier()
            P.flush()


def _consts():
    cq = np.zeros((128, NCQ), np.float32)
    i = np.arange(128)
    cq[:, 0:128] = np.eye(128, dtype=np.float32)
    strict = (i[:, None] < i[None, :]).astype(np.float32)
    incl = (i[:, None] <= i[None, :]).astype(np.float32)
    cq[:, 128:256] = strict
    cq[:, 256:384] = incl
    cq[:, 384:512] = strict
    cq[:, 512:640] = incl
    cq[:, 640:768] = (i[:, None] > i[None, :]).astype(np.float32)
    cq[:, 768:896] = ((i[:, None] // 64) == (i[None, :] // 64)).astype(np.float32)
    cq[:, 896:1024] = 1.0
    gam = 1.0 - 2.0 ** (-5.0 - np.arange(8, dtype=np.float64))
    xi = gam[None, :] ** (i[:, None] + 1.0)
    kz = 0.125 * gam[None, :] ** (-(i[:, None] + 1.0))
    gC = np.zeros((128, 4))
    for ct in range(4):
        for p in range(128):
            gC[p, ct] = gam[2 * ct + p // 64] ** 128.0
    invf = (10000.0 ** (-np.arange(32, dtype=np.float32) / 32.0)).astype(np.float32)
    return cq, xi.astype(np.float32), kz.astype(np.float32), gC.astype(np.float32), np.broadcast_to(invf[None], (128, 32))


def _col(v):
    v = np.asarray(v, np.float32).reshape(-1)
    return v.reshape(-1, 128).T


def make_inputs(inp):
    cq, xi, kz, gC, invf = _consts()
    cols = np.zeros((128, NCOLS), np.float32)
    cols[:, 0:8] = _col(inp["norm_mix"][0])
    cols[:, 8:16] = _col(inp["norm_xattn"][0])
    cols[:, 16:24] = _col(inp["norm_mem"][0])
    cols[:, 24:32] = _col(inp["norm_mlp"][0])
    cols[:, 32:36] = _col(inp["ret_gn_w"][0])
    cols[:, 36:40] = _col(inp["ret_gn_b"][0])
    mu = np.asarray(inp["rwkv_mu"][0], np.float32)
    cols[:, 40:54] = _col(mu[:1792])
    cols[:, 54] = mu[1696:1824]
    cols[:, 55:59] = _col(inp["rwkv_w0"][0])
    cols[:, 59:63] = _col(inp["rwkv_a0"][0])
    cols[:, 63:67] = _col(inp["rwkv_k_k"][0])
    cols[:, 67:71] = _col(inp["rwkv_k_a"][0])
    cols[:, 71:75] = _col(inp["rwkv_r_k"][0])
    cols[:, 75:79] = _col(inp["rwkv_gn_w"][0])
    cols[:, 79:83] = _col(inp["rwkv_gn_b"][0])
    cols[:, 83:91] = xi
    cols[:, 91:99] = kz
    cols[:, 99:103] = gC
    cols[:, 103:135] = invf
    shared = {
        "cols": cols, "cq": cq,
        "gfin": np.ascontiguousarray(inp["norm_final"], np.float32),
        "w_in": np.ascontiguousarray(inp["w_in"][0]),
        "w_up": np.ascontiguousarray(inp["rwkv_w_up"][0]),
        "a_up": np.ascontiguousarray(inp["rwkv_a_up"][0]),
        "g_up": np.ascontiguousarray(inp["rwkv_g_up"][0]),
        "w_out": np.ascontiguousarray(inp["w_out"][0]),
        "wq": np.ascontiguousarray(inp["xattn_w_q"][0]),
        "wkv": np.ascontiguousarray(inp["xattn_w_kv"][0]),
        "wo": np.ascontiguousarray(inp["xattn_w_o"][0]),
        "mlp_up": np.ascontiguousarray(inp["mlp_w_up"][0]),
        "mlp_down": np.ascontiguousarray(inp["mlp_w_down"][0]),
    }
    maps = []
    for b in range(8):
        m = dict(shared)
        m["x"] = np.ascontiguousarray(inp["x"][b], np.float32)
        m["mem"] = np.ascontiguousarray(inp["mem"][b], np.float32)
        m["pos"] = np.ascontiguousarray(np.asarray(inp["positions"][b], np.int32).reshape(NT, 128).T)
        maps.append(m)
    return maps


_NC_CACHE = {}


def kernel(**inputs):
    inp = {k: np.asarray(v) for k, v in inputs.items()}
    maps = make_inputs(inp)
    if "nc" not in _NC_CACHE:
        _NC_CACHE["nc"] = build()
    res = run_bass_kernel_spmd(_NC_CACHE["nc"], maps, core_ids=list(range(8)))
    return np.stack([np.asarray(r["out"], np.float32) for r in res.results], axis=0)
```

```python
import contextlib
import math
import numpy as np
import concourse.bass as bass
import concourse.mybir as mybir
from concourse.bass_utils import run_bass_kernel_spmd

F32 = mybir.dt.float32
BF16 = mybir.dt.bfloat16
I32 = mybir.dt.int32
AF = mybir.ActivationFunctionType
ALU = mybir.AluOpType
AX = mybir.AxisListType

T = 2048
D = 1024
NT = 16
KC = 8
NCOLS = 135
NCQ = 1024
CUT = 99
COMPUTE = ("pe", "act", "dve", "pool")
ISSUERS = ("pe", "act", "dve", "pool", "sp")


class Prog:
    def __init__(self, nc, st, n_dma=12, self_sync=True):
        self.nc = nc
        self.n_dma = n_dma
        self.self_sync = self_sync
        self.chans = list(COMPUTE) + [f"d{i}" for i in range(n_dma)]
        self.sems = {c: st.enter_context(nc.semaphore(f"s_{c}")) for c in self.chans}
        self.streams = {e: [] for e in ISSUERS}
        self.count = {c: 0 for c in self.chans}
        self.clock = {e: {} for e in ISSUERS}
        self.snap = {}
        self.wr = {}
        self.rd = {}
        self.rr = 0
        self.nops = 0
        self.nwaits = 0

    @staticmethod
    def _need(needs, d):
        for c, n in d.items():
            if needs.get(c, 0) < n:
                needs[c] = n

    def op(self, eng, fn, reads=(), writes=(), dma=False):
        needs = {}
        for k in reads:
            self._need(needs, self.wr.get(k, {}))
        for k in writes:
            self._need(needs, self.wr.get(k, {}))
            self._need(needs, self.rd.get(k, {}))
        if dma:
            chan = f"d{self.rr % self.n_dma}"
            self.rr += 1
            if self.count[chan]:
                self._need(needs, {chan: self.count[chan]})
        else:
            chan = eng
        clk = self.clock[eng]
        waits = []
        for c, n in needs.items():
            if c == eng and (eng == "pe" or not self.self_sync):
                continue
            if clk.get(c, 0) < n:
                waits.append((c, n))
                for c2, n2 in self.snap[(c, n)].items():
                    if clk.get(c2, 0) < n2:
                        clk[c2] = n2
        self.nwaits += len(waits)
        self.nops += 1
        n_new = self.count[chan] + 1
        self.count[chan] = n_new
        s = dict(clk)
        s[chan] = n_new
        self.snap[(chan, n_new)] = s
        if eng == "pe" and chan == "pe":
            clk["pe"] = n_new
        wset = set(writes)
        for k in wset:
            self.wr[k] = {chan: n_new}
            self.rd[k] = {}
        for k in reads:
            if k in wset:
                continue
            self.rd.setdefault(k, {})[chan] = n_new
        self.streams[eng].append((waits, fn, chan))

    def barrier(self):
        allk = {c: n for c, n in self.count.items() if n}
        for e in ISSUERS:
            clk = self.clock[e]
            waits = []
            for c, n in allk.items():
                if c == e and e == "pe":
                    continue
                if clk.get(c, 0) < n:
                    waits.append((c, n))
            if waits:
                self.streams[e].append((waits, None, None))
        for e in ISSUERS:
            for c, n in allk.items():
                if self.clock[e].get(c, 0) < n:
                    self.clock[e][c] = n

    def flush(self):
        nc = self.nc
        streams = self.streams
        if not any(streams[e] for e in ISSUERS):
            return
        self.streams = {e: [] for e in ISSUERS}
        sems = self.sems

        def val(c, n):
            return n * 16 if c.startswith("d") else n

        def run(engname):
            def body(e):
                for waits, fn, chan in streams[engname]:
                    for c, n in waits:
                        e.wait_ge(sems[c], val(c, n))
                    if fn is not None:
                        fn(e).then_inc(sems[chan], 16 if chan.startswith("d") else 1)
            return body

        with nc.Block() as block:
            block.tensor(run("pe"))
            block.scalar(run("act"))
            block.vector(run("dve"))
            block.gpsimd(run("pool"))
            block.sync(run("sp"))


class Ring:
    def __init__(self, kb, name, n, shape, dtype, st=None, psum=False):
        st = st or kb.st
        alloc = kb.nc.psum_tensor if psum else kb.nc.sbuf_tensor
        self.name = name
        self.t = [st.enter_context(alloc(f"rg_{name}{i}", shape, dtype)) for i in range(n)]
        self.i = 0

    def next(self):
        i = self.i % len(self.t)
        self.i += 1
        return self.t[i], f"{self.name}.{i}"


class ViewRing:
    def __init__(self, aps, keys):
        self.t, self.k, self.i = aps, keys, 0

    def next(self):
        i = self.i % len(self.t)
        self.i += 1
        return self.t[i], self.k[i]


class Banks:
    def __init__(self, pp):
        import collections
        self.pp = pp
        self.free = collections.deque(range(8))

    def get(self):
        return self.free.popleft()

    def put(self, i):
        self.free.append(i)

    def f32(self, i):
        return self.pp[:, i, :]

    def bf(self, i):
        return self.pp[:, i, :].bitcast(BF16)

    @staticmethod
    def key(i):
        return f"ps{i}"


def interleave(gens):
    gens = list(gens)
    while gens:
        for g in list(gens):
            try:
                next(g)
            except StopIteration:
                gens.remove(g)


class KB:
    def __init__(self, nc, P, st):
        self.nc, self.P, self.st = nc, P, st
        self.flip = 0

    def sb(self, name, shape, dtype, st=None):
        return (st or self.st).enter_context(self.nc.sbuf_tensor("sb_" + name, shape, dtype))

    def mm(self, out, lhsT, rhs, start, stop, r, w):
        self.P.op("pe", lambda e: e.matmul(out, lhsT=lhsT, rhs=rhs, start=start, stop=stop), r, w)

    def tr(self, out, in_, r, w):
        idb = self.identb[:]
        self.P.op("pe", lambda e: e.transpose(out=out, in_=in_, identity=idb), list(r) + ["identb"], w)

    def act(self, out, in_, func, r, w, scale=1.0, bias=0.0, accum=None):
        kw = {}
        if accum is not None:
            kw["accum_out"] = accum
        self.P.op("act", lambda e: e.activation(out=out, in_=in_, func=func, bias=bias, scale=scale, **kw), r, w)

    def tt(self, eng, out, in0, in1, op, r, w):
        self.P.op(eng, lambda e: e.tensor_tensor(out=out, in0=in0, in1=in1, op=op), r, w)

    def ts(self, eng, out, in0, s1, s2, op0, op1, r, w):
        if s2 is None:
            self.P.op(eng, lambda e: e.tensor_scalar(out=out, in0=in0, scalar1=s1, scalar2=None, op0=op0), r, w)
        else:
            self.P.op(eng, lambda e: e.tensor_scalar(out=out, in0=in0, scalar1=s1, scalar2=s2, op0=op0, op1=op1), r, w)

    def stt(self, out, in0, sc, in1, op0, op1, r, w):
        self.P.op("dve", lambda e: e.scalar_tensor_tensor(out=out, in0=in0, scalar=sc, in1=in1, op0=op0, op1=op1), r, w)

    def cp(self, eng, out, in_, r, w):
        if eng == "act":
            self.P.op("act", lambda e: e.activation(out=out, in_=in_, func=AF.Copy), r, w)
        else:
            self.P.op(eng, lambda e: e.tensor_copy(out=out, in_=in_), r, w)

    def cpalt(self, out, in_, r, w):
        self.flip ^= 1
        self.cp("act" if self.flip else "dve", out, in_, r, w)

    def red(self, out, in_, op, r, w):
        self.P.op("dve", lambda e: e.tensor_reduce(out=out, in_=in_, axis=AX.X, op=op), r, w)

    def dma(self, eng, out, in_, r, w):
        self.P.op(eng, lambda e: e.dma_start(out=out, in_=in_), r, w, dma=True)


def build(stages="ABCD", dbg=False):
    nc = bass.Bass("TRN2", target_bir_lowering=False)

    def dram(n, s, d, kind="ExternalInput"):
        return nc.dram_tensor(n, s, d, kind=kind).ap()

    x_d = dram("x", [T, D], F32)
    mem_d = dram("mem", [256, D], F32)
    pos_d = dram("pos", [128, NT], I32)
    cols_d = dram("cols", [128, NCOLS], F32)
    cq_d = dram("cq", [128, NCQ], F32)
    gfin_d = dram("gfin", [D], F32)
    w_in_d = dram("w_in", [D, 3872], F32)
    wup_d = dram("w_up", [64, 512], F32)
    aup_d = dram("a_up", [64, 512], F32)
    gup_d = dram("g_up", [160, 512], F32)
    w_out_d = dram("w_out", [D, D], F32)
    wq_d = dram("wq", [D, D], F32)
    wkv_d = dram("wkv", [D, 2 * D], F32)
    wo_d = dram("wo", [D, D], F32)
    wupm_d = dram("mlp_up", [D, 4 * D], F32)
    wdn_d = dram("mlp_down", [4 * D, D], F32)
    out_d = dram("out", [T, D], F32, kind="ExternalOutput")
    if dbg:
        dbg_yT = dram("dbg_yT", [128, KC, T], BF16, kind="ExternalOutput")
        dbg_hT = dram("dbg_hT", [128, KC, T], BF16, kind="ExternalOutput")

    def wview(w):
        return w.rearrange("(kc p) n -> p kc n", p=128)

    with contextlib.ExitStack() as st:
        P = Prog(nc, st)
        kb = KB(nc, P, st)
        cols = kb.sb("cols", [128, NCOLS], F32)
        dc = kb.sb("dcols", [128, 40], F32)
        cqf = kb.sb("cqf", [128, NCQ], F32)
        identb = kb.sb("identb", [128, 128], BF16)
        bonesb = kb.sb("bonesb", [128, 128], BF16)
        onesb = kb.sb("onesb", [128, 128], BF16)
        mhalf = kb.sb("mhalf", [128, 256], F32)
        hT = kb.sb("hT", [128, KC, T], BF16)
        yT = kb.sb("yT", [128, 4, T], BF16)
        WA = kb.sb("WA", [128, 16384], BF16)
        ss = kb.sb("ss", [128, 3 * NT], F32)
        kb.identb = identb
        pp = st.enter_context(nc.psum_tensor("pp", [128, 8, 512], F32))
        L_BK = Banks(pp)
        psA = ViewRing([pp[:, i, :] for i in range(6)], [f"ps{i}" for i in range(6)])
        psT = ViewRing([pp[:, i, :].bitcast(BF16) for i in (6, 7)], ["ps6", "ps7"])
        mask4 = cqf[:, 128:640]
        maskI = cqf[:, 256:384]
        maskLs = cqf[:, 640:768]
        identf = cqf[:, 0:128]

        kb.dma("sp", cols[:], cols_d, [], ["cols"])
        kb.dma("sp", cqf[:], cq_d, [], ["cqf"])
        kb.dma("pool", identb[:], cq_d[:, 0:128], [], ["identb"])
        kb.dma("pool", bonesb[:], cq_d[:, 768:896], [], ["bonesb"])
        kb.dma("pool", onesb[:], cq_d[:, 896:1024], [], ["onesb"])
        P.op("dve", lambda e: e.memset(mhalf[:], -0.5), [], ["mhalf"])
        kb.ts("dve", dc[:, 0:15], cols[:, 40:55], -1.0, 1.0, ALU.mult, ALU.add, ["cols"], ["dc"])
        kb.ts("dve", dc[:, 15:19], cols[:, 55:59], 0.5, None, ALU.mult, None, ["cols"], ["dc"])
        kb.ts("dve", dc[:, 19:23], cols[:, 59:63], 0.5, None, ALU.mult, None, ["cols"], ["dc"])
        kb.ts("dve", dc[:, 23:27], cols[:, 67:71], -1.0, None, ALU.mult, None, ["cols"], ["dc"])
        kb.ts("dve", dc[:, 27:31], cols[:, 67:71], -1.0, 1.0, ALU.mult, ALU.add, ["cols"], ["dc"])

        def norm_T(src, nblk, tpb, gcol0, dst, dstkey, sscol0, hb, sqj):
            for blk in range(nblk):
                hbs = []
                for tl in range(tpb):
                    t = blk * tpb + tl
                    xa, xk = src(t)
                    c = sscol0 + t
                    kb.act(sqj[:], xa, AF.Square, [xk], ["sqj", f"ss{c}"], accum=ss[:, c:c + 1])
                    kb.ts("dve", ss[:, c:c + 1], ss[:, c:c + 1], 1.0 / D, 1e-6, ALU.mult, ALU.add, [f"ss{c}"], [f"ss{c}"])
                    kb.tt("pool", ss[:, c:c + 1], ss[:, c:c + 1], mhalf[:, 0:1], ALU.pow, [f"ss{c}", "mhalf"], [f"ss{c}"])
                    h, hk = hb.next()
                    kb.act(h[:], xa, AF.Copy, [xk, f"ss{c}"], [hk], scale=ss[:, c:c + 1])
                    hbs.append((h, hk))
                for kc in range(KC):
                    bank, bk = psT.next()
                    for tl, (h, hk) in enumerate(hbs):
                        kb.tr(bank[:, tl * 128:(tl + 1) * 128], h[:, kc * 128:(kc + 1) * 128], [hk], [bk])
                    n = tpb * 128
                    o = dst[:, kc, blk * n:(blk + 1) * n]
                    kb.flip ^= 1
                    if kb.flip:
                        kb.act(o, bank[:, 0:n], AF.Copy, [bk, "cols"], [dstkey(blk)], scale=cols[:, gcol0 + kc:gcol0 + kc + 1])
                    else:
                        kb.ts("dve", o, bank[:, 0:n], cols[:, gcol0 + kc:gcol0 + kc + 1], None, ALU.mult, None, [bk, "cols"], [dstkey(blk)])

        def load_w(dst, src, key, eng="pool"):
            kb.dma(eng, dst, src, [], [key])

        Wv8 = lambda ncol: WA[:, 0:KC * ncol].rearrange("p (k n) -> p k n", k=KC)

        Wr = Wv8(2048)
        BK = L_BK

        def acquire(n=1):
            spins = 0
            while len(BK.free) < n:
                spins += 1
                assert spins < 100000, "PSUM bank pool deadlock"
                yield
            return [BK.get() for _ in range(n)]

        def rr(gens):
            gens = list(gens)
            while gens:
                for g in list(gens):
                    try:
                        next(g)
                    except StopIteration:
                        gens.remove(g)
                yield

        def run(g):
            for _ in g:
                pass

        if "B" not in stages:
            with contextlib.ExitStack() as sa:
                xring = Ring(kb, "xr", 3, [128, D], F32, st=sa)
                hb = Ring(kb, "hb", 8, [128, D], BF16, st=sa)
                sqj = kb.sb("sqj", [128, D], BF16, st=sa)

                def srcx(t):
                    xa, xk = xring.next()
                    kb.dma("sp", xa[:], x_d[t * 128:(t + 1) * 128, :], [], [xk])
                    return xa[:], xk
                norm_T(srcx, 4, 4, 0, hT, lambda b: f"hT.{b}", 0, hb, sqj)
                P.barrier()
                P.flush()
            P.op("dve", lambda e: e.memset(yT[:, 0:4, :], 0.0), [], [f"yT.{t}.0" for t in range(NT)])
        else:
            for gi in range(4):
                load_w(Wr[:, :, gi * 512:(gi + 1) * 512], wview(w_in_d)[:, :, gi * 512:(gi + 1) * 512], f"W.{gi}")
            with contextlib.ExitStack() as sbk:
                cos_t = kb.sb("cos_t", [128, NT, 32], F32, st=sbk)
                sin_t = kb.sb("sin_t", [128, NT, 32], F32, st=sbk)
                srope = sbk
                if True:
                    posi = kb.sb("posi", [128, NT], I32, st=srope)
                    posf = kb.sb("posf", [128, NT], F32, st=srope)
                    ang = kb.sb("ang", [128, NT, 32], F32, st=srope)
                    ra = kb.sb("ra", [128, NT, 32], F32, st=srope)
                    rb_ = kb.sb("rb_", [128, NT, 32], F32, st=srope)
                    ri = kb.sb("ri", [128, NT, 32], I32, st=srope)
                    kb.dma("sp", posi[:], pos_d, [], ["posi"])
                    kb.cp("dve", posf[:], posi[:], ["posi"], ["posf"])
                    kb.tt("dve", ang[:], posf[:].unsqueeze(2).to_broadcast([128, NT, 32]),
                          cols[:, 103:135].unsqueeze(1).to_broadcast([128, NT, 32]), ALU.mult, ["posf", "cols"], ["ang"])
                    C1 = 6.28125
                    C2 = 2.0 * math.pi - C1
                    for tab, shift, nm in ((sin_t, 0.0, "sin"), (cos_t, 0.5 * math.pi, "cos")):
                        kb.ts("dve", ra[:], ang[:], shift, 1.0 / (2.0 * math.pi), ALU.add, ALU.mult, ["ang"], ["ra"])
                        kb.cp("dve", ri[:], ra[:], ["ra"], ["ri"])
                        kb.cp("dve", rb_[:], ri[:], ["ri"], ["rb"])
                        kb.ts("dve", ra[:], ang[:], shift, None, ALU.add, None, ["ang"], ["ra"])
                        kb.stt(ra[:], rb_[:], -C1, ra[:], ALU.mult, ALU.add, ["rb", "ra"], ["ra"])
                        kb.stt(ra[:], rb_[:], -C2, ra[:], ALU.mult, ALU.add, ["rb", "ra"], ["ra"])
                        kb.ts("dve", rb_[:], ra[:], math.pi, -2.0 * math.pi, ALU.is_gt, ALU.mult, ["ra"], ["rb"])
                        kb.tt("dve", ra[:], ra[:], rb_[:], ALU.add, ["ra", "rb"], ["ra"])
                        kb.ts("dve", rb_[:], ra[:], -math.pi, 2.0 * math.pi, ALU.is_lt, ALU.mult, ["ra"], ["rb"])
                        kb.tt("dve", ra[:], ra[:], rb_[:], ALU.add, ["ra", "rb"], ["ra"])
                        kb.act(tab[:], ra[:], AF.Sin, ["ra"], [nm])

                sb_ = lambda n, shp, d: kb.sb(n, shp, d, st=sbk)
                W = 2
                xr_p = [sb_(f"xr{i}", [128, D], F32) for i in range(2)]
                hb_p = [sb_(f"hb{i}", [128, D], BF16) for i in range(4)]
                sqj = sb_("sqj", [128, D], BF16)
                qT_p = [sb_(f"qT{i}", [128, 4, 2, 512], BF16) for i in range(2)]
                for i in range(2):
                    P.op("dve", lambda e, i=i: e.memset(qT_p[i][:], 0.0), [], [f"qT{i}.{j}" for j in range(4)])
                kT_p = [sb_(f"kT{i}", [128, 4, 512], BF16) for i in range(2)]
                ktok_p = [sb_(f"ktok{i}", [128, 4, 512], BF16) for i in range(2)]
                vtok_p = [sb_(f"vtok{i}", [128, 4, 512], BF16) for i in range(2)]
                gateT_p = [sb_(f"gateT{i}", [128, 4, 512], BF16) for i in range(2)]
                xs_p = [sb_(f"xs{i}", [128, 512], F32) for i in range(W)]
                A_p = [sb_(f"rA{i}", [128, 512], F32) for i in range(W)]
                B_p = [sb_(f"rB{i}", [128, 512], F32) for i in range(W)]
                qtok_p = [sb_(f"qtok{i}", [128, 512], BF16) for i in range(W)]
                th_p = [sb_(f"thr{i}", [128, 512], F32) for i in range(2)]
                sT_p = [sb_(f"sT{i}", [128, 8, 128], BF16) for i in range(W)]
                y32_p = [sb_(f"y32{i}", [128, 512], F32) for i in range(W)]
                sq_p = [sb_(f"sqr{i}", [128, 512], F32) for i in range(W)]
                ynb_p = [sb_(f"ynb{i}", [128, 512], BF16) for i in range(W)]
                yaff_p = [sb_(f"yaff{i}", [128, 2, 128], F32) for i in range(W)]
                stat_p = [sb_(f"stat{i}", [128, 64], F32) for i in range(W)]
                R32 = sb_("R32", [128, 4, 64], F32)
                Rbf = sb_("Rbf", [128, 5, 4, 64], BF16)
                P.op("dve", lambda e: e.memset(R32[:], 0.0), [], ["R32"])

                def gen_normA(blk):
                    for tl in range(4):
                        t = blk * 4 + tl
                        xa, xk = xr_p[t % 2], f"xr.{t % 2}"
                        kb.dma("sp", xa[:], x_d[t * 128:(t + 1) * 128, :], [], [xk])
                        kb.act(sqj[:], xa[:], AF.Square, [xk], ["sqj", f"ss{t}"], accum=ss[:, t:t + 1])
                        yield
                        kb.ts("dve", ss[:, t:t + 1], ss[:, t:t + 1], 1.0 / D, 1e-6, ALU.mult, ALU.add, [f"ss{t}"], [f"ss{t}"])
                        yield
                        kb.tt("pool", ss[:, t:t + 1], ss[:, t:t + 1], mhalf[:, 0:1], ALU.pow, [f"ss{t}", "mhalf"], [f"ss{t}"])
                        yield
                        kb.act(hb_p[tl][:], xa[:], AF.Copy, [xk, f"ss{t}"], [f"hb.{tl}"], scale=ss[:, t:t + 1])
                        yield
                    for kc in range(KC):
                        (b,) = yield from acquire(1)
                        bankT, bk = BK.bf(b), BK.key(b)
                        for tl in range(4):
                            kb.tr(bankT[:, tl * 128:(tl + 1) * 128], hb_p[tl][:, kc * 128:(kc + 1) * 128], [f"hb.{tl}"], [bk])
                        yield
                        o = hT[:, kc, blk * 512:(blk + 1) * 512]
                        if kc % 2:
                            kb.act(o, bankT[:, 0:512], AF.Copy, [bk, "cols"], [f"hT.{blk}"], scale=cols[:, kc:kc + 1])
                        else:
                            kb.ts("dve", o, bankT[:, 0:512], cols[:, kc:kc + 1], None, ALU.mult, None, [bk, "cols"], [f"hT.{blk}"])
                        BK.put(b)
                        yield

                def post_gen(slot, b, eps, wcol0, bcol0, finish):
                    bank, bk = BK.f32(b), BK.key(b)
                    y32, k1 = y32_p[slot], f"y32.{slot}"
                    kb.cp("act", y32[:], bank, [bk], [k1])
                    BK.put(b)
                    yield
                    stt_, ks = stat_p[slot], f"stat.{slot}"
                    v3 = lambda a: a.rearrange("p (h d) -> p h d", d=64)
                    bc = lambda a: a.unsqueeze(2).to_broadcast([128, 8, 64])
                    kb.red(stt_[:, 0:8], v3(y32[:]), ALU.add, [k1], [ks])
                    sq, k2 = sq_p[slot], f"sq.{slot}"
                    kb.act(sq[:], y32[:], AF.Square, [k1], [k2])
                    yield
                    kb.red(stt_[:, 8:16], v3(sq[:]), ALU.add, [k2], [ks])
                    yield
                    kb.ts("dve", stt_[:, 16:24], stt_[:, 0:8], 1.0 / 64, None, ALU.mult, None, [ks], [ks])
                    yield
                    kb.tt("dve", stt_[:, 24:32], stt_[:, 16:24], stt_[:, 16:24], ALU.mult, [ks], [ks])
                    kb.ts("dve", stt_[:, 32:40], stt_[:, 8:16], 1.0 / 64, eps, ALU.mult, ALU.add, [ks], [ks])
                    yield
                    kb.tt("dve", stt_[:, 40:48], stt_[:, 32:40], stt_[:, 24:32], ALU.subtract, [ks], [ks])
                    yield
                    kb.tt("pool", stt_[:, 48:56], stt_[:, 40:48], mhalf[:, 0:8], ALU.pow, [ks, "mhalf"], [ks])
                    yield
                    kb.stt(stt_[:, 56:64], stt_[:, 16:24], -1.0, stt_[:, 48:56], ALU.mult, ALU.mult, [ks], [ks])
                    kb.tt("pool", v3(y32[:]), v3(y32[:]), bc(stt_[:, 48:56]), ALU.mult, [k1, ks], [k1])
                    yield
                    ynb, k3 = ynb_p[slot], f"ynb.{slot}"
                    kb.tt("dve", v3(ynb[:]), v3(y32[:]), bc(stt_[:, 56:64]), ALU.add, [k1, ks], [k3])
                    yield
                    (b2,) = yield from acquire(1)
                    bankT, kt = BK.bf(b2), BK.key(b2)
                    for ct in range(4):
                        kb.tr(bankT[:, ct * 128:(ct + 1) * 128], ynb[:, ct * 128:(ct + 1) * 128], [k3], [kt])
                    yield
                    for ct in range(4):
                        ya, k4 = yaff_p[slot][:, ct % 2, :], f"yaff.{slot}.{ct % 2}"
                        kb.act(ya, bankT[:, ct * 128:(ct + 1) * 128], AF.Identity, [kt, "cols"], [k4],
                               scale=cols[:, wcol0 + ct:wcol0 + ct + 1], bias=cols[:, bcol0 + ct:bcol0 + ct + 1])
                        finish(ct, ya, k4)
                        if ct == 3:
                            BK.put(b2)
                        yield

                def qk_chain(slot, blk, tl, wi):
                    par = blk % 2
                    t = blk * 4 + tl
                    tok = slice(t * 128, (t + 1) * 128)
                    hk = f"hT.{blk}"
                    tabcol = 83 if wi == 0 else 91
                    (b,) = yield from acquire(1)
                    bank, bk = BK.f32(b), BK.key(b)
                    for kc in range(KC):
                        kb.mm(bank, hT[:, kc, tok], Wr[:, kc, wi * 512:(wi + 1) * 512], kc == 0, kc == KC - 1, [hk, f"W.{wi}"], [bk])
                    yield
                    xs, kx = xs_p[slot], f"xs.{slot}"
                    kb.cp("act", xs[:], bank, [bk], [kx])
                    BK.put(b)
                    yield
                    A, ka = A_p[slot], f"rA.{slot}"
                    B, kbk = B_p[slot], f"rB.{slot}"
                    v4 = lambda a: a.rearrange("p (h two f) -> p h two f", two=2, f=32)
                    cs4 = cos_t[:, t, :].unsqueeze(1).unsqueeze(1).to_broadcast([128, 8, 2, 32])
                    sn3 = sin_t[:, t, :].unsqueeze(1).to_broadcast([128, 8, 32])
                    kb.tt("dve", v4(A[:]), v4(xs[:]), cs4, ALU.mult, [kx, "cos"], [ka])
                    kb.tt("pool", v4(B[:])[:, :, 0, :], v4(xs[:])[:, :, 1, :], sn3, ALU.mult, [kx, "sin"], [kbk])
                    kb.tt("pool", v4(B[:])[:, :, 1, :], v4(xs[:])[:, :, 0, :], sn3, ALU.mult, [kx, "sin"], [kbk])
                    yield
                    kb.tt("dve", v4(A[:])[:, :, 0, :], v4(A[:])[:, :, 0, :], v4(B[:])[:, :, 0, :], ALU.subtract, [ka, kbk], [ka])
                    kb.tt("dve", v4(A[:])[:, :, 1, :], v4(A[:])[:, :, 1, :], v4(B[:])[:, :, 1, :], ALU.add, [ka, kbk], [ka])
                    yield
                    scl = cols[:, tabcol:tabcol + 8].unsqueeze(2).to_broadcast([128, 8, 64])
                    A3 = A[:].rearrange("p (h d) -> p h d", d=64)
                    if wi == 0:
                        qtv, kq = qtok_p[slot][:], f"qtok.{slot}"
                    else:
                        qtv, kq = ktok_p[par][:, tl, :], f"ktok{par}.{tl}"
                    kb.tt("dve", qtv.rearrange("p (h d) -> p h d", d=64), A3, scl, ALU.mult, [ka, "cols"], [kq])
                    yield
                    (b2,) = yield from acquire(1)
                    bankT, kt = BK.bf(b2), BK.key(b2)
                    for ct in range(4):
                        kb.tr(bankT[:, ct * 128:(ct + 1) * 128], qtv[:, ct * 128:(ct + 1) * 128], [kq], [kt])
                    yield
                    bv = bankT[:, 0:512].rearrange("p (c n) -> p c n", c=4)
                    if wi == 0:
                        kb.cp("act", qT_p[par][0:64, :, 0, tl * 128:(tl + 1) * 128], bv[0:64], [kt], [f"qT{par}.{tl}"])
                        kb.cp("dve", qT_p[par][64:128, :, 1, tl * 128:(tl + 1) * 128], bv[64:128], [kt], [f"qT{par}.{tl}"])
                    else:
                        kb.cpalt(kT_p[par][:, :, tl * 128:(tl + 1) * 128], bv, [kt], [f"kT{par}.{tl}"])
                    BK.put(b2)
                    yield

                def v_chain(blk, tl):
                    par = blk % 2
                    t = blk * 4 + tl
                    tok = slice(t * 128, (t + 1) * 128)
                    (b,) = yield from acquire(1)
                    bank, bk = BK.f32(b), BK.key(b)
                    for kc in range(KC):
                        kb.mm(bank, hT[:, kc, tok], Wr[:, kc, 1024:1536], kc == 0, kc == KC - 1, [f"hT.{blk}", "W.2"], [bk])
                    yield
                    kb.cpalt(vtok_p[par][:, tl, :], bank, [bk], [f"vtok{par}.{tl}"])
                    BK.put(b)
                    yield

                def g_chain(blk, ct):
                    par = blk % 2
                    (b,) = yield from acquire(1)
                    bank, bk = BK.f32(b), BK.key(b)
                    for kc in range(KC):
                        kb.mm(bank, Wr[:, kc, 1536 + ct * 128:1536 + (ct + 1) * 128], hT[:, kc, blk * 512:(blk + 1) * 512],
                              kc == 0, kc == KC - 1, [f"hT.{blk}", "W.3"], [bk])
                    yield
                    th, kth = th_p[ct % 2], f"thr.{ct % 2}"
                    kb.act(th[:], bank, AF.Tanh, [bk], [kth], scale=0.5)
                    yield
                    kb.ts("pool", th[:], th[:], 0.5, 0.5, ALU.mult, ALU.add, [kth], [kth])
                    yield
                    kb.tt("dve", gateT_p[par][:, ct, :], th[:], bank, ALU.mult, [kth, bk], [f"gateT{par}.{ct}"])
                    BK.put(b)
                    yield

                def gen_proj(blk):
                    for k in range(4):
                        yield from rr([qk_chain((2 * k) % W, blk, k, 0), qk_chain((2 * k + 1) % W, blk, k, 1), v_chain(blk, k), g_chain(blk, k)])

                def chunk_chain(slot, blk, tl):
                    par = blk % 2
                    qT, kT_, vtok, gateT = qT_p[par], kT_p[par], vtok_p[par], gateT_p[par]
                    t = blk * 4 + tl
                    tok = slice(t * 128, (t + 1) * 128)
                    tl_ = slice(tl * 128, (tl + 1) * 128)
                    sT, ksT = sT_p[slot], f"sT.{slot}"
                    bs = yield from acquire(2)
                    for g4 in range(2):
                        bank, bk = BK.f32(bs[g4]), BK.key(bs[g4])
                        for hh in range(4):
                            h = 4 * g4 + hh
                            kb.mm(bank[:, hh * 128:(hh + 1) * 128], kT_[:, h // 2, tl_], qT[:, h // 2, h % 2, tl_], True, True,
                                  [f"kT{par}.{tl}", f"qT{par}.{tl}"], [bk])
                    yield
                    for g4 in range(2):
                        bank, bk = BK.f32(bs[g4]), BK.key(bs[g4])
                        kb.tt("dve", sT[:, 4 * g4:4 * g4 + 4, :], bank.rearrange("p (c n) -> p c n", c=4),
                              maskI.unsqueeze(1).to_broadcast([128, 4, 128]), ALU.mult, [bk, "cqf"], [ksT])
                        BK.put(bs[g4])
                        yield
                    (bo,) = yield from acquire(1)
                    bankO, ko = BK.f32(bo), BK.key(bo)
                    for h in range(8):
                        kb.mm(bankO[:, 64 * h:64 * h + 64], sT[:, h, :], vtok[:, tl, 64 * h:64 * h + 64], True, t == 0,
                              [ksT, f"vtok{par}.{tl}"], [ko])
                        if t > 0:
                            kb.mm(bankO[:, 64 * h:64 * h + 64], qT[:, h // 2, h % 2, tl_], Rbf[:, t % 5, h // 2, :], False, True,
                                  [f"qT{par}.{tl}", f"Rbf.{t % 5}"], [ko])
                    yield

                    def fin(ct, ya, k4):
                        kb.tt("dve", yT[:, ct, tok], ya, gateT[:, ct, tl_], ALU.mult, [k4, f"gateT{par}.{ct}"], [f"yT.{t}.0"])
                    yield from post_gen(slot, bo, 1e-5, 32, 36, fin)

                def gen_chunks(blk):
                    par = blk % 2
                    ktok, vtok = ktok_p[par], vtok_p[par]
                    for tl in range(4):
                        t = blk * 4 + tl
                        if t == NT - 1:
                            continue
                        (b,) = yield from acquire(1)
                        bankK, kk_ = BK.f32(b), BK.key(b)
                        for ct in range(4):
                            kb.mm(bankK[:, ct * 128:(ct + 1) * 128], ktok[:, tl, ct * 128:(ct + 1) * 128], vtok[:, tl, ct * 128:(ct + 1) * 128],
                                  True, True, [f"ktok{par}.{tl}", f"vtok{par}.{tl}"], [kk_])
                        yield
                        for hh in range(2):
                            b0 = 64 * hh
                            kb.tt("dve", R32[b0:b0 + 64, :, :], bankK[b0:b0 + 64, :].rearrange("p (c n) -> p c n", c=4)[:, :, b0:b0 + 64],
                                  R32[b0:b0 + 64, :, :], ALU.add, [kk_, "R32"], ["R32"])
                        BK.put(b)
                        yield
                        kb.tt("dve", R32[:], R32[:], cols[:, 99:103].unsqueeze(2).to_broadcast([128, 4, 64]), ALU.mult, ["R32", "cols"], ["R32"])
                        yield
                        kb.cp("act", Rbf[:, (t + 1) % 5, :, :], R32[:], ["R32"], [f"Rbf.{(t + 1) % 5}"])
                        yield
                    yield from rr([chunk_chain(0, blk, 0), chunk_chain(1, blk, 1)])
                    yield from rr([chunk_chain(0, blk, 2), chunk_chain(1, blk, 3)])

                run(gen_normA(0))
                interleave([gen_proj(0), gen_normA(1)])
                for blk in range(4):
                    if blk == 3 and "C" in stages:
                        Wk = Wv8(1824)
                        kb.dma("pool", Wk[:, :, :], wview(w_in_d)[:, :, 2048:3872], [], ["Wk", "W.0", "W.1", "W.2", "W.3"])
                    gs = [gen_chunks(blk)]
                    if blk + 1 < 4:
                        gs.append(gen_proj(blk + 1))
                    if blk + 2 < 4:
                        gs.append(gen_normA(blk + 2))
                    interleave(gs)
                if dbg:
                    kb.dma("sp", dbg_hT, hT[:], [f"hT.{b}" for b in range(4)], [])
                P.barrier()
                P.flush()

        if "C" in stages:
            stage_C(nc, P, kb, locals())
        else:
            P.op("dve", lambda e: e.memset(hT[:, 0:4, :], 0.0), [f"hT.{b}" for b in range(4)], [f"yT.{t}.1" for t in range(NT)])

        if dbg:
            kb.dma("sp", dbg_yT[:, 0:4, :], yT[:], [f"yT.{t}.0" for t in range(NT)], [])
            kb.dma("sp", dbg_yT[:, 4:8, :], hT[:, 0:4, :], [f"yT.{t}.1" for t in range(NT)], [])

        if "D" in stages:
            stage_D(nc, P, kb, locals())
        P.barrier()
        P.flush()
    return nc


def stage_C(nc, P, kb, L):
    cols, dc, cqf, hT, WA, mhalf, bonesb, BK = (L[k] for k in ("cols", "dc", "cqf", "hT", "WA", "mhalf", "bonesb", "L_BK"))
    wup_d, aup_d, gup_d, w_in_d = L["wup_d"], L["aup_d"], L["gup_d"], L["w_in_d"]
    mask4, maskLs, identf = L["mask4"], L["maskLs"], L["identf"]
    onesf = cqf[:, 896:1024]
    Wk = WA[:, 0:KC * 1824].rearrange("p (k n) -> p k n", k=KC)
    CL = 0.5 * math.exp(-0.5)
    NB = 256
    NBLK = T // NB
    with contextlib.ExitStack() as sc:
        if "B" not in L["stages"]:
            kb.dma("pool", Wk[:, :, :], w_in_d.rearrange("(kc p) n -> p kc n", p=128)[:, :, 2048:3872], [], ["Wk"])
        sb = lambda n, s, d: kb.sb(n, s, d, st=sc)
        wupz = sb("wupz", [128, 512], BF16)
        aupz = sb("aupz", [128, 512], BF16)
        gup1b = sb("gup1b", [128, 512], BF16)
        gup2z = sb("gup2z", [128, 512], BF16)
        for tz in (wupz, aupz, gup2z):
            P.op("dve", lambda e, tz=tz: e.memset(tz[:], 0.0), [], ["wupb", "gupb"])
        kb.dma("pool", wupz[0:64, :], wup_d, [], ["wupb"])
        kb.dma("pool", aupz[64:128, :], aup_d, [], ["wupb"])
        kb.dma("pool", gup1b[:], gup_d[0:128, :], [], ["gupb"])
        kb.dma("pool", gup2z[96:128, :], gup_d[128:160, :], [], ["gupb"])
        RKblk = sb("RKblk", [128, 4, 128], BF16)
        for ct in range(4):
            kb.ts("dve", RKblk[:, ct, :], cqf[:, 768:896], cols[:, 71 + ct:72 + ct], None, ALU.mult, None, ["cqf", "cols"], ["RKblk"])
        pT = sb("pT", [128, 15, NB + 1], F32)
        P.op("dve", lambda e: e.memset(pT[:], 0.0), [], [f"pT.{i}" for i in range(15)])
        carry = sb("carry", [128, 15], F32)
        NTMP = 10
        T_p = [[sb(f"T{c}_{i}", [128, NB], F32) for i in range(NTMP)] for c in range(2)]
        sh_p = [sb(f"shT{i}", [128, NB], F32) for i in range(2)]
        kk2_p = [sb(f"kk2{i}", [128, NB], BF16) for i in range(2)]
        twa = sb("twa", [128, NB], BF16)
        sg1 = sb("sg1", [128, NB], BF16)
        sg2 = sb("sg2", [128, NB], BF16)
        ARt = sb("ARt", [128, 4, 2, 2, 2, 128], BF16)
        P.op("dve", lambda e: e.memset(ARt[:], 0.0), [], [f"ARt.{i}" for i in range(4)])
        KT = sb("KT", [128, 4, NB], BF16)
        BnT = sb("BnT", [128, 4, NB], BF16)
        rkb = sb("rkb", [128, 4, NB], BF16)
        vbf_p = [sb(f"vbf{i}", [128, 4, NB], BF16) for i in range(2)]
        gT = sb("gT", [128, 4, NB], BF16)
        Ktok = sb("Ktok", [128, 2, 512], BF16)
        Btok = sb("Btok", [128, 2, 512], BF16)
        Vtok = sb("Vtok", [128, 2, 512], BF16)
        PCt = sb("PCt", [128, 4, 2], F32)
        MTs = [sb(f"MT{i}", [128, 8, 512], BF16) for i in range(2)]
        mraw_p = [sb(f"mraw{i}", [128, 512], BF16) for i in range(2)]
        mask4b = sb("mask4b", [128, 512], BF16)
        kb.dma("pool", mask4b[:], L["cq_d"][:, 128:640], [], ["mask4b"])
        Xm = [sb(f"Xm{i}", [128, 8, 128], BF16) for i in range(2)]
        Nm = [sb(f"Nm{i}", [128, 8, 128], BF16) for i in range(2)]
        Lm = [sb(f"Lm{i}", [128, 8, 128], BF16) for i in range(2)]
        TTs = [sb(f"TT{i}", [128, 8, 128], BF16) for i in range(2)]
        RHSs = [sb(f"RHSb{i}", [128, 512], BF16) for i in range(2)]
        Ubs = [sb(f"Ub{i}", [128, 512], BF16) for i in range(2)]
        S32 = sb("S32", [128, 4, 64], F32)
        Sbf = sb("Sbf", [128, 4, 64], BF16)
        P.op("dve", lambda e: e.memset(S32[:], 0.0), [], ["S32"])
        y32 = sb("cy32", [128, 512], F32)
        sq = sb("csq", [128, 512], F32)
        ynb = sb("cynb", [128, 512], BF16)
        yaff = sb("cyaff", [128, 4, 128], F32)
        t1b = sb("ct1", [128, 4, 128], F32)
        stt_ = sb("cstat", [128, 64], F32)

        def gen_AB(blk):
            tok0 = blk * NB
            hk = f"hT.{tok0 // 512}"
            for i0 in range(0, 15, 2):
                b = BK.get()
                bank, bk = BK.f32(b), BK.key(b)
                idxs = [i for i in (i0, i0 + 1) if i < 15]
                for j, idx in enumerate(idxs):
                    c0 = idx * 128 if idx < 14 else 1696
                    for kc in range(KC):
                        kb.mm(bank[:, j * NB:(j + 1) * NB], Wk[:, kc, c0:c0 + 128], hT[:, kc, tok0:tok0 + NB],
                              kc == 0, kc == KC - 1, [hk, "Wk"] + (["hTu"] if kc >= 4 else []), [bk])
                yield
                for j, idx in enumerate(idxs):
                    kb.cp("act", pT[:, idx, 1:NB + 1], bank[:, j * NB:(j + 1) * NB], [bk], [f"pT.{idx}"])
                BK.put(b)
                yield
            allp = [f"pT.{i}" for i in range(15)]
            kb.cp("dve", carry[:], pT[:, :, NB], allp, ["carry"])
            yield
            for idx in range(15):
                tmp, ktm = sh_p[idx % 2], f"shT.{idx % 2}"
                kb.act(tmp[:], pT[:, idx, 0:NB], AF.Copy, [f"pT.{idx}", "cols"], [ktm], scale=cols[:, 40 + idx:41 + idx])
                yield
                if 8 <= idx < 12:
                    kb.stt(vbf_p[blk % 2][:, idx - 8, :], pT[:, idx, 1:NB + 1], dc[:, idx:idx + 1], tmp[:], ALU.mult, ALU.add,
                           [f"pT.{idx}", "dc", ktm, "carry"], [f"vbf{blk % 2}.{idx - 8}"])
                else:
                    kb.stt(pT[:, idx, 1:NB + 1], pT[:, idx, 1:NB + 1], dc[:, idx:idx + 1], tmp[:], ALU.mult, ALU.add,
                           [f"pT.{idx}", "dc", ktm, "carry"], [f"pT.{idx}"])
                yield
            kb.cp("dve", pT[:, :, 0], carry[:], ["carry"], allp)
            yield

        ps = lambda idx: pT[:, idx, 1:NB + 1]

        def gen_lora(blk):
            kb.act(twa[0:64, :], pT[0:64, 12, 1:NB + 1], AF.Tanh, ["pT.12"], ["twa"])
            kb.cp("dve", twa[64:128, :], pT[64:128, 12, 1:NB + 1], ["pT.12"], ["twa"])
            yield
            th, kth = sh_p[0], "shT.0"
            kb.act(th[:], ps(13), AF.Tanh, ["pT.13"], [kth], scale=0.5)
            th2, kth2 = sh_p[1], "shT.1"
            kb.act(th2[:], ps(14), AF.Tanh, ["pT.14"], [kth2], scale=0.5)
            yield
            kb.ts("dve", sg1[:], th[:], 0.5, 0.5, ALU.mult, ALU.add, [kth], ["sg1"])
            kb.ts("pool", sg2[:], th2[:], 0.5, 0.5, ALU.mult, ALU.add, [kth2], ["sg2"])
            yield

        def gen_ct(ch, blk, ct):
            Tb = T_p[ch]
            tk = lambda i: f"T{ch}.{i}"
            cs = slice(ct * 128, (ct + 1) * 128)
            r_, k_, v_ = ps(ct), ps(4 + ct), ps(8 + ct)
            kr, kk_, kv = f"pT.{ct}", f"pT.{4 + ct}", f"pT.{8 + ct}"
            bz, bg = BK.get(), BK.get()
            bankZ, kz = BK.f32(bz), BK.key(bz)
            bankG, kg = BK.f32(bg), BK.key(bg)
            kb.mm(bankZ[:, 0:NB], wupz[:, cs], twa[:], True, True, ["wupb", "twa"], [kz])
            kb.mm(bankZ[:, NB:2 * NB], aupz[:, cs], twa[:], True, True, ["wupb", "twa"], [kz])
            kb.mm(bankG[:, 0:NB], gup1b[:, cs], sg1[:], True, False, ["gupb", "sg1"], [kg])
            kb.mm(bankG[:, 0:NB], gup2z[:, cs], sg2[:], False, True, ["gupb", "sg2"], [kg])
            yield
            thw, tha = Tb[0], Tb[1]
            kb.act(thw[:], bankZ[:, 0:NB], AF.Tanh, [kz, "dc"], [tk(0)], scale=0.5, bias=dc[:, 15 + ct:16 + ct])
            kb.act(tha[:], bankZ[:, NB:2 * NB], AF.Tanh, [kz, "dc"], [tk(1)], scale=0.5, bias=dc[:, 19 + ct:20 + ct])
            BK.put(bz)
            kk = Tb[4]
            kb.ts("pool", kk[:], k_, cols[:, 63 + ct:64 + ct], 0.0, ALU.mult, ALU.add, [kk_, "cols"], [tk(4)])
            yield
            kb.cp("act", gT[:, ct, :], bankG[:, 0:NB], [kg], [f"gT.{ct}"])
            BK.put(bg)
            kk2, k_kk2 = kk2_p[ch], f"kk2.{ch}"
            kb.act(kk2[:], kk[:], AF.Square, [tk(4)], [k_kk2])
            ld = Tb[0]
            kb.ts("dve", ld[:], thw[:], -CL, -CL, ALU.mult, ALU.add, [tk(0)], [tk(0)])
            yield
            bn_ = BK.get()
            bankN, kn = BK.f32(bn_), BK.key(bn_)
            kb.mm(bankN[:, 0:NB], bonesb[:], kk2[:], True, True, ["bonesb", k_kk2], [kn])
            cum = Tb[2]
            for c in range(2):
                ld_c, cum_c = ld[:, c * 128:(c + 1) * 128], cum[:, c * 128:(c + 1) * 128]
                P.op("dve", lambda e, cum_c=cum_c, ld_c=ld_c: e.tensor_tensor_scan(out=cum_c, data0=onesf, data1=ld_c, initial=0.0, op0=ALU.mult, op1=ALU.add),
                     [tk(0), "cqf"], [tk(2)])
            an = Tb[1]
            kb.ts("pool", an[:], tha[:], -0.5, -0.5, ALU.mult, ALU.add, [tk(1)], [tk(1)])
            yield
            sn = Tb[5]
            kb.act(sn[:], bankN[:, 0:NB], AF.Sqrt, [kn], [tk(5)])
            BK.put(bn_)
            cumex = Tb[3]
            kb.tt("pool", cumex[:], cum[:], ld[:], ALU.subtract, [tk(2), tk(0)], [tk(3)])
            yield
            ep, en, ex = Tb[6], Tb[7], Tb[8]
            kb.act(ep[:], cum[:], AF.Exp, [tk(2)], [tk(6)])
            kb.act(en[:], cum[:], AF.Exp, [tk(2)], [tk(7)], scale=-1.0)
            kb.ts("dve", sn[:], sn[:], 1e-12, None, ALU.max, None, [tk(5)], [tk(5)])
            tmp2 = Tb[9]
            kb.ts("pool", tmp2[:], an[:], dc[:, 23 + ct:24 + ct], dc[:, 27 + ct:28 + ct], ALU.mult, ALU.add, [tk(1), "dc"], [tk(9)])
            yield
            kb.act(ex[:], cumex[:], AF.Exp, [tk(3)], [tk(8)])
            P.op("dve", lambda e: e.reciprocal(out=sn[:], in_=sn[:]), [tk(5)], [tk(5)])
            k2 = Tb[9]
            kb.tt("pool", k2[:], k_, tmp2[:], ALU.mult, [kk_, tk(9)], [tk(9)])
            yield
            kkn = Tb[4]
            kb.tt("dve", kkn[:], kk[:], sn[:], ALU.mult, [tk(4), tk(5)], [tk(4)])
            c3 = lambda a: a.rearrange("p (c n) -> p c n", c=2)
            kb.cp("pool", PCt[:, ct, :], c3(ep[:])[:, :, 127], [tk(6)], [f"PCt.{ct}"])
            yield
            bn = Tb[5]
            kb.tt("pool", bn[:], kkn[:], an[:], ALU.mult, [tk(4), tk(1), tk(5)], [tk(5)])
            kb.tt("dve", KT[:, ct, :], k2[:], en[:], ALU.mult, [tk(9), tk(7)], [f"KT.{ct}"])
            yield
            for hh in range(2):
                pp = slice(64 * hh, 64 * hh + 64)
                kb.tt("dve", ARt[pp, ct, :, hh, 1, :], c3(r_)[pp], c3(ep[:])[pp], ALU.mult, [kr, tk(6)], [f"ARt.{ct}"])
                kb.tt("dve" if hh == 0 else "pool", ARt[pp, ct, :, hh, 0, :], c3(kkn[:])[pp], c3(ex[:])[pp], ALU.mult, [tk(4), tk(8)], [f"ARt.{ct}"])
                yield
            kb.tt("dve", BnT[:, ct, :], bn[:], en[:], ALU.mult, [tk(5), tk(7)], [f"BnT.{ct}"])
            kb.tt("pool", rkb[:, ct, :], r_, k2[:], ALU.mult, [kr, tk(9)], [f"rkb.{ct}"])
            yield

        def gen_tok(blk):
            for c in range(2):
                cs = slice(c * 128, (c + 1) * 128)
                for src, dst, nm, sk in ((KT, Ktok, "KT", "KT"), (BnT, Btok, "BnT", "BnT"), (vbf_p[blk % 2], Vtok, "vbf", f"vbf{blk % 2}")):
                    b = BK.get()
                    bankT, kt = BK.bf(b), BK.key(b)
                    for ct in range(4):
                        kb.tr(bankT[:, ct * 128:(ct + 1) * 128], src[:, ct, cs], [f"{sk}.{ct}"], [kt])
                    yield
                    kb.cpalt(dst[:, c, :], bankT[:, 0:512], [kt], [f"{nm}tok.{c}"])
                    BK.put(b)
                    yield

        def gen_F1(blk, c):
            cs = slice(c * 128, (c + 1) * 128)
            MT = MTs[c]
            for h in range(8):
                ct = h // 2
                b = BK.get()
                bankM, km = BK.f32(b), BK.key(b)
                rhsAR = ARt[:, ct, c, h % 2, :, :].rearrange("p a b -> p (a b)")
                kb.mm(bankM[:, 0:256], BnT[:, ct, cs], rhsAR, True, True, [f"BnT.{ct}", f"ARt.{ct}"], [km])
                kb.mm(bankM[:, 256:512], KT[:, ct, cs], rhsAR, True, True, [f"KT.{ct}", f"ARt.{ct}"], [km])
                yield
                if h % 2 == 0:
                    kb.tt("dve", MT[:, h, :], bankM, mask4, ALU.mult, [km, "cqf"], [f"MT{c}.{h}"])
                    BK.put(b)
                else:
                    mr, kmr = mraw_p[(h // 2) % 2], f"mraw.{(h // 2) % 2}"
                    kb.cp("act", mr[:], bankM, [km], [kmr])
                    BK.put(b)
                    yield
                    kb.tt("pool", MT[:, h, :], mr[:], mask4b[:], ALU.mult, [kmr, "mask4b"], [f"MT{c}.{h}"])
                yield
            for g4 in range(2):
                b = BK.get()
                bankL, kl = BK.f32(b), BK.key(b)
                for hh in range(4):
                    h = 4 * g4 + hh
                    kb.mm(bankL[:, hh * 128:(hh + 1) * 128], ARt[:, h // 2, c, h % 2, 0, :], BnT[:, h // 2, cs], True, True,
                          [f"ARt.{h // 2}", f"BnT.{h // 2}"], [kl])
                yield
                kb.tt("dve", Lm[0][:, 4 * g4:4 * g4 + 4, :], bankL.rearrange("p (c n) -> p c n", c=4),
                      maskLs.unsqueeze(1).to_broadcast([128, 4, 128]), ALU.mult, [kl, "cqf"], [f"Lm0.{g4}"])
                BK.put(b)
                hs = slice(4 * g4, 4 * g4 + 4)
                mtk = [f"MT{c}.{h}" for h in range(4 * g4, 4 * g4 + 4)]
                kb.cp("act", Nm[0][:, hs, :], MT[:, hs, 0:128], mtk, [f"Nm0.{g4}"])
                kb.tt("pool", Xm[0][:, hs, :], MT[:, hs, 0:128], identf.unsqueeze(1).to_broadcast([128, 4, 128]), ALU.add,
                      mtk + ["cqf"], [f"Xm0.{g4}"])
                yield

        def gen_inv_group(c, g4):
            hs = slice(4 * g4, 4 * g4 + 4)
            v4 = lambda bank: bank.rearrange("p (c n) -> p c n", c=4)
            cur = 0
            for lvl in range(7):
                nxt = cur ^ 1
                kN, kL, kX = f"Nm{cur}.{g4}", f"Lm{cur}.{g4}", f"Xm{cur}.{g4}"
                last = lvl == 6
                Xdst, kXd = (TTs[c], f"TT{c}.{g4}") if last else (Xm[nxt], f"Xm{nxt}.{g4}")
                bx = ba = bb = None
                if lvl >= 1:
                    bx = BK.get()
                    bankX, kbx = BK.f32(bx), BK.key(bx)
                    for hh in range(4):
                        h = 4 * g4 + hh
                        kb.mm(bankX[:, hh * 128:(hh + 1) * 128], Lm[cur][:, h, :], Xm[cur][:, h, :], True, True, [kL, kX], [kbx])
                if not last:
                    ba, bb = BK.get(), BK.get()
                    bankA, kba = BK.f32(ba), BK.key(ba)
                    bankB, kbb = BK.f32(bb), BK.key(bb)
                    for hh in range(4):
                        h = 4 * g4 + hh
                        kb.mm(bankA[:, hh * 128:(hh + 1) * 128], Lm[cur][:, h, :], Nm[cur][:, h, :], True, True, [kL, kN], [kba])
                    for hh in range(4):
                        h = 4 * g4 + hh
                        kb.mm(bankB[:, hh * 128:(hh + 1) * 128], Nm[cur][:, h, :], Lm[cur][:, h, :], True, True, [kL, kN], [kbb])
                yield
                if lvl >= 1:
                    kb.tt("dve", Xdst[:, hs, :], v4(bankX), Xm[cur][:, hs, :], ALU.add, [kbx, kX], [kXd])
                    BK.put(bx)
                else:
                    kb.cp("pool", Xdst[:, hs, :], Xm[cur][:, hs, :], [kX], [kXd])
                if not last:
                    kb.cp("act", Nm[nxt][:, hs, :], v4(bankA), [kba], [f"Nm{nxt}.{g4}"])
                    BK.put(ba)
                    if g4 == 0:
                        kb.cp("act", Lm[nxt][:, hs, :], v4(bankB), [kbb], [f"Lm{nxt}.{g4}"])
                    else:
                        kb.cp("dve", Lm[nxt][:, hs, :], v4(bankB), [kbb], [f"Lm{nxt}.{g4}"])
                    BK.put(bb)
                yield
                cur = nxt

        def gen_inv(blk, c):
            gs = [gen_inv_group(c, 0), gen_inv_group(c, 1)]
            while gs:
                for g in list(gs):
                    try:
                        next(g)
                    except StopIteration:
                        gs.remove(g)
                yield

        def gen_seqpost(blk, c):
            n_ch = blk * 2 + c
            cs = slice(c * 128, (c + 1) * 128)
            tok = slice(blk * NB + c * 128, blk * NB + (c + 1) * 128)
            MT, TT, RHSb, Ub = MTs[c], TTs[c], RHSs[c], Ubs[c]
            kTT = [f"TT{c}.0", f"TT{c}.1"]
            kRH, kUb = f"RHSb{c}", f"Ub{c}"
            br = BK.get()
            bankR, kr_ = BK.f32(br), BK.key(br)
            for h in range(8):
                ct = h // 2
                o = bankR[:, 64 * h:64 * h + 64]
                if n_ch > 0:
                    kb.mm(o, ARt[:, ct, c, h % 2, 0, :], Sbf[:, ct, :], True, False, [f"ARt.{ct}", "Sbf"], [kr_])
                kb.mm(o, MT[:, h, 256:384], Vtok[:, c, 64 * h:64 * h + 64], n_ch == 0, True, [f"MT{c}.{h}", f"vbftok.{c}"], [kr_])
            yield
            kb.cp("act", RHSb[:], bankR, [kr_], [kRH])
            BK.put(br)
            yield
            bu = BK.get()
            bankU, ku = BK.f32(bu), BK.key(bu)
            for h in range(8):
                kb.mm(bankU[:, 64 * h:64 * h + 64], TT[:, h, :], RHSb[:, 64 * h:64 * h + 64], True, True, kTT + [kRH], [ku])
            yield
            kb.cp("dve", Ub[:], bankU, [ku], [kUb])
            BK.put(bu)
            yield
            if n_ch < NT - 1:
                bs_ = BK.get()
                bankS, ks_ = BK.f32(bs_), BK.key(bs_)
                for ct in range(4):
                    o = bankS[:, ct * 128:(ct + 1) * 128]
                    kb.mm(o, Btok[:, c, ct * 128:(ct + 1) * 128], Ub[:, ct * 128:(ct + 1) * 128], True, False, [f"BnTtok.{c}", kUb], [ks_])
                    kb.mm(o, Ktok[:, c, ct * 128:(ct + 1) * 128], Vtok[:, c, ct * 128:(ct + 1) * 128], False, True, [f"KTtok.{c}", f"vbftok.{c}"], [ks_])
            by = BK.get()
            bankY, ky = BK.f32(by), BK.key(by)
            for h in range(8):
                ct = h // 2
                o = bankY[:, 64 * h:64 * h + 64]
                if n_ch > 0:
                    kb.mm(o, ARt[:, ct, c, h % 2, 1, :], Sbf[:, ct, :], True, False, [f"ARt.{ct}", "Sbf"], [ky])
                kb.mm(o, MT[:, h, 128:256], Ub[:, 64 * h:64 * h + 64], n_ch == 0, False, [f"MT{c}.{h}", kUb], [ky])
                kb.mm(o, MT[:, h, 384:512], Vtok[:, c, 64 * h:64 * h + 64], False, True, [f"MT{c}.{h}", f"vbftok.{c}"], [ky])
            yield
            if n_ch < NT - 1:
                for hh in range(2):
                    b0 = 64 * hh
                    kb.tt("dve", S32[b0:b0 + 64, :, :], bankS[b0:b0 + 64, :].rearrange("p (c n) -> p c n", c=4)[:, :, b0:b0 + 64],
                          S32[b0:b0 + 64, :, :], ALU.add, [ks_, "S32"], ["S32"])
                BK.put(bs_)
                kb.tt("dve", S32[:], S32[:], PCt[:, :, c].unsqueeze(2).to_broadcast([128, 4, 64]), ALU.mult,
                      ["S32"] + [f"PCt.{ct}" for ct in range(4)], ["S32"])
                kb.cp("act", Sbf[:], S32[:], ["S32"], ["Sbf"])
                yield
            k1, k2, ks, k3 = "cy32", "csq", "cstat", "cynb"
            kb.cp("act", y32[:], bankY, [ky], [k1])
            BK.put(by)
            yield
            v3 = lambda a: a.rearrange("p (h d) -> p h d", d=64)
            bc = lambda a: a.unsqueeze(2).to_broadcast([128, 8, 64])
            kb.red(stt_[:, 0:8], v3(y32[:]), ALU.add, [k1], [ks])
            kb.act(sq[:], y32[:], AF.Square, [k1], [k2])
            yield
            kb.red(stt_[:, 8:16], v3(sq[:]), ALU.add, [k2], [ks])
            yield
            kb.ts("dve", stt_[:, 16:24], stt_[:, 0:8], 1.0 / 64, None, ALU.mult, None, [ks], [ks])
            yield
            kb.tt("dve", stt_[:, 24:32], stt_[:, 16:24], stt_[:, 16:24], ALU.mult, [ks], [ks])
            kb.ts("dve", stt_[:, 32:40], stt_[:, 8:16], 1.0 / 64, 64e-5, ALU.mult, ALU.add, [ks], [ks])
            yield
            kb.tt("dve", stt_[:, 40:48], stt_[:, 32:40], stt_[:, 24:32], ALU.subtract, [ks], [ks])
            yield
            kb.tt("pool", stt_[:, 48:56], stt_[:, 40:48], mhalf[:, 0:8], ALU.pow, [ks, "mhalf"], [ks])
            yield
            kb.stt(stt_[:, 56:64], stt_[:, 16:24], -1.0, stt_[:, 48:56], ALU.mult, ALU.mult, [ks], [ks])
            kb.tt("pool", v3(y32[:]), v3(y32[:]), bc(stt_[:, 48:56]), ALU.mult, [k1, ks], [k1])
            yield
            kb.tt("dve", v3(ynb[:]), v3(y32[:]), bc(stt_[:, 56:64]), ALU.add, [k1, ks], [k3])
            yield
            b2 = BK.get()
            bankT, kt = BK.bf(b2), BK.key(b2)
            for ct in range(4):
                kb.tr(bankT[:, ct * 128:(ct + 1) * 128], ynb[:, ct * 128:(ct + 1) * 128], [k3], [kt])
            bbo = BK.get()
            bankBo, kbo = BK.f32(bbo), BK.key(bbo)
            for ct in range(4):
                kb.mm(bankBo[:, ct * 128:(ct + 1) * 128], RKblk[:, ct, :], rkb[:, ct, cs], True, True, ["RKblk", f"rkb.{ct}"], [kbo])
            yield
            for ct in range(4):
                kb.act(yaff[:, ct, :], bankT[:, ct * 128:(ct + 1) * 128], AF.Identity, [kt, "cols"], ["cyaff"],
                       scale=cols[:, 75 + ct:76 + ct], bias=cols[:, 79 + ct:80 + ct])
            kb.tt("dve", t1b[:], bankBo.rearrange("p (c n) -> p c n", c=4), vbf_p[blk % 2][:, :, cs], ALU.mult,
                  [kbo] + [f"vbf{blk % 2}.{ct}" for ct in range(4)], ["ct1"])
            BK.put(b2)
            BK.put(bbo)
            yield
            kb.tt("pool", t1b[:], t1b[:], yaff[:], ALU.add, ["ct1", "cyaff"], ["ct1"])
            yield
            kb.tt("dve", hT[:, 0:4, tok], t1b[:], gT[:, :, cs], ALU.mult, ["ct1"] + [f"gT.{ct}" for ct in range(4)],
                  [f"yT.{n_ch}.1", f"hT.{n_ch // 4}"])
            yield

        def gen_CD(blk):
            yield from gen_lora(blk)
            for pair in range(2):
                gs = [gen_ct(0, blk, 2 * pair), gen_ct(1, blk, 2 * pair + 1)]
                while gs:
                    for g in list(gs):
                        try:
                            next(g)
                        except StopIteration:
                            gs.remove(g)
                    yield
            yield from gen_tok(blk)

        def run(g):
            for _ in g:
                pass

        run(gen_AB(0))
        run(gen_CD(0))
        for blk in range(NBLK):
            run(gen_F1(blk, 0))
            interleave([gen_inv(blk, 0), gen_F1(blk, 1)])
            interleave([gen_seqpost(blk, 0), gen_inv(blk, 1)])
            if blk + 1 < NBLK:
                interleave([gen_seqpost(blk, 1), gen_AB(blk + 1)])
                if blk + 1 == NBLK - 1 and "D" in L["stages"]:
                    wv = lambda w: w.rearrange("(kc p) n -> p kc n", p=128)
                    Wout = WA[:, 0:8192].rearrange("p (k n) -> p k n", k=KC)
                    WkvK = WA[:, 8192:16384].rearrange("p (k n) -> p k n", k=KC)
                    WkvV = hT[:, 4:8, :].rearrange("p a (b n) -> p (a b) n", n=1024)
                    kb.dma("pool", Wout, wv(L["w_out_d"]), [], ["WA.0", "Wk"])
                    kb.dma("pool", WkvK, wv(L["wkv_d"])[:, :, 0:1024], [], ["WA.1", "Wk"])
                    kb.dma("pool", WkvV, wv(L["wkv_d"])[:, :, 1024:2048], [], ["WkvV", "hTu"])
                run(gen_CD(blk + 1))
            else:
                run(gen_seqpost(blk, 1))
        P.barrier()
        P.flush()


def stage_D(nc, P, kb, L):
    cols, cqf, hT, yT, WA, mhalf, onesb, ss, BK = (L[k] for k in ("cols", "cqf", "hT", "yT", "WA", "mhalf", "onesb", "ss", "L_BK"))
    x_d, mem_d, gfin_d, out_d = L["x_d"], L["mem_d"], L["gfin_d"], L["out_d"]
    w_out_d, wq_d, wkv_d, wo_d, wupm_d, wdn_d = (L[k] for k in ("w_out_d", "wq_d", "wkv_d", "wo_d", "wupm_d", "wdn_d"))
    wview = lambda w: w.rearrange("(kc p) n -> p kc n", p=128)

    def run(g):
        for _ in g:
            pass

    with contextlib.ExitStack() as sd:
        sb = lambda n, s, d, st=sd: kb.sb(n, s, d, st=st)
        xres = sb("xres", [128, NT, D], F32)
        gfin = sb("gfin", [128, D], F32)
        kTm = sb("kTm", [128, KC, 256], BF16)
        vmem = sb("vmem", [128, 2, D], BF16)
        kmx = sb("kmx", [128, 4], F32)
        hb_p = [sb(f"dhb{i}", [128, D], BF16) for i in range(4)]
        sqj = sb("dsqj", [128, D], BF16)
        W0 = WA[:, 0:8192].rearrange("p (k n) -> p k n", k=KC)
        W1 = WA[:, 8192:16384].rearrange("p (k n) -> p k n", k=KC)
        Wout, WkvK = W0, W1
        WkvV = hT[:, 4:8, :].rearrange("p a (b n) -> p (a b) n", n=1024)
        xv = x_d.rearrange("(t p) d -> p t d", p=128)
        kb.dma("sp", xres[:, 0:4, :], xv[:, 0:4, :], [], [f"xres.{t}" for t in range(0, 4)])
        if "C" not in L["stages"]:
            kb.dma("pool", Wout, wview(w_out_d), [], ["WA.0"])
            kb.dma("pool", WkvK, wview(wkv_d)[:, :, 0:1024], [], ["WA.1"])
            kb.dma("pool", WkvV, wview(wkv_d)[:, :, 1024:2048], [], ["WkvV"])
        kb.dma("sp", gfin[:], gfin_d.partition_broadcast(128), [], ["gfin"])

        def gen_norm(tiles, src, gcol0, dst_fn, dkey, sscol0):
            for tl, t in enumerate(tiles):
                xa, xk = src(t)
                c = sscol0 + t
                xks = xk if isinstance(xk, list) else [xk]
                kb.act(sqj[:], xa, AF.Square, xks, ["sqj", f"ss{c}"], accum=ss[:, c:c + 1])
                yield
                kb.ts("dve", ss[:, c:c + 1], ss[:, c:c + 1], 1.0 / D, 1e-6, ALU.mult, ALU.add, [f"ss{c}"], [f"ss{c}"])
                yield
                kb.tt("pool", ss[:, c:c + 1], ss[:, c:c + 1], mhalf[:, 0:1], ALU.pow, [f"ss{c}", "mhalf"], [f"ss{c}"])
                yield
                kb.act(hb_p[tl][:], xa, AF.Copy, xks + [f"ss{c}"], [f"dhb.{tl}"], scale=ss[:, c:c + 1])
                yield
            n = len(tiles) * 128
            for kc in range(KC):
                b = BK.get()
                bankT, bk = BK.bf(b), BK.key(b)
                for tl in range(len(tiles)):
                    kb.tr(bankT[:, tl * 128:(tl + 1) * 128], hb_p[tl][:, kc * 128:(kc + 1) * 128], [f"dhb.{tl}"], [bk])
                yield
                o = dst_fn(kc)
                dk = dkey if isinstance(dkey, list) else [dkey]
                if kc % 2:
                    kb.act(o, bankT[:, 0:n], AF.Copy, [bk, "cols"], dk, scale=cols[:, gcol0 + kc:gcol0 + kc + 1])
                else:
                    kb.ts("dve", o, bankT[:, 0:n], cols[:, gcol0 + kc:gcol0 + kc + 1], None, ALU.mult, None, [bk, "cols"], dk)
                BK.put(b)
                yield

        oTb_t = sb("oTb", [128, KC * 512], BF16)
        aT_t = sb("aT_all", [128, 8 * 512], BF16)
        if True:
            memx = [oTb_t[:, 2048 * i:2048 * (i + 1)].bitcast(F32) for i in range(2)]
            mxk = [[f"oTb.{k}" for k in range(4 * i, 4 * i + 4)] for i in range(2)]
            memT = aT_t[:, 0:2048].rearrange("p (k n) -> p k n", k=KC)
            k2m = aT_t[:, 2048:4096].rearrange("p (k n) -> p k n", k=KC)
            kmT = [f"aT.{k}" for k in range(4)]
            kk2 = [f"aT.{k}" for k in range(4, 8)]
            for i in range(2):
                kb.dma("sp", memx[i], mem_d[i * 128:(i + 1) * 128, :], [], mxk[i])
            for i in range(1, 4):
                kb.dma("sp", xres[:, 4 * i:4 * i + 4, :], xv[:, 4 * i:4 * i + 4, :], [], [f"xres.{t}" for t in range(4 * i, 4 * i + 4)])

            dstate = {"v_done": False, "t3_done": False}

            def gen_memkv():
                yield from gen_norm([0, 1], lambda t: (memx[t], mxk[t]), 16, lambda kc: memT[:, kc, :], kmT, 16)
                for mt in range(2):
                    for hf in range(2):
                        b = BK.get()
                        bank, bk = BK.f32(b), BK.key(b)
                        for kc in range(KC):
                            kb.mm(bank, memT[:, kc, mt * 128:(mt + 1) * 128], WkvV[:, kc, hf * 512:(hf + 1) * 512],
                                  kc == 0, kc == KC - 1, ["WkvV"] + kmT, [bk])
                        yield
                        kb.cpalt(vmem[:, mt, hf * 512:(hf + 1) * 512], bank, [bk], ["vmem"])
                        BK.put(b)
                        yield
                dstate["v_done"] = True
                for ft in range(KC):
                    b = BK.get()
                    bank, bk = BK.f32(b), BK.key(b)
                    for kc in range(KC):
                        kb.mm(bank[:, 0:256], WkvK[:, kc, ft * 128:(ft + 1) * 128], memT[:, kc, :], kc == 0, kc == KC - 1, ["WA.1"] + kmT, [bk])
                    yield
                    kb.cpalt(kTm[:, ft, :], bank[:, 0:256], [bk], ["kTm"])
                    BK.put(b)
                    yield
                kb.act(k2m, kTm[:], AF.Square, ["kTm"], kk2)
                yield
                for h in range(4):
                    b = BK.get()
                    bank, bk = BK.f32(b), BK.key(b)
                    kb.mm(bank[:, 0:256], onesb[:], k2m[:, 2 * h, :], True, False, ["onesb"] + kk2, [bk])
                    kb.mm(bank[:, 0:256], onesb[:], k2m[:, 2 * h + 1, :], False, True, ["onesb"] + kk2, [bk])
                    yield
                    kb.red(kmx[:, h:h + 1], bank[:, 0:256], ALU.max, [bk], ["kmx"])
                    BK.put(b)
                    yield

            def gen_wout():
                for t in range(NT):
                    tok = slice(t * 128, (t + 1) * 128)
                    for hf in range(2):
                        b = BK.get()
                        bank, bk = BK.f32(b), BK.key(b)
                        for kc in range(KC):
                            ysrc = yT[:, kc, tok] if kc < 4 else hT[:, kc - 4, tok]
                            kb.mm(bank, ysrc, Wout[:, kc, hf * 512:(hf + 1) * 512], kc == 0, kc == KC - 1,
                                  [f"yT.{t}.0", f"yT.{t}.1", "WA.0", f"hT.{t // 4}"], [bk])
                        yield
                        xs = xres[:, t, hf * 512:(hf + 1) * 512]
                        kb.tt("dve", xs, bank, xs, ALU.add, [bk, f"xres.{t}"], [f"xres.{t}"])
                        BK.put(b)
                        yield
                    if t == 3:
                        dstate["t3_done"] = True

            srcx = lambda t: (xres[:, t, :], f"xres.{t}")

            def gen_norm0():
                while not (dstate["v_done"] and dstate["t3_done"]):
                    yield
                yield from gen_norm([0, 1, 2, 3], srcx, 8, lambda kc: hT[:, kc, 0:512], ["hT.0", "WkvV"], 0)

            interleave([gen_memkv(), gen_wout(), gen_norm0()])
            kb.dma("pool", W1, wview(wq_d), [], ["WA.1"])
            kb.dma("pool", W0, wview(wo_d), [], ["WA.0"])

        srcx = lambda t: (xres[:, t, :], f"xres.{t}")
        norm_x = lambda blk: gen_norm(list(range(4 * blk, 4 * blk + 4)), srcx, 8, lambda kc: hT[:, kc, blk * 512:(blk + 1) * 512], [f"hT.{blk}", "WkvV"], 0)
        norm_m = lambda blk: gen_norm(list(range(4 * blk, 4 * blk + 4)), srcx, 24, lambda kc: hT[:, kc, blk * 512:(blk + 1) * 512], f"hT.{blk}", 16)
        with contextlib.ExitStack() as s2:
            qTb = yT[:, 0:2, :].rearrange("p a (b n) -> p (a b) n", n=512)
            PT = yT[:, 2:4, :].rearrange("p a (b n) -> p (a b) n", n=512)
            oTb = oTb_t[:].rearrange("p (k n) -> p k n", k=KC)
            yall = [f"yT.{t}.0" for t in range(NT)]
            q2b_p = [sb(f"q2b{i}", [128, 2, 512], BF16, st=s2) for i in range(4)]
            nb_p = [sb(f"nb{i}", [128, 2], F32, st=s2) for i in range(4)]
            rc_p = [sb(f"rc{i}", [128, 512], F32, st=s2) for i in range(4)]

            def gen_head(sl, h):
                qk = [f"qTb.{2 * h}", f"qTb.{2 * h + 1}"]
                q2b, kq2 = q2b_p[sl], f"q2b.{sl}"
                kb.act(q2b[:], qTb[:, 2 * h:2 * h + 2, :], AF.Square, qk, [kq2])
                yield
                b = BK.get()
                bankQ, kq = BK.f32(b), BK.key(b)
                kb.mm(bankQ, onesb[:], q2b[:, 0, :], True, False, ["onesb", kq2], [kq])
                kb.mm(bankQ, onesb[:], q2b[:, 1, :], False, True, ["onesb", kq2], [kq])
                yield
                nb, kbs = nb_p[sl], f"nb.{sl}"
                kb.red(nb[:, 0:1], bankQ, ALU.max, [kq], [kbs])
                BK.put(b)
                yield
                kb.ts("dve", nb[:, 1:2], nb[:, 0:1], kmx[:, h:h + 1], -1.0 / 32, ALU.add, ALU.mult, [kbs, "kmx"], [kbs])
                yield
                for mt in range(2):
                    b = BK.get()
                    bankS, ksb = BK.f32(b), BK.key(b)
                    ms = slice(mt * 128, (mt + 1) * 128)
                    kb.mm(bankS, kTm[:, 2 * h, ms], qTb[:, 2 * h, :], True, False, ["kTm", qk[0]], [ksb])
                    kb.mm(bankS, kTm[:, 2 * h + 1, ms], qTb[:, 2 * h + 1, :], False, True, ["kTm", qk[1]], [ksb])
                    yield
                    kb.act(PT[:, 2 * h + mt, :], bankS, AF.Exp, [ksb, kbs], [f"PT.{2 * h + mt}"] + (yall if blk == 0 else []), scale=1.0 / 16, bias=nb[:, 1:2])
                    BK.put(b)
                    yield
                pk = [f"PT.{2 * h}", f"PT.{2 * h + 1}"]
                b = BK.get()
                bankRs, krs = BK.f32(b), BK.key(b)
                kb.mm(bankRs, onesb[:], PT[:, 2 * h, :], True, False, ["onesb", pk[0]], [krs])
                kb.mm(bankRs, onesb[:], PT[:, 2 * h + 1, :], False, True, ["onesb", pk[1]], [krs])
                yield
                rc, krc = rc_p[sl], f"rc.{sl}"
                P.op("dve", lambda e: e.reciprocal(out=rc[:], in_=bankRs), [krs], [krc])
                BK.put(b)
                yield
                for dt_ in range(2):
                    ft = 2 * h + dt_
                    b = BK.get()
                    bankO, kob = BK.f32(b), BK.key(b)
                    kb.mm(bankO, vmem[:, 0, ft * 128:(ft + 1) * 128], PT[:, 2 * h, :], True, False, ["vmem", pk[0]], [kob])
                    kb.mm(bankO, vmem[:, 1, ft * 128:(ft + 1) * 128], PT[:, 2 * h + 1, :], False, True, ["vmem", pk[1]], [kob])
                    yield
                    kb.tt("dve", oTb[:, ft, :], bankO, rc[:], ALU.mult, [kob, krc], [f"oTb.{ft}"])
                    BK.put(b)
                    yield

            def gen_attn(blk):
                bs = slice(blk * 512, (blk + 1) * 512)
                for ft in range(KC):
                    b = BK.get()
                    bank, bk = BK.f32(b), BK.key(b)
                    for kc in range(KC):
                        kb.mm(bank, W1[:, kc, ft * 128:(ft + 1) * 128], hT[:, kc, bs], kc == 0, kc == KC - 1, ["WA.1", f"hT.{blk}"], [bk])
                    yield
                    kb.cpalt(qTb[:, ft, :], bank, [bk], [f"qTb.{ft}"] + (yall if blk == 0 else []))
                    BK.put(b)
                    yield
                if blk == 3:
                    mlp_load(0)
                for pair in range(1):
                    gs = [gen_head(h, h) for h in range(4)]
                    while gs:
                        for g in list(gs):
                            try:
                                next(g)
                            except StopIteration:
                                gs.remove(g)
                        yield
                for tl in range(4):
                    t = blk * 4 + tl
                    for hf in range(2):
                        b = BK.get()
                        bank, bk = BK.f32(b), BK.key(b)
                        for kc in range(KC):
                            kb.mm(bank, oTb[:, kc, tl * 128:(tl + 1) * 128], W0[:, kc, hf * 512:(hf + 1) * 512], kc == 0, kc == KC - 1,
                                  [f"oTb.{kc}", "WA.0"], [bk])
                        yield
                        xs = xres[:, t, hf * 512:(hf + 1) * 512]
                        kb.tt("dve", xs, bank, xs, ALU.add, [bk, f"xres.{t}"], [f"xres.{t}"])
                        BK.put(b)
                        yield

            def norm_worker(blk):
                if blk + 1 < 4:
                    yield from norm_x(blk + 1)
                if blk - 1 >= 0:
                    yield from norm_m(blk - 1)

            s3 = s2
            aT_all = aT_t[:].rearrange("p (k n) -> p k n", k=8)
            rl_p = [sb(f"rl{i}", [128, 512], BF16, st=s3) for i in range(2)]
            o_p = [oTb_t[:, 2048 * i:2048 * (i + 1)].bitcast(F32) for i in range(2)]
            upv = wupm_d.rearrange("(kc p) n -> p kc n", p=128)
            dnv = wdn_d.rearrange("(f p) n -> p f n", p=128)
            ov = out_d.rearrange("(t p) d -> p t d", p=128)

            def mlp_w(j):
                s = (j + 1) % 2
                Wu = WA[:, s * 8192:s * 8192 + 4096].rearrange("p (k n) -> p k n", k=KC)
                Wd = WA[:, s * 8192 + 4096:(s + 1) * 8192].rearrange("p (f n) -> p f n", f=4)
                return s, Wu, Wd

            def mlp_load(j):
                s, Wu, Wd = mlp_w(j)
                kb.dma("pool", Wu, upv[:, :, j * 512:(j + 1) * 512], [], [f"WA.{s}"])
                kb.dma("pool", Wd, dnv[:, 4 * j:4 * j + 4, :], [], [f"WA.{s}"])

            for blk in range(4):
                interleave([gen_attn(blk), norm_worker(blk)])
            run(norm_m(3))

            NJ = 8
            ai = 0
            ri = 0
            for j in range(NJ):
                s, Wu, Wd = mlp_w(j)
                if j > 0:
                    mlp_load(j)
                for blk in range(4):
                    bs = slice(blk * 512, (blk + 1) * 512)
                    ab = (ai % 2) * 4
                    ai += 1
                    for fft in range(4):
                        b = BK.get()
                        bank, bk = BK.f32(b), BK.key(b)
                        for kc in range(KC):
                            kb.mm(bank, Wu[:, kc, fft * 128:(fft + 1) * 128], hT[:, kc, bs], kc == 0, kc == KC - 1, [f"WA.{s}", f"hT.{blk}"], [bk])
                        rl, krl = rl_p[ri % 2], f"rl.{ri % 2}"
                        ri += 1
                        kb.act(rl[:], bank, AF.Relu, [bk], [krl])
                        BK.put(b)
                        kb.tt("dve", aT_all[:, ab + fft, :], rl[:], rl[:], ALU.mult, [krl], [f"aT.{ab + fft}"])
                    for tl in range(4):
                        t = blk * 4 + tl
                        for hf in range(2):
                            b = BK.get()
                            bank, bk = BK.f32(b), BK.key(b)
                            for fft in range(4):
                                kb.mm(bank, aT_all[:, ab + fft, tl * 128:(tl + 1) * 128], Wd[:, fft, hf * 512:(hf + 1) * 512], fft == 0, fft == 3,
                                      [f"aT.{ab + fft}", f"WA.{s}"], [bk])
                            xs = xres[:, t, hf * 512:(hf + 1) * 512]
                            kb.tt("dve", xs, bank, xs, ALU.add, [bk, f"xres.{t}"], [f"xres.{t}"])
                            BK.put(b)
                    if j == NJ - 1:
                        for fb in ([blk - 1] if blk > 0 else []) + ([3] if blk == 3 else []):
                            for t in range(4 * fb, 4 * fb + 4):
                                c = 32 + t
                                kb.act(sqj[:], xres[:, t, :], AF.Square, [f"xres.{t}"], ["sqj", f"ss{c}"], accum=ss[:, c:c + 1])
                                kb.ts("dve", ss[:, c:c + 1], ss[:, c:c + 1], 1.0 / D, 1e-6, ALU.mult, ALU.add, [f"ss{c}"], [f"ss{c}"])
                                kb.tt("pool", ss[:, c:c + 1], ss[:, c:c + 1], mhalf[:, 0:1], ALU.pow, [f"ss{c}", "mhalf"], [f"ss{c}"])
                                ot, ko = o_p[t % 2], f"ot.{t % 2}"
                                kb.stt(ot, xres[:, t, :], ss[:, c:c + 1], gfin[:], ALU.mult, ALU.mult, [f"xres.{t}", f"ss{c}", "gfin"],
                                       [ko] + [f"oTb.{k}" for k in range(KC)])
                                kb.dma("sp", ov[:, t, :], ot, [ko], [f"out.{t}"])
            P.barrier()
            P.flush()


def _consts():
    cq = np.zeros((128, NCQ), np.float32)
    i = np.arange(128)
    cq[:, 0:128] = np.eye(128, dtype=np.float32)
    strict = (i[:, None] < i[None, :]).astype(np.float32)
    incl = (i[:, None] <= i[None, :]).astype(np.float32)
    cq[:, 128:256] = strict
    cq[:, 256:384] = incl
    cq[:, 384:512] = strict
    cq[:, 512:640] = incl
    cq[:, 640:768] = (i[:, None] > i[None, :]).astype(np.float32)
    cq[:, 768:896] = ((i[:, None] // 64) == (i[None, :] // 64)).astype(np.float32)
    cq[:, 896:1024] = 1.0
    gam = 1.0 - 2.0 ** (-5.0 - np.arange(8, dtype=np.float64))
    xi = gam[None, :] ** (i[:, None] + 1.0)
    kz = 0.125 * gam[None, :] ** (-(i[:, None] + 1.0))
    gC = np.zeros((128, 4))
    for ct in range(4):
        for p in range(128):
            gC[p, ct] = gam[2 * ct + p // 64] ** 128.0
    invf = (10000.0 ** (-np.arange(32, dtype=np.float32) / 32.0)).astype(np.float32)
    return cq, xi.astype(np.float32), kz.astype(np.float32), gC.astype(np.float32), np.broadcast_to(invf[None], (128, 32))


def _col(v):
    v = np.asarray(v, np.float32).reshape(-1)
    return v.reshape(-1, 128).T


def make_inputs(inp):
    cq, xi, kz, gC, invf = _consts()
    cols = np.zeros((128, NCOLS), np.float32)
    cols[:, 0:8] = _col(inp["norm_mix"][0])
    cols[:, 8:16] = _col(inp["norm_xattn"][0])
    cols[:, 16:24] = _col(inp["norm_mem"][0])
    cols[:, 24:32] = _col(inp["norm_mlp"][0])
    cols[:, 32:36] = _col(inp["ret_gn_w"][0])
    cols[:, 36:40] = _col(inp["ret_gn_b"][0])
    mu = np.asarray(inp["rwkv_mu"][0], np.float32)
    cols[:, 40:54] = _col(mu[:1792])
    cols[:, 54] = mu[1696:1824]
    cols[:, 55:59] = _col(inp["rwkv_w0"][0])
    cols[:, 59:63] = _col(inp["rwkv_a0"][0])
    cols[:, 63:67] = _col(inp["rwkv_k_k"][0])
    cols[:, 67:71] = _col(inp["rwkv_k_a"][0])
    cols[:, 71:75] = _col(inp["rwkv_r_k"][0])
    cols[:, 75:79] = _col(inp["rwkv_gn_w"][0])
    cols[:, 79:83] = _col(inp["rwkv_gn_b"][0])
    cols[:, 83:91] = xi
    cols[:, 91:99] = kz
    cols[:, 99:103] = gC
    cols[:, 103:135] = invf
    shared = {
        "cols": cols, "cq": cq,
        "gfin": np.ascontiguousarray(inp["norm_final"], np.float32),
        "w_in": np.ascontiguousarray(inp["w_in"][0]),
        "w_up": np.ascontiguousarray(inp["rwkv_w_up"][0]),
        "a_up": np.ascontiguousarray(inp["rwkv_a_up"][0]),
        "g_up": np.ascontiguousarray(inp["rwkv_g_up"][0]),
        "w_out": np.ascontiguousarray(inp["w_out"][0]),
        "wq": np.ascontiguousarray(inp["xattn_w_q"][0]),
        "wkv": np.ascontiguousarray(inp["xattn_w_kv"][0]),
        "wo": np.ascontiguousarray(inp["xattn_w_o"][0]),
        "mlp_up": np.ascontiguousarray(inp["mlp_w_up"][0]),
        "mlp_down": np.ascontiguousarray(inp["mlp_w_down"][0]),
    }
    maps = []
    for b in range(8):
        m = dict(shared)
        m["x"] = np.ascontiguousarray(inp["x"][b], np.float32)
        m["mem"] = np.ascontiguousarray(inp["mem"][b], np.float32)
        m["pos"] = np.ascontiguousarray(np.asarray(inp["positions"][b], np.int32).reshape(NT, 128).T)
        maps.append(m)
    return maps


_NC_CACHE = {}


def kernel(**inputs):
    inp = {k: np.asarray(v) for k, v in inputs.items()}
    maps = make_inputs(inp)
    if "nc" not in _NC_CACHE:
        _NC_CACHE["nc"] = build()
    res = run_bass_kernel_spmd(_NC_CACHE["nc"], maps, core_ids=list(range(8)))
    return np.stack([np.asarray(r["out"], np.float32) for r in res.results], axis=0)
```

```python
import contextlib
import math
import numpy as np
import concourse.bass as bass
import concourse.mybir as mybir
from concourse.bass_utils import run_bass_kernel_spmd

F32 = mybir.dt.float32
BF16 = mybir.dt.bfloat16
I32 = mybir.dt.int32
AF = mybir.ActivationFunctionType
ALU = mybir.AluOpType
AX = mybir.AxisListType

T = 2048
D = 1024
NT = 16
KC = 8
NCOLS = 135
NCQ = 1024
CUT = 99
COMPUTE = ("pe", "act", "dve", "pool")
ISSUERS = ("pe", "act", "dve", "pool", "sp")


class Prog:
    def __init__(self, nc, st, n_dma=12, self_sync=True):
        self.nc = nc
        self.n_dma = n_dma
        self.self_sync = self_sync
        self.chans = list(COMPUTE) + [f"d{i}" for i in range(n_dma)]
        self.sems = {c: st.enter_context(nc.semaphore(f"s_{c}")) for c in self.chans}
        self.streams = {e: [] for e in ISSUERS}
        self.count = {c: 0 for c in self.chans}
        self.clock = {e: {} for e in ISSUERS}
        self.snap = {}
        self.wr = {}
        self.rd = {}
        self.rr = 0
        self.nops = 0
        self.nwaits = 0

    @staticmethod
    def _need(needs, d):
        for c, n in d.items():
            if needs.get(c, 0) < n:
                needs[c] = n

    def op(self, eng, fn, reads=(), writes=(), dma=False):
        needs = {}
        for k in reads:
            self._need(needs, self.wr.get(k, {}))
        for k in writes:
            self._need(needs, self.wr.get(k, {}))
            self._need(needs, self.rd.get(k, {}))
        if dma:
            chan = f"d{self.rr % self.n_dma}"
            self.rr += 1
            if self.count[chan]:
                self._need(needs, {chan: self.count[chan]})
        else:
            chan = eng
        clk = self.clock[eng]
        waits = []
        for c, n in needs.items():
            if c == eng and (eng == "pe" or not self.self_sync):
                continue
            if clk.get(c, 0) < n:
                waits.append((c, n))
                for c2, n2 in self.snap[(c, n)].items():
                    if clk.get(c2, 0) < n2:
                        clk[c2] = n2
        self.nwaits += len(waits)
        self.nops += 1
        n_new = self.count[chan] + 1
        self.count[chan] = n_new
        s = dict(clk)
        s[chan] = n_new
        self.snap[(chan, n_new)] = s
        if eng == "pe" and chan == "pe":
            clk["pe"] = n_new
        wset = set(writes)
        for k in wset:
            self.wr[k] = {chan: n_new}
            self.rd[k] = {}
        for k in reads:
            if k in wset:
                continue
            self.rd.setdefault(k, {})[chan] = n_new
        self.streams[eng].append((waits, fn, chan))

    def barrier(self):
        allk = {c: n for c, n in self.count.items() if n}
        for e in ISSUERS:
            clk = self.clock[e]
            waits = []
            for c, n in allk.items():
                if c == e and e == "pe":
                    continue
                if clk.get(c, 0) < n:
                    waits.append((c, n))
            if waits:
                self.streams[e].append((waits, None, None))
        for e in ISSUERS:
            for c, n in allk.items():
                if self.clock[e].get(c, 0) < n:
                    self.clock[e][c] = n

    def flush(self):
        nc = self.nc
        streams = self.streams
        if not any(streams[e] for e in ISSUERS):
            return
        self.streams = {e: [] for e in ISSUERS}
        sems = self.sems

        def val(c, n):
            return n * 16 if c.startswith("d") else n

        def run(engname):
            def body(e):
                for waits, fn, chan in streams[engname]:
                    for c, n in waits:
                        e.wait_ge(sems[c], val(c, n))
                    if fn is not None:
                        fn(e).then_inc(sems[chan], 16 if chan.startswith("d") else 1)
            return body

        with nc.Block() as block:
            block.tensor(run("pe"))
            block.scalar(run("act"))
            block.vector(run("dve"))
            block.gpsimd(run("pool"))
            block.sync(run("sp"))


class Ring:
    def __init__(self, kb, name, n, shape, dtype, st=None, psum=False):
        st = st or kb.st
        alloc = kb.nc.psum_tensor if psum else kb.nc.sbuf_tensor
        self.name = name
        self.t = [st.enter_context(alloc(f"rg_{name}{i}", shape, dtype)) for i in range(n)]
        self.i = 0

    def next(self):
        i = self.i % len(self.t)
        self.i += 1
        return self.t[i], f"{self.name}.{i}"


class ViewRing:
    def __init__(self, aps, keys):
        self.t, self.k, self.i = aps, keys, 0

    def next(self):
        i = self.i % len(self.t)
        self.i += 1
        return self.t[i], self.k[i]


class Banks:
    def __init__(self, pp):
        import collections
        self.pp = pp
        self.free = collections.deque(range(8))

    def get(self):
        return self.free.popleft()

    def put(self, i):
        self.free.append(i)

    def f32(self, i):
        return self.pp[:, i, :]

    def bf(self, i):
        return self.pp[:, i, :].bitcast(BF16)

    @staticmethod
    def key(i):
        return f"ps{i}"


def interleave(gens):
    gens = list(gens)
    while gens:
        for g in list(gens):
            try:
                next(g)
            except StopIteration:
                gens.remove(g)


class KB:
    def __init__(self, nc, P, st):
        self.nc, self.P, self.st = nc, P, st
        self.flip = 0

    def sb(self, name, shape, dtype, st=None):
        return (st or self.st).enter_context(self.nc.sbuf_tensor("sb_" + name, shape, dtype))

    def mm(self, out, lhsT, rhs, start, stop, r, w):
        self.P.op("pe", lambda e: e.matmul(out, lhsT=lhsT, rhs=rhs, start=start, stop=stop), r, w)

    def tr(self, out, in_, r, w):
        idb = self.identb[:]
        self.P.op("pe", lambda e: e.transpose(out=out, in_=in_, identity=idb), list(r) + ["identb"], w)

    def act(self, out, in_, func, r, w, scale=1.0, bias=0.0, accum=None):
        kw = {}
        if accum is not None:
            kw["accum_out"] = accum
        self.P.op("act", lambda e: e.activation(out=out, in_=in_, func=func, bias=bias, scale=scale, **kw), r, w)

    def tt(self, eng, out, in0, in1, op, r, w):
        self.P.op(eng, lambda e: e.tensor_tensor(out=out, in0=in0, in1=in1, op=op), r, w)

    def ts(self, eng, out, in0, s1, s2, op0, op1, r, w):
        if s2 is None:
            self.P.op(eng, lambda e: e.tensor_scalar(out=out, in0=in0, scalar1=s1, scalar2=None, op0=op0), r, w)
        else:
            self.P.op(eng, lambda e: e.tensor_scalar(out=out, in0=in0, scalar1=s1, scalar2=s2, op0=op0, op1=op1), r, w)

    def stt(self, out, in0, sc, in1, op0, op1, r, w):
        self.P.op("dve", lambda e: e.scalar_tensor_tensor(out=out, in0=in0, scalar=sc, in1=in1, op0=op0, op1=op1), r, w)

    def cp(self, eng, out, in_, r, w):
        if eng == "act":
            self.P.op("act", lambda e: e.activation(out=out, in_=in_, func=AF.Copy), r, w)
        else:
            self.P.op(eng, lambda e: e.tensor_copy(out=out, in_=in_), r, w)

    def cpalt(self, out, in_, r, w):
        self.flip ^= 1
        self.cp("act" if self.flip else "dve", out, in_, r, w)

    def red(self, out, in_, op, r, w):
        self.P.op("dve", lambda e: e.tensor_reduce(out=out, in_=in_, axis=AX.X, op=op), r, w)

    def dma(self, eng, out, in_, r, w):
        self.P.op(eng, lambda e: e.dma_start(out=out, in_=in_), r, w, dma=True)


def build(stages="ABCD", dbg=False):
    nc = bass.Bass("TRN2", target_bir_lowering=False)

    def dram(n, s, d, kind="ExternalInput"):
        return nc.dram_tensor(n, s, d, kind=kind).ap()

    x_d = dram("x", [T, D], F32)
    mem_d = dram("mem", [256, D], F32)
    pos_d = dram("pos", [128, NT], I32)
    cols_d = dram("cols", [128, NCOLS], F32)
    cq_d = dram("cq", [128, NCQ], F32)
    gfin_d = dram("gfin", [D], F32)
    w_in_d = dram("w_in", [D, 3872], F32)
    wup_d = dram("w_up", [64, 512], F32)
    aup_d = dram("a_up", [64, 512], F32)
    gup_d = dram("g_up", [160, 512], F32)
    w_out_d = dram("w_out", [D, D], F32)
    wq_d = dram("wq", [D, D], F32)
    wkv_d = dram("wkv", [D, 2 * D], F32)
    wo_d = dram("wo", [D, D], F32)
    wupm_d = dram("mlp_up", [D, 4 * D], F32)
    wdn_d = dram("mlp_down", [4 * D, D], F32)
    out_d = dram("out", [T, D], F32, kind="ExternalOutput")
    if dbg:
        dbg_yT = dram("dbg_yT", [128, KC, T], BF16, kind="ExternalOutput")
        dbg_hT = dram("dbg_hT", [128, KC, T], BF16, kind="ExternalOutput")

    def wview(w):
        return w.rearrange("(kc p) n -> p kc n", p=128)

    with contextlib.ExitStack() as st:
        P = Prog(nc, st)
        kb = KB(nc, P, st)
        cols = kb.sb("cols", [128, NCOLS], F32)
        dc = kb.sb("dcols", [128, 40], F32)
        cqf = kb.sb("cqf", [128, NCQ], F32)
        identb = kb.sb("identb", [128, 128], BF16)
        bonesb = kb.sb("bonesb", [128, 128], BF16)
        onesb = kb.sb("onesb", [128, 128], BF16)
        mhalf = kb.sb("mhalf", [128, 256], F32)
        hT = kb.sb("hT", [128, KC, T], BF16)
        yT = kb.sb("yT", [128, 4, T], BF16)
        WA = kb.sb("WA", [128, 16384], BF16)
        ss = kb.sb("ss", [128, 3 * NT], F32)
        kb.identb = identb
        pp = st.enter_context(nc.psum_tensor("pp", [128, 8, 512], F32))
        L_BK = Banks(pp)
        psA = ViewRing([pp[:, i, :] for i in range(6)], [f"ps{i}" for i in range(6)])
        psT = ViewRing([pp[:, i, :].bitcast(BF16) for i in (6, 7)], ["ps6", "ps7"])
        mask4 = cqf[:, 128:640]
        maskI = cqf[:, 256:384]
        maskLs = cqf[:, 640:768]
        identf = cqf[:, 0:128]

        kb.dma("sp", cols[:], cols_d, [], ["cols"])
        kb.dma("sp", cqf[:], cq_d, [], ["cqf"])
        kb.dma("pool", identb[:], cq_d[:, 0:128], [], ["identb"])
        kb.dma("pool", bonesb[:], cq_d[:, 768:896], [], ["bonesb"])
        kb.dma("pool", onesb[:], cq_d[:, 896:1024], [], ["onesb"])
        P.op("dve", lambda e: e.memset(mhalf[:], -0.5), [], ["mhalf"])
        kb.ts("dve", dc[:, 0:15], cols[:, 40:55], -1.0, 1.0, ALU.mult, ALU.add, ["cols"], ["dc"])
        kb.ts("dve", dc[:, 15:19], cols[:, 55:59], 0.5, None, ALU.mult, None, ["cols"], ["dc"])
        kb.ts("dve", dc[:, 19:23], cols[:, 59:63], 0.5, None, ALU.mult, None, ["cols"], ["dc"])
        kb.ts("dve", dc[:, 23:27], cols[:, 67:71], -1.0, None, ALU.mult, None, ["cols"], ["dc"])
        kb.ts("dve", dc[:, 27:31], cols[:, 67:71], -1.0, 1.0, ALU.mult, ALU.add, ["cols"], ["dc"])

        def norm_T(src, nblk, tpb, gcol0, dst, dstkey, sscol0, hb, sqj):
            for blk in range(nblk):
                hbs = []
                for tl in range(tpb):
                    t = blk * tpb + tl
                    xa, xk = src(t)
                    c = sscol0 + t
                    kb.act(sqj[:], xa, AF.Square, [xk], ["sqj", f"ss{c}"], accum=ss[:, c:c + 1])
                    kb.ts("dve", ss[:, c:c + 1], ss[:, c:c + 1], 1.0 / D, 1e-6, ALU.mult, ALU.add, [f"ss{c}"], [f"ss{c}"])
                    kb.tt("pool", ss[:, c:c + 1], ss[:, c:c + 1], mhalf[:, 0:1], ALU.pow, [f"ss{c}", "mhalf"], [f"ss{c}"])
                    h, hk = hb.next()
                    kb.act(h[:], xa, AF.Copy, [xk, f"ss{c}"], [hk], scale=ss[:, c:c + 1])
                    hbs.append((h, hk))
                for kc in range(KC):
                    bank, bk = psT.next()
                    for tl, (h, hk) in enumerate(hbs):
                        kb.tr(bank[:, tl * 128:(tl + 1) * 128], h[:, kc * 128:(kc + 1) * 128], [hk], [bk])
                    n = tpb * 128
                    o = dst[:, kc, blk * n:(blk + 1) * n]
                    kb.flip ^= 1
                    if kb.flip:
                        kb.act(o, bank[:, 0:n], AF.Copy, [bk, "cols"], [dstkey(blk)], scale=cols[:, gcol0 + kc:gcol0 + kc + 1])
                    else:
                        kb.ts("dve", o, bank[:, 0:n], cols[:, gcol0 + kc:gcol0 + kc + 1], None, ALU.mult, None, [bk, "cols"], [dstkey(blk)])

        def load_w(dst, src, key, eng="pool"):
            kb.dma(eng, dst, src, [], [key])

        Wv8 = lambda ncol: WA[:, 0:KC * ncol].rearrange("p (k n) -> p k n", k=KC)

        Wr = Wv8(2048)
        BK = L_BK

        def acquire(n=1):
            spins = 0
            while len(BK.free) < n:
                spins += 1
                assert spins < 100000, "PSUM bank pool deadlock"
                yield
            return [BK.get() for _ in range(n)]

        def rr(gens):
            gens = list(gens)
            while gens:
                for g in list(gens):
                    try:
                        next(g)
                    except StopIteration:
                        gens.remove(g)
                yield

        def run(g):
            for _ in g:
                pass

        if "B" not in stages:
            with contextlib.ExitStack() as sa:
                xring = Ring(kb, "xr", 3, [128, D], F32, st=sa)
                hb = Ring(kb, "hb", 8, [128, D], BF16, st=sa)
                sqj = kb.sb("sqj", [128, D], BF16, st=sa)

                def srcx(t):
                    xa, xk = xring.next()
                    kb.dma("sp", xa[:], x_d[t * 128:(t + 1) * 128, :], [], [xk])
                    return xa[:], xk
                norm_T(srcx, 4, 4, 0, hT, lambda b: f"hT.{b}", 0, hb, sqj)
                P.barrier()
                P.flush()
            P.op("dve", lambda e: e.memset(yT[:, 0:4, :], 0.0), [], [f"yT.{t}.0" for t in range(NT)])
        else:
            for gi in range(4):
                load_w(Wr[:, :, gi * 512:(gi + 1) * 512], wview(w_in_d)[:, :, gi * 512:(gi + 1) * 512], f"W.{gi}")
            with contextlib.ExitStack() as sbk:
                cos_t = kb.sb("cos_t", [128, NT, 32], F32, st=sbk)
                sin_t = kb.sb("sin_t", [128, NT, 32], F32, st=sbk)
                srope = sbk
                if True:
                    posi = kb.sb("posi", [128, NT], I32, st=srope)
                    posf = kb.sb("posf", [128, NT], F32, st=srope)
                    ang = kb.sb("ang", [128, NT, 32], F32, st=srope)
                    ra = kb.sb("ra", [128, NT, 32], F32, st=srope)
                    rb_ = kb.sb("rb_", [128, NT, 32], F32, st=srope)
                    ri = kb.sb("ri", [128, NT, 32], I32, st=srope)
                    kb.dma("sp", posi[:], pos_d, [], ["posi"])
                    kb.cp("dve", posf[:], posi[:], ["posi"], ["posf"])
                    kb.tt("dve", ang[:], posf[:].unsqueeze(2).to_broadcast([128, NT, 32]),
                          cols[:, 103:135].unsqueeze(1).to_broadcast([128, NT, 32]), ALU.mult, ["posf", "cols"], ["ang"])
                    C1 = 6.28125
                    C2 = 2.0 * math.pi - C1
                    for tab, shift, nm in ((sin_t, 0.0, "sin"), (cos_t, 0.5 * math.pi, "cos")):
                        kb.ts("dve", ra[:], ang[:], shift, 1.0 / (2.0 * math.pi), ALU.add, ALU.mult, ["ang"], ["ra"])
                        kb.cp("dve", ri[:], ra[:], ["ra"], ["ri"])
                        kb.cp("dve", rb_[:], ri[:], ["ri"], ["rb"])
                        kb.ts("dve", ra[:], ang[:], shift, None, ALU.add, None, ["ang"], ["ra"])
                        kb.stt(ra[:], rb_[:], -C1, ra[:], ALU.mult, ALU.add, ["rb", "ra"], ["ra"])
                        kb.stt(ra[:], rb_[:], -C2, ra[:], ALU.mult, ALU.add, ["rb", "ra"], ["ra"])
                        kb.ts("dve", rb_[:], ra[:], math.pi, -2.0 * math.pi, ALU.is_gt, ALU.mult, ["ra"], ["rb"])
                        kb.tt("dve", ra[:], ra[:], rb_[:], ALU.add, ["ra", "rb"], ["ra"])
                        kb.ts("dve", rb_[:], ra[:], -math.pi, 2.0 * math.pi, ALU.is_lt, ALU.mult, ["ra"], ["rb"])
                        kb.tt("dve", ra[:], ra[:], rb_[:], ALU.add, ["ra", "rb"], ["ra"])
                        kb.act(tab[:], ra[:], AF.Sin, ["ra"], [nm])

                sb_ = lambda n, shp, d: kb.sb(n, shp, d, st=sbk)
                W = 2
                xr_p = [sb_(f"xr{i}", [128, D], F32) for i in range(2)]
                hb_p = [sb_(f"hb{i}", [128, D], BF16) for i in range(4)]
                sqj = sb_("sqj", [128, D], BF16)
                qT_p = [sb_(f"qT{i}", [128, 4, 2, 512], BF16) for i in range(2)]
                for i in range(2):
                    P.op("dve", lambda e, i=i: e.memset(qT_p[i][:], 0.0), [], [f"qT{i}.{j}" for j in range(4)])
                kT_p = [sb_(f"kT{i}", [128, 4, 512], BF16) for i in range(2)]
                ktok_p = [sb_(f"ktok{i}", [128, 4, 512], BF16) for i in range(2)]
                vtok_p = [sb_(f"vtok{i}", [128, 4, 512], BF16) for i in range(2)]
                gateT_p = [sb_(f"gateT{i}", [128, 4, 512], BF16) for i in range(2)]
                xs_p = [sb_(f"xs{i}", [128, 512], F32) for i in range(W)]
                A_p = [sb_(f"rA{i}", [128, 512], F32) for i in range(W)]
                B_p = [sb_(f"rB{i}", [128, 512], F32) for i in range(W)]
                qtok_p = [sb_(f"qtok{i}", [128, 512], BF16) for i in range(W)]
                th_p = [sb_(f"thr{i}", [128, 512], F32) for i in range(2)]
                sT_p = [sb_(f"sT{i}", [128, 8, 128], BF16) for i in range(W)]
                y32_p = [sb_(f"y32{i}", [128, 512], F32) for i in range(W)]
                sq_p = [sb_(f"sqr{i}", [128, 512], F32) for i in range(W)]
                ynb_p = [sb_(f"ynb{i}", [128, 512], BF16) for i in range(W)]
                yaff_p = [sb_(f"yaff{i}", [128, 2, 128], F32) for i in range(W)]
                stat_p = [sb_(f"stat{i}", [128, 64], F32) for i in range(W)]
                R32 = sb_("R32", [128, 4, 64], F32)
                Rbf = sb_("Rbf", [128, 5, 4, 64], BF16)
                P.op("dve", lambda e: e.memset(R32[:], 0.0), [], ["R32"])

                def gen_normA(blk):
                    for tl in range(4):
                        t = blk * 4 + tl
                        xa, xk = xr_p[t % 2], f"xr.{t % 2}"
                        kb.dma("sp", xa[:], x_d[t * 128:(t + 1) * 128, :], [], [xk])
                        kb.act(sqj[:], xa[:], AF.Square, [xk], ["sqj", f"ss{t}"], accum=ss[:, t:t + 1])
                        yield
                        kb.ts("dve", ss[:, t:t + 1], ss[:, t:t + 1], 1.0 / D, 1e-6, ALU.mult, ALU.add, [f"ss{t}"], [f"ss{t}"])
                        yield
                        kb.tt("pool", ss[:, t:t + 1], ss[:, t:t + 1], mhalf[:, 0:1], ALU.pow, [f"ss{t}", "mhalf"], [f"ss{t}"])
                        yield
                        kb.act(hb_p[tl][:], xa[:], AF.Copy, [xk, f"ss{t}"], [f"hb.{tl}"], scale=ss[:, t:t + 1])
                        yield
                    for kc in range(KC):
                        (b,) = yield from acquire(1)
                        bankT, bk = BK.bf(b), BK.key(b)
                        for tl in range(4):
                            kb.tr(bankT[:, tl * 128:(tl + 1) * 128], hb_p[tl][:, kc * 128:(kc + 1) * 128], [f"hb.{tl}"], [bk])
                        yield
                        o = hT[:, kc, blk * 512:(blk + 1) * 512]
                        if kc % 2:
                            kb.act(o, bankT[:, 0:512], AF.Copy, [bk, "cols"], [f"hT.{blk}"], scale=cols[:, kc:kc + 1])
                        else:
                            kb.ts("dve", o, bankT[:, 0:512], cols[:, kc:kc + 1], None, ALU.mult, None, [bk, "cols"], [f"hT.{blk}"])
                        BK.put(b)
                        yield

                def post_gen(slot, b, eps, wcol0, bcol0, finish):
                    bank, bk = BK.f32(b), BK.key(b)
                    y32, k1 = y32_p[slot], f"y32.{slot}"
                    kb.cp("act", y32[:], bank, [bk], [k1])
                    BK.put(b)
                    yield
                    stt_, ks = stat_p[slot], f"stat.{slot}"
                    v3 = lambda a: a.rearrange("p (h d) -> p h d", d=64)
                    bc = lambda a: a.unsqueeze(2).to_broadcast([128, 8, 64])
                    kb.red(stt_[:, 0:8], v3(y32[:]), ALU.add, [k1], [ks])
                    sq, k2 = sq_p[slot], f"sq.{slot}"
                    kb.act(sq[:], y32[:], AF.Square, [k1], [k2])
                    yield
                    kb.red(stt_[:, 8:16], v3(sq[:]), ALU.add, [k2], [ks])
                    yield
                    kb.ts("dve", stt_[:, 16:24], stt_[:, 0:8], 1.0 / 64, None, ALU.mult, None, [ks], [ks])
                    yield
                    kb.tt("dve", stt_[:, 24:32], stt_[:, 16:24], stt_[:, 16:24], ALU.mult, [ks], [ks])
                    kb.ts("dve", stt_[:, 32:40], stt_[:, 8:16], 1.0 / 64, eps, ALU.mult, ALU.add, [ks], [ks])
                    yield
                    kb.tt("dve", stt_[:, 40:48], stt_[:, 32:40], stt_[:, 24:32], ALU.subtract, [ks], [ks])
                    yield
                    kb.tt("pool", stt_[:, 48:56], stt_[:, 40:48], mhalf[:, 0:8], ALU.pow, [ks, "mhalf"], [ks])
                    yield
                    kb.stt(stt_[:, 56:64], stt_[:, 16:24], -1.0, stt_[:, 48:56], ALU.mult, ALU.mult, [ks], [ks])
                    kb.tt("pool", v3(y32[:]), v3(y32[:]), bc(stt_[:, 48:56]), ALU.mult, [k1, ks], [k1])
                    yield
                    ynb, k3 = ynb_p[slot], f"ynb.{slot}"
                    kb.tt("dve", v3(ynb[:]), v3(y32[:]), bc(stt_[:, 56:64]), ALU.add, [k1, ks], [k3])
                    yield
                    (b2,) = yield from acquire(1)
                    bankT, kt = BK.bf(b2), BK.key(b2)
                    for ct in range(4):
                        kb.tr(bankT[:, ct * 128:(ct + 1) * 128], ynb[:, ct * 128:(ct + 1) * 128], [k3], [kt])
                    yield
                    for ct in range(4):
                        ya, k4 = yaff_p[slot][:, ct % 2, :], f"yaff.{slot}.{ct % 2}"
                        kb.act(ya, bankT[:, ct * 128:(ct + 1) * 128], AF.Identity, [kt, "cols"], [k4],
                               scale=cols[:, wcol0 + ct:wcol0 + ct + 1], bias=cols[:, bcol0 + ct:bcol0 + ct + 1])
                        finish(ct, ya, k4)
                        if ct == 3:
                            BK.put(b2)
                        yield

                def qk_chain(slot, blk, tl, wi):
                    par = blk % 2
                    t = blk * 4 + tl
                    tok = slice(t * 128, (t + 1) * 128)
                    hk = f"hT.{blk}"
                    tabcol = 83 if wi == 0 else 91
                    (b,) = yield from acquire(1)
                    bank, bk = BK.f32(b), BK.key(b)
                    for kc in range(KC):
                        kb.mm(bank, hT[:, kc, tok], Wr[:, kc, wi * 512:(wi + 1) * 512], kc == 0, kc == KC - 1, [hk, f"W.{wi}"], [bk])
                    yield
                    xs, kx = xs_p[slot], f"xs.{slot}"
                    kb.cp("act", xs[:], bank, [bk], [kx])
                    BK.put(b)
                    yield
                    A, ka = A_p[slot], f"rA.{slot}"
                    B, kbk = B_p[slot], f"rB.{slot}"
                    v4 = lambda a: a.rearrange("p (h two f) -> p h two f", two=2, f=32)
                    cs4 = cos_t[:, t, :].unsqueeze(1).unsqueeze(1).to_broadcast([128, 8, 2, 32])
                    sn3 = sin_t[:, t, :].unsqueeze(1).to_broadcast([128, 8, 32])
                    kb.tt("dve", v4(A[:]), v4(xs[:]), cs4, ALU.mult, [kx, "cos"], [ka])
                    kb.tt("pool", v4(B[:])[:, :, 0, :], v4(xs[:])[:, :, 1, :], sn3, ALU.mult, [kx, "sin"], [kbk])
                    kb.tt("pool", v4(B[:])[:, :, 1, :], v4(xs[:])[:, :, 0, :], sn3, ALU.mult, [kx, "sin"], [kbk])
                    yield
                    kb.tt("dve", v4(A[:])[:, :, 0, :], v4(A[:])[:, :, 0, :], v4(B[:])[:, :, 0, :], ALU.subtract, [ka, kbk], [ka])
                    kb.tt("dve", v4(A[:])[:, :, 1, :], v4(A[:])[:, :, 1, :], v4(B[:])[:, :, 1, :], ALU.add, [ka, kbk], [ka])
                    yield
                    scl = cols[:, tabcol:tabcol + 8].unsqueeze(2).to_broadcast([128, 8, 64])
                    A3 = A[:].rearrange("p (h d) -> p h d", d=64)
                    if wi == 0:
                        qtv, kq = qtok_p[slot][:], f"qtok.{slot}"
                    else:
                        qtv, kq = ktok_p[par][:, tl, :], f"ktok{par}.{tl}"
                    kb.tt("dve", qtv.rearrange("p (h d) -> p h d", d=64), A3, scl, ALU.mult, [ka, "cols"], [kq])
                    yield
                    (b2,) = yield from acquire(1)
                    bankT, kt = BK.bf(b2), BK.key(b2)
                    for ct in range(4):
                        kb.tr(bankT[:, ct * 128:(ct + 1) * 128], qtv[:, ct * 128:(ct + 1) * 128], [kq], [kt])
                    yield
                    bv = bankT[:, 0:512].rearrange("p (c n) -> p c n", c=4)
                    if wi == 0:
                        kb.cp("act", qT_p[par][0:64, :, 0, tl * 128:(tl + 1) * 128], bv[0:64], [kt], [f"qT{par}.{tl}"])
                        kb.cp("dve", qT_p[par][64:128, :, 1, tl * 128:(tl + 1) * 128], bv[64:128], [kt], [f"qT{par}.{tl}"])
                    else:
                        kb.cpalt(kT_p[par][:, :, tl * 128:(tl + 1) * 128], bv, [kt], [f"kT{par}.{tl}"])
                    BK.put(b2)
                    yield

                def v_chain(blk, tl):
                    par = blk % 2
                    t = blk * 4 + tl
                    tok = slice(t * 128, (t + 1) * 128)
                    (b,) = yield from acquire(1)
                    bank, bk = BK.f32(b), BK.key(b)
                    for kc in range(KC):
                        kb.mm(bank, hT[:, kc, tok], Wr[:, kc, 1024:1536], kc == 0, kc == KC - 1, [f"hT.{blk}", "W.2"], [bk])
                    yield
                    kb.cpalt(vtok_p[par][:, tl, :], bank, [bk], [f"vtok{par}.{tl}"])
                    BK.put(b)
                    yield

                def g_chain(blk, ct):
                    par = blk % 2
                    (b,) = yield from acquire(1)
                    bank, bk = BK.f32(b), BK.key(b)
                    for kc in range(KC):
                        kb.mm(bank, Wr[:, kc, 1536 + ct * 128:1536 + (ct + 1) * 128], hT[:, kc, blk * 512:(blk + 1) * 512],
                              kc == 0, kc == KC - 1, [f"hT.{blk}", "W.3"], [bk])
                    yield
                    th, kth = th_p[ct % 2], f"thr.{ct % 2}"
                    kb.act(th[:], bank, AF.Tanh, [bk], [kth], scale=0.5)
                    yield
                    kb.ts("pool", th[:], th[:], 0.5, 0.5, ALU.mult, ALU.add, [kth], [kth])
                    yield
                    kb.tt("dve", gateT_p[par][:, ct, :], th[:], bank, ALU.mult, [kth, bk], [f"gateT{par}.{ct}"])
                    BK.put(b)
                    yield

                def gen_proj(blk):
                    for k in range(4):
                        yield from rr([qk_chain((2 * k) % W, blk, k, 0), qk_chain((2 * k + 1) % W, blk, k, 1), v_chain(blk, k), g_chain(blk, k)])

                def chunk_chain(slot, blk, tl):
                    par = blk % 2
                    qT, kT_, vtok, gateT = qT_p[par], kT_p[par], vtok_p[par], gateT_p[par]
                    t = blk * 4 + tl
                    tok = slice(t * 128, (t + 1) * 128)
                    tl_ = slice(tl * 128, (tl + 1) * 128)
                    sT, ksT = sT_p[slot], f"sT.{slot}"
                    bs = yield from acquire(2)
                    for g4 in range(2):
                        bank, bk = BK.f32(bs[g4]), BK.key(bs[g4])
                        for hh in range(4):
                            h = 4 * g4 + hh
                            kb.mm(bank[:, hh * 128:(hh + 1) * 128], kT_[:, h // 2, tl_], qT[:, h // 2, h % 2, tl_], True, True,
                                  [f"kT{par}.{tl}", f"qT{par}.{tl}"], [bk])
                    yield
                    for g4 in range(2):
                        bank, bk = BK.f32(bs[g4]), BK.key(bs[g4])
                        kb.tt("dve", sT[:, 4 * g4:4 * g4 + 4, :], bank.rearrange("p (c n) -> p c n", c=4),
                              maskI.unsqueeze(1).to_broadcast([128, 4, 128]), ALU.mult, [bk, "cqf"], [ksT])
                        BK.put(bs[g4])
                        yield
                    (bo,) = yield from acquire(1)
                    bankO, ko = BK.f32(bo), BK.key(bo)
                    for h in range(8):
                        kb.mm(bankO[:, 64 * h:64 * h + 64], sT[:, h, :], vtok[:, tl, 64 * h:64 * h + 64], True, t == 0,
                              [ksT, f"vtok{par}.{tl}"], [ko])
                        if t > 0:
                            kb.mm(bankO[:, 64 * h:64 * h + 64], qT[:, h // 2, h % 2, tl_], Rbf[:, t % 5, h // 2, :], False, True,
                                  [f"qT{par}.{tl}", f"Rbf.{t % 5}"], [ko])
                    yield

                    def fin(ct, ya, k4):
                        kb.tt("dve", yT[:, ct, tok], ya, gateT[:, ct, tl_], ALU.mult, [k4, f"gateT{par}.{ct}"], [f"yT.{t}.0"])
                    yield from post_gen(slot, bo, 1e-5, 32, 36, fin)

                def gen_chunks(blk):
                    par = blk % 2
                    ktok, vtok = ktok_p[par], vtok_p[par]
                    for tl in range(4):
                        t = blk * 4 + tl
                        if t == NT - 1:
                            continue
                        (b,) = yield from acquire(1)
                        bankK, kk_ = BK.f32(b), BK.key(b)
                        for ct in range(4):
                            kb.mm(bankK[:, ct * 128:(ct + 1) * 128], ktok[:, tl, ct * 128:(ct + 1) * 128], vtok[:, tl, ct * 128:(ct + 1) * 128],
                                  True, True, [f"ktok{par}.{tl}", f"vtok{par}.{tl}"], [kk_])
                        yield
                        for hh in range(2):
                            b0 = 64 * hh
                            kb.tt("dve", R32[b0:b0 + 64, :, :], bankK[b0:b0 + 64, :].rearrange("p (c n) -> p c n", c=4)[:, :, b0:b0 + 64],
                                  R32[b0:b0 + 64, :, :], ALU.add, [kk_, "R32"], ["R32"])
                        BK.put(b)
                        yield
                        kb.tt("dve", R32[:], R32[:], cols[:, 99:103].unsqueeze(2).to_broadcast([128, 4, 64]), ALU.mult, ["R32", "cols"], ["R32"])
                        yield
                        kb.cp("act", Rbf[:, (t + 1) % 5, :, :], R32[:], ["R32"], [f"Rbf.{(t + 1) % 5}"])
                        yield
                    yield from rr([chunk_chain(0, blk, 0), chunk_chain(1, blk, 1)])
                    yield from rr([chunk_chain(0, blk, 2), chunk_chain(1, blk, 3)])

                run(gen_normA(0))
                interleave([gen_proj(0), gen_normA(1)])
                for blk in range(4):
                    if blk == 3 and "C" in stages:
                        Wk = Wv8(1824)
                        kb.dma("pool", Wk[:, :, :], wview(w_in_d)[:, :, 2048:3872], [], ["Wk", "W.0", "W.1", "W.2", "W.3"])
                    gs = [gen_chunks(blk)]
                    if blk + 1 < 4:
                        gs.append(gen_proj(blk + 1))
                    if blk + 2 < 4:
                        gs.append(gen_normA(blk + 2))
                    interleave(gs)
                if dbg:
                    kb.dma("sp", dbg_hT, hT[:], [f"hT.{b}" for b in range(4)], [])
                P.barrier()
                P.flush()

        if "C" in stages:
            stage_C(nc, P, kb, locals())
        else:
            P.op("dve", lambda e: e.memset(hT[:, 0:4, :], 0.0), [f"hT.{b}" for b in range(4)], [f"yT.{t}.1" for t in range(NT)])

        if dbg:
            kb.dma("sp", dbg_yT[:, 0:4, :], yT[:], [f"yT.{t}.0" for t in range(NT)], [])
            kb.dma("sp", dbg_yT[:, 4:8, :], hT[:, 0:4, :], [f"yT.{t}.1" for t in range(NT)], [])

        if "D" in stages:
            stage_D(nc, P, kb, locals())
        P.barrier()
        P.flush()
    return nc


def stage_C(nc, P, kb, L):
    cols, dc, cqf, hT, WA, mhalf, bonesb, BK = (L[k] for k in ("cols", "dc", "cqf", "hT", "WA", "mhalf", "bonesb", "L_BK"))
    wup_d, aup_d, gup_d, w_in_d = L["wup_d"], L["aup_d"], L["gup_d"], L["w_in_d"]
    mask4, maskLs, identf = L["mask4"], L["maskLs"], L["identf"]
    onesf = cqf[:, 896:1024]
    Wk = WA[:, 0:KC * 1824].rearrange("p (k n) -> p k n", k=KC)
    CL = 0.5 * math.exp(-0.5)
    NB = 256
    NBLK = T // NB
    with contextlib.ExitStack() as sc:
        if "B" not in L["stages"]:
            kb.dma("pool", Wk[:, :, :], w_in_d.rearrange("(kc p) n -> p kc n", p=128)[:, :, 2048:3872], [], ["Wk"])
        sb = lambda n, s, d: kb.sb(n, s, d, st=sc)
        wupz = sb("wupz", [128, 512], BF16)
        aupz = sb("aupz", [128, 512], BF16)
        gup1b = sb("gup1b", [128, 512], BF16)
        gup2z = sb("gup2z", [128, 512], BF16)
        for tz in (wupz, aupz, gup2z):
            P.op("dve", lambda e, tz=tz: e.memset(tz[:], 0.0), [], ["wupb", "gupb"])
        kb.dma("pool", wupz[0:64, :], wup_d, [], ["wupb"])
        kb.dma("pool", aupz[64:128, :], aup_d, [], ["wupb"])
        kb.dma("pool", gup1b[:], gup_d[0:128, :], [], ["gupb"])
        kb.dma("pool", gup2z[96:128, :], gup_d[128:160, :], [], ["gupb"])
        RKblk = sb("RKblk", [128, 4, 128], BF16)
        for ct in range(4):
            kb.ts("dve", RKblk[:, ct, :], cqf[:, 768:896], cols[:, 71 + ct:72 + ct], None, ALU.mult, None, ["cqf", "cols"], ["RKblk"])
        pT = sb("pT", [128, 15, NB + 1], F32)
        P.op("dve", lambda e: e.memset(pT[:], 0.0), [], [f"pT.{i}" for i in range(15)])
        carry = sb("carry", [128, 15], F32)
        NTMP = 10
        T_p = [[sb(f"T{c}_{i}", [128, NB], F32) for i in range(NTMP)] for c in range(2)]
        sh_p = [sb(f"shT{i}", [128, NB], F32) for i in range(2)]
        kk2_p = [sb(f"kk2{i}", [128, NB], BF16) for i in range(2)]
        twa = sb("twa", [128, NB], BF16)
        sg1 = sb("sg1", [128, NB], BF16)
        sg2 = sb("sg2", [128, NB], BF16)
        ARt = sb("ARt", [128, 4, 2, 2, 2, 128], BF16)
        P.op("dve", lambda e: e.memset(ARt[:], 0.0), [], [f"ARt.{i}" for i in range(4)])
        KT = sb("KT", [128, 4, NB], BF16)
        BnT = sb("BnT", [128, 4, NB], BF16)
        rkb = sb("rkb", [128, 4, NB], BF16)
        vbf_p = [sb(f"vbf{i}", [128, 4, NB], BF16) for i in range(2)]
        gT = sb("gT", [128, 4, NB], BF16)
        Ktok = sb("Ktok", [128, 2, 512], BF16)
        Btok = sb("Btok", [128, 2, 512], BF16)
        Vtok = sb("Vtok", [128, 2, 512], BF16)
        PCt = sb("PCt", [128, 4, 2], F32)
        MTs = [sb(f"MT{i}", [128, 8, 512], BF16) for i in range(2)]
        mraw_p = [sb(f"mraw{i}", [128, 512], BF16) for i in range(2)]
        mask4b = sb("mask4b", [128, 512], BF16)
        kb.dma("pool", mask4b[:], L["cq_d"][:, 128:640], [], ["mask4b"])
        Xm = [sb(f"Xm{i}", [128, 8, 128], BF16) for i in range(2)]
        Nm = [sb(f"Nm{i}", [128, 8, 128], BF16) for i in range(2)]
        Lm = [sb(f"Lm{i}", [128, 8, 128], BF16) for i in range(2)]
        TTs = [sb(f"TT{i}", [128, 8, 128], BF16) for i in range(2)]
        RHSs = [sb(f"RHSb{i}", [128, 512], BF16) for i in range(2)]
        Ubs = [sb(f"Ub{i}", [128, 512], BF16) for i in range(2)]
        S32 = sb("S32", [128, 4, 64], F32)
        Sbf = sb("Sbf", [128, 4, 64], BF16)
        P.op("dve", lambda e: e.memset(S32[:], 0.0), [], ["S32"])
        y32 = sb("cy32", [128, 512], F32)
        sq = sb("csq", [128, 512], F32)
        ynb = sb("cynb", [128, 512], BF16)
        yaff = sb("cyaff", [128, 4, 128], F32)
        t1b = sb("ct1", [128, 4, 128], F32)
        stt_ = sb("cstat", [128, 64], F32)

        def gen_AB(blk):
            tok0 = blk * NB
            hk = f"hT.{tok0 // 512}"
            for i0 in range(0, 15, 2):
                b = BK.get()
                bank, bk = BK.f32(b), BK.key(b)
                idxs = [i for i in (i0, i0 + 1) if i < 15]
                for j, idx in enumerate(idxs):
                    c0 = idx * 128 if idx < 14 else 1696
                    for kc in range(KC):
                        kb.mm(bank[:, j * NB:(j + 1) * NB], Wk[:, kc, c0:c0 + 128], hT[:, kc, tok0:tok0 + NB],
                              kc == 0, kc == KC - 1, [hk, "Wk"] + (["hTu"] if kc >= 4 else []), [bk])
                yield
                for j, idx in enumerate(idxs):
                    kb.cp("act", pT[:, idx, 1:NB + 1], bank[:, j * NB:(j + 1) * NB], [bk], [f"pT.{idx}"])
                BK.put(b)
                yield
            allp = [f"pT.{i}" for i in range(15)]
            kb.cp("dve", carry[:], pT[:, :, NB], allp, ["carry"])
            yield
            for idx in range(15):
                tmp, ktm = sh_p[idx % 2], f"shT.{idx % 2}"
                kb.act(tmp[:], pT[:, idx, 0:NB], AF.Copy, [f"pT.{idx}", "cols"], [ktm], scale=cols[:, 40 + idx:41 + idx])
                yield
                if 8 <= idx < 12:
                    kb.stt(vbf_p[blk % 2][:, idx - 8, :], pT[:, idx, 1:NB + 1], dc[:, idx:idx + 1], tmp[:], ALU.mult, ALU.add,
                           [f"pT.{idx}", "dc", ktm, "carry"], [f"vbf{blk % 2}.{idx - 8}"])
                else:
                    kb.stt(pT[:, idx, 1:NB + 1], pT[:, idx, 1:NB + 1], dc[:, idx:idx + 1], tmp[:], ALU.mult, ALU.add,
                           [f"pT.{idx}", "dc", ktm, "carry"], [f"pT.{idx}"])
                yield
            kb.cp("dve", pT[:, :, 0], carry[:], ["carry"], allp)
            yield

        ps = lambda idx: pT[:, idx, 1:NB + 1]

        def gen_lora(blk):
            kb.act(twa[0:64, :], pT[0:64, 12, 1:NB + 1], AF.Tanh, ["pT.12"], ["twa"])
            kb.cp("dve", twa[64:128, :], pT[64:128, 12, 1:NB + 1], ["pT.12"], ["twa"])
            yield
            th, kth = sh_p[0], "shT.0"
            kb.act(th[:], ps(13), AF.Tanh, ["pT.13"], [kth], scale=0.5)
            th2, kth2 = sh_p[1], "shT.1"
            kb.act(th2[:], ps(14), AF.Tanh, ["pT.14"], [kth2], scale=0.5)
            yield
            kb.ts("dve", sg1[:], th[:], 0.5, 0.5, ALU.mult, ALU.add, [kth], ["sg1"])
            kb.ts("pool", sg2[:], th2[:], 0.5, 0.5, ALU.mult, ALU.add, [kth2], ["sg2"])
            yield

        def gen_ct(ch, blk, ct):
            Tb = T_p[ch]
            tk = lambda i: f"T{ch}.{i}"
            cs = slice(ct * 128, (ct + 1) * 128)
            r_, k_, v_ = ps(ct), ps(4 + ct), ps(8 + ct)
            kr, kk_, kv = f"pT.{ct}", f"pT.{4 + ct}", f"pT.{8 + ct}"
            bz, bg = BK.get(), BK.get()
            bankZ, kz = BK.f32(bz), BK.key(bz)
            bankG, kg = BK.f32(bg), BK.key(bg)
            kb.mm(bankZ[:, 0:NB], wupz[:, cs], twa[:], True, True, ["wupb", "twa"], [kz])
            kb.mm(bankZ[:, NB:2 * NB], aupz[:, cs], twa[:], True, True, ["wupb", "twa"], [kz])
            kb.mm(bankG[:, 0:NB], gup1b[:, cs], sg1[:], True, False, ["gupb", "sg1"], [kg])
            kb.mm(bankG[:, 0:NB], gup2z[:, cs], sg2[:], False, True, ["gupb", "sg2"], [kg])
            yield
            thw, tha = Tb[0], Tb[1]
            kb.act(thw[:], bankZ[:, 0:NB], AF.Tanh, [kz, "dc"], [tk(0)], scale=0.5, bias=dc[:, 15 + ct:16 + ct])
            kb.act(tha[:], bankZ[:, NB:2 * NB], AF.Tanh, [kz, "dc"], [tk(1)], scale=0.5, bias=dc[:, 19 + ct:20 + ct])
            BK.put(bz)
            kk = Tb[4]
            kb.ts("dve", kk[:], k_, cols[:, 63 + ct:64 + ct], None, ALU.mult, None, [kk_, "cols"], [tk(4)])
            yield
            kb.cp("act", gT[:, ct, :], bankG[:, 0:NB], [kg], [f"gT.{ct}"])
            BK.put(bg)
            kk2, k_kk2 = kk2_p[ch], f"kk2.{ch}"
            kb.act(kk2[:], kk[:], AF.Square, [tk(4)], [k_kk2])
            ld = Tb[0]
            kb.ts("dve", ld[:], thw[:], -CL, -CL, ALU.mult, ALU.add, [tk(0)], [tk(0)])
            yield
            bn_ = BK.get()
            bankN, kn = BK.f32(bn_), BK.key(bn_)
            kb.mm(bankN[:, 0:NB], bonesb[:], kk2[:], True, True, ["bonesb", k_kk2], [kn])
            cum = Tb[2]
            for c in range(2):
                ld_c, cum_c = ld[:, c * 128:(c + 1) * 128], cum[:, c * 128:(c + 1) * 128]
                P.op("dve", lambda e, cum_c=cum_c, ld_c=ld_c: e.tensor_tensor_scan(out=cum_c, data0=onesf, data1=ld_c, initial=0.0, op0=ALU.mult, op1=ALU.add),
                     [tk(0), "cqf"], [tk(2)])
            an = Tb[1]
            kb.ts("pool", an[:], tha[:], -0.5, -0.5, ALU.mult, ALU.add, [tk(1)], [tk(1)])
            yield
            sn = Tb[5]
            kb.act(sn[:], bankN[:, 0:NB], AF.Sqrt, [kn], [tk(5)])
            BK.put(bn_)
            cumex = Tb[3]
            kb.tt("dve", cumex[:], cum[:], ld[:], ALU.subtract, [tk(2), tk(0)], [tk(3)])
            yield
            ep, en, ex = Tb[6], Tb[7], Tb[8]
            kb.act(ep[:], cum[:], AF.Exp, [tk(2)], [tk(6)])
            kb.act(en[:], cum[:], AF.Exp, [tk(2)], [tk(7)], scale=-1.0)
            kb.ts("dve", sn[:], sn[:], 1e-12, None, ALU.max, None, [tk(5)], [tk(5)])
            tmp2 = Tb[9]
            kb.ts("pool", tmp2[:], an[:], dc[:, 23 + ct:24 + ct], dc[:, 27 + ct:28 + ct], ALU.mult, ALU.add, [tk(1), "dc"], [tk(9)])
            yield
            kb.act(ex[:], cumex[:], AF.Exp, [tk(3)], [tk(8)])
            P.op("dve", lambda e: e.reciprocal(out=sn[:], in_=sn[:]), [tk(5)], [tk(5)])
            k2 = Tb[9]
            kb.tt("pool", k2[:], k_, tmp2[:], ALU.mult, [kk_, tk(9)], [tk(9)])
            yield
            kkn = Tb[4]
            kb.tt("dve", kkn[:], kk[:], sn[:], ALU.mult, [tk(4), tk(5)], [tk(4)])
            c3 = lambda a: a.rearrange("p (c n) -> p c n", c=2)
            kb.cp("pool", PCt[:, ct, :], c3(ep[:])[:, :, 127], [tk(6)], [f"PCt.{ct}"])
            yield
            bn = Tb[5]
            kb.tt("dve", bn[:], kkn[:], an[:], ALU.mult, [tk(4), tk(1), tk(5)], [tk(5)])
            kb.tt("dve", KT[:, ct, :], k2[:], en[:], ALU.mult, [tk(9), tk(7)], [f"KT.{ct}"])
            yield
            for hh in range(2):
                pp = slice(64 * hh, 64 * hh + 64)
                kb.tt("dve", ARt[pp, ct, :, hh, 1, :], c3(r_)[pp], c3(ep[:])[pp], ALU.mult, [kr, tk(6)], [f"ARt.{ct}"])
                kb.tt("dve" if hh == 0 else "pool", ARt[pp, ct, :, hh, 0, :], c3(kkn[:])[pp], c3(ex[:])[pp], ALU.mult, [tk(4), tk(8)], [f"ARt.{ct}"])
                yield
            kb.tt("dve", BnT[:, ct, :], bn[:], en[:], ALU.mult, [tk(5), tk(7)], [f"BnT.{ct}"])
            kb.tt("pool", rkb[:, ct, :], r_, k2[:], ALU.mult, [kr, tk(9)], [f"rkb.{ct}"])
            yield

        def gen_tok(blk):
            for c in range(2):
                cs = slice(c * 128, (c + 1) * 128)
                for src, dst, nm, sk in ((KT, Ktok, "KT", "KT"), (BnT, Btok, "BnT", "BnT"), (vbf_p[blk % 2], Vtok, "vbf", f"vbf{blk % 2}")):
                    b = BK.get()
                    bankT, kt = BK.bf(b), BK.key(b)
                    for ct in range(4):
                        kb.tr(bankT[:, ct * 128:(ct + 1) * 128], src[:, ct, cs], [f"{sk}.{ct}"], [kt])
                    yield
                    kb.cpalt(dst[:, c, :], bankT[:, 0:512], [kt], [f"{nm}tok.{c}"])
                    BK.put(b)
                    yield

        def gen_F1(blk, c):
            cs = slice(c * 128, (c + 1) * 128)
            MT = MTs[c]
            for h in range(8):
                ct = h // 2
                b = BK.get()
                bankM, km = BK.f32(b), BK.key(b)
                rhsAR = ARt[:, ct, c, h % 2, :, :].rearrange("p a b -> p (a b)")
                kb.mm(bankM[:, 0:256], BnT[:, ct, cs], rhsAR, True, True, [f"BnT.{ct}", f"ARt.{ct}"], [km])
                kb.mm(bankM[:, 256:512], KT[:, ct, cs], rhsAR, True, True, [f"KT.{ct}", f"ARt.{ct}"], [km])
                yield
                if h % 2 == 0:
                    kb.tt("dve", MT[:, h, :], bankM, mask4, ALU.mult, [km, "cqf"], [f"MT{c}.{h}"])
                    BK.put(b)
                else:
                    mr, kmr = mraw_p[(h // 2) % 2], f"mraw.{(h // 2) % 2}"
                    kb.cp("act", mr[:], bankM, [km], [kmr])
                    BK.put(b)
                    yield
                    kb.tt("pool", MT[:, h, :], mr[:], mask4b[:], ALU.mult, [kmr, "mask4b"], [f"MT{c}.{h}"])
                yield
            for g4 in range(2):
                b = BK.get()
                bankL, kl = BK.f32(b), BK.key(b)
                for hh in range(4):
                    h = 4 * g4 + hh
                    kb.mm(bankL[:, hh * 128:(hh + 1) * 128], ARt[:, h // 2, c, h % 2, 0, :], BnT[:, h // 2, cs], True, True,
                          [f"ARt.{h // 2}", f"BnT.{h // 2}"], [kl])
                yield
                kb.tt("dve", Lm[0][:, 4 * g4:4 * g4 + 4, :], bankL.rearrange("p (c n) -> p c n", c=4),
                      maskLs.unsqueeze(1).to_broadcast([128, 4, 128]), ALU.mult, [kl, "cqf"], [f"Lm0.{g4}"])
                BK.put(b)
                hs = slice(4 * g4, 4 * g4 + 4)
                mtk = [f"MT{c}.{h}" for h in range(4 * g4, 4 * g4 + 4)]
                kb.cp("act", Nm[0][:, hs, :], MT[:, hs, 0:128], mtk, [f"Nm0.{g4}"])
                kb.tt("pool", Xm[0][:, hs, :], MT[:, hs, 0:128], identf.unsqueeze(1).to_broadcast([128, 4, 128]), ALU.add,
                      mtk + ["cqf"], [f"Xm0.{g4}"])
                yield

        def gen_inv_group(c, g4):
            hs = slice(4 * g4, 4 * g4 + 4)
            v4 = lambda bank: bank.rearrange("p (c n) -> p c n", c=4)
            cur = 0
            for lvl in range(7):
                nxt = cur ^ 1
                kN, kL, kX = f"Nm{cur}.{g4}", f"Lm{cur}.{g4}", f"Xm{cur}.{g4}"
                last = lvl == 6
                Xdst, kXd = (TTs[c], f"TT{c}.{g4}") if last else (Xm[nxt], f"Xm{nxt}.{g4}")
                bx = ba = bb = None
                if lvl >= 1:
                    bx = BK.get()
                    bankX, kbx = BK.f32(bx), BK.key(bx)
                    for hh in range(4):
                        h = 4 * g4 + hh
                        kb.mm(bankX[:, hh * 128:(hh + 1) * 128], Lm[cur][:, h, :], Xm[cur][:, h, :], True, True, [kL, kX], [kbx])
                if not last:
                    ba, bb = BK.get(), BK.get()
                    bankA, kba = BK.f32(ba), BK.key(ba)
                    bankB, kbb = BK.f32(bb), BK.key(bb)
                    for hh in range(4):
                        h = 4 * g4 + hh
                        kb.mm(bankA[:, hh * 128:(hh + 1) * 128], Lm[cur][:, h, :], Nm[cur][:, h, :], True, True, [kL, kN], [kba])
                    for hh in range(4):
                        h = 4 * g4 + hh
                        kb.mm(bankB[:, hh * 128:(hh + 1) * 128], Nm[cur][:, h, :], Lm[cur][:, h, :], True, True, [kL, kN], [kbb])
                yield
                if lvl >= 1:
                    kb.tt("dve", Xdst[:, hs, :], v4(bankX), Xm[cur][:, hs, :], ALU.add, [kbx, kX], [kXd])
                    BK.put(bx)
                else:
                    kb.cp("pool", Xdst[:, hs, :], Xm[cur][:, hs, :], [kX], [kXd])
                if not last:
                    kb.cp("act", Nm[nxt][:, hs, :], v4(bankA), [kba], [f"Nm{nxt}.{g4}"])
                    BK.put(ba)
                    if g4 == 0:
                        kb.cp("act", Lm[nxt][:, hs, :], v4(bankB), [kbb], [f"Lm{nxt}.{g4}"])
                    else:
                        kb.cp("dve", Lm[nxt][:, hs, :], v4(bankB), [kbb], [f"Lm{nxt}.{g4}"])
                    BK.put(bb)
                yield
                cur = nxt

        def gen_inv(blk, c):
            gs = [gen_inv_group(c, 0), gen_inv_group(c, 1)]
            while gs:
                for g in list(gs):
                    try:
                        next(g)
                    except StopIteration:
                        gs.remove(g)
                yield

        def gen_seqpost(blk, c):
            n_ch = blk * 2 + c
            cs = slice(c * 128, (c + 1) * 128)
            tok = slice(blk * NB + c * 128, blk * NB + (c + 1) * 128)
            MT, TT, RHSb, Ub = MTs[c], TTs[c], RHSs[c], Ubs[c]
            kTT = [f"TT{c}.0", f"TT{c}.1"]
            kRH, kUb = f"RHSb{c}", f"Ub{c}"
            br = BK.get()
            bankR, kr_ = BK.f32(br), BK.key(br)
            for h in range(8):
                ct = h // 2
                o = bankR[:, 64 * h:64 * h + 64]
                if n_ch > 0:
                    kb.mm(o, ARt[:, ct, c, h % 2, 0, :], Sbf[:, ct, :], True, False, [f"ARt.{ct}", "Sbf"], [kr_])
                kb.mm(o, MT[:, h, 256:384], Vtok[:, c, 64 * h:64 * h + 64], n_ch == 0, True, [f"MT{c}.{h}", f"vbftok.{c}"], [kr_])
            yield
            kb.cp("act", RHSb[:], bankR, [kr_], [kRH])
            BK.put(br)
            yield
            bu = BK.get()
            bankU, ku = BK.f32(bu), BK.key(bu)
            for h in range(8):
                kb.mm(bankU[:, 64 * h:64 * h + 64], TT[:, h, :], RHSb[:, 64 * h:64 * h + 64], True, True, kTT + [kRH], [ku])
            yield
            kb.cp("dve", Ub[:], bankU, [ku], [kUb])
            BK.put(bu)
            yield
            if n_ch < NT - 1:
                bs_ = BK.get()
                bankS, ks_ = BK.f32(bs_), BK.key(bs_)
                for ct in range(4):
                    o = bankS[:, ct * 128:(ct + 1) * 128]
                    kb.mm(o, Btok[:, c, ct * 128:(ct + 1) * 128], Ub[:, ct * 128:(ct + 1) * 128], True, False, [f"BnTtok.{c}", kUb], [ks_])
                    kb.mm(o, Ktok[:, c, ct * 128:(ct + 1) * 128], Vtok[:, c, ct * 128:(ct + 1) * 128], False, True, [f"KTtok.{c}", f"vbftok.{c}"], [ks_])
            by = BK.get()
            bankY, ky = BK.f32(by), BK.key(by)
            for h in range(8):
                ct = h // 2
                o = bankY[:, 64 * h:64 * h + 64]
                if n_ch > 0:
                    kb.mm(o, ARt[:, ct, c, h % 2, 1, :], Sbf[:, ct, :], True, False, [f"ARt.{ct}", "Sbf"], [ky])
                kb.mm(o, MT[:, h, 128:256], Ub[:, 64 * h:64 * h + 64], n_ch == 0, False, [f"MT{c}.{h}", kUb], [ky])
                kb.mm(o, MT[:, h, 384:512], Vtok[:, c, 64 * h:64 * h + 64], False, True, [f"MT{c}.{h}", f"vbftok.{c}"], [ky])
            yield
            if n_ch < NT - 1:
                for hh in range(2):
                    b0 = 64 * hh
                    kb.tt("dve", S32[b0:b0 + 64, :, :], bankS[b0:b0 + 64, :].rearrange("p (c n) -> p c n", c=4)[:, :, b0:b0 + 64],
                          S32[b0:b0 + 64, :, :], ALU.add, [ks_, "S32"], ["S32"])
                BK.put(bs_)
                kb.tt("dve", S32[:], S32[:], PCt[:, :, c].unsqueeze(2).to_broadcast([128, 4, 64]), ALU.mult,
                      ["S32"] + [f"PCt.{ct}" for ct in range(4)], ["S32"])
                kb.cp("act", Sbf[:], S32[:], ["S32"], ["Sbf"])
                yield
            k1, k2, ks, k3 = "cy32", "csq", "cstat", "cynb"
            kb.cp("act", y32[:], bankY, [ky], [k1])
            BK.put(by)
            yield
            v3 = lambda a: a.rearrange("p (h d) -> p h d", d=64)
            bc = lambda a: a.unsqueeze(2).to_broadcast([128, 8, 64])
            kb.red(stt_[:, 0:8], v3(y32[:]), ALU.add, [k1], [ks])
            kb.act(sq[:], y32[:], AF.Square, [k1], [k2])
            yield
            kb.red(stt_[:, 8:16], v3(sq[:]), ALU.add, [k2], [ks])
            yield
            kb.ts("dve", stt_[:, 16:24], stt_[:, 0:8], 1.0 / 64, None, ALU.mult, None, [ks], [ks])
            yield
            kb.tt("dve", stt_[:, 24:32], stt_[:, 16:24], stt_[:, 16:24], ALU.mult, [ks], [ks])
            kb.ts("dve", stt_[:, 32:40], stt_[:, 8:16], 1.0 / 64, 64e-5, ALU.mult, ALU.add, [ks], [ks])
            yield
            kb.tt("dve", stt_[:, 40:48], stt_[:, 32:40], stt_[:, 24:32], ALU.subtract, [ks], [ks])
            yield
            kb.tt("pool", stt_[:, 48:56], stt_[:, 40:48], mhalf[:, 0:8], ALU.pow, [ks, "mhalf"], [ks])
            yield
            kb.stt(stt_[:, 56:64], stt_[:, 16:24], -1.0, stt_[:, 48:56], ALU.mult, ALU.mult, [ks], [ks])
            kb.tt("pool", v3(y32[:]), v3(y32[:]), bc(stt_[:, 48:56]), ALU.mult, [k1, ks], [k1])
            yield
            kb.tt("dve", v3(ynb[:]), v3(y32[:]), bc(stt_[:, 56:64]), ALU.add, [k1, ks], [k3])
            yield
            b2 = BK.get()
            bankT, kt = BK.bf(b2), BK.key(b2)
            for ct in range(4):
                kb.tr(bankT[:, ct * 128:(ct + 1) * 128], ynb[:, ct * 128:(ct + 1) * 128], [k3], [kt])
            bbo = BK.get()
            bankBo, kbo = BK.f32(bbo), BK.key(bbo)
            for ct in range(4):
                kb.mm(bankBo[:, ct * 128:(ct + 1) * 128], RKblk[:, ct, :], rkb[:, ct, cs], True, True, ["RKblk", f"rkb.{ct}"], [kbo])
            yield
            for ct in range(4):
                kb.act(yaff[:, ct, :], bankT[:, ct * 128:(ct + 1) * 128], AF.Identity, [kt, "cols"], ["cyaff"],
                       scale=cols[:, 75 + ct:76 + ct], bias=cols[:, 79 + ct:80 + ct])
            kb.tt("dve", t1b[:], bankBo.rearrange("p (c n) -> p c n", c=4), vbf_p[blk % 2][:, :, cs], ALU.mult,
                  [kbo] + [f"vbf{blk % 2}.{ct}" for ct in range(4)], ["ct1"])
            BK.put(b2)
            BK.put(bbo)
            yield
            kb.tt("pool", t1b[:], t1b[:], yaff[:], ALU.add, ["ct1", "cyaff"], ["ct1"])
            yield
            kb.tt("dve", hT[:, 0:4, tok], t1b[:], gT[:, :, cs], ALU.mult, ["ct1"] + [f"gT.{ct}" for ct in range(4)],
                  [f"yT.{n_ch}.1", f"hT.{n_ch // 4}"])
            yield

        def gen_CD(blk):
            yield from gen_lora(blk)
            for pair in range(2):
                gs = [gen_ct(0, blk, 2 * pair), gen_ct(1, blk, 2 * pair + 1)]
                while gs:
                    for g in list(gs):
                        try:
                            next(g)
                        except StopIteration:
                            gs.remove(g)
                    yield
            yield from gen_tok(blk)

        def run(g):
            for _ in g:
                pass

        run(gen_AB(0))
        run(gen_CD(0))
        for blk in range(NBLK):
            run(gen_F1(blk, 0))
            interleave([gen_inv(blk, 0), gen_F1(blk, 1)])
            interleave([gen_seqpost(blk, 0), gen_inv(blk, 1)])
            if blk + 1 < NBLK:
                interleave([gen_seqpost(blk, 1), gen_AB(blk + 1)])
                if blk + 1 == NBLK - 1 and "D" in L["stages"]:
                    wv = lambda w: w.rearrange("(kc p) n -> p kc n", p=128)
                    Wout = WA[:, 0:8192].rearrange("p (k n) -> p k n", k=KC)
                    WkvK = WA[:, 8192:16384].rearrange("p (k n) -> p k n", k=KC)
                    WkvV = hT[:, 4:8, :].rearrange("p a (b n) -> p (a b) n", n=1024)
                    kb.dma("pool", Wout, wv(L["w_out_d"]), [], ["WA.0", "Wk"])
                    kb.dma("pool", WkvK, wv(L["wkv_d"])[:, :, 0:1024], [], ["WA.1", "Wk"])
                    kb.dma("pool", WkvV, wv(L["wkv_d"])[:, :, 1024:2048], [], ["WkvV", "hTu"])
                run(gen_CD(blk + 1))
            else:
                run(gen_seqpost(blk, 1))
        P.barrier()
        P.flush()


def stage_D(nc, P, kb, L):
    cols, cqf, hT, yT, WA, mhalf, onesb, ss, BK = (L[k] for k in ("cols", "cqf", "hT", "yT", "WA", "mhalf", "onesb", "ss", "L_BK"))
    x_d, mem_d, gfin_d, out_d = L["x_d"], L["mem_d"], L["gfin_d"], L["out_d"]
    w_out_d, wq_d, wkv_d, wo_d, wupm_d, wdn_d = (L[k] for k in ("w_out_d", "wq_d", "wkv_d", "wo_d", "wupm_d", "wdn_d"))
    wview = lambda w: w.rearrange("(kc p) n -> p kc n", p=128)

    def run(g):
        for _ in g:
            pass

    with contextlib.ExitStack() as sd:
        sb = lambda n, s, d, st=sd: kb.sb(n, s, d, st=st)
        xres = sb("xres", [128, NT, D], F32)
        gfin = sb("gfin", [128, D], F32)
        kTm = sb("kTm", [128, KC, 256], BF16)
        vmem = sb("vmem", [128, 2, D], BF16)
        kmx = sb("kmx", [128, 4], F32)
        hb_p = [sb(f"dhb{i}", [128, D], BF16) for i in range(4)]
        sqj = sb("dsqj", [128, D], BF16)
        W0 = WA[:, 0:8192].rearrange("p (k n) -> p k n", k=KC)
        W1 = WA[:, 8192:16384].rearrange("p (k n) -> p k n", k=KC)
        Wout, WkvK = W0, W1
        WkvV = hT[:, 4:8, :].rearrange("p a (b n) -> p (a b) n", n=1024)
        xv = x_d.rearrange("(t p) d -> p t d", p=128)
        kb.dma("sp", xres[:, 0:4, :], xv[:, 0:4, :], [], [f"xres.{t}" for t in range(0, 4)])
        if "C" not in L["stages"]:
            kb.dma("pool", Wout, wview(w_out_d), [], ["WA.0"])
            kb.dma("pool", WkvK, wview(wkv_d)[:, :, 0:1024], [], ["WA.1"])
            kb.dma("pool", WkvV, wview(wkv_d)[:, :, 1024:2048], [], ["WkvV"])
        kb.dma("sp", gfin[:], gfin_d.partition_broadcast(128), [], ["gfin"])

        def gen_norm(tiles, src, gcol0, dst_fn, dkey, sscol0):
            for tl, t in enumerate(tiles):
                xa, xk = src(t)
                c = sscol0 + t
                xks = xk if isinstance(xk, list) else [xk]
                kb.act(sqj[:], xa, AF.Square, xks, ["sqj", f"ss{c}"], accum=ss[:, c:c + 1])
                yield
                kb.ts("dve", ss[:, c:c + 1], ss[:, c:c + 1], 1.0 / D, 1e-6, ALU.mult, ALU.add, [f"ss{c}"], [f"ss{c}"])
                yield
                kb.tt("pool", ss[:, c:c + 1], ss[:, c:c + 1], mhalf[:, 0:1], ALU.pow, [f"ss{c}", "mhalf"], [f"ss{c}"])
                yield
                kb.act(hb_p[tl][:], xa, AF.Copy, xks + [f"ss{c}"], [f"dhb.{tl}"], scale=ss[:, c:c + 1])
                yield
            n = len(tiles) * 128
            for kc in range(KC):
                b = BK.get()
                bankT, bk = BK.bf(b), BK.key(b)
                for tl in range(len(tiles)):
                    kb.tr(bankT[:, tl * 128:(tl + 1) * 128], hb_p[tl][:, kc * 128:(kc + 1) * 128], [f"dhb.{tl}"], [bk])
                yield
                o = dst_fn(kc)
                dk = dkey if isinstance(dkey, list) else [dkey]
                if kc % 2:
                    kb.act(o, bankT[:, 0:n], AF.Copy, [bk, "cols"], dk, scale=cols[:, gcol0 + kc:gcol0 + kc + 1])
                else:
                    kb.ts("dve", o, bankT[:, 0:n], cols[:, gcol0 + kc:gcol0 + kc + 1], None, ALU.mult, None, [bk, "cols"], dk)
                BK.put(b)
                yield

        oTb_t = sb("oTb", [128, KC * 512], BF16)
        aT_t = sb("aT_all", [128, 8 * 512], BF16)
        if True:
            memx = [oTb_t[:, 2048 * i:2048 * (i + 1)].bitcast(F32) for i in range(2)]
            mxk = [[f"oTb.{k}" for k in range(4 * i, 4 * i + 4)] for i in range(2)]
            memT = aT_t[:, 0:2048].rearrange("p (k n) -> p k n", k=KC)
            k2m = aT_t[:, 2048:4096].rearrange("p (k n) -> p k n", k=KC)
            kmT = [f"aT.{k}" for k in range(4)]
            kk2 = [f"aT.{k}" for k in range(4, 8)]
            for i in range(2):
                kb.dma("sp", memx[i], mem_d[i * 128:(i + 1) * 128, :], [], mxk[i])
            for i in range(1, 4):
                kb.dma("sp", xres[:, 4 * i:4 * i + 4, :], xv[:, 4 * i:4 * i + 4, :], [], [f"xres.{t}" for t in range(4 * i, 4 * i + 4)])

            dstate = {"v_done": False, "t3_done": False}

            def gen_memkv():
                yield from gen_norm([0, 1], lambda t: (memx[t], mxk[t]), 16, lambda kc: memT[:, kc, :], kmT, 16)
                for mt in range(2):
                    for hf in range(2):
                        b = BK.get()
                        bank, bk = BK.f32(b), BK.key(b)
                        for kc in range(KC):
                            kb.mm(bank, memT[:, kc, mt * 128:(mt + 1) * 128], WkvV[:, kc, hf * 512:(hf + 1) * 512],
                                  kc == 0, kc == KC - 1, ["WkvV"] + kmT, [bk])
                        yield
                        kb.cpalt(vmem[:, mt, hf * 512:(hf + 1) * 512], bank, [bk], ["vmem"])
                        BK.put(b)
                        yield
                dstate["v_done"] = True
                for ft in range(KC):
                    b = BK.get()
                    bank, bk = BK.f32(b), BK.key(b)
                    for kc in range(KC):
                        kb.mm(bank[:, 0:256], WkvK[:, kc, ft * 128:(ft + 1) * 128], memT[:, kc, :], kc == 0, kc == KC - 1, ["WA.1"] + kmT, [bk])
                    yield
                    kb.cpalt(kTm[:, ft, :], bank[:, 0:256], [bk], ["kTm"])
                    BK.put(b)
                    yield
                kb.act(k2m, kTm[:], AF.Square, ["kTm"], kk2)
                yield
                for h in range(4):
                    b = BK.get()
                    bank, bk = BK.f32(b), BK.key(b)
                    kb.mm(bank[:, 0:256], onesb[:], k2m[:, 2 * h, :], True, False, ["onesb"] + kk2, [bk])
                    kb.mm(bank[:, 0:256], onesb[:], k2m[:, 2 * h + 1, :], False, True, ["onesb"] + kk2, [bk])
                    yield
                    kb.red(kmx[:, h:h + 1], bank[:, 0:256], ALU.max, [bk], ["kmx"])
                    BK.put(b)
                    yield

            def gen_wout():
                for t in range(NT):
                    tok = slice(t * 128, (t + 1) * 128)
                    for hf in range(2):
                        b = BK.get()
                        bank, bk = BK.f32(b), BK.key(b)
                        for kc in range(KC):
                            ysrc = yT[:, kc, tok] if kc < 4 else hT[:, kc - 4, tok]
                            kb.mm(bank, ysrc, Wout[:, kc, hf * 512:(hf + 1) * 512], kc == 0, kc == KC - 1,
                                  [f"yT.{t}.0", f"yT.{t}.1", "WA.0", f"hT.{t // 4}"], [bk])
                        yield
                        xs = xres[:, t, hf * 512:(hf + 1) * 512]
                        kb.tt("dve", xs, bank, xs, ALU.add, [bk, f"xres.{t}"], [f"xres.{t}"])
                        BK.put(b)
                        yield
                    if t == 3:
                        dstate["t3_done"] = True

            srcx = lambda t: (xres[:, t, :], f"xres.{t}")

            def gen_norm0():
                while not (dstate["v_done"] and dstate["t3_done"]):
                    yield
                yield from gen_norm([0, 1, 2, 3], srcx, 8, lambda kc: hT[:, kc, 0:512], ["hT.0", "WkvV"], 0)

            interleave([gen_memkv(), gen_wout(), gen_norm0()])
            kb.dma("pool", W1, wview(wq_d), [], ["WA.1"])
            kb.dma("pool", W0, wview(wo_d), [], ["WA.0"])

        srcx = lambda t: (xres[:, t, :], f"xres.{t}")
        norm_x = lambda blk: gen_norm(list(range(4 * blk, 4 * blk + 4)), srcx, 8, lambda kc: hT[:, kc, blk * 512:(blk + 1) * 512], [f"hT.{blk}", "WkvV"], 0)
        norm_m = lambda blk: gen_norm(list(range(4 * blk, 4 * blk + 4)), srcx, 24, lambda kc: hT[:, kc, blk * 512:(blk + 1) * 512], f"hT.{blk}", 16)
        with contextlib.ExitStack() as s2:
            qTb = yT[:, 0:2, :].rearrange("p a (b n) -> p (a b) n", n=512)
            PT = yT[:, 2:4, :].rearrange("p a (b n) -> p (a b) n", n=512)
            oTb = oTb_t[:].rearrange("p (k n) -> p k n", k=KC)
            yall = [f"yT.{t}.0" for t in range(NT)]
            q2b_p = [sb(f"q2b{i}", [128, 2, 512], BF16, st=s2) for i in range(4)]
            nb_p = [sb(f"nb{i}", [128, 2], F32, st=s2) for i in range(4)]
            rc_p = [sb(f"rc{i}", [128, 512], F32, st=s2) for i in range(4)]

            def gen_head(sl, h):
                qk = [f"qTb.{2 * h}", f"qTb.{2 * h + 1}"]
                q2b, kq2 = q2b_p[sl], f"q2b.{sl}"
                kb.act(q2b[:], qTb[:, 2 * h:2 * h + 2, :], AF.Square, qk, [kq2])
                yield
                b = BK.get()
                bankQ, kq = BK.f32(b), BK.key(b)
                kb.mm(bankQ, onesb[:], q2b[:, 0, :], True, False, ["onesb", kq2], [kq])
                kb.mm(bankQ, onesb[:], q2b[:, 1, :], False, True, ["onesb", kq2], [kq])
                yield
                nb, kbs = nb_p[sl], f"nb.{sl}"
                kb.red(nb[:, 0:1], bankQ, ALU.max, [kq], [kbs])
                BK.put(b)
                yield
                kb.ts("dve", nb[:, 1:2], nb[:, 0:1], kmx[:, h:h + 1], -1.0 / 32, ALU.add, ALU.mult, [kbs, "kmx"], [kbs])
                yield
                for mt in range(2):
                    b = BK.get()
                    bankS, ksb = BK.f32(b), BK.key(b)
                    ms = slice(mt * 128, (mt + 1) * 128)
                    kb.mm(bankS, kTm[:, 2 * h, ms], qTb[:, 2 * h, :], True, False, ["kTm", qk[0]], [ksb])
                    kb.mm(bankS, kTm[:, 2 * h + 1, ms], qTb[:, 2 * h + 1, :], False, True, ["kTm", qk[1]], [ksb])
                    yield
                    kb.act(PT[:, 2 * h + mt, :], bankS, AF.Exp, [ksb, kbs], [f"PT.{2 * h + mt}"] + (yall if blk == 0 else []), scale=1.0 / 16, bias=nb[:, 1:2])
                    BK.put(b)
                    yield
                pk = [f"PT.{2 * h}", f"PT.{2 * h + 1}"]
                b = BK.get()
                bankRs, krs = BK.f32(b), BK.key(b)
                kb.mm(bankRs, onesb[:], PT[:, 2 * h, :], True, False, ["onesb", pk[0]], [krs])
                kb.mm(bankRs, onesb[:], PT[:, 2 * h + 1, :], False, True, ["onesb", pk[1]], [krs])
                yield
                rc, krc = rc_p[sl], f"rc.{sl}"
                P.op("dve", lambda e: e.reciprocal(out=rc[:], in_=bankRs), [krs], [krc])
                BK.put(b)
                yield
                for dt_ in range(2):
                    ft = 2 * h + dt_
                    b = BK.get()
                    bankO, kob = BK.f32(b), BK.key(b)
                    kb.mm(bankO, vmem[:, 0, ft * 128:(ft + 1) * 128], PT[:, 2 * h, :], True, False, ["vmem", pk[0]], [kob])
                    kb.mm(bankO, vmem[:, 1, ft * 128:(ft + 1) * 128], PT[:, 2 * h + 1, :], False, True, ["vmem", pk[1]], [kob])
                    yield
                    kb.tt("dve", oTb[:, ft, :], bankO, rc[:], ALU.mult, [kob, krc], [f"oTb.{ft}"])
                    BK.put(b)
                    yield

            def gen_attn(blk):
                bs = slice(blk * 512, (blk + 1) * 512)
                for ft in range(KC):
                    b = BK.get()
                    bank, bk = BK.f32(b), BK.key(b)
                    for kc in range(KC):
                        kb.mm(bank, W1[:, kc, ft * 128:(ft + 1) * 128], hT[:, kc, bs], kc == 0, kc == KC - 1, ["WA.1", f"hT.{blk}"], [bk])
                    yield
                    kb.cpalt(qTb[:, ft, :], bank, [bk], [f"qTb.{ft}"] + (yall if blk == 0 else []))
                    BK.put(b)
                    yield
                if blk == 3:
                    mlp_load(0)
                for pair in range(1):
                    gs = [gen_head(h, h) for h in range(4)]
                    while gs:
                        for g in list(gs):
                            try:
                                next(g)
                            except StopIteration:
                                gs.remove(g)
                        yield
                for tl in range(4):
                    t = blk * 4 + tl
                    for hf in range(2):
                        b = BK.get()
                        bank, bk = BK.f32(b), BK.key(b)
                        for kc in range(KC):
                            kb.mm(bank, oTb[:, kc, tl * 128:(tl + 1) * 128], W0[:, kc, hf * 512:(hf + 1) * 512], kc == 0, kc == KC - 1,
                                  [f"oTb.{kc}", "WA.0"], [bk])
                        yield
                        xs = xres[:, t, hf * 512:(hf + 1) * 512]
                        kb.tt("dve", xs, bank, xs, ALU.add, [bk, f"xres.{t}"], [f"xres.{t}"])
                        BK.put(b)
                        yield

            def norm_worker(blk):
                if blk + 1 < 4:
                    yield from norm_x(blk + 1)
                if blk - 1 >= 0:
                    yield from norm_m(blk - 1)

            s3 = s2
            aT_all = aT_t[:].rearrange("p (k n) -> p k n", k=8)
            rl_p = [sb(f"rl{i}", [128, 512], BF16, st=s3) for i in range(2)]
            o_p = [oTb_t[:, 2048 * i:2048 * (i + 1)].bitcast(F32) for i in range(2)]
            upv = wupm_d.rearrange("(kc p) n -> p kc n", p=128)
            dnv = wdn_d.rearrange("(f p) n -> p f n", p=128)
            ov = out_d.rearrange("(t p) d -> p t d", p=128)

            def mlp_w(j):
                s = (j + 1) % 2
                Wu = WA[:, s * 8192:s * 8192 + 4096].rearrange("p (k n) -> p k n", k=KC)
                Wd = WA[:, s * 8192 + 4096:(s + 1) * 8192].rearrange("p (f n) -> p f n", f=4)
                return s, Wu, Wd

            def mlp_load(j):
                s, Wu, Wd = mlp_w(j)
                kb.dma("pool", Wu, upv[:, :, j * 512:(j + 1) * 512], [], [f"WA.{s}"])
                kb.dma("pool", Wd, dnv[:, 4 * j:4 * j + 4, :], [], [f"WA.{s}"])

            for blk in range(4):
                interleave([gen_attn(blk), norm_worker(blk)])
            run(norm_m(3))

            NJ = 8
            ai = 0
            ri = 0
            for j in range(NJ):
                s, Wu, Wd = mlp_w(j)
                if j > 0:
                    mlp_load(j)
                for blk in range(4):
                    bs = slice(blk * 512, (blk + 1) * 512)
                    ab = (ai % 2) * 4
                    ai += 1
                    for fft in range(4):
                        b = BK.get()
                        bank, bk = BK.f32(b), BK.key(b)
                        for kc in range(KC):
                            kb.mm(bank, Wu[:, kc, fft * 128:(fft + 1) * 128], hT[:, kc, bs], kc == 0, kc == KC - 1, [f"WA.{s}", f"hT.{blk}"], [bk])
                        rl, krl = rl_p[ri % 2], f"rl.{ri % 2}"
                        ri += 1
                        kb.act(rl[:], bank, AF.Relu, [bk], [krl])
                        BK.put(b)
                        kb.tt("dve", aT_all[:, ab + fft, :], rl[:], rl[:], ALU.mult, [krl], [f"aT.{ab + fft}"])
                    for tl in range(4):
                        t = blk * 4 + tl
                        for hf in range(2):
                            b = BK.get()
                            bank, bk = BK.f32(b), BK.key(b)
                            for fft in range(4):
                                kb.mm(bank, aT_all[:, ab + fft, tl * 128:(tl + 1) * 128], Wd[:, fft, hf * 512:(hf + 1) * 512], fft == 0, fft == 3,
                                      [f"aT.{ab + fft}", f"WA.{s}"], [bk])
                            xs = xres[:, t, hf * 512:(hf + 1) * 512]
                            kb.tt("dve", xs, bank, xs, ALU.add, [bk, f"xres.{t}"], [f"xres.{t}"])
                            BK.put(b)
                    if j == NJ - 1:
                        for fb in ([blk - 1] if blk > 0 else []) + ([3] if blk == 3 else []):
                            for t in range(4 * fb, 4 * fb + 4):
                                c = 32 + t
                                kb.act(sqj[:], xres[:, t, :], AF.Square, [f"xres.{t}"], ["sqj", f"ss{c}"], accum=ss[:, c:c + 1])
                                kb.ts("dve", ss[:, c:c + 1], ss[:, c:c + 1], 1.0 / D, 1e-6, ALU.mult, ALU.add, [f"ss{c}"], [f"ss{c}"])
                                kb.tt("pool", ss[:, c:c + 1], ss[:, c:c + 1], mhalf[:, 0:1], ALU.pow, [f"ss{c}", "mhalf"], [f"ss{c}"])
                                ot, ko = o_p[t % 2], f"ot.{t % 2}"
                                kb.stt(ot, xres[:, t, :], ss[:, c:c + 1], gfin[:], ALU.mult, ALU.mult, [f"xres.{t}", f"ss{c}", "gfin"],
                                       [ko] + [f"oTb.{k}" for k in range(KC)])
                                kb.dma("sp", ov[:, t, :], ot, [ko], [f"out.{t}"])
            P.barrier()
            P.flush()


def _consts():
    cq = np.zeros((128, NCQ), np.float32)
    i = np.arange(128)
    cq[:, 0:128] = np.eye(128, dtype=np.float32)
    strict = (i[:, None] < i[None, :]).astype(np.float32)
    incl = (i[:, None] <= i[None, :]).astype(np.float32)
    cq[:, 128:256] = strict
    cq[:, 256:384] = incl
    cq[:, 384:512] = strict
    cq[:, 512:640] = incl
    cq[:, 640:768] = (i[:, None] > i[None, :]).astype(np.float32)
    cq[:, 768:896] = ((i[:, None] // 64) == (i[None, :] // 64)).astype(np.float32)
    cq[:, 896:1024] = 1.0
    gam = 1.0 - 2.0 ** (-5.0 - np.arange(8, dtype=np.float64))
    xi = gam[None, :] ** (i[:, None] + 1.0)
    kz = 0.125 * gam[None, :] ** (-(i[:, None] + 1.0))
    gC = np.zeros((128, 4))
    for ct in range(4):
        for p in range(128):
            gC[p, ct] = gam[2 * ct + p // 64] ** 128.0
    invf = (10000.0 ** (-np.arange(32, dtype=np.float32) / 32.0)).astype(np.float32)
    return cq, xi.astype(np.float32), kz.astype(np.float32), gC.astype(np.float32), np.broadcast_to(invf[None], (128, 32))


def _col(v):
    v = np.asarray(v, np.float32).reshape(-1)
    return v.reshape(-1, 128).T


def make_inputs(inp):
    cq, xi, kz, gC, invf = _consts()
    cols = np.zeros((128, NCOLS), np.float32)
    cols[:, 0:8] = _col(inp["norm_mix"][0])
    cols[:, 8:16] = _col(inp["norm_xattn"][0])
    cols[:, 16:24] = _col(inp["norm_mem"][0])
    cols[:, 24:32] = _col(inp["norm_mlp"][0])
    cols[:, 32:36] = _col(inp["ret_gn_w"][0])
    cols[:, 36:40] = _col(inp["ret_gn_b"][0])
    mu = np.asarray(inp["rwkv_mu"][0], np.float32)
    cols[:, 40:54] = _col(mu[:1792])
    cols[:, 54] = mu[1696:1824]
    cols[:, 55:59] = _col(inp["rwkv_w0"][0])
    cols[:, 59:63] = _col(inp["rwkv_a0"][0])
    cols[:, 63:67] = _col(inp["rwkv_k_k"][0])
    cols[:, 67:71] = _col(inp["rwkv_k_a"][0])
    cols[:, 71:75] = _col(inp["rwkv_r_k"][0])
    cols[:, 75:79] = _col(inp["rwkv_gn_w"][0])
    cols[:, 79:83] = _col(inp["rwkv_gn_b"][0])
    cols[:, 83:91] = xi
    cols[:, 91:99] = kz
    cols[:, 99:103] = gC
    cols[:, 103:135] = invf
    shared = {
        "cols": cols, "cq": cq,
        "gfin": np.ascontiguousarray(inp["norm_final"], np.float32),
        "w_in": np.ascontiguousarray(inp["w_in"][0]),
        "w_up": np.ascontiguousarray(inp["rwkv_w_up"][0]),
        "a_up": np.ascontiguousarray(inp["rwkv_a_up"][0]),
        "g_up": np.ascontiguousarray(inp["rwkv_g_up"][0]),
        "w_out": np.ascontiguousarray(inp["w_out"][0]),
        "wq": np.ascontiguousarray(inp["xattn_w_q"][0]),
        "wkv": np.ascontiguousarray(inp["xattn_w_kv"][0]),
        "wo": np.ascontiguousarray(inp["xattn_w_o"][0]),
        "mlp_up": np.ascontiguousarray(inp["mlp_w_up"][0]),
        "mlp_down": np.ascontiguousarray(inp["mlp_w_down"][0]),
    }
    maps = []
    for b in range(8):
        m = dict(shared)
        m["x"] = np.ascontiguousarray(inp["x"][b], np.float32)
        m["mem"] = np.ascontiguousarray(inp["mem"][b], np.float32)
        m["pos"] = np.ascontiguousarray(np.asarray(inp["positions"][b], np.int32).reshape(NT, 128).T)
        maps.append(m)
    return maps


_NC_CACHE = {}


def kernel(**inputs):
    inp = {k: np.asarray(v) for k, v in inputs.items()}
    maps = make_inputs(inp)
    if "nc" not in _NC_CACHE:
        _NC_CACHE["nc"] = build()
    res = run_bass_kernel_spmd(_NC_CACHE["nc"], maps, core_ids=list(range(8)))
    return np.stack([np.asarray(r["out"], np.float32) for r in res.results], axis=0)
```

```python
import contextlib
import math
import numpy as np
import concourse.bass as bass
import concourse.mybir as mybir
from concourse.bass_utils import run_bass_kernel_spmd

F32 = mybir.dt.float32
BF16 = mybir.dt.bfloat16
I32 = mybir.dt.int32
AF = mybir.ActivationFunctionType
ALU = mybir.AluOpType
AX = mybir.AxisListType

T = 2048
D = 1024
NT = 16
KC = 8
NCOLS = 135
NCQ = 1024
CUT = 99
COMPUTE = ("pe", "act", "dve", "pool")
ISSUERS = ("pe", "act", "dve", "pool", "sp")


class Prog:
    def __init__(self, nc, st, n_dma=12, self_sync=True):
        self.nc = nc
        self.n_dma = n_dma
        self.self_sync = self_sync
        self.chans = list(COMPUTE) + [f"d{i}" for i in range(n_dma)]
        self.sems = {c: st.enter_context(nc.semaphore(f"s_{c}")) for c in self.chans}
        self.streams = {e: [] for e in ISSUERS}
        self.count = {c: 0 for c in self.chans}
        self.clock = {e: {} for e in ISSUERS}
        self.snap = {}
        self.wr = {}
        self.rd = {}
        self.rr = 0
        self.nops = 0
        self.nwaits = 0

    @staticmethod
    def _need(needs, d):
        for c, n in d.items():
            if needs.get(c, 0) < n:
                needs[c] = n

    def op(self, eng, fn, reads=(), writes=(), dma=False):
        needs = {}
        for k in reads:
            self._need(needs, self.wr.get(k, {}))
        for k in writes:
            self._need(needs, self.wr.get(k, {}))
            self._need(needs, self.rd.get(k, {}))
        if dma:
            chan = f"d{self.rr % self.n_dma}"
            self.rr += 1
            if self.count[chan]:
                self._need(needs, {chan: self.count[chan]})
        else:
            chan = eng
        clk = self.clock[eng]
        waits = []
        for c, n in needs.items():
            if c == eng and (eng == "pe" or not self.self_sync):
                continue
            if clk.get(c, 0) < n:
                waits.append((c, n))
                for c2, n2 in self.snap[(c, n)].items():
                    if clk.get(c2, 0) < n2:
                        clk[c2] = n2
        self.nwaits += len(waits)
        self.nops += 1
        n_new = self.count[chan] + 1
        self.count[chan] = n_new
        s = dict(clk)
        s[chan] = n_new
        self.snap[(chan, n_new)] = s
        if eng == "pe" and chan == "pe":
            clk["pe"] = n_new
        wset = set(writes)
        for k in wset:
            self.wr[k] = {chan: n_new}
            self.rd[k] = {}
        for k in reads:
            if k in wset:
                continue
            self.rd.setdefault(k, {})[chan] = n_new
        self.streams[eng].append((waits, fn, chan))

    def barrier(self):
        allk = {c: n for c, n in self.count.items() if n}
        for e in ISSUERS:
            clk = self.clock[e]
            waits = []
            for c, n in allk.items():
                if c == e and e == "pe":
                    continue
                if clk.get(c, 0) < n:
                    waits.append((c, n))
            if waits:
                self.streams[e].append((waits, None, None))
        for e in ISSUERS:
            for c, n in allk.items():
                if self.clock[e].get(c, 0) < n:
                    self.clock[e][c] = n

    def flush(self):
        nc = self.nc
        streams = self.streams
        if not any(streams[e] for e in ISSUERS):
            return
        self.streams = {e: [] for e in ISSUERS}
        sems = self.sems

        def val(c, n):
            return n * 16 if c.startswith("d") else n

        def run(engname):
            def body(e):
                for waits, fn, chan in streams[engname]:
                    for c, n in waits:
                        e.wait_ge(sems[c], val(c, n))
                    if fn is not None:
                        fn(e).then_inc(sems[chan], 16 if chan.startswith("d") else 1)
            return body

        with nc.Block() as block:
            block.tensor(run("pe"))
            block.scalar(run("act"))
            block.vector(run("dve"))
            block.gpsimd(run("pool"))
            block.sync(run("sp"))


class Ring:
    def __init__(self, kb, name, n, shape, dtype, st=None, psum=False):
        st = st or kb.st
        alloc = kb.nc.psum_tensor if psum else kb.nc.sbuf_tensor
        self.name = name
        self.t = [st.enter_context(alloc(f"rg_{name}{i}", shape, dtype)) for i in range(n)]
        self.i = 0

    def next(self):
        i = self.i % len(self.t)
        self.i += 1
        return self.t[i], f"{self.name}.{i}"


class ViewRing:
    def __init__(self, aps, keys):
        self.t, self.k, self.i = aps, keys, 0

    def next(self):
        i = self.i % len(self.t)
        self.i += 1
        return self.t[i], self.k[i]


class Banks:
    def __init__(self, pp):
        import collections
        self.pp = pp
        self.free = collections.deque(range(8))

    def get(self):
        return self.free.popleft()

    def put(self, i):
        self.free.append(i)

    def f32(self, i):
        return self.pp[:, i, :]

    def bf(self, i):
        return self.pp[:, i, :].bitcast(BF16)

    @staticmethod
    def key(i):
        return f"ps{i}"


def interleave(gens):
    gens = list(gens)
    while gens:
        for g in list(gens):
            try:
                next(g)
            except StopIteration:
                gens.remove(g)


class KB:
    def __init__(self, nc, P, st):
        self.nc, self.P, self.st = nc, P, st
        self.flip = 0

    def sb(self, name, shape, dtype, st=None):
        return (st or self.st).enter_context(self.nc.sbuf_tensor("sb_" + name, shape, dtype))

    def mm(self, out, lhsT, rhs, start, stop, r, w):
        self.P.op("pe", lambda e: e.matmul(out, lhsT=lhsT, rhs=rhs, start=start, stop=stop), r, w)

    def tr(self, out, in_, r, w):
        idb = self.identb[:]
        self.P.op("pe", lambda e: e.transpose(out=out, in_=in_, identity=idb), list(r) + ["identb"], w)

    def act(self, out, in_, func, r, w, scale=1.0, bias=0.0, accum=None):
        kw = {}
        if accum is not None:
            kw["accum_out"] = accum
        self.P.op("act", lambda e: e.activation(out=out, in_=in_, func=func, bias=bias, scale=scale, **kw), r, w)

    def tt(self, eng, out, in0, in1, op, r, w):
        self.P.op(eng, lambda e: e.tensor_tensor(out=out, in0=in0, in1=in1, op=op), r, w)

    def ts(self, eng, out, in0, s1, s2, op0, op1, r, w):
        if s2 is None:
            self.P.op(eng, lambda e: e.tensor_scalar(out=out, in0=in0, scalar1=s1, scalar2=None, op0=op0), r, w)
        else:
            self.P.op(eng, lambda e: e.tensor_scalar(out=out, in0=in0, scalar1=s1, scalar2=s2, op0=op0, op1=op1), r, w)

    def stt(self, out, in0, sc, in1, op0, op1, r, w):
        self.P.op("dve", lambda e: e.scalar_tensor_tensor(out=out, in0=in0, scalar=sc, in1=in1, op0=op0, op1=op1), r, w)

    def cp(self, eng, out, in_, r, w):
        if eng == "act":
            self.P.op("act", lambda e: e.activation(out=out, in_=in_, func=AF.Copy), r, w)
        else:
            self.P.op(eng, lambda e: e.tensor_copy(out=out, in_=in_), r, w)

    def cpalt(self, out, in_, r, w):
        self.flip ^= 1
        self.cp("act" if self.flip else "dve", out, in_, r, w)

    def red(self, out, in_, op, r, w):
        self.P.op("dve", lambda e: e.tensor_reduce(out=out, in_=in_, axis=AX.X, op=op), r, w)

    def dma(self, eng, out, in_, r, w):
        self.P.op(eng, lambda e: e.dma_start(out=out, in_=in_), r, w, dma=True)


def build(stages="ABCD", dbg=False):
    nc = bass.Bass("TRN2", target_bir_lowering=False)

    def dram(n, s, d, kind="ExternalInput"):
        return nc.dram_tensor(n, s, d, kind=kind).ap()

    x_d = dram("x", [T, D], F32)
    mem_d = dram("mem", [256, D], F32)
    pos_d = dram("pos", [128, NT], I32)
    cols_d = dram("cols", [128, NCOLS], F32)
    cq_d = dram("cq", [128, NCQ], F32)
    gfin_d = dram("gfin", [D], F32)
    w_in_d = dram("w_in", [D, 3872], F32)
    wup_d = dram("w_up", [64, 512], F32)
    aup_d = dram("a_up", [64, 512], F32)
    gup_d = dram("g_up", [160, 512], F32)
    w_out_d = dram("w_out", [D, D], F32)
    wq_d = dram("wq", [D, D], F32)
    wkv_d = dram("wkv", [D, 2 * D], F32)
    wo_d = dram("wo", [D, D], F32)
    wupm_d = dram("mlp_up", [D, 4 * D], F32)
    wdn_d = dram("mlp_down", [4 * D, D], F32)
    out_d = dram("out", [T, D], F32, kind="ExternalOutput")
    if dbg:
        dbg_yT = dram("dbg_yT", [128, KC, T], BF16, kind="ExternalOutput")
        dbg_hT = dram("dbg_hT", [128, KC, T], BF16, kind="ExternalOutput")

    def wview(w):
        return w.rearrange("(kc p) n -> p kc n", p=128)

    with contextlib.ExitStack() as st:
        P = Prog(nc, st)
        kb = KB(nc, P, st)
        cols = kb.sb("cols", [128, NCOLS], F32)
        dc = kb.sb("dcols", [128, 40], F32)
        cqf = kb.sb("cqf", [128, NCQ], F32)
        identb = kb.sb("identb", [128, 128], BF16)
        bonesb = kb.sb("bonesb", [128, 128], BF16)
        onesb = kb.sb("onesb", [128, 128], BF16)
        mhalf = kb.sb("mhalf", [128, 256], F32)
        hT = kb.sb("hT", [128, KC, T], BF16)
        yT = kb.sb("yT", [128, 4, T], BF16)
        WA = kb.sb("WA", [128, 16384], BF16)
        ss = kb.sb("ss", [128, 3 * NT], F32)
        kb.identb = identb
        pp = st.enter_context(nc.psum_tensor("pp", [128, 8, 512], F32))
        L_BK = Banks(pp)
        psA = ViewRing([pp[:, i, :] for i in range(6)], [f"ps{i}" for i in range(6)])
        psT = ViewRing([pp[:, i, :].bitcast(BF16) for i in (6, 7)], ["ps6", "ps7"])
        mask4 = cqf[:, 128:640]
        maskI = cqf[:, 256:384]
        maskLs = cqf[:, 640:768]
        identf = cqf[:, 0:128]

        kb.dma("sp", cols[:], cols_d, [], ["cols"])
        kb.dma("sp", cqf[:], cq_d, [], ["cqf"])
        kb.dma("pool", identb[:], cq_d[:, 0:128], [], ["identb"])
        kb.dma("pool", bonesb[:], cq_d[:, 768:896], [], ["bonesb"])
        kb.dma("pool", onesb[:], cq_d[:, 896:1024], [], ["onesb"])
        P.op("dve", lambda e: e.memset(mhalf[:], -0.5), [], ["mhalf"])
        kb.ts("dve", dc[:, 0:15], cols[:, 40:55], -1.0, 1.0, ALU.mult, ALU.add, ["cols"], ["dc"])
        kb.ts("dve", dc[:, 15:19], cols[:, 55:59], 0.5, None, ALU.mult, None, ["cols"], ["dc"])
        kb.ts("dve", dc[:, 19:23], cols[:, 59:63], 0.5, None, ALU.mult, None, ["cols"], ["dc"])
        kb.ts("dve", dc[:, 23:27], cols[:, 67:71], -1.0, None, ALU.mult, None, ["cols"], ["dc"])
        kb.ts("dve", dc[:, 27:31], cols[:, 67:71], -1.0, 1.0, ALU.mult, ALU.add, ["cols"], ["dc"])

        def norm_T(src, nblk, tpb, gcol0, dst, dstkey, sscol0, hb, sqj):
            for blk in range(nblk):
                hbs = []
                for tl in range(tpb):
                    t = blk * tpb + tl
                    xa, xk = src(t)
                    c = sscol0 + t
                    kb.act(sqj[:], xa, AF.Square, [xk], ["sqj", f"ss{c}"], accum=ss[:, c:c + 1])
                    kb.ts("dve", ss[:, c:c + 1], ss[:, c:c + 1], 1.0 / D, 1e-6, ALU.mult, ALU.add, [f"ss{c}"], [f"ss{c}"])
                    kb.tt("pool", ss[:, c:c + 1], ss[:, c:c + 1], mhalf[:, 0:1], ALU.pow, [f"ss{c}", "mhalf"], [f"ss{c}"])
                    h, hk = hb.next()
                    kb.act(h[:], xa, AF.Copy, [xk, f"ss{c}"], [hk], scale=ss[:, c:c + 1])
                    hbs.append((h, hk))
                for kc in range(KC):
                    bank, bk = psT.next()
                    for tl, (h, hk) in enumerate(hbs):
                        kb.tr(bank[:, tl * 128:(tl + 1) * 128], h[:, kc * 128:(kc + 1) * 128], [hk], [bk])
                    n = tpb * 128
                    o = dst[:, kc, blk * n:(blk + 1) * n]
                    kb.flip ^= 1
                    if kb.flip:
                        kb.act(o, bank[:, 0:n], AF.Copy, [bk, "cols"], [dstkey(blk)], scale=cols[:, gcol0 + kc:gcol0 + kc + 1])
                    else:
                        kb.ts("dve", o, bank[:, 0:n], cols[:, gcol0 + kc:gcol0 + kc + 1], None, ALU.mult, None, [bk, "cols"], [dstkey(blk)])

        def load_w(dst, src, key, eng="pool"):
            kb.dma(eng, dst, src, [], [key])

        Wv8 = lambda ncol: WA[:, 0:KC * ncol].rearrange("p (k n) -> p k n", k=KC)

        Wr = Wv8(2048)
        BK = L_BK

        def acquire(n=1):
            spins = 0
            while len(BK.free) < n:
                spins += 1
                assert spins < 100000, "PSUM bank pool deadlock"
                yield
            return [BK.get() for _ in range(n)]

        def rr(gens):
            gens = list(gens)
            while gens:
                for g in list(gens):
                    try:
                        next(g)
                    except StopIteration:
                        gens.remove(g)
                yield

        def run(g):
            for _ in g:
                pass

        if "B" not in stages:
            with contextlib.ExitStack() as sa:
                xring = Ring(kb, "xr", 3, [128, D], F32, st=sa)
                hb = Ring(kb, "hb", 8, [128, D], BF16, st=sa)
                sqj = kb.sb("sqj", [128, D], BF16, st=sa)

                def srcx(t):
                    xa, xk = xring.next()
                    kb.dma("sp", xa[:], x_d[t * 128:(t + 1) * 128, :], [], [xk])
                    return xa[:], xk
                norm_T(srcx, 4, 4, 0, hT, lambda b: f"hT.{b}", 0, hb, sqj)
                P.barrier()
                P.flush()
            P.op("dve", lambda e: e.memset(yT[:, 0:4, :], 0.0), [], [f"yT.{t}.0" for t in range(NT)])
        else:
            for gi in range(4):
                load_w(Wr[:, :, gi * 512:(gi + 1) * 512], wview(w_in_d)[:, :, gi * 512:(gi + 1) * 512], f"W.{gi}")
            with contextlib.ExitStack() as sbk:
                cos_t = kb.sb("cos_t", [128, NT, 32], F32, st=sbk)
                sin_t = kb.sb("sin_t", [128, NT, 32], F32, st=sbk)
                srope = sbk
                if True:
                    posi = kb.sb("posi", [128, NT], I32, st=srope)
                    posf = kb.sb("posf", [128, NT], F32, st=srope)
                    ang = kb.sb("ang", [128, NT, 32], F32, st=srope)
                    ra = kb.sb("ra", [128, NT, 32], F32, st=srope)
                    rb_ = kb.sb("rb_", [128, NT, 32], F32, st=srope)
                    ri = kb.sb("ri", [128, NT, 32], I32, st=srope)
                    kb.dma("sp", posi[:], pos_d, [], ["posi"])
                    kb.cp("dve", posf[:], posi[:], ["posi"], ["posf"])
                    kb.tt("dve", ang[:], posf[:].unsqueeze(2).to_broadcast([128, NT, 32]),
                          cols[:, 103:135].unsqueeze(1).to_broadcast([128, NT, 32]), ALU.mult, ["posf", "cols"], ["ang"])
                    C1 = 6.28125
                    C2 = 2.0 * math.pi - C1
                    for tab, shift, nm in ((sin_t, 0.0, "sin"), (cos_t, 0.5 * math.pi, "cos")):
                        kb.ts("dve", ra[:], ang[:], shift, 1.0 / (2.0 * math.pi), ALU.add, ALU.mult, ["ang"], ["ra"])
                        kb.cp("dve", ri[:], ra[:], ["ra"], ["ri"])
                        kb.cp("dve", rb_[:], ri[:], ["ri"], ["rb"])
                        kb.ts("dve", ra[:], ang[:], shift, None, ALU.add, None, ["ang"], ["ra"])
                        kb.stt(ra[:], rb_[:], -C1, ra[:], ALU.mult, ALU.add, ["rb", "ra"], ["ra"])
                        kb.stt(ra[:], rb_[:], -C2, ra[:], ALU.mult, ALU.add, ["rb", "ra"], ["ra"])
                        kb.ts("dve", rb_[:], ra[:], math.pi, -2.0 * math.pi, ALU.is_gt, ALU.mult, ["ra"], ["rb"])
                        kb.tt("dve", ra[:], ra[:], rb_[:], ALU.add, ["ra", "rb"], ["ra"])
                        kb.ts("dve", rb_[:], ra[:], -math.pi, 2.0 * math.pi, ALU.is_lt, ALU.mult, ["ra"], ["rb"])
                        kb.tt("dve", ra[:], ra[:], rb_[:], ALU.add, ["ra", "rb"], ["ra"])
                        kb.act(tab[:], ra[:], AF.Sin, ["ra"], [nm])

                sb_ = lambda n, shp, d: kb.sb(n, shp, d, st=sbk)
                W = 2
                xr_p = [sb_(f"xr{i}", [128, D], F32) for i in range(2)]
                hb_p = [sb_(f"hb{i}", [128, D], BF16) for i in range(4)]
                sqj = sb_("sqj", [128, D], BF16)
                qT_p = [sb_(f"qT{i}", [128, 4, 2, 512], BF16) for i in range(2)]
                for i in range(2):
                    P.op("dve", lambda e, i=i: e.memset(qT_p[i][:], 0.0), [], [f"qT{i}.{j}" for j in range(4)])
                kT_p = [sb_(f"kT{i}", [128, 4, 512], BF16) for i in range(2)]
                ktok_p = [sb_(f"ktok{i}", [128, 4, 512], BF16) for i in range(2)]
                vtok_p = [sb_(f"vtok{i}", [128, 4, 512], BF16) for i in range(2)]
                gateT_p = [sb_(f"gateT{i}", [128, 4, 512], BF16) for i in range(2)]
                xs_p = [sb_(f"xs{i}", [128, 512], F32) for i in range(W)]
                A_p = [sb_(f"rA{i}", [128, 512], F32) for i in range(W)]
                B_p = [sb_(f"rB{i}", [128, 512], F32) for i in range(W)]
                qtok_p = [sb_(f"qtok{i}", [128, 512], BF16) for i in range(W)]
                th_p = [sb_(f"thr{i}", [128, 512], F32) for i in range(2)]
                sT_p = [sb_(f"sT{i}", [128, 8, 128], BF16) for i in range(W)]
                y32_p = [sb_(f"y32{i}", [128, 512], F32) for i in range(W)]
                sq_p = [sb_(f"sqr{i}", [128, 512], F32) for i in range(W)]
                ynb_p = [sb_(f"ynb{i}", [128, 512], BF16) for i in range(W)]
                yaff_p = [sb_(f"yaff{i}", [128, 2, 128], F32) for i in range(W)]
                stat_p = [sb_(f"stat{i}", [128, 64], F32) for i in range(W)]
                R32 = sb_("R32", [128, 4, 64], F32)
                Rbf = sb_("Rbf", [128, 5, 4, 64], BF16)
                P.op("dve", lambda e: e.memset(R32[:], 0.0), [], ["R32"])

                def gen_normA(blk):
                    for tl in range(4):
                        t = blk * 4 + tl
                        xa, xk = xr_p[t % 2], f"xr.{t % 2}"
                        kb.dma("sp", xa[:], x_d[t * 128:(t + 1) * 128, :], [], [xk])
                        kb.act(sqj[:], xa[:], AF.Square, [xk], ["sqj", f"ss{t}"], accum=ss[:, t:t + 1])
                        yield
                        kb.ts("dve", ss[:, t:t + 1], ss[:, t:t + 1], 1.0 / D, 1e-6, ALU.mult, ALU.add, [f"ss{t}"], [f"ss{t}"])
                        yield
                        kb.tt("pool", ss[:, t:t + 1], ss[:, t:t + 1], mhalf[:, 0:1], ALU.pow, [f"ss{t}", "mhalf"], [f"ss{t}"])
                        yield
                        kb.act(hb_p[tl][:], xa[:], AF.Copy, [xk, f"ss{t}"], [f"hb.{tl}"], scale=ss[:, t:t + 1])
                        yield
                    for kc in range(KC):
                        (b,) = yield from acquire(1)
                        bankT, bk = BK.bf(b), BK.key(b)
                        for tl in range(4):
                            kb.tr(bankT[:, tl * 128:(tl + 1) * 128], hb_p[tl][:, kc * 128:(kc + 1) * 128], [f"hb.{tl}"], [bk])
                        yield
                        o = hT[:, kc, blk * 512:(blk + 1) * 512]
                        if kc % 2:
                            kb.act(o, bankT[:, 0:512], AF.Copy, [bk, "cols"], [f"hT.{blk}"], scale=cols[:, kc:kc + 1])
                        else:
                            kb.ts("dve", o, bankT[:, 0:512], cols[:, kc:kc + 1], None, ALU.mult, None, [bk, "cols"], [f"hT.{blk}"])
                        BK.put(b)
                        yield

                def post_gen(slot, b, eps, wcol0, bcol0, finish):
                    bank, bk = BK.f32(b), BK.key(b)
                    y32, k1 = y32_p[slot], f"y32.{slot}"
                    kb.cp("act", y32[:], bank, [bk], [k1])
                    BK.put(b)
                    yield
                    stt_, ks = stat_p[slot], f"stat.{slot}"
                    v3 = lambda a: a.rearrange("p (h d) -> p h d", d=64)
                    bc = lambda a: a.unsqueeze(2).to_broadcast([128, 8, 64])
                    kb.red(stt_[:, 0:8], v3(y32[:]), ALU.add, [k1], [ks])
                    sq, k2 = sq_p[slot], f"sq.{slot}"
                    kb.act(sq[:], y32[:], AF.Square, [k1], [k2])
                    yield
                    kb.red(stt_[:, 8:16], v3(sq[:]), ALU.add, [k2], [ks])
                    yield
                    kb.ts("dve", stt_[:, 16:24], stt_[:, 0:8], 1.0 / 64, None, ALU.mult, None, [ks], [ks])
                    yield
                    kb.tt("dve", stt_[:, 24:32], stt_[:, 16:24], stt_[:, 16:24], ALU.mult, [ks], [ks])
                    kb.ts("dve", stt_[:, 32:40], stt_[:, 8:16], 1.0 / 64, eps, ALU.mult, ALU.add, [ks], [ks])
                    yield
                    kb.tt("dve", stt_[:, 40:48], stt_[:, 32:40], stt_[:, 24:32], ALU.subtract, [ks], [ks])
                    yield
                    kb.tt("pool", stt_[:, 48:56], stt_[:, 40:48], mhalf[:, 0:8], ALU.pow, [ks, "mhalf"], [ks])
                    yield
                    kb.stt(stt_[:, 56:64], stt_[:, 16:24], -1.0, stt_[:, 48:56], ALU.mult, ALU.mult, [ks], [ks])
                    kb.tt("dve", v3(y32[:]), v3(y32[:]), bc(stt_[:, 48:56]), ALU.mult, [k1, ks], [k1])
                    yield
                    ynb, k3 = ynb_p[slot], f"ynb.{slot}"
                    kb.tt("dve", v3(ynb[:]), v3(y32[:]), bc(stt_[:, 56:64]), ALU.add, [k1, ks], [k3])
                    yield
                    (b2,) = yield from acquire(1)
                    bankT, kt = BK.bf(b2), BK.key(b2)
                    for ct in range(4):
                        kb.tr(bankT[:, ct * 128:(ct + 1) * 128], ynb[:, ct * 128:(ct + 1) * 128], [k3], [kt])
                    yield
                    for ct in range(4):
                        ya, k4 = yaff_p[slot][:, ct % 2, :], f"yaff.{slot}.{ct % 2}"
                        kb.act(ya, bankT[:, ct * 128:(ct + 1) * 128], AF.Identity, [kt, "cols"], [k4],
                               scale=cols[:, wcol0 + ct:wcol0 + ct + 1], bias=cols[:, bcol0 + ct:bcol0 + ct + 1])
                        finish(ct, ya, k4)
                        if ct == 3:
                            BK.put(b2)
                        yield

                def qk_chain(slot, blk, tl, wi):
                    par = blk % 2
                    t = blk * 4 + tl
                    tok = slice(t * 128, (t + 1) * 128)
                    hk = f"hT.{blk}"
                    tabcol = 83 if wi == 0 else 91
                    (b,) = yield from acquire(1)
                    bank, bk = BK.f32(b), BK.key(b)
                    for kc in range(KC):
                        kb.mm(bank, hT[:, kc, tok], Wr[:, kc, wi * 512:(wi + 1) * 512], kc == 0, kc == KC - 1, [hk, f"W.{wi}"], [bk])
                    yield
                    xs, kx = xs_p[slot], f"xs.{slot}"
                    kb.cp("act", xs[:], bank, [bk], [kx])
                    BK.put(b)
                    yield
                    A, ka = A_p[slot], f"rA.{slot}"
                    B, kbk = B_p[slot], f"rB.{slot}"
                    v4 = lambda a: a.rearrange("p (h two f) -> p h two f", two=2, f=32)
                    cs4 = cos_t[:, t, :].unsqueeze(1).unsqueeze(1).to_broadcast([128, 8, 2, 32])
                    sn3 = sin_t[:, t, :].unsqueeze(1).to_broadcast([128, 8, 32])
                    kb.tt("dve", v4(A[:]), v4(xs[:]), cs4, ALU.mult, [kx, "cos"], [ka])
                    kb.tt("pool", v4(B[:])[:, :, 0, :], v4(xs[:])[:, :, 1, :], sn3, ALU.mult, [kx, "sin"], [kbk])
                    kb.tt("pool", v4(B[:])[:, :, 1, :], v4(xs[:])[:, :, 0, :], sn3, ALU.mult, [kx, "sin"], [kbk])
                    yield
                    kb.tt("dve", v4(A[:])[:, :, 0, :], v4(A[:])[:, :, 0, :], v4(B[:])[:, :, 0, :], ALU.subtract, [ka, kbk], [ka])
                    kb.tt("dve", v4(A[:])[:, :, 1, :], v4(A[:])[:, :, 1, :], v4(B[:])[:, :, 1, :], ALU.add, [ka, kbk], [ka])
                    yield
                    scl = cols[:, tabcol:tabcol + 8].unsqueeze(2).to_broadcast([128, 8, 64])
                    A3 = A[:].rearrange("p (h d) -> p h d", d=64)
                    if wi == 0:
                        qtv, kq = qtok_p[slot][:], f"qtok.{slot}"
                    else:
                        qtv, kq = ktok_p[par][:, tl, :], f"ktok{par}.{tl}"
                    kb.tt("dve", qtv.rearrange("p (h d) -> p h d", d=64), A3, scl, ALU.mult, [ka, "cols"], [kq])
                    yield
                    (b2,) = yield from acquire(1)
                    bankT, kt = BK.bf(b2), BK.key(b2)
                    for ct in range(4):
                        kb.tr(bankT[:, ct * 128:(ct + 1) * 128], qtv[:, ct * 128:(ct + 1) * 128], [kq], [kt])
                    yield
                    bv = bankT[:, 0:512].rearrange("p (c n) -> p c n", c=4)
                    if wi == 0:
                        kb.cp("act", qT_p[par][0:64, :, 0, tl * 128:(tl + 1) * 128], bv[0:64], [kt], [f"qT{par}.{tl}"])
                        kb.cp("dve", qT_p[par][64:128, :, 1, tl * 128:(tl + 1) * 128], bv[64:128], [kt], [f"qT{par}.{tl}"])
                    else:
                        kb.cpalt(kT_p[par][:, :, tl * 128:(tl + 1) * 128], bv, [kt], [f"kT{par}.{tl}"])
                    BK.put(b2)
                    yield

                def v_chain(blk, tl):
                    par = blk % 2
                    t = blk * 4 + tl
                    tok = slice(t * 128, (t + 1) * 128)
                    (b,) = yield from acquire(1)
                    bank, bk = BK.f32(b), BK.key(b)
                    for kc in range(KC):
                        kb.mm(bank, hT[:, kc, tok], Wr[:, kc, 1024:1536], kc == 0, kc == KC - 1, [f"hT.{blk}", "W.2"], [bk])
                    yield
                    kb.cpalt(vtok_p[par][:, tl, :], bank, [bk], [f"vtok{par}.{tl}"])
                    BK.put(b)
                    yield

                def g_chain(blk, ct):
                    par = blk % 2
                    (b,) = yield from acquire(1)
                    bank, bk = BK.f32(b), BK.key(b)
                    for kc in range(KC):
                        kb.mm(bank, Wr[:, kc, 1536 + ct * 128:1536 + (ct + 1) * 128], hT[:, kc, blk * 512:(blk + 1) * 512],
                              kc == 0, kc == KC - 1, [f"hT.{blk}", "W.3"], [bk])
                    yield
                    th, kth = th_p[ct % 2], f"thr.{ct % 2}"
                    kb.act(th[:], bank, AF.Tanh, [bk], [kth], scale=0.5)
                    yield
                    kb.ts("pool", th[:], th[:], 0.5, 0.5, ALU.mult, ALU.add, [kth], [kth])
                    yield
                    kb.tt("dve", gateT_p[par][:, ct, :], th[:], bank, ALU.mult, [kth, bk], [f"gateT{par}.{ct}"])
                    BK.put(b)
                    yield

                def gen_proj(blk):
                    for k in range(4):
                        yield from rr([qk_chain((2 * k) % W, blk, k, 0), qk_chain((2 * k + 1) % W, blk, k, 1), v_chain(blk, k), g_chain(blk, k)])

                def chunk_chain(slot, blk, tl):
                    par = blk % 2
                    qT, kT_, vtok, gateT = qT_p[par], kT_p[par], vtok_p[par], gateT_p[par]
                    t = blk * 4 + tl
                    tok = slice(t * 128, (t + 1) * 128)
                    tl_ = slice(tl * 128, (tl + 1) * 128)
                    sT, ksT = sT_p[slot], f"sT.{slot}"
                    bs = yield from acquire(2)
                    for g4 in range(2):
                        bank, bk = BK.f32(bs[g4]), BK.key(bs[g4])
                        for hh in range(4):
                            h = 4 * g4 + hh
                            kb.mm(bank[:, hh * 128:(hh + 1) * 128], kT_[:, h // 2, tl_], qT[:, h // 2, h % 2, tl_], True, True,
                                  [f"kT{par}.{tl}", f"qT{par}.{tl}"], [bk])
                    yield
                    for g4 in range(2):
                        bank, bk = BK.f32(bs[g4]), BK.key(bs[g4])
                        kb.tt("dve", sT[:, 4 * g4:4 * g4 + 4, :], bank.rearrange("p (c n) -> p c n", c=4),
                              maskI.unsqueeze(1).to_broadcast([128, 4, 128]), ALU.mult, [bk, "cqf"], [ksT])
                        BK.put(bs[g4])
                        yield
                    (bo,) = yield from acquire(1)
                    bankO, ko = BK.f32(bo), BK.key(bo)
                    for h in range(8):
                        kb.mm(bankO[:, 64 * h:64 * h + 64], sT[:, h, :], vtok[:, tl, 64 * h:64 * h + 64], True, t == 0,
                              [ksT, f"vtok{par}.{tl}"], [ko])
                        if t > 0:
                            kb.mm(bankO[:, 64 * h:64 * h + 64], qT[:, h // 2, h % 2, tl_], Rbf[:, t % 5, h // 2, :], False, True,
                                  [f"qT{par}.{tl}", f"Rbf.{t % 5}"], [ko])
                    yield

                    def fin(ct, ya, k4):
                        kb.tt("dve", yT[:, ct, tok], ya, gateT[:, ct, tl_], ALU.mult, [k4, f"gateT{par}.{ct}"], [f"yT.{t}.0"])
                    yield from post_gen(slot, bo, 1e-5, 32, 36, fin)

                def gen_chunks(blk):
                    par = blk % 2
                    ktok, vtok = ktok_p[par], vtok_p[par]
                    for tl in range(4):
                        t = blk * 4 + tl
                        if t == NT - 1:
                            continue
                        (b,) = yield from acquire(1)
                        bankK, kk_ = BK.f32(b), BK.key(b)
                        for ct in range(4):
                            kb.mm(bankK[:, ct * 128:(ct + 1) * 128], ktok[:, tl, ct * 128:(ct + 1) * 128], vtok[:, tl, ct * 128:(ct + 1) * 128],
                                  True, True, [f"ktok{par}.{tl}", f"vtok{par}.{tl}"], [kk_])
                        yield
                        for hh in range(2):
                            b0 = 64 * hh
                            kb.tt("dve", R32[b0:b0 + 64, :, :], bankK[b0:b0 + 64, :].rearrange("p (c n) -> p c n", c=4)[:, :, b0:b0 + 64],
                                  R32[b0:b0 + 64, :, :], ALU.add, [kk_, "R32"], ["R32"])
                        BK.put(b)
                        yield
                        kb.tt("dve", R32[:], R32[:], cols[:, 99:103].unsqueeze(2).to_broadcast([128, 4, 64]), ALU.mult, ["R32", "cols"], ["R32"])
                        yield
                        kb.cp("act", Rbf[:, (t + 1) % 5, :, :], R32[:], ["R32"], [f"Rbf.{(t + 1) % 5}"])
                        yield
                    yield from rr([chunk_chain(0, blk, 0), chunk_chain(1, blk, 1)])
                    yield from rr([chunk_chain(0, blk, 2), chunk_chain(1, blk, 3)])

                run(gen_normA(0))
                interleave([gen_proj(0), gen_normA(1)])
                for blk in range(4):
                    if blk == 3 and "C" in stages:
                        Wk = Wv8(1824)
                        kb.dma("pool", Wk[:, :, :], wview(w_in_d)[:, :, 2048:3872], [], ["Wk", "W.0", "W.1", "W.2", "W.3"])
                    gs = [gen_chunks(blk)]
                    if blk + 1 < 4:
                        gs.append(gen_proj(blk + 1))
                    if blk + 2 < 4:
                        gs.append(gen_normA(blk + 2))
                    interleave(gs)
                if dbg:
                    kb.dma("sp", dbg_hT, hT[:], [f"hT.{b}" for b in range(4)], [])
                P.barrier()
                P.flush()

        if "C" in stages:
            stage_C(nc, P, kb, locals())
        else:
            P.op("dve", lambda e: e.memset(hT[:, 0:4, :], 0.0), [f"hT.{b}" for b in range(4)], [f"yT.{t}.1" for t in range(NT)])

        if dbg:
            kb.dma("sp", dbg_yT[:, 0:4, :], yT[:], [f"yT.{t}.0" for t in range(NT)], [])
            kb.dma("sp", dbg_yT[:, 4:8, :], hT[:, 0:4, :], [f"yT.{t}.1" for t in range(NT)], [])

        if "D" in stages:
            stage_D(nc, P, kb, locals())
        P.barrier()
        P.flush()
    return nc


def stage_C(nc, P, kb, L):
    cols, dc, cqf, hT, WA, mhalf, bonesb, BK = (L[k] for k in ("cols", "dc", "cqf", "hT", "WA", "mhalf", "bonesb", "L_BK"))
    wup_d, aup_d, gup_d, w_in_d = L["wup_d"], L["aup_d"], L["gup_d"], L["w_in_d"]
    mask4, maskLs, identf = L["mask4"], L["maskLs"], L["identf"]
    onesf = cqf[:, 896:1024]
    Wk = WA[:, 0:KC * 1824].rearrange("p (k n) -> p k n", k=KC)
    CL = 0.5 * math.exp(-0.5)
    NB = 256
    NBLK = T // NB
    with contextlib.ExitStack() as sc:
        if "B" not in L["stages"]:
            kb.dma("pool", Wk[:, :, :], w_in_d.rearrange("(kc p) n -> p kc n", p=128)[:, :, 2048:3872], [], ["Wk"])
        sb = lambda n, s, d: kb.sb(n, s, d, st=sc)
        wupz = sb("wupz", [128, 512], BF16)
        aupz = sb("aupz", [128, 512], BF16)
        gup1b = sb("gup1b", [128, 512], BF16)
        gup2z = sb("gup2z", [128, 512], BF16)
        for tz in (wupz, aupz, gup2z):
            P.op("dve", lambda e, tz=tz: e.memset(tz[:], 0.0), [], ["wupb", "gupb"])
        kb.dma("pool", wupz[0:64, :], wup_d, [], ["wupb"])
        kb.dma("pool", aupz[64:128, :], aup_d, [], ["wupb"])
        kb.dma("pool", gup1b[:], gup_d[0:128, :], [], ["gupb"])
        kb.dma("pool", gup2z[96:128, :], gup_d[128:160, :], [], ["gupb"])
        RKblk = sb("RKblk", [128, 4, 128], BF16)
        for ct in range(4):
            kb.ts("dve", RKblk[:, ct, :], cqf[:, 768:896], cols[:, 71 + ct:72 + ct], None, ALU.mult, None, ["cqf", "cols"], ["RKblk"])
        pT = sb("pT", [128, 15, NB + 1], F32)
        P.op("dve", lambda e: e.memset(pT[:], 0.0), [], [f"pT.{i}" for i in range(15)])
        carry = sb("carry", [128, 15], F32)
        NTMP = 10
        T_p = [[sb(f"T{c}_{i}", [128, NB], F32) for i in range(NTMP)] for c in range(2)]
        sh_p = [sb(f"shT{i}", [128, NB], F32) for i in range(2)]
        kk2_p = [sb(f"kk2{i}", [128, NB], BF16) for i in range(2)]
        twa = sb("twa", [128, NB], BF16)
        sg1 = sb("sg1", [128, NB], BF16)
        sg2 = sb("sg2", [128, NB], BF16)
        ARt = sb("ARt", [128, 4, 2, 2, 2, 128], BF16)
        P.op("dve", lambda e: e.memset(ARt[:], 0.0), [], [f"ARt.{i}" for i in range(4)])
        KT = sb("KT", [128, 4, NB], BF16)
        BnT = sb("BnT", [128, 4, NB], BF16)
        rkb = sb("rkb", [128, 4, NB], BF16)
        vbf_p = [sb(f"vbf{i}", [128, 4, NB], BF16) for i in range(2)]
        gT = sb("gT", [128, 4, NB], BF16)
        Ktok = sb("Ktok", [128, 2, 512], BF16)
        Btok = sb("Btok", [128, 2, 512], BF16)
        Vtok = sb("Vtok", [128, 2, 512], BF16)
        PCt = sb("PCt", [128, 4, 2], F32)
        MTs = [sb(f"MT{i}", [128, 8, 512], BF16) for i in range(2)]
        mraw_p = [sb(f"mraw{i}", [128, 512], BF16) for i in range(2)]
        mask4b = sb("mask4b", [128, 512], BF16)
        kb.dma("pool", mask4b[:], L["cq_d"][:, 128:640], [], ["mask4b"])
        Xm = [sb(f"Xm{i}", [128, 8, 128], BF16) for i in range(2)]
        Nm = [sb(f"Nm{i}", [128, 8, 128], BF16) for i in range(2)]
        Lm = [sb(f"Lm{i}", [128, 8, 128], BF16) for i in range(2)]
        TTs = [sb(f"TT{i}", [128, 8, 128], BF16) for i in range(2)]
        RHSs = [sb(f"RHSb{i}", [128, 512], BF16) for i in range(2)]
        Ubs = [sb(f"Ub{i}", [128, 512], BF16) for i in range(2)]
        S32 = sb("S32", [128, 4, 64], F32)
        Sbf = sb("Sbf", [128, 4, 64], BF16)
        P.op("dve", lambda e: e.memset(S32[:], 0.0), [], ["S32"])
        y32 = sb("cy32", [128, 512], F32)
        sq = sb("csq", [128, 512], F32)
        ynb = sb("cynb", [128, 512], BF16)
        yaff = sb("cyaff", [128, 4, 128], F32)
        t1b = sb("ct1", [128, 4, 128], F32)
        stt_ = sb("cstat", [128, 64], F32)

        def gen_AB(blk):
            tok0 = blk * NB
            hk = f"hT.{tok0 // 512}"
            for i0 in range(0, 15, 2):
                b = BK.get()
                bank, bk = BK.f32(b), BK.key(b)
                idxs = [i for i in (i0, i0 + 1) if i < 15]
                for j, idx in enumerate(idxs):
                    c0 = idx * 128 if idx < 14 else 1696
                    for kc in range(KC):
                        kb.mm(bank[:, j * NB:(j + 1) * NB], Wk[:, kc, c0:c0 + 128], hT[:, kc, tok0:tok0 + NB],
                              kc == 0, kc == KC - 1, [hk, "Wk"] + (["hTu"] if kc >= 4 else []), [bk])
                yield
                for j, idx in enumerate(idxs):
                    kb.cp("act", pT[:, idx, 1:NB + 1], bank[:, j * NB:(j + 1) * NB], [bk], [f"pT.{idx}"])
                BK.put(b)
                yield
            allp = [f"pT.{i}" for i in range(15)]
            kb.cp("dve", carry[:], pT[:, :, NB], allp, ["carry"])
            yield
            for idx in range(15):
                tmp, ktm = sh_p[idx % 2], f"shT.{idx % 2}"
                kb.act(tmp[:], pT[:, idx, 0:NB], AF.Copy, [f"pT.{idx}", "cols"], [ktm], scale=cols[:, 40 + idx:41 + idx])
                yield
                if 8 <= idx < 12:
                    kb.stt(vbf_p[blk % 2][:, idx - 8, :], pT[:, idx, 1:NB + 1], dc[:, idx:idx + 1], tmp[:], ALU.mult, ALU.add,
                           [f"pT.{idx}", "dc", ktm, "carry"], [f"vbf{blk % 2}.{idx - 8}"])
                else:
                    kb.stt(pT[:, idx, 1:NB + 1], pT[:, idx, 1:NB + 1], dc[:, idx:idx + 1], tmp[:], ALU.mult, ALU.add,
                           [f"pT.{idx}", "dc", ktm, "carry"], [f"pT.{idx}"])
                yield
            kb.cp("dve", pT[:, :, 0], carry[:], ["carry"], allp)
            yield

        ps = lambda idx: pT[:, idx, 1:NB + 1]

        def gen_lora(blk):
            kb.act(twa[0:64, :], pT[0:64, 12, 1:NB + 1], AF.Tanh, ["pT.12"], ["twa"])
            kb.cp("dve", twa[64:128, :], pT[64:128, 12, 1:NB + 1], ["pT.12"], ["twa"])
            yield
            th, kth = sh_p[0], "shT.0"
            kb.act(th[:], ps(13), AF.Tanh, ["pT.13"], [kth], scale=0.5)
            th2, kth2 = sh_p[1], "shT.1"
            kb.act(th2[:], ps(14), AF.Tanh, ["pT.14"], [kth2], scale=0.5)
            yield
            kb.ts("dve", sg1[:], th[:], 0.5, 0.5, ALU.mult, ALU.add, [kth], ["sg1"])
            kb.ts("pool", sg2[:], th2[:], 0.5, 0.5, ALU.mult, ALU.add, [kth2], ["sg2"])
            yield

        def gen_ct(ch, blk, ct):
            Tb = T_p[ch]
            tk = lambda i: f"T{ch}.{i}"
            cs = slice(ct * 128, (ct + 1) * 128)
            r_, k_, v_ = ps(ct), ps(4 + ct), ps(8 + ct)
            kr, kk_, kv = f"pT.{ct}", f"pT.{4 + ct}", f"pT.{8 + ct}"
            bz, bg = BK.get(), BK.get()
            bankZ, kz = BK.f32(bz), BK.key(bz)
            bankG, kg = BK.f32(bg), BK.key(bg)
            kb.mm(bankZ[:, 0:NB], wupz[:, cs], twa[:], True, True, ["wupb", "twa"], [kz])
            kb.mm(bankZ[:, NB:2 * NB], aupz[:, cs], twa[:], True, True, ["wupb", "twa"], [kz])
            kb.mm(bankG[:, 0:NB], gup1b[:, cs], sg1[:], True, False, ["gupb", "sg1"], [kg])
            kb.mm(bankG[:, 0:NB], gup2z[:, cs], sg2[:], False, True, ["gupb", "sg2"], [kg])
            yield
            thw, tha = Tb[0], Tb[1]
            kb.act(thw[:], bankZ[:, 0:NB], AF.Tanh, [kz, "dc"], [tk(0)], scale=0.5, bias=dc[:, 15 + ct:16 + ct])
            kb.act(tha[:], bankZ[:, NB:2 * NB], AF.Tanh, [kz, "dc"], [tk(1)], scale=0.5, bias=dc[:, 19 + ct:20 + ct])
            BK.put(bz)
            kk = Tb[4]
            kb.ts("pool", kk[:], k_, cols[:, 63 + ct:64 + ct], 0.0, ALU.mult, ALU.add, [kk_, "cols"], [tk(4)])
            yield
            kb.cp("act", gT[:, ct, :], bankG[:, 0:NB], [kg], [f"gT.{ct}"])
            BK.put(bg)
            kk2, k_kk2 = kk2_p[ch], f"kk2.{ch}"
            kb.act(kk2[:], kk[:], AF.Square, [tk(4)], [k_kk2])
            ld = Tb[0]
            kb.ts("dve", ld[:], thw[:], -CL, -CL, ALU.mult, ALU.add, [tk(0)], [tk(0)])
            yield
            bn_ = BK.get()
            bankN, kn = BK.f32(bn_), BK.key(bn_)
            kb.mm(bankN[:, 0:NB], bonesb[:], kk2[:], True, True, ["bonesb", k_kk2], [kn])
            cum = Tb[2]
            for c in range(2):
                ld_c, cum_c = ld[:, c * 128:(c + 1) * 128], cum[:, c * 128:(c + 1) * 128]
                P.op("dve", lambda e, cum_c=cum_c, ld_c=ld_c: e.tensor_tensor_scan(out=cum_c, data0=onesf, data1=ld_c, initial=0.0, op0=ALU.mult, op1=ALU.add),
                     [tk(0), "cqf"], [tk(2)])
            an = Tb[1]
            kb.ts("pool", an[:], tha[:], -0.5, -0.5, ALU.mult, ALU.add, [tk(1)], [tk(1)])
            yield
            sn = Tb[5]
            kb.act(sn[:], bankN[:, 0:NB], AF.Sqrt, [kn], [tk(5)])
            BK.put(bn_)
            cumex = Tb[3]
            kb.tt("pool", cumex[:], cum[:], ld[:], ALU.subtract, [tk(2), tk(0)], [tk(3)])
            yield
            ep, en, ex = Tb[6], Tb[7], Tb[8]
            kb.act(ep[:], cum[:], AF.Exp, [tk(2)], [tk(6)])
            kb.act(en[:], cum[:], AF.Exp, [tk(2)], [tk(7)], scale=-1.0)
            kb.ts("dve", sn[:], sn[:], 1e-12, None, ALU.max, None, [tk(5)], [tk(5)])
            tmp2 = Tb[9]
            kb.ts("pool", tmp2[:], an[:], dc[:, 23 + ct:24 + ct], dc[:, 27 + ct:28 + ct], ALU.mult, ALU.add, [tk(1), "dc"], [tk(9)])
            yield
            kb.act(ex[:], cumex[:], AF.Exp, [tk(3)], [tk(8)])
            P.op("dve", lambda e: e.reciprocal(out=sn[:], in_=sn[:]), [tk(5)], [tk(5)])
            k2 = Tb[9]
            kb.tt("pool", k2[:], k_, tmp2[:], ALU.mult, [kk_, tk(9)], [tk(9)])
            yield
            kkn = Tb[4]
            kb.tt("dve", kkn[:], kk[:], sn[:], ALU.mult, [tk(4), tk(5)], [tk(4)])
            c3 = lambda a: a.rearrange("p (c n) -> p c n", c=2)
            kb.cp("pool", PCt[:, ct, :], c3(ep[:])[:, :, 127], [tk(6)], [f"PCt.{ct}"])
            yield
            bn = Tb[5]
            kb.tt("pool", bn[:], kkn[:], an[:], ALU.mult, [tk(4), tk(1), tk(5)], [tk(5)])
            kb.tt("dve", KT[:, ct, :], k2[:], en[:], ALU.mult, [tk(9), tk(7)], [f"KT.{ct}"])
            yield
            for hh in range(2):
                pp = slice(64 * hh, 64 * hh + 64)
                kb.tt("dve", ARt[pp, ct, :, hh, 1, :], c3(r_)[pp], c3(ep[:])[pp], ALU.mult, [kr, tk(6)], [f"ARt.{ct}"])
                kb.tt("dve" if hh == 0 else "pool", ARt[pp, ct, :, hh, 0, :], c3(kkn[:])[pp], c3(ex[:])[pp], ALU.mult, [tk(4), tk(8)], [f"ARt.{ct}"])
                yield
            kb.tt("dve", BnT[:, ct, :], bn[:], en[:], ALU.mult, [tk(5), tk(7)], [f"BnT.{ct}"])
            kb.tt("pool", rkb[:, ct, :], r_, k2[:], ALU.mult, [kr, tk(9)], [f"rkb.{ct}"])
            yield

        def gen_tok(blk):
            for c in range(2):
                cs = slice(c * 128, (c + 1) * 128)
                for src, dst, nm, sk in ((KT, Ktok, "KT", "KT"), (BnT, Btok, "BnT", "BnT"), (vbf_p[blk % 2], Vtok, "vbf", f"vbf{blk % 2}")):
                    b = BK.get()
                    bankT, kt = BK.bf(b), BK.key(b)
                    for ct in range(4):
                        kb.tr(bankT[:, ct * 128:(ct + 1) * 128], src[:, ct, cs], [f"{sk}.{ct}"], [kt])
                    yield
                    kb.cpalt(dst[:, c, :], bankT[:, 0:512], [kt], [f"{nm}tok.{c}"])
                    BK.put(b)
                    yield

        def gen_F1(blk, c):
            cs = slice(c * 128, (c + 1) * 128)
            MT = MTs[c]
            for h in range(8):
                ct = h // 2
                b = BK.get()
                bankM, km = BK.f32(b), BK.key(b)
                rhsAR = ARt[:, ct, c, h % 2, :, :].rearrange("p a b -> p (a b)")
                kb.mm(bankM[:, 0:256], BnT[:, ct, cs], rhsAR, True, True, [f"BnT.{ct}", f"ARt.{ct}"], [km])
                kb.mm(bankM[:, 256:512], KT[:, ct, cs], rhsAR, True, True, [f"KT.{ct}", f"ARt.{ct}"], [km])
                yield
                if True:
                    kb.tt("dve", MT[:, h, :], bankM, mask4, ALU.mult, [km, "cqf"], [f"MT{c}.{h}"])
                    BK.put(b)
                else:
                    mr, kmr = mraw_p[(h // 2) % 2], f"mraw.{(h // 2) % 2}"
                    kb.cp("act", mr[:], bankM, [km], [kmr])
                    BK.put(b)
                    yield
                    kb.tt("pool", MT[:, h, :], mr[:], mask4b[:], ALU.mult, [kmr, "mask4b"], [f"MT{c}.{h}"])
                yield
            for g4 in range(2):
                b = BK.get()
                bankL, kl = BK.f32(b), BK.key(b)
                for hh in range(4):
                    h = 4 * g4 + hh
                    kb.mm(bankL[:, hh * 128:(hh + 1) * 128], ARt[:, h // 2, c, h % 2, 0, :], BnT[:, h // 2, cs], True, True,
                          [f"ARt.{h // 2}", f"BnT.{h // 2}"], [kl])
                yield
                kb.tt("dve", Lm[0][:, 4 * g4:4 * g4 + 4, :], bankL.rearrange("p (c n) -> p c n", c=4),
                      maskLs.unsqueeze(1).to_broadcast([128, 4, 128]), ALU.mult, [kl, "cqf"], [f"Lm0.{g4}"])
                BK.put(b)
                hs = slice(4 * g4, 4 * g4 + 4)
                mtk = [f"MT{c}.{h}" for h in range(4 * g4, 4 * g4 + 4)]
                kb.tt("dve", Xm[1][:, hs, :], MT[:, hs, 0:128], identf.unsqueeze(1).to_broadcast([128, 4, 128]), ALU.add,
                      mtk + ["cqf"], [f"Xm1.{g4}"])
                yield

        def gen_inv_group(c, g4):
            hs = slice(4 * g4, 4 * g4 + 4)
            v4 = lambda bank: bank.rearrange("p (c n) -> p c n", c=4)
            cur = 0
            for lvl in range(7):
                nxt = cur ^ 1
                kN, kL, kX = f"Nm{cur}.{g4}", f"Lm{cur}.{g4}", f"Xm{cur}.{g4}"
                if lvl == 0:
                    Ncur = MTs[c][:, :, 0:128]
                    kNl = [f"MT{c}.{h}" for h in range(4 * g4, 4 * g4 + 4)]
                else:
                    Ncur, kNl = Nm[cur], [kN]
                last = lvl == 6
                Xdst, kXd = (TTs[c], f"TT{c}.{g4}") if last else (Xm[nxt], f"Xm{nxt}.{g4}")
                bx = ba = bb = None
                if lvl >= 1:
                    bx = BK.get()
                    bankX, kbx = BK.f32(bx), BK.key(bx)
                    for hh in range(4):
                        h = 4 * g4 + hh
                        kb.mm(bankX[:, hh * 128:(hh + 1) * 128], Lm[cur][:, h, :], Xm[cur][:, h, :], True, True, [kL, kX], [kbx])
                if not last:
                    ba, bb = BK.get(), BK.get()
                    bankA, kba = BK.f32(ba), BK.key(ba)
                    bankB, kbb = BK.f32(bb), BK.key(bb)
                    for hh in range(4):
                        h = 4 * g4 + hh
                        kb.mm(bankA[:, hh * 128:(hh + 1) * 128], Lm[cur][:, h, :], Ncur[:, h, :], True, True, [kL] + kNl, [kba])
                    for hh in range(4):
                        h = 4 * g4 + hh
                        kb.mm(bankB[:, hh * 128:(hh + 1) * 128], Ncur[:, h, :], Lm[cur][:, h, :], True, True, [kL] + kNl, [kbb])
                yield
                if lvl >= 1:
                    kb.tt("dve", Xdst[:, hs, :], v4(bankX), Xm[cur][:, hs, :], ALU.add, [kbx, kX], [kXd])
                    BK.put(bx)
                else:
                    pass
                if not last:
                    kb.cp("act", Nm[nxt][:, hs, :], v4(bankA), [kba], [f"Nm{nxt}.{g4}"])
                    BK.put(ba)
                    if g4 == 0:
                        kb.cp("act", Lm[nxt][:, hs, :], v4(bankB), [kbb], [f"Lm{nxt}.{g4}"])
                    else:
                        kb.cp("dve", Lm[nxt][:, hs, :], v4(bankB), [kbb], [f"Lm{nxt}.{g4}"])
                    BK.put(bb)
                yield
                cur = nxt

        def gen_inv(blk, c):
            gs = [gen_inv_group(c, 0), gen_inv_group(c, 1)]
            while gs:
                for g in list(gs):
                    try:
                        next(g)
                    except StopIteration:
                        gs.remove(g)
                yield

        def gen_seqpost(blk, c):
            n_ch = blk * 2 + c
            cs = slice(c * 128, (c + 1) * 128)
            tok = slice(blk * NB + c * 128, blk * NB + (c + 1) * 128)
            MT, TT, RHSb, Ub = MTs[c], TTs[c], RHSs[c], Ubs[c]
            kTT = [f"TT{c}.0", f"TT{c}.1"]
            kRH, kUb = f"RHSb{c}", f"Ub{c}"
            br = BK.get()
            bankR, kr_ = BK.f32(br), BK.key(br)
            for h in range(8):
                ct = h // 2
                o = bankR[:, 64 * h:64 * h + 64]
                if n_ch > 0:
                    kb.mm(o, ARt[:, ct, c, h % 2, 0, :], Sbf[:, ct, :], True, False, [f"ARt.{ct}", "Sbf"], [kr_])
                kb.mm(o, MT[:, h, 256:384], Vtok[:, c, 64 * h:64 * h + 64], n_ch == 0, True, [f"MT{c}.{h}", f"vbftok.{c}"], [kr_])
            yield
            kb.cp("act", RHSb[:], bankR, [kr_], [kRH])
            BK.put(br)
            yield
            bu = BK.get()
            bankU, ku = BK.f32(bu), BK.key(bu)
            for h in range(8):
                kb.mm(bankU[:, 64 * h:64 * h + 64], TT[:, h, :], RHSb[:, 64 * h:64 * h + 64], True, True, kTT + [kRH], [ku])
            yield
            kb.cp("dve", Ub[:], bankU, [ku], [kUb])
            BK.put(bu)
            yield
            if n_ch < NT - 1:
                bs_ = BK.get()
                bankS, ks_ = BK.f32(bs_), BK.key(bs_)
                for ct in range(4):
                    o = bankS[:, ct * 128:(ct + 1) * 128]
                    kb.mm(o, Btok[:, c, ct * 128:(ct + 1) * 128], Ub[:, ct * 128:(ct + 1) * 128], True, False, [f"BnTtok.{c}", kUb], [ks_])
                    kb.mm(o, Ktok[:, c, ct * 128:(ct + 1) * 128], Vtok[:, c, ct * 128:(ct + 1) * 128], False, True, [f"KTtok.{c}", f"vbftok.{c}"], [ks_])
            by = BK.get()
            bankY, ky = BK.f32(by), BK.key(by)
            for h in range(8):
                ct = h // 2
                o = bankY[:, 64 * h:64 * h + 64]
                if n_ch > 0:
                    kb.mm(o, ARt[:, ct, c, h % 2, 1, :], Sbf[:, ct, :], True, False, [f"ARt.{ct}", "Sbf"], [ky])
                kb.mm(o, MT[:, h, 128:256], Ub[:, 64 * h:64 * h + 64], n_ch == 0, False, [f"MT{c}.{h}", kUb], [ky])
                kb.mm(o, MT[:, h, 384:512], Vtok[:, c, 64 * h:64 * h + 64], False, True, [f"MT{c}.{h}", f"vbftok.{c}"], [ky])
            yield
            if n_ch < NT - 1:
                for hh in range(2):
                    b0 = 64 * hh
                    kb.tt("dve", S32[b0:b0 + 64, :, :], bankS[b0:b0 + 64, :].rearrange("p (c n) -> p c n", c=4)[:, :, b0:b0 + 64],
                          S32[b0:b0 + 64, :, :], ALU.add, [ks_, "S32"], ["S32"])
                BK.put(bs_)
                kb.tt("dve", S32[:], S32[:], PCt[:, :, c].unsqueeze(2).to_broadcast([128, 4, 64]), ALU.mult,
                      ["S32"] + [f"PCt.{ct}" for ct in range(4)], ["S32"])
                kb.cp("act", Sbf[:], S32[:], ["S32"], ["Sbf"])
                yield
            k1, k2, ks, k3 = "cy32", "csq", "cstat", "cynb"
            kb.cp("act", y32[:], bankY, [ky], [k1])
            BK.put(by)
            yield
            v3 = lambda a: a.rearrange("p (h d) -> p h d", d=64)
            bc = lambda a: a.unsqueeze(2).to_broadcast([128, 8, 64])
            kb.red(stt_[:, 0:8], v3(y32[:]), ALU.add, [k1], [ks])
            kb.act(sq[:], y32[:], AF.Square, [k1], [k2])
            yield
            kb.red(stt_[:, 8:16], v3(sq[:]), ALU.add, [k2], [ks])
            yield
            kb.ts("dve", stt_[:, 16:24], stt_[:, 0:8], 1.0 / 64, None, ALU.mult, None, [ks], [ks])
            yield
            kb.tt("dve", stt_[:, 24:32], stt_[:, 16:24], stt_[:, 16:24], ALU.mult, [ks], [ks])
            kb.ts("dve", stt_[:, 32:40], stt_[:, 8:16], 1.0 / 64, 64e-5, ALU.mult, ALU.add, [ks], [ks])
            yield
            kb.tt("dve", stt_[:, 40:48], stt_[:, 32:40], stt_[:, 24:32], ALU.subtract, [ks], [ks])
            yield
            kb.tt("pool", stt_[:, 48:56], stt_[:, 40:48], mhalf[:, 0:8], ALU.pow, [ks, "mhalf"], [ks])
            yield
            kb.stt(stt_[:, 56:64], stt_[:, 16:24], -1.0, stt_[:, 48:56], ALU.mult, ALU.mult, [ks], [ks])
            kb.tt("dve", v3(y32[:]), v3(y32[:]), bc(stt_[:, 48:56]), ALU.mult, [k1, ks], [k1])
            yield
            kb.tt("dve", v3(ynb[:]), v3(y32[:]), bc(stt_[:, 56:64]), ALU.add, [k1, ks], [k3])
            yield
            b2 = BK.get()
            bankT, kt = BK.bf(b2), BK.key(b2)
            for ct in range(4):
                kb.tr(bankT[:, ct * 128:(ct + 1) * 128], ynb[:, ct * 128:(ct + 1) * 128], [k3], [kt])
            bbo = BK.get()
            bankBo, kbo = BK.f32(bbo), BK.key(bbo)
            for ct in range(4):
                kb.mm(bankBo[:, ct * 128:(ct + 1) * 128], RKblk[:, ct, :], rkb[:, ct, cs], True, True, ["RKblk", f"rkb.{ct}"], [kbo])
            yield
            for ct in range(4):
                kb.act(yaff[:, ct, :], bankT[:, ct * 128:(ct + 1) * 128], AF.Identity, [kt, "cols"], ["cyaff"],
                       scale=cols[:, 75 + ct:76 + ct], bias=cols[:, 79 + ct:80 + ct])
            kb.tt("dve", t1b[:], bankBo.rearrange("p (c n) -> p c n", c=4), vbf_p[blk % 2][:, :, cs], ALU.mult,
                  [kbo] + [f"vbf{blk % 2}.{ct}" for ct in range(4)], ["ct1"])
            BK.put(b2)
            BK.put(bbo)
            yield
            kb.tt("dve", t1b[:], t1b[:], yaff[:], ALU.add, ["ct1", "cyaff"], ["ct1"])
            yield
            kb.tt("dve", hT[:, 0:4, tok], t1b[:], gT[:, :, cs], ALU.mult, ["ct1"] + [f"gT.{ct}" for ct in range(4)],
                  [f"yT.{n_ch}.1", f"hT.{n_ch // 4}"])
            yield

        def gen_CD(blk):
            yield from gen_lora(blk)
            for pair in range(2):
                gs = [gen_ct(0, blk, 2 * pair), gen_ct(1, blk, 2 * pair + 1)]
                while gs:
                    for g in list(gs):
                        try:
                            next(g)
                        except StopIteration:
                            gs.remove(g)
                    yield
            yield from gen_tok(blk)

        def run(g):
            for _ in g:
                pass

        run(gen_AB(0))
        run(gen_CD(0))
        for blk in range(NBLK):
            run(gen_F1(blk, 0))
            interleave([gen_inv(blk, 0), gen_F1(blk, 1)])
            interleave([gen_seqpost(blk, 0), gen_inv(blk, 1)])
            if blk + 1 < NBLK:
                interleave([gen_seqpost(blk, 1), gen_AB(blk + 1)])
                if blk + 1 == NBLK - 1 and "D" in L["stages"]:
                    wv = lambda w: w.rearrange("(kc p) n -> p kc n", p=128)
                    Wout = WA[:, 0:8192].rearrange("p (k n) -> p k n", k=KC)
                    WkvK = WA[:, 8192:16384].rearrange("p (k n) -> p k n", k=KC)
                    WkvV = hT[:, 4:8, :].rearrange("p a (b n) -> p (a b) n", n=1024)
                    kb.dma("pool", Wout, wv(L["w_out_d"]), [], ["WA.0", "Wk"])
                    kb.dma("pool", WkvK, wv(L["wkv_d"])[:, :, 0:1024], [], ["WA.1", "Wk"])
                    kb.dma("pool", WkvV, wv(L["wkv_d"])[:, :, 1024:2048], [], ["WkvV", "hTu"])
                run(gen_CD(blk + 1))
            else:
                run(gen_seqpost(blk, 1))
        P.barrier()
        P.flush()


def stage_D(nc, P, kb, L):
    cols, cqf, hT, yT, WA, mhalf, onesb, ss, BK = (L[k] for k in ("cols", "cqf", "hT", "yT", "WA", "mhalf", "onesb", "ss", "L_BK"))
    x_d, mem_d, gfin_d, out_d = L["x_d"], L["mem_d"], L["gfin_d"], L["out_d"]
    w_out_d, wq_d, wkv_d, wo_d, wupm_d, wdn_d = (L[k] for k in ("w_out_d", "wq_d", "wkv_d", "wo_d", "wupm_d", "wdn_d"))
    wview = lambda w: w.rearrange("(kc p) n -> p kc n", p=128)

    def run(g):
        for _ in g:
            pass

    with contextlib.ExitStack() as sd:
        sb = lambda n, s, d, st=sd: kb.sb(n, s, d, st=st)
        xres = sb("xres", [128, NT, D], F32)
        gfin = sb("gfin", [128, D], F32)
        kTm = sb("kTm", [128, KC, 256], BF16)
        vmem = sb("vmem", [128, 2, D], BF16)
        kmx = sb("kmx", [128, 4], F32)
        hb_p = [sb(f"dhb{i}", [128, D], BF16) for i in range(4)]
        sqj = sb("dsqj", [128, D], BF16)
        W0 = WA[:, 0:8192].rearrange("p (k n) -> p k n", k=KC)
        W1 = WA[:, 8192:16384].rearrange("p (k n) -> p k n", k=KC)
        Wout, WkvK = W0, W1
        WkvV = hT[:, 4:8, :].rearrange("p a (b n) -> p (a b) n", n=1024)
        xv = x_d.rearrange("(t p) d -> p t d", p=128)
        kb.dma("sp", xres[:, 0:4, :], xv[:, 0:4, :], [], [f"xres.{t}" for t in range(0, 4)])
        if "C" not in L["stages"]:
            kb.dma("pool", Wout, wview(w_out_d), [], ["WA.0"])
            kb.dma("pool", WkvK, wview(wkv_d)[:, :, 0:1024], [], ["WA.1"])
            kb.dma("pool", WkvV, wview(wkv_d)[:, :, 1024:2048], [], ["WkvV"])
        kb.dma("sp", gfin[:], gfin_d.partition_broadcast(128), [], ["gfin"])

        def gen_norm(tiles, src, gcol0, dst_fn, dkey, sscol0):
            for tl, t in enumerate(tiles):
                xa, xk = src(t)
                c = sscol0 + t
                xks = xk if isinstance(xk, list) else [xk]
                kb.act(sqj[:], xa, AF.Square, xks, ["sqj", f"ss{c}"], accum=ss[:, c:c + 1])
                yield
                kb.ts("dve", ss[:, c:c + 1], ss[:, c:c + 1], 1.0 / D, 1e-6, ALU.mult, ALU.add, [f"ss{c}"], [f"ss{c}"])
                yield
                kb.tt("pool", ss[:, c:c + 1], ss[:, c:c + 1], mhalf[:, 0:1], ALU.pow, [f"ss{c}", "mhalf"], [f"ss{c}"])
                yield
                kb.act(hb_p[tl][:], xa, AF.Copy, xks + [f"ss{c}"], [f"dhb.{tl}"], scale=ss[:, c:c + 1])
                yield
            n = len(tiles) * 128
            for kc in range(KC):
                b = BK.get()
                bankT, bk = BK.bf(b), BK.key(b)
                for tl in range(len(tiles)):
                    kb.tr(bankT[:, tl * 128:(tl + 1) * 128], hb_p[tl][:, kc * 128:(kc + 1) * 128], [f"dhb.{tl}"], [bk])
                yield
                o = dst_fn(kc)
                dk = dkey if isinstance(dkey, list) else [dkey]
                if kc % 2:
                    kb.act(o, bankT[:, 0:n], AF.Copy, [bk, "cols"], dk, scale=cols[:, gcol0 + kc:gcol0 + kc + 1])
                else:
                    kb.ts("dve", o, bankT[:, 0:n], cols[:, gcol0 + kc:gcol0 + kc + 1], None, ALU.mult, None, [bk, "cols"], dk)
                BK.put(b)
                yield

        oTb_t = sb("oTb", [128, KC * 512], BF16)
        aT_t = sb("aT_all", [128, 8 * 512], BF16)
        if True:
            memx = [oTb_t[:, 2048 * i:2048 * (i + 1)].bitcast(F32) for i in range(2)]
            mxk = [[f"oTb.{k}" for k in range(4 * i, 4 * i + 4)] for i in range(2)]
            memT = aT_t[:, 0:2048].rearrange("p (k n) -> p k n", k=KC)
            k2m = aT_t[:, 2048:4096].rearrange("p (k n) -> p k n", k=KC)
            kmT = [f"aT.{k}" for k in range(4)]
            kk2 = [f"aT.{k}" for k in range(4, 8)]
            for i in range(2):
                kb.dma("sp", memx[i], mem_d[i * 128:(i + 1) * 128, :], [], mxk[i])
            for i in range(1, 4):
                kb.dma("sp", xres[:, 4 * i:4 * i + 4, :], xv[:, 4 * i:4 * i + 4, :], [], [f"xres.{t}" for t in range(4 * i, 4 * i + 4)])

            dstate = {"v_done": False, "t3_done": False}

            def gen_memkv():
                yield from gen_norm([0, 1], lambda t: (memx[t], mxk[t]), 16, lambda kc: memT[:, kc, :], kmT, 16)
                for mt in range(2):
                    for hf in range(2):
                        b = BK.get()
                        bank, bk = BK.f32(b), BK.key(b)
                        for kc in range(KC):
                            kb.mm(bank, memT[:, kc, mt * 128:(mt + 1) * 128], WkvV[:, kc, hf * 512:(hf + 1) * 512],
                                  kc == 0, kc == KC - 1, ["WkvV"] + kmT, [bk])
                        yield
                        kb.cpalt(vmem[:, mt, hf * 512:(hf + 1) * 512], bank, [bk], ["vmem"])
                        BK.put(b)
                        yield
                dstate["v_done"] = True
                for ft in range(KC):
                    b = BK.get()
                    bank, bk = BK.f32(b), BK.key(b)
                    for kc in range(KC):
                        kb.mm(bank[:, 0:256], WkvK[:, kc, ft * 128:(ft + 1) * 128], memT[:, kc, :], kc == 0, kc == KC - 1, ["WA.1"] + kmT, [bk])
                    yield
                    kb.cpalt(kTm[:, ft, :], bank[:, 0:256], [bk], ["kTm"])
                    BK.put(b)
                    yield
                kb.act(k2m, kTm[:], AF.Square, ["kTm"], kk2)
                yield
                for h in range(4):
                    b = BK.get()
                    bank, bk = BK.f32(b), BK.key(b)
                    kb.mm(bank[:, 0:256], onesb[:], k2m[:, 2 * h, :], True, False, ["onesb"] + kk2, [bk])
                    kb.mm(bank[:, 0:256], onesb[:], k2m[:, 2 * h + 1, :], False, True, ["onesb"] + kk2, [bk])
                    yield
                    kb.red(kmx[:, h:h + 1], bank[:, 0:256], ALU.max, [bk], ["kmx"])
                    BK.put(b)
                    yield

            def gen_wout():
                for t in range(NT):
                    tok = slice(t * 128, (t + 1) * 128)
                    for hf in range(2):
                        b = BK.get()
                        bank, bk = BK.f32(b), BK.key(b)
                        for kc in range(KC):
                            ysrc = yT[:, kc, tok] if kc < 4 else hT[:, kc - 4, tok]
                            kb.mm(bank, ysrc, Wout[:, kc, hf * 512:(hf + 1) * 512], kc == 0, kc == KC - 1,
                                  [f"yT.{t}.0", f"yT.{t}.1", "WA.0", f"hT.{t // 4}"], [bk])
                        yield
                        xs = xres[:, t, hf * 512:(hf + 1) * 512]
                        kb.tt("dve", xs, bank, xs, ALU.add, [bk, f"xres.{t}"], [f"xres.{t}"])
                        BK.put(b)
                        yield
                    if t == 3:
                        dstate["t3_done"] = True

            srcx = lambda t: (xres[:, t, :], f"xres.{t}")

            def gen_norm0():
                while not (dstate["v_done"] and dstate["t3_done"]):
                    yield
                yield from gen_norm([0, 1, 2, 3], srcx, 8, lambda kc: hT[:, kc, 0:512], ["hT.0", "WkvV"], 0)

            interleave([gen_memkv(), gen_wout(), gen_norm0()])
            kb.dma("pool", W1, wview(wq_d), [], ["WA.1"])
            kb.dma("pool", W0, wview(wo_d), [], ["WA.0"])

        srcx = lambda t: (xres[:, t, :], f"xres.{t}")
        norm_x = lambda blk: gen_norm(list(range(4 * blk, 4 * blk + 4)), srcx, 8, lambda kc: hT[:, kc, blk * 512:(blk + 1) * 512], [f"hT.{blk}", "WkvV"], 0)
        norm_m = lambda blk: gen_norm(list(range(4 * blk, 4 * blk + 4)), srcx, 24, lambda kc: hT[:, kc, blk * 512:(blk + 1) * 512], f"hT.{blk}", 16)
        with contextlib.ExitStack() as s2:
            qTb = yT[:, 0:2, :].rearrange("p a (b n) -> p (a b) n", n=512)
            PT = yT[:, 2:4, :].rearrange("p a (b n) -> p (a b) n", n=512)
            oTb = oTb_t[:].rearrange("p (k n) -> p k n", k=KC)
            yall = [f"yT.{t}.0" for t in range(NT)]
            q2b_p = [sb(f"q2b{i}", [128, 2, 512], BF16, st=s2) for i in range(4)]
            nb_p = [sb(f"nb{i}", [128, 2], F32, st=s2) for i in range(4)]
            rc_p = [sb(f"rc{i}", [128, 512], F32, st=s2) for i in range(4)]

            def gen_head(sl, h):
                qk = [f"qTb.{2 * h}", f"qTb.{2 * h + 1}"]
                q2b, kq2 = q2b_p[sl], f"q2b.{sl}"
                kb.act(q2b[:], qTb[:, 2 * h:2 * h + 2, :], AF.Square, qk, [kq2])
                yield
                b = BK.get()
                bankQ, kq = BK.f32(b), BK.key(b)
                kb.mm(bankQ, onesb[:], q2b[:, 0, :], True, False, ["onesb", kq2], [kq])
                kb.mm(bankQ, onesb[:], q2b[:, 1, :], False, True, ["onesb", kq2], [kq])
                yield
                nb, kbs = nb_p[sl], f"nb.{sl}"
                kb.red(nb[:, 0:1], bankQ, ALU.max, [kq], [kbs])
                BK.put(b)
                yield
                kb.ts("dve", nb[:, 1:2], nb[:, 0:1], kmx[:, h:h + 1], -1.0 / 32, ALU.add, ALU.mult, [kbs, "kmx"], [kbs])
                yield
                for mt in range(2):
                    b = BK.get()
                    bankS, ksb = BK.f32(b), BK.key(b)
                    ms = slice(mt * 128, (mt + 1) * 128)
                    kb.mm(bankS, kTm[:, 2 * h, ms], qTb[:, 2 * h, :], True, False, ["kTm", qk[0]], [ksb])
                    kb.mm(bankS, kTm[:, 2 * h + 1, ms], qTb[:, 2 * h + 1, :], False, True, ["kTm", qk[1]], [ksb])
                    yield
                    kb.act(PT[:, 2 * h + mt, :], bankS, AF.Exp, [ksb, kbs], [f"PT.{2 * h + mt}"] + (yall if blk == 0 else []), scale=1.0 / 16, bias=nb[:, 1:2])
                    BK.put(b)
                    yield
                pk = [f"PT.{2 * h}", f"PT.{2 * h + 1}"]
                b = BK.get()
                bankRs, krs = BK.f32(b), BK.key(b)
                kb.mm(bankRs, onesb[:], PT[:, 2 * h, :], True, False, ["onesb", pk[0]], [krs])
                kb.mm(bankRs, onesb[:], PT[:, 2 * h + 1, :], False, True, ["onesb", pk[1]], [krs])
                yield
                rc, krc = rc_p[sl], f"rc.{sl}"
                P.op("dve", lambda e: e.reciprocal(out=rc[:], in_=bankRs), [krs], [krc])
                BK.put(b)
                yield
                for dt_ in range(2):
                    ft = 2 * h + dt_
                    b = BK.get()
                    bankO, kob = BK.f32(b), BK.key(b)
                    kb.mm(bankO, vmem[:, 0, ft * 128:(ft + 1) * 128], PT[:, 2 * h, :], True, False, ["vmem", pk[0]], [kob])
                    kb.mm(bankO, vmem[:, 1, ft * 128:(ft + 1) * 128], PT[:, 2 * h + 1, :], False, True, ["vmem", pk[1]], [kob])
                    yield
                    kb.tt("dve", oTb[:, ft, :], bankO, rc[:], ALU.mult, [kob, krc], [f"oTb.{ft}"])
                    BK.put(b)
                    yield

            def gen_attn(blk):
                bs = slice(blk * 512, (blk + 1) * 512)
                for ft in range(KC):
                    b = BK.get()
                    bank, bk = BK.f32(b), BK.key(b)
                    for kc in range(KC):
                        kb.mm(bank, W1[:, kc, ft * 128:(ft + 1) * 128], hT[:, kc, bs], kc == 0, kc == KC - 1, ["WA.1", f"hT.{blk}"], [bk])
                    yield
                    kb.cpalt(qTb[:, ft, :], bank, [bk], [f"qTb.{ft}"] + (yall if blk == 0 else []))
                    BK.put(b)
                    yield
                if blk == 3:
                    mlp_load(0)
                for pair in range(1):
                    gs = [gen_head(h, h) for h in range(4)]
                    while gs:
                        for g in list(gs):
                            try:
                                next(g)
                            except StopIteration:
                                gs.remove(g)
                        yield
                for tl in range(4):
                    t = blk * 4 + tl
                    for hf in range(2):
                        b = BK.get()
                        bank, bk = BK.f32(b), BK.key(b)
                        for kc in range(KC):
                            kb.mm(bank, oTb[:, kc, tl * 128:(tl + 1) * 128], W0[:, kc, hf * 512:(hf + 1) * 512], kc == 0, kc == KC - 1,
                                  [f"oTb.{kc}", "WA.0"], [bk])
                        yield
                        xs = xres[:, t, hf * 512:(hf + 1) * 512]
                        kb.tt("dve", xs, bank, xs, ALU.add, [bk, f"xres.{t}"], [f"xres.{t}"])
                        BK.put(b)
                        yield

            def norm_worker(blk):
                if blk + 1 < 4:
                    yield from norm_x(blk + 1)
                if blk - 1 >= 0:
                    yield from norm_m(blk - 1)

            s3 = s2
            aT_all = aT_t[:].rearrange("p (k n) -> p k n", k=8)
            rl_p = [sb(f"rl{i}", [128, 512], BF16, st=s3) for i in range(2)]
            o_p = [oTb_t[:, 2048 * i:2048 * (i + 1)].bitcast(F32) for i in range(2)]
            upv = wupm_d.rearrange("(kc p) n -> p kc n", p=128)
            dnv = wdn_d.rearrange("(f p) n -> p f n", p=128)
            ov = out_d.rearrange("(t p) d -> p t d", p=128)

            def mlp_w(j):
                s = (j + 1) % 2
                Wu = WA[:, s * 8192:s * 8192 + 4096].rearrange("p (k n) -> p k n", k=KC)
                Wd = WA[:, s * 8192 + 4096:(s + 1) * 8192].rearrange("p (f n) -> p f n", f=4)
                return s, Wu, Wd

            def mlp_load(j):
                s, Wu, Wd = mlp_w(j)
                kb.dma("pool", Wu, upv[:, :, j * 512:(j + 1) * 512], [], [f"WA.{s}"])
                kb.dma("pool", Wd, dnv[:, 4 * j:4 * j + 4, :], [], [f"WA.{s}"])

            for blk in range(4):
                interleave([gen_attn(blk), norm_worker(blk)])
            run(norm_m(3))

            NJ = 8
            ai = 0
            ri = 0
            for j in range(NJ):
                s, Wu, Wd = mlp_w(j)
                if j > 0:
                    mlp_load(j)
                for blk in range(4):
                    bs = slice(blk * 512, (blk + 1) * 512)
                    ab = (ai % 2) * 4
                    ai += 1
                    for fft in range(4):
                        b = BK.get()
                        bank, bk = BK.f32(b), BK.key(b)
                        for kc in range(KC):
                            kb.mm(bank, Wu[:, kc, fft * 128:(fft + 1) * 128], hT[:, kc, bs], kc == 0, kc == KC - 1, [f"WA.{s}", f"hT.{blk}"], [bk])
                        rl, krl = rl_p[ri % 2], f"rl.{ri % 2}"
                        ri += 1
                        kb.act(rl[:], bank, AF.Relu, [bk], [krl])
                        BK.put(b)
                        kb.tt("dve", aT_all[:, ab + fft, :], rl[:], rl[:], ALU.mult, [krl], [f"aT.{ab + fft}"])
                    for tl in range(4):
                        t = blk * 4 + tl
                        for hf in range(2):
                            b = BK.get()
                            bank, bk = BK.f32(b), BK.key(b)
                            for fft in range(4):
                                kb.mm(bank, aT_all[:, ab + fft, tl * 128:(tl + 1) * 128], Wd[:, fft, hf * 512:(hf + 1) * 512], fft == 0, fft == 3,
                                      [f"aT.{ab + fft}", f"WA.{s}"], [bk])
                            xs = xres[:, t, hf * 512:(hf + 1) * 512]
                            kb.tt("dve", xs, bank, xs, ALU.add, [bk, f"xres.{t}"], [f"xres.{t}"])
                            BK.put(b)
                    if j == NJ - 1:
                        for fb in ([blk - 1] if blk > 0 else []) + ([3] if blk == 3 else []):
                            for t in range(4 * fb, 4 * fb + 4):
                                c = 32 + t
                                kb.act(sqj[:], xres[:, t, :], AF.Square, [f"xres.{t}"], ["sqj", f"ss{c}"], accum=ss[:, c:c + 1])
                                kb.ts("dve", ss[:, c:c + 1], ss[:, c:c + 1], 1.0 / D, 1e-6, ALU.mult, ALU.add, [f"ss{c}"], [f"ss{c}"])
                                kb.tt("pool", ss[:, c:c + 1], ss[:, c:c + 1], mhalf[:, 0:1], ALU.pow, [f"ss{c}", "mhalf"], [f"ss{c}"])
                                ot, ko = o_p[t % 2], f"ot.{t % 2}"
                                kb.stt(ot, xres[:, t, :], ss[:, c:c + 1], gfin[:], ALU.mult, ALU.mult, [f"xres.{t}", f"ss{c}", "gfin"],
                                       [ko] + [f"oTb.{k}" for k in range(KC)])
                                kb.dma("sp", ov[:, t, :], ot, [ko], [f"out.{t}"])
            P.barrier()
            P.flush()


def _consts():
    cq = np.zeros((128, NCQ), np.float32)
    i = np.arange(128)
    cq[:, 0:128] = np.eye(128, dtype=np.float32)
    strict = (i[:, None] < i[None, :]).astype(np.float32)
    incl = (i[:, None] <= i[None, :]).astype(np.float32)
    cq[:, 128:256] = strict
    cq[:, 256:384] = incl
    cq[:, 384:512] = strict
    cq[:, 512:640] = incl
    cq[:, 640:768] = (i[:, None] > i[None, :]).astype(np.float32)
    cq[:, 768:896] = ((i[:, None] // 64) == (i[None, :] // 64)).astype(np.float32)
    cq[:, 896:1024] = 1.0
    gam = 1.0 - 2.0 ** (-5.0 - np.arange(8, dtype=np.float64))
    xi = gam[None, :] ** (i[:, None] + 1.0)
    kz = 0.125 * gam[None, :] ** (-(i[:, None] + 1.0))
    gC = np.zeros((128, 4))
    for ct in range(4):
        for p in range(128):
            gC[p, ct] = gam[2 * ct + p // 64] ** 128.0
    invf = (10000.0 ** (-np.arange(32, dtype=np.float32) / 32.0)).astype(np.float32)
    return cq, xi.astype(np.float32), kz.astype(np.float32), gC.astype(np.float32), np.broadcast_to(invf[None], (128, 32))


def _col(v):
    v = np.asarray(v, np.float32).reshape(-1)
    return v.reshape(-1, 128).T


def make_inputs(inp):
    cq, xi, kz, gC, invf = _consts()
    cols = np.zeros((128, NCOLS), np.float32)
    cols[:, 0:8] = _col(inp["norm_mix"][0])
    cols[:, 8:16] = _col(inp["norm_xattn"][0])
    cols[:, 16:24] = _col(inp["norm_mem"][0])
    cols[:, 24:32] = _col(inp["norm_mlp"][0])
    cols[:, 32:36] = _col(inp["ret_gn_w"][0])
    cols[:, 36:40] = _col(inp["ret_gn_b"][0])
    mu = np.asarray(inp["rwkv_mu"][0], np.float32)
    cols[:, 40:54] = _col(mu[:1792])
    cols[:, 54] = mu[1696:1824]
    cols[:, 55:59] = _col(inp["rwkv_w0"][0])
    cols[:, 59:63] = _col(inp["rwkv_a0"][0])
    cols[:, 63:67] = _col(inp["rwkv_k_k"][0])
    cols[:, 67:71] = _col(inp["rwkv_k_a"][0])
    cols[:, 71:75] = _col(inp["rwkv_r_k"][0])
    cols[:, 75:79] = _col(inp["rwkv_gn_w"][0])
    cols[:, 79:83] = _col(inp["rwkv_gn_b"][0])
    cols[:, 83:91] = xi
    cols[:, 91:99] = kz
    cols[:, 99:103] = gC
    cols[:, 103:135] = invf
    shared = {
        "cols": cols, "cq": cq,
        "gfin": np.ascontiguousarray(inp["norm_final"], np.float32),
        "w_in": np.ascontiguousarray(inp["w_in"][0]),
        "w_up": np.ascontiguousarray(inp["rwkv_w_up"][0]),
        "a_up": np.ascontiguousarray(inp["rwkv_a_up"][0]),
        "g_up": np.ascontiguousarray(inp["rwkv_g_up"][0]),
        "w_out": np.ascontiguousarray(inp["w_out"][0]),
        "wq": np.ascontiguousarray(inp["xattn_w_q"][0]),
        "wkv": np.ascontiguousarray(inp["xattn_w_kv"][0]),
        "wo": np.ascontiguousarray(inp["xattn_w_o"][0]),
        "mlp_up": np.ascontiguousarray(inp["mlp_w_up"][0]),
        "mlp_down": np.ascontiguousarray(inp["mlp_w_down"][0]),
    }
    maps = []
    for b in range(8):
        m = dict(shared)
        m["x"] = np.ascontiguousarray(inp["x"][b], np.float32)
        m["mem"] = np.ascontiguousarray(inp["mem"][b], np.float32)
        m["pos"] = np.ascontiguousarray(np.asarray(inp["positions"][b], np.int32).reshape(NT, 128).T)
        maps.append(m)
    return maps


_NC_CACHE = {}


def kernel(**inputs):
    inp = {k: np.asarray(v) for k, v in inputs.items()}
    maps = make_inputs(inp)
    if "nc" not in _NC_CACHE:
        _NC_CACHE["nc"] = build()
    res = run_bass_kernel_spmd(_NC_CACHE["nc"], maps, core_ids=list(range(8)))
    return np.stack([np.asarray(r["out"], np.float32) for r in res.results], axis=0)
```

```python
import contextlib
import math
import numpy as np
import concourse.bass as bass
import concourse.mybir as mybir
from concourse.bass_utils import run_bass_kernel_spmd

F32 = mybir.dt.float32
BF16 = mybir.dt.bfloat16
I32 = mybir.dt.int32
AF = mybir.ActivationFunctionType
ALU = mybir.AluOpType
AX = mybir.AxisListType

T = 2048
D = 1024
NT = 16
KC = 8
NCOLS = 135
NCQ = 1024
CUT = 99
COMPUTE = ("pe", "act", "dve", "pool")
ISSUERS = ("pe", "act", "dve", "pool", "sp")


class Prog:
    def __init__(self, nc, st, n_dma=12, self_sync=True):
        self.nc = nc
        self.n_dma = n_dma
        self.self_sync = self_sync
        self.chans = list(COMPUTE) + [f"d{i}" for i in range(n_dma)]
        self.sems = {c: st.enter_context(nc.semaphore(f"s_{c}")) for c in self.chans}
        self.streams = {e: [] for e in ISSUERS}
        self.count = {c: 0 for c in self.chans}
        self.clock = {e: {} for e in ISSUERS}
        self.snap = {}
        self.wr = {}
        self.rd = {}
        self.rr = 0
        self.nops = 0
        self.nwaits = 0

    @staticmethod
    def _need(needs, d):
        for c, n in d.items():
            if needs.get(c, 0) < n:
                needs[c] = n

    def op(self, eng, fn, reads=(), writes=(), dma=False):
        needs = {}
        for k in reads:
            self._need(needs, self.wr.get(k, {}))
        for k in writes:
            self._need(needs, self.wr.get(k, {}))
            self._need(needs, self.rd.get(k, {}))
        if dma:
            chan = f"d{self.rr % self.n_dma}"
            self.rr += 1
            if self.count[chan]:
                self._need(needs, {chan: self.count[chan]})
        else:
            chan = eng
        clk = self.clock[eng]
        waits = []
        for c, n in needs.items():
            if c == eng and (eng == "pe" or not self.self_sync):
                continue
            if clk.get(c, 0) < n:
                waits.append((c, n))
                for c2, n2 in self.snap[(c, n)].items():
                    if clk.get(c2, 0) < n2:
                        clk[c2] = n2
        self.nwaits += len(waits)
        self.nops += 1
        n_new = self.count[chan] + 1
        self.count[chan] = n_new
        s = dict(clk)
        s[chan] = n_new
        self.snap[(chan, n_new)] = s
        if eng == "pe" and chan == "pe":
            clk["pe"] = n_new
        wset = set(writes)
        for k in wset:
            self.wr[k] = {chan: n_new}
            self.rd[k] = {}
        for k in reads:
            if k in wset:
                continue
            self.rd.setdefault(k, {})[chan] = n_new
        self.streams[eng].append((waits, fn, chan))

    def barrier(self):
        allk = {c: n for c, n in self.count.items() if n}
        for e in ISSUERS:
            clk = self.clock[e]
            waits = []
            for c, n in allk.items():
                if c == e and e == "pe":
                    continue
                if clk.get(c, 0) < n:
                    waits.append((c, n))
            if waits:
                self.streams[e].append((waits, None, None))
        for e in ISSUERS:
            for c, n in allk.items():
                if self.clock[e].get(c, 0) < n:
                    self.clock[e][c] = n

    def flush(self):
        nc = self.nc
        streams = self.streams
        if not any(streams[e] for e in ISSUERS):
            return
        self.streams = {e: [] for e in ISSUERS}
        sems = self.sems

        def val(c, n):
            return n * 16 if c.startswith("d") else n

        def run(engname):
            def body(e):
                for waits, fn, chan in streams[engname]:
                    for c, n in waits:
                        e.wait_ge(sems[c], val(c, n))
                    if fn is not None:
                        fn(e).then_inc(sems[chan], 16 if chan.startswith("d") else 1)
            return body

        with nc.Block() as block:
            block.tensor(run("pe"))
            block.scalar(run("act"))
            block.vector(run("dve"))
            block.gpsimd(run("pool"))
            block.sync(run("sp"))


class Ring:
    def __init__(self, kb, name, n, shape, dtype, st=None, psum=False):
        st = st or kb.st
        alloc = kb.nc.psum_tensor if psum else kb.nc.sbuf_tensor
        self.name = name
        self.t = [st.enter_context(alloc(f"rg_{name}{i}", shape, dtype)) for i in range(n)]
        self.i = 0

    def next(self):
        i = self.i % len(self.t)
        self.i += 1
        return self.t[i], f"{self.name}.{i}"


class ViewRing:
    def __init__(self, aps, keys):
        self.t, self.k, self.i = aps, keys, 0

    def next(self):
        i = self.i % len(self.t)
        self.i += 1
        return self.t[i], self.k[i]


class Banks:
    def __init__(self, pp):
        import collections
        self.pp = pp
        self.free = collections.deque(range(8))

    def get(self):
        return self.free.popleft()

    def put(self, i):
        self.free.append(i)

    def f32(self, i):
        return self.pp[:, i, :]

    def bf(self, i):
        return self.pp[:, i, :].bitcast(BF16)

    @staticmethod
    def key(i):
        return f"ps{i}"


def interleave(gens):
    gens = list(gens)
    while gens:
        for g in list(gens):
            try:
                next(g)
            except StopIteration:
                gens.remove(g)


class KB:
    def __init__(self, nc, P, st):
        self.nc, self.P, self.st = nc, P, st
        self.flip = 0

    def sb(self, name, shape, dtype, st=None):
        return (st or self.st).enter_context(self.nc.sbuf_tensor("sb_" + name, shape, dtype))

    def mm(self, out, lhsT, rhs, start, stop, r, w):
        self.P.op("pe", lambda e: e.matmul(out, lhsT=lhsT, rhs=rhs, start=start, stop=stop), r, w)

    def tr(self, out, in_, r, w):
        idb = self.identb[:]
        self.P.op("pe", lambda e: e.transpose(out=out, in_=in_, identity=idb), list(r) + ["identb"], w)

    def act(self, out, in_, func, r, w, scale=1.0, bias=0.0, accum=None):
        kw = {}
        if accum is not None:
            kw["accum_out"] = accum
        self.P.op("act", lambda e: e.activation(out=out, in_=in_, func=func, bias=bias, scale=scale, **kw), r, w)

    def tt(self, eng, out, in0, in1, op, r, w):
        self.P.op(eng, lambda e: e.tensor_tensor(out=out, in0=in0, in1=in1, op=op), r, w)

    def ts(self, eng, out, in0, s1, s2, op0, op1, r, w):
        if s2 is None:
            self.P.op(eng, lambda e: e.tensor_scalar(out=out, in0=in0, scalar1=s1, scalar2=None, op0=op0), r, w)
        else:
            self.P.op(eng, lambda e: e.tensor_scalar(out=out, in0=in0, scalar1=s1, scalar2=s2, op0=op0, op1=op1), r, w)

    def stt(self, out, in0, sc, in1, op0, op1, r, w):
        self.P.op("dve", lambda e: e.scalar_tensor_tensor(out=out, in0=in0, scalar=sc, in1=in1, op0=op0, op1=op1), r, w)

    def cp(self, eng, out, in_, r, w):
        if eng == "act":
            self.P.op("act", lambda e: e.activation(out=out, in_=in_, func=AF.Copy), r, w)
        else:
            self.P.op(eng, lambda e: e.tensor_copy(out=out, in_=in_), r, w)

    def cpalt(self, out, in_, r, w):
        self.flip ^= 1
        self.cp("act" if self.flip else "dve", out, in_, r, w)

    def red(self, out, in_, op, r, w):
        self.P.op("dve", lambda e: e.tensor_reduce(out=out, in_=in_, axis=AX.X, op=op), r, w)

    def dma(self, eng, out, in_, r, w):
        self.P.op(eng, lambda e: e.dma_start(out=out, in_=in_), r, w, dma=True)


def build(stages="ABCD", dbg=False):
    nc = bass.Bass("TRN2", target_bir_lowering=False)

    def dram(n, s, d, kind="ExternalInput"):
        return nc.dram_tensor(n, s, d, kind=kind).ap()

    x_d = dram("x", [T, D], F32)
    mem_d = dram("mem", [256, D], F32)
    pos_d = dram("pos", [128, NT], I32)
    cols_d = dram("cols", [128, NCOLS], F32)
    cq_d = dram("cq", [128, NCQ], F32)
    gfin_d = dram("gfin", [D], F32)
    w_in_d = dram("w_in", [D, 3872], F32)
    wup_d = dram("w_up", [64, 512], F32)
    aup_d = dram("a_up", [64, 512], F32)
    gup_d = dram("g_up", [160, 512], F32)
    w_out_d = dram("w_out", [D, D], F32)
    wq_d = dram("wq", [D, D], F32)
    wkv_d = dram("wkv", [D, 2 * D], F32)
    wo_d = dram("wo", [D, D], F32)
    wupm_d = dram("mlp_up", [D, 4 * D], F32)
    wdn_d = dram("mlp_down", [4 * D, D], F32)
    out_d = dram("out", [T, D], F32, kind="ExternalOutput")
    if dbg:
        dbg_yT = dram("dbg_yT", [128, KC, T], BF16, kind="ExternalOutput")
        dbg_hT = dram("dbg_hT", [128, KC, T], BF16, kind="ExternalOutput")

    def wview(w):
        return w.rearrange("(kc p) n -> p kc n", p=128)

    with contextlib.ExitStack() as st:
        P = Prog(nc, st)
        kb = KB(nc, P, st)
        cols = kb.sb("cols", [128, NCOLS], F32)
        dc = kb.sb("dcols", [128, 40], F32)
        cqf = kb.sb("cqf", [128, NCQ], F32)
        identb = kb.sb("identb", [128, 128], BF16)
        bonesb = kb.sb("bonesb", [128, 128], BF16)
        onesb = kb.sb("onesb", [128, 128], BF16)
        mhalf = kb.sb("mhalf", [128, 256], F32)
        hT = kb.sb("hT", [128, KC, T], BF16)
        yT = kb.sb("yT", [128, 4, T], BF16)
        WA = kb.sb("WA", [128, 16384], BF16)
        ss = kb.sb("ss", [128, 3 * NT], F32)
        kb.identb = identb
        pp = st.enter_context(nc.psum_tensor("pp", [128, 8, 512], F32))
        L_BK = Banks(pp)
        psA = ViewRing([pp[:, i, :] for i in range(6)], [f"ps{i}" for i in range(6)])
        psT = ViewRing([pp[:, i, :].bitcast(BF16) for i in (6, 7)], ["ps6", "ps7"])
        mask4 = cqf[:, 128:640]
        maskI = cqf[:, 256:384]
        maskLs = cqf[:, 640:768]
        identf = cqf[:, 0:128]

        kb.dma("sp", cols[:], cols_d, [], ["cols"])
        kb.dma("sp", cqf[:], cq_d, [], ["cqf"])
        kb.dma("pool", identb[:], cq_d[:, 0:128], [], ["identb"])
        kb.dma("pool", bonesb[:], cq_d[:, 768:896], [], ["bonesb"])
        kb.dma("pool", onesb[:], cq_d[:, 896:1024], [], ["onesb"])
        P.op("dve", lambda e: e.memset(mhalf[:], -0.5), [], ["mhalf"])
        kb.ts("dve", dc[:, 0:15], cols[:, 40:55], -1.0, 1.0, ALU.mult, ALU.add, ["cols"], ["dc"])
        kb.ts("dve", dc[:, 15:19], cols[:, 55:59], 0.5, None, ALU.mult, None, ["cols"], ["dc"])
        kb.ts("dve", dc[:, 19:23], cols[:, 59:63], 0.5, None, ALU.mult, None, ["cols"], ["dc"])
        kb.ts("dve", dc[:, 23:27], cols[:, 67:71], -1.0, None, ALU.mult, None, ["cols"], ["dc"])
        kb.ts("dve", dc[:, 27:31], cols[:, 67:71], -1.0, 1.0, ALU.mult, ALU.add, ["cols"], ["dc"])

        def norm_T(src, nblk, tpb, gcol0, dst, dstkey, sscol0, hb, sqj):
            for blk in range(nblk):
                hbs = []
                for tl in range(tpb):
                    t = blk * tpb + tl
                    xa, xk = src(t)
                    c = sscol0 + t
                    kb.act(sqj[:], xa, AF.Square, [xk], ["sqj", f"ss{c}"], accum=ss[:, c:c + 1])
                    kb.ts("dve", ss[:, c:c + 1], ss[:, c:c + 1], 1.0 / D, 1e-6, ALU.mult, ALU.add, [f"ss{c}"], [f"ss{c}"])
                    kb.tt("pool", ss[:, c:c + 1], ss[:, c:c + 1], mhalf[:, 0:1], ALU.pow, [f"ss{c}", "mhalf"], [f"ss{c}"])
                    h, hk = hb.next()
                    kb.act(h[:], xa, AF.Copy, [xk, f"ss{c}"], [hk], scale=ss[:, c:c + 1])
                    hbs.append((h, hk))
                for kc in range(KC):
                    bank, bk = psT.next()
                    for tl, (h, hk) in enumerate(hbs):
                        kb.tr(bank[:, tl * 128:(tl + 1) * 128], h[:, kc * 128:(kc + 1) * 128], [hk], [bk])
                    n = tpb * 128
                    o = dst[:, kc, blk * n:(blk + 1) * n]
                    kb.flip ^= 1
                    if kb.flip:
                        kb.act(o, bank[:, 0:n], AF.Copy, [bk, "cols"], [dstkey(blk)], scale=cols[:, gcol0 + kc:gcol0 + kc + 1])
                    else:
                        kb.ts("dve", o, bank[:, 0:n], cols[:, gcol0 + kc:gcol0 + kc + 1], None, ALU.mult, None, [bk, "cols"], [dstkey(blk)])

        def load_w(dst, src, key, eng="pool"):
            kb.dma(eng, dst, src, [], [key])

        Wv8 = lambda ncol: WA[:, 0:KC * ncol].rearrange("p (k n) -> p k n", k=KC)

        Wr = Wv8(2048)
        BK = L_BK

        def acquire(n=1):
            spins = 0
            while len(BK.free) < n:
                spins += 1
                assert spins < 100000, "PSUM bank pool deadlock"
                yield
            return [BK.get() for _ in range(n)]

        def rr(gens):
            gens = list(gens)
            while gens:
                for g in list(gens):
                    try:
                        next(g)
                    except StopIteration:
                        gens.remove(g)
                yield

        def run(g):
            for _ in g:
                pass

        if "B" not in stages:
            with contextlib.ExitStack() as sa:
                xring = Ring(kb, "xr", 3, [128, D], F32, st=sa)
                hb = Ring(kb, "hb", 8, [128, D], BF16, st=sa)
                sqj = kb.sb("sqj", [128, D], BF16, st=sa)

                def srcx(t):
                    xa, xk = xring.next()
                    kb.dma("sp", xa[:], x_d[t * 128:(t + 1) * 128, :], [], [xk])
                    return xa[:], xk
                norm_T(srcx, 4, 4, 0, hT, lambda b: f"hT.{b}", 0, hb, sqj)
                P.barrier()
                P.flush()
            P.op("dve", lambda e: e.memset(yT[:, 0:4, :], 0.0), [], [f"yT.{t}.0" for t in range(NT)])
        else:
            for gi in range(4):
                load_w(Wr[:, :, gi * 512:(gi + 1) * 512], wview(w_in_d)[:, :, gi * 512:(gi + 1) * 512], f"W.{gi}")
            with contextlib.ExitStack() as sbk:
                cos_t = kb.sb("cos_t", [128, NT, 32], F32, st=sbk)
                sin_t = kb.sb("sin_t", [128, NT, 32], F32, st=sbk)
                srope = sbk
                if True:
                    posi = kb.sb("posi", [128, NT], I32, st=srope)
                    posf = kb.sb("posf", [128, NT], F32, st=srope)
                    ang = kb.sb("ang", [128, NT, 32], F32, st=srope)
                    ra = kb.sb("ra", [128, NT, 32], F32, st=srope)
                    rb_ = kb.sb("rb_", [128, NT, 32], F32, st=srope)
                    ri = kb.sb("ri", [128, NT, 32], I32, st=srope)
                    kb.dma("sp", posi[:], pos_d, [], ["posi"])
                    kb.cp("dve", posf[:], posi[:], ["posi"], ["posf"])
                    kb.tt("dve", ang[:], posf[:].unsqueeze(2).to_broadcast([128, NT, 32]),
                          cols[:, 103:135].unsqueeze(1).to_broadcast([128, NT, 32]), ALU.mult, ["posf", "cols"], ["ang"])
                    C1 = 6.28125
                    C2 = 2.0 * math.pi - C1
                    for tab, shift, nm in ((sin_t, 0.0, "sin"), (cos_t, 0.5 * math.pi, "cos")):
                        kb.ts("dve", ra[:], ang[:], shift, 1.0 / (2.0 * math.pi), ALU.add, ALU.mult, ["ang"], ["ra"])
                        kb.cp("dve", ri[:], ra[:], ["ra"], ["ri"])
                        kb.cp("dve", rb_[:], ri[:], ["ri"], ["rb"])
                        kb.ts("dve", ra[:], ang[:], shift, None, ALU.add, None, ["ang"], ["ra"])
                        kb.stt(ra[:], rb_[:], -C1, ra[:], ALU.mult, ALU.add, ["rb", "ra"], ["ra"])
                        kb.stt(ra[:], rb_[:], -C2, ra[:], ALU.mult, ALU.add, ["rb", "ra"], ["ra"])
                        kb.ts("dve", rb_[:], ra[:], math.pi, -2.0 * math.pi, ALU.is_gt, ALU.mult, ["ra"], ["rb"])
                        kb.tt("dve", ra[:], ra[:], rb_[:], ALU.add, ["ra", "rb"], ["ra"])
                        kb.ts("dve", rb_[:], ra[:], -math.pi, 2.0 * math.pi, ALU.is_lt, ALU.mult, ["ra"], ["rb"])
                        kb.tt("dve", ra[:], ra[:], rb_[:], ALU.add, ["ra", "rb"], ["ra"])
                        kb.act(tab[:], ra[:], AF.Sin, ["ra"], [nm])

                sb_ = lambda n, shp, d: kb.sb(n, shp, d, st=sbk)
                W = 2
                xr_p = [sb_(f"xr{i}", [128, D], F32) for i in range(2)]
                hb_p = [sb_(f"hb{i}", [128, D], BF16) for i in range(4)]
                sqj = sb_("sqj", [128, D], BF16)
                qT_p = [sb_(f"qT{i}", [128, 4, 2, 512], BF16) for i in range(2)]
                for i in range(2):
                    P.op("dve", lambda e, i=i: e.memset(qT_p[i][:], 0.0), [], [f"qT{i}.{j}" for j in range(4)])
                kT_p = [sb_(f"kT{i}", [128, 4, 512], BF16) for i in range(2)]
                ktok_p = [sb_(f"ktok{i}", [128, 4, 512], BF16) for i in range(2)]
                vtok_p = [sb_(f"vtok{i}", [128, 4, 512], BF16) for i in range(2)]
                gateT_p = [sb_(f"gateT{i}", [128, 4, 512], BF16) for i in range(2)]
                xs_p = [sb_(f"xs{i}", [128, 512], F32) for i in range(W)]
                A_p = [sb_(f"rA{i}", [128, 512], F32) for i in range(W)]
                B_p = [sb_(f"rB{i}", [128, 512], F32) for i in range(W)]
                qtok_p = [sb_(f"qtok{i}", [128, 512], BF16) for i in range(W)]
                th_p = [sb_(f"thr{i}", [128, 512], F32) for i in range(2)]
                sT_p = [sb_(f"sT{i}", [128, 8, 128], BF16) for i in range(W)]
                y32_p = [sb_(f"y32{i}", [128, 512], F32) for i in range(W)]
                sq_p = [sb_(f"sqr{i}", [128, 512], F32) for i in range(W)]
                ynb_p = [sb_(f"ynb{i}", [128, 512], BF16) for i in range(W)]
                yaff_p = [sb_(f"yaff{i}", [128, 2, 128], F32) for i in range(W)]
                stat_p = [sb_(f"stat{i}", [128, 64], F32) for i in range(W)]
                R32 = sb_("R32", [128, 4, 64], F32)
                Rbf = sb_("Rbf", [128, 5, 4, 64], BF16)
                P.op("dve", lambda e: e.memset(R32[:], 0.0), [], ["R32"])

                def gen_normA(blk):
                    for tl in range(4):
                        t = blk * 4 + tl
                        xa, xk = xr_p[t % 2], f"xr.{t % 2}"
                        kb.dma("sp", xa[:], x_d[t * 128:(t + 1) * 128, :], [], [xk])
                        kb.act(sqj[:], xa[:], AF.Square, [xk], ["sqj", f"ss{t}"], accum=ss[:, t:t + 1])
                        yield
                        kb.ts("dve", ss[:, t:t + 1], ss[:, t:t + 1], 1.0 / D, 1e-6, ALU.mult, ALU.add, [f"ss{t}"], [f"ss{t}"])
                        yield
                        kb.tt("pool", ss[:, t:t + 1], ss[:, t:t + 1], mhalf[:, 0:1], ALU.pow, [f"ss{t}", "mhalf"], [f"ss{t}"])
                        yield
                        kb.act(hb_p[tl][:], xa[:], AF.Copy, [xk, f"ss{t}"], [f"hb.{tl}"], scale=ss[:, t:t + 1])
                        yield
                    for kc in range(KC):
                        (b,) = yield from acquire(1)
                        bankT, bk = BK.bf(b), BK.key(b)
                        for tl in range(4):
                            kb.tr(bankT[:, tl * 128:(tl + 1) * 128], hb_p[tl][:, kc * 128:(kc + 1) * 128], [f"hb.{tl}"], [bk])
                        yield
                        o = hT[:, kc, blk * 512:(blk + 1) * 512]
                        if kc % 2:
                            kb.act(o, bankT[:, 0:512], AF.Copy, [bk, "cols"], [f"hT.{blk}"], scale=cols[:, kc:kc + 1])
                        else:
                            kb.ts("dve", o, bankT[:, 0:512], cols[:, kc:kc + 1], None, ALU.mult, None, [bk, "cols"], [f"hT.{blk}"])
                        BK.put(b)
                        yield

                def post_gen(slot, b, eps, wcol0, bcol0, finish):
                    bank, bk = BK.f32(b), BK.key(b)
                    y32, k1 = y32_p[slot], f"y32.{slot}"
                    kb.cp("act", y32[:], bank, [bk], [k1])
                    BK.put(b)
                    yield
                    stt_, ks = stat_p[slot], f"stat.{slot}"
                    v3 = lambda a: a.rearrange("p (h d) -> p h d", d=64)
                    bc = lambda a: a.unsqueeze(2).to_broadcast([128, 8, 64])
                    kb.red(stt_[:, 0:8], v3(y32[:]), ALU.add, [k1], [ks])
                    sq, k2 = sq_p[slot], f"sq.{slot}"
                    kb.act(sq[:], y32[:], AF.Square, [k1], [k2])
                    yield
                    kb.red(stt_[:, 8:16], v3(sq[:]), ALU.add, [k2], [ks])
                    yield
                    kb.tt("dve", stt_[:, 24:32], stt_[:, 0:8], stt_[:, 0:8], ALU.mult, [ks], [ks])
                    kb.ts("dve", stt_[:, 32:40], stt_[:, 8:16], 1.0 / 64, eps, ALU.mult, ALU.add, [ks], [ks])
                    yield
                    kb.stt(stt_[:, 40:48], stt_[:, 24:32], -1.0 / 4096, stt_[:, 32:40], ALU.mult, ALU.add, [ks], [ks])
                    yield
                    kb.tt("pool", stt_[:, 48:56], stt_[:, 40:48], mhalf[:, 0:8], ALU.pow, [ks, "mhalf"], [ks])
                    yield
                    kb.stt(stt_[:, 56:64], stt_[:, 0:8], -1.0 / 64, stt_[:, 48:56], ALU.mult, ALU.mult, [ks], [ks])
                    kb.tt("dve", v3(y32[:]), v3(y32[:]), bc(stt_[:, 48:56]), ALU.mult, [k1, ks], [k1])
                    yield
                    ynb, k3 = ynb_p[slot], f"ynb.{slot}"
                    kb.tt("dve", v3(ynb[:]), v3(y32[:]), bc(stt_[:, 56:64]), ALU.add, [k1, ks], [k3])
                    yield
                    (b2,) = yield from acquire(1)
                    bankT, kt = BK.bf(b2), BK.key(b2)
                    for ct in range(4):
                        kb.tr(bankT[:, ct * 128:(ct + 1) * 128], ynb[:, ct * 128:(ct + 1) * 128], [k3], [kt])
                    yield
                    for ct in range(4):
                        ya, k4 = yaff_p[slot][:, ct % 2, :], f"yaff.{slot}.{ct % 2}"
                        kb.act(ya, bankT[:, ct * 128:(ct + 1) * 128], AF.Identity, [kt, "cols"], [k4],
                               scale=cols[:, wcol0 + ct:wcol0 + ct + 1], bias=cols[:, bcol0 + ct:bcol0 + ct + 1])
                        finish(ct, ya, k4)
                        if ct == 3:
                            BK.put(b2)
                        yield

                def qk_chain(slot, blk, tl, wi):
                    par = blk % 2
                    t = blk * 4 + tl
                    tok = slice(t * 128, (t + 1) * 128)
                    hk = f"hT.{blk}"
                    tabcol = 83 if wi == 0 else 91
                    (b,) = yield from acquire(1)
                    bank, bk = BK.f32(b), BK.key(b)
                    for kc in range(KC):
                        kb.mm(bank, hT[:, kc, tok], Wr[:, kc, wi * 512:(wi + 1) * 512], kc == 0, kc == KC - 1, [hk, f"W.{wi}"], [bk])
                    yield
                    xs, kx = xs_p[slot], f"xs.{slot}"
                    kb.cp("act", xs[:], bank, [bk], [kx])
                    BK.put(b)
                    yield
                    A, ka = A_p[slot], f"rA.{slot}"
                    B, kbk = B_p[slot], f"rB.{slot}"
                    v4 = lambda a: a.rearrange("p (h two f) -> p h two f", two=2, f=32)
                    cs4 = cos_t[:, t, :].unsqueeze(1).unsqueeze(1).to_broadcast([128, 8, 2, 32])
                    sn3 = sin_t[:, t, :].unsqueeze(1).to_broadcast([128, 8, 32])
                    kb.tt("dve", v4(A[:]), v4(xs[:]), cs4, ALU.mult, [kx, "cos"], [ka])
                    kb.tt("pool", v4(B[:])[:, :, 0, :], v4(xs[:])[:, :, 1, :], sn3, ALU.mult, [kx, "sin"], [kbk])
                    kb.tt("pool", v4(B[:])[:, :, 1, :], v4(xs[:])[:, :, 0, :], sn3, ALU.mult, [kx, "sin"], [kbk])
                    yield
                    kb.tt("dve", v4(A[:])[:, :, 0, :], v4(A[:])[:, :, 0, :], v4(B[:])[:, :, 0, :], ALU.subtract, [ka, kbk], [ka])
                    kb.tt("dve", v4(A[:])[:, :, 1, :], v4(A[:])[:, :, 1, :], v4(B[:])[:, :, 1, :], ALU.add, [ka, kbk], [ka])
                    yield
                    scl = cols[:, tabcol:tabcol + 8].unsqueeze(2).to_broadcast([128, 8, 64])
                    A3 = A[:].rearrange("p (h d) -> p h d", d=64)
                    if wi == 0:
                        qtv, kq = qtok_p[slot][:], f"qtok.{slot}"
                    else:
                        qtv, kq = ktok_p[par][:, tl, :], f"ktok{par}.{tl}"
                    kb.tt("dve", qtv.rearrange("p (h d) -> p h d", d=64), A3, scl, ALU.mult, [ka, "cols"], [kq])
                    yield
                    (b2,) = yield from acquire(1)
                    bankT, kt = BK.bf(b2), BK.key(b2)
                    for ct in range(4):
                        kb.tr(bankT[:, ct * 128:(ct + 1) * 128], qtv[:, ct * 128:(ct + 1) * 128], [kq], [kt])
                    yield
                    bv = bankT[:, 0:512].rearrange("p (c n) -> p c n", c=4)
                    if wi == 0:
                        kb.cp("act", qT_p[par][0:64, :, 0, tl * 128:(tl + 1) * 128], bv[0:64], [kt], [f"qT{par}.{tl}"])
                        kb.cp("dve", qT_p[par][64:128, :, 1, tl * 128:(tl + 1) * 128], bv[64:128], [kt], [f"qT{par}.{tl}"])
                    else:
                        kb.cpalt(kT_p[par][:, :, tl * 128:(tl + 1) * 128], bv, [kt], [f"kT{par}.{tl}"])
                    BK.put(b2)
                    yield

                def v_chain(blk, tl):
                    par = blk % 2
                    t = blk * 4 + tl
                    tok = slice(t * 128, (t + 1) * 128)
                    (b,) = yield from acquire(1)
                    bank, bk = BK.f32(b), BK.key(b)
                    for kc in range(KC):
                        kb.mm(bank, hT[:, kc, tok], Wr[:, kc, 1024:1536], kc == 0, kc == KC - 1, [f"hT.{blk}", "W.2"], [bk])
                    yield
                    kb.cpalt(vtok_p[par][:, tl, :], bank, [bk], [f"vtok{par}.{tl}"])
                    BK.put(b)
                    yield

                def g_chain(blk, ct):
                    par = blk % 2
                    (b,) = yield from acquire(1)
                    bank, bk = BK.f32(b), BK.key(b)
                    for kc in range(KC):
                        kb.mm(bank, Wr[:, kc, 1536 + ct * 128:1536 + (ct + 1) * 128], hT[:, kc, blk * 512:(blk + 1) * 512],
                              kc == 0, kc == KC - 1, [f"hT.{blk}", "W.3"], [bk])
                    yield
                    th, kth = th_p[ct % 2], f"thr.{ct % 2}"
                    kb.act(th[:], bank, AF.Tanh, [bk], [kth], scale=0.5)
                    yield
                    kb.ts("pool", th[:], th[:], 0.5, 0.5, ALU.mult, ALU.add, [kth], [kth])
                    yield
                    kb.tt("dve", gateT_p[par][:, ct, :], th[:], bank, ALU.mult, [kth, bk], [f"gateT{par}.{ct}"])
                    BK.put(b)
                    yield

                def gen_proj(blk):
                    for k in range(4):
                        yield from rr([qk_chain((2 * k) % W, blk, k, 0), qk_chain((2 * k + 1) % W, blk, k, 1), v_chain(blk, k), g_chain(blk, k)])

                def chunk_chain(slot, blk, tl):
                    par = blk % 2
                    qT, kT_, vtok, gateT = qT_p[par], kT_p[par], vtok_p[par], gateT_p[par]
                    t = blk * 4 + tl
                    tok = slice(t * 128, (t + 1) * 128)
                    tl_ = slice(tl * 128, (tl + 1) * 128)
                    sT, ksT = sT_p[slot], f"sT.{slot}"
                    bs = yield from acquire(2)
                    for g4 in range(2):
                        bank, bk = BK.f32(bs[g4]), BK.key(bs[g4])
                        for hh in range(4):
                            h = 4 * g4 + hh
                            kb.mm(bank[:, hh * 128:(hh + 1) * 128], kT_[:, h // 2, tl_], qT[:, h // 2, h % 2, tl_], True, True,
                                  [f"kT{par}.{tl}", f"qT{par}.{tl}"], [bk])
                    yield
                    for g4 in range(2):
                        bank, bk = BK.f32(bs[g4]), BK.key(bs[g4])
                        kb.tt("dve", sT[:, 4 * g4:4 * g4 + 4, :], bank.rearrange("p (c n) -> p c n", c=4),
                              maskI.unsqueeze(1).to_broadcast([128, 4, 128]), ALU.mult, [bk, "cqf"], [ksT])
                        BK.put(bs[g4])
                        yield
                    (bo,) = yield from acquire(1)
                    bankO, ko = BK.f32(bo), BK.key(bo)
                    for h in range(8):
                        kb.mm(bankO[:, 64 * h:64 * h + 64], sT[:, h, :], vtok[:, tl, 64 * h:64 * h + 64], True, t == 0,
                              [ksT, f"vtok{par}.{tl}"], [ko])
                        if t > 0:
                            kb.mm(bankO[:, 64 * h:64 * h + 64], qT[:, h // 2, h % 2, tl_], Rbf[:, t % 5, h // 2, :], False, True,
                                  [f"qT{par}.{tl}", f"Rbf.{t % 5}"], [ko])
                    yield

                    def fin(ct, ya, k4):
                        kb.tt("dve", yT[:, ct, tok], ya, gateT[:, ct, tl_], ALU.mult, [k4, f"gateT{par}.{ct}"], [f"yT.{t}.0"])
                    yield from post_gen(slot, bo, 1e-5, 32, 36, fin)

                def gen_chunks(blk):
                    par = blk % 2
                    ktok, vtok = ktok_p[par], vtok_p[par]
                    for tl in range(4):
                        t = blk * 4 + tl
                        if t == NT - 1:
                            continue
                        (b,) = yield from acquire(1)
                        bankK, kk_ = BK.f32(b), BK.key(b)
                        for ct in range(4):
                            kb.mm(bankK[:, ct * 128:(ct + 1) * 128], ktok[:, tl, ct * 128:(ct + 1) * 128], vtok[:, tl, ct * 128:(ct + 1) * 128],
                                  True, True, [f"ktok{par}.{tl}", f"vtok{par}.{tl}"], [kk_])
                        yield
                        for hh in range(2):
                            b0 = 64 * hh
                            kb.tt("dve", R32[b0:b0 + 64, :, :], bankK[b0:b0 + 64, :].rearrange("p (c n) -> p c n", c=4)[:, :, b0:b0 + 64],
                                  R32[b0:b0 + 64, :, :], ALU.add, [kk_, "R32"], ["R32"])
                        BK.put(b)
                        yield
                        kb.tt("dve", R32[:], R32[:], cols[:, 99:103].unsqueeze(2).to_broadcast([128, 4, 64]), ALU.mult, ["R32", "cols"], ["R32"])
                        yield
                        kb.cp("act", Rbf[:, (t + 1) % 5, :, :], R32[:], ["R32"], [f"Rbf.{(t + 1) % 5}"])
                        yield
                    yield from rr([chunk_chain(0, blk, 0), chunk_chain(1, blk, 1)])
                    yield from rr([chunk_chain(0, blk, 2), chunk_chain(1, blk, 3)])

                run(gen_normA(0))
                interleave([gen_proj(0), gen_normA(1)])
                for blk in range(4):
                    if blk == 3 and "C" in stages:
                        Wk = Wv8(1824)
                        kb.dma("pool", Wk[:, :, :], wview(w_in_d)[:, :, 2048:3872], [], ["Wk", "W.0", "W.1", "W.2", "W.3"])
                    gs = [gen_chunks(blk)]
                    if blk + 1 < 4:
                        gs.append(gen_proj(blk + 1))
                    if blk + 2 < 4:
                        gs.append(gen_normA(blk + 2))
                    interleave(gs)
                if dbg:
                    kb.dma("sp", dbg_hT, hT[:], [f"hT.{b}" for b in range(4)], [])
                P.barrier()
                P.flush()

        if "C" in stages:
            stage_C(nc, P, kb, locals())
        else:
            P.op("dve", lambda e: e.memset(hT[:, 0:4, :], 0.0), [f"hT.{b}" for b in range(4)], [f"yT.{t}.1" for t in range(NT)])

        if dbg:
            kb.dma("sp", dbg_yT[:, 0:4, :], yT[:], [f"yT.{t}.0" for t in range(NT)], [])
            kb.dma("sp", dbg_yT[:, 4:8, :], hT[:, 0:4, :], [f"yT.{t}.1" for t in range(NT)], [])

        if "D" in stages:
            stage_D(nc, P, kb, locals())
        P.barrier()
        P.flush()
    return nc


def stage_C(nc, P, kb, L):
    cols, dc, cqf, hT, WA, mhalf, bonesb, BK = (L[k] for k in ("cols", "dc", "cqf", "hT", "WA", "mhalf", "bonesb", "L_BK"))
    wup_d, aup_d, gup_d, w_in_d = L["wup_d"], L["aup_d"], L["gup_d"], L["w_in_d"]
    mask4, maskLs, identf = L["mask4"], L["maskLs"], L["identf"]
    onesf = cqf[:, 896:1024]
    Wk = WA[:, 0:KC * 1824].rearrange("p (k n) -> p k n", k=KC)
    CL = 0.5 * math.exp(-0.5)
    NB = 256
    NBLK = T // NB
    with contextlib.ExitStack() as sc:
        if "B" not in L["stages"]:
            kb.dma("pool", Wk[:, :, :], w_in_d.rearrange("(kc p) n -> p kc n", p=128)[:, :, 2048:3872], [], ["Wk"])
        sb = lambda n, s, d: kb.sb(n, s, d, st=sc)
        wupz = sb("wupz", [128, 512], BF16)
        aupz = sb("aupz", [128, 512], BF16)
        gup1b = sb("gup1b", [128, 512], BF16)
        gup2z = sb("gup2z", [128, 512], BF16)
        for tz in (wupz, aupz, gup2z):
            P.op("dve", lambda e, tz=tz: e.memset(tz[:], 0.0), [], ["wupb", "gupb"])
        kb.dma("pool", wupz[0:64, :], wup_d, [], ["wupb"])
        kb.dma("pool", aupz[64:128, :], aup_d, [], ["wupb"])
        kb.dma("pool", gup1b[:], gup_d[0:128, :], [], ["gupb"])
        kb.dma("pool", gup2z[96:128, :], gup_d[128:160, :], [], ["gupb"])
        RKblk = sb("RKblk", [128, 4, 128], BF16)
        for ct in range(4):
            kb.ts("dve", RKblk[:, ct, :], cqf[:, 768:896], cols[:, 71 + ct:72 + ct], None, ALU.mult, None, ["cqf", "cols"], ["RKblk"])
        pT = sb("pT", [128, 15, NB + 1], F32)
        P.op("dve", lambda e: e.memset(pT[:], 0.0), [], [f"pT.{i}" for i in range(15)])
        carry = sb("carry", [128, 15], F32)
        NTMP = 10
        T_p = [[sb(f"T{c}_{i}", [128, NB], F32) for i in range(NTMP)] for c in range(2)]
        sh_p = [sb(f"shT{i}", [128, NB], F32) for i in range(2)]
        kk2_p = [sb(f"kk2{i}", [128, NB], BF16) for i in range(2)]
        twa = sb("twa", [128, NB], BF16)
        sg1 = sb("sg1", [128, NB], BF16)
        sg2 = sb("sg2", [128, NB], BF16)
        ARt = sb("ARt", [128, 4, 2, 2, 2, 128], BF16)
        P.op("dve", lambda e: e.memset(ARt[:], 0.0), [], [f"ARt.{i}" for i in range(4)])
        KT = sb("KT", [128, 4, NB], BF16)
        BnT = sb("BnT", [128, 4, NB], BF16)
        rkb = sb("rkb", [128, 4, NB], BF16)
        vbf_p = [sb(f"vbf{i}", [128, 4, NB], BF16) for i in range(2)]
        gT = sb("gT", [128, 4, NB], BF16)
        Ktok = sb("Ktok", [128, 2, 512], BF16)
        Btok = sb("Btok", [128, 2, 512], BF16)
        Vtok = sb("Vtok", [128, 2, 512], BF16)
        PCt = sb("PCt", [128, 4, 2], F32)
        MTs = [sb(f"MT{i}", [128, 8, 512], BF16) for i in range(2)]
        mraw_p = [sb(f"mraw{i}", [128, 512], BF16) for i in range(2)]
        mask4b = sb("mask4b", [128, 512], BF16)
        kb.dma("pool", mask4b[:], L["cq_d"][:, 128:640], [], ["mask4b"])
        Xm = [sb(f"Xm{i}", [128, 8, 128], BF16) for i in range(2)]
        Nm = [sb(f"Nm{i}", [128, 8, 128], BF16) for i in range(2)]
        Lm = [sb(f"Lm{i}", [128, 8, 128], BF16) for i in range(2)]
        TTs = [sb(f"TT{i}", [128, 8, 128], BF16) for i in range(2)]
        RHSs = [sb(f"RHSb{i}", [128, 512], BF16) for i in range(2)]
        Ubs = [sb(f"Ub{i}", [128, 512], BF16) for i in range(2)]
        S32 = sb("S32", [128, 4, 64], F32)
        Sbf = sb("Sbf", [128, 4, 64], BF16)
        P.op("dve", lambda e: e.memset(S32[:], 0.0), [], ["S32"])
        y32 = sb("cy32", [128, 512], F32)
        sq = sb("csq", [128, 512], F32)
        ynb = sb("cynb", [128, 512], BF16)
        yaff = sb("cyaff", [128, 4, 128], F32)
        t1b = sb("ct1", [128, 4, 128], F32)
        stt_ = sb("cstat", [128, 64], F32)

        def gen_AB(blk):
            tok0 = blk * NB
            hk = f"hT.{tok0 // 512}"
            for i0 in range(0, 15, 2):
                b = BK.get()
                bank, bk = BK.f32(b), BK.key(b)
                idxs = [i for i in (i0, i0 + 1) if i < 15]
                for j, idx in enumerate(idxs):
                    c0 = idx * 128 if idx < 14 else 1696
                    for kc in range(KC):
                        kb.mm(bank[:, j * NB:(j + 1) * NB], Wk[:, kc, c0:c0 + 128], hT[:, kc, tok0:tok0 + NB],
                              kc == 0, kc == KC - 1, [hk, "Wk"] + (["hTu"] if kc >= 4 else []), [bk])
                yield
                for j, idx in enumerate(idxs):
                    kb.cp("act", pT[:, idx, 1:NB + 1], bank[:, j * NB:(j + 1) * NB], [bk], [f"pT.{idx}"])
                BK.put(b)
                yield
            allp = [f"pT.{i}" for i in range(15)]
            kb.cp("dve", carry[:], pT[:, :, NB], allp, ["carry"])
            yield
            for idx in range(15):
                tmp, ktm = sh_p[idx % 2], f"shT.{idx % 2}"
                kb.act(tmp[:], pT[:, idx, 0:NB], AF.Copy, [f"pT.{idx}", "cols"], [ktm], scale=cols[:, 40 + idx:41 + idx])
                yield
                if 8 <= idx < 12:
                    kb.stt(vbf_p[blk % 2][:, idx - 8, :], pT[:, idx, 1:NB + 1], dc[:, idx:idx + 1], tmp[:], ALU.mult, ALU.add,
                           [f"pT.{idx}", "dc", ktm, "carry"], [f"vbf{blk % 2}.{idx - 8}"])
                else:
                    kb.stt(pT[:, idx, 1:NB + 1], pT[:, idx, 1:NB + 1], dc[:, idx:idx + 1], tmp[:], ALU.mult, ALU.add,
                           [f"pT.{idx}", "dc", ktm, "carry"], [f"pT.{idx}"])
                yield
            kb.cp("dve", pT[:, :, 0], carry[:], ["carry"], allp)
            yield

        ps = lambda idx: pT[:, idx, 1:NB + 1]

        def gen_lora(blk):
            kb.act(twa[0:64, :], pT[0:64, 12, 1:NB + 1], AF.Tanh, ["pT.12"], ["twa"])
            kb.cp("dve", twa[64:128, :], pT[64:128, 12, 1:NB + 1], ["pT.12"], ["twa"])
            yield
            th, kth = sh_p[0], "shT.0"
            kb.act(th[:], ps(13), AF.Tanh, ["pT.13"], [kth], scale=0.5)
            th2, kth2 = sh_p[1], "shT.1"
            kb.act(th2[:], ps(14), AF.Tanh, ["pT.14"], [kth2], scale=0.5)
            yield
            kb.ts("dve", sg1[:], th[:], 0.5, 0.5, ALU.mult, ALU.add, [kth], ["sg1"])
            kb.ts("pool", sg2[:], th2[:], 0.5, 0.5, ALU.mult, ALU.add, [kth2], ["sg2"])
            yield

        def gen_ct(ch, blk, ct):
            Tb = T_p[ch]
            tk = lambda i: f"T{ch}.{i}"
            cs = slice(ct * 128, (ct + 1) * 128)
            r_, k_, v_ = ps(ct), ps(4 + ct), ps(8 + ct)
            kr, kk_, kv = f"pT.{ct}", f"pT.{4 + ct}", f"pT.{8 + ct}"
            bz, bg = BK.get(), BK.get()
            bankZ, kz = BK.f32(bz), BK.key(bz)
            bankG, kg = BK.f32(bg), BK.key(bg)
            kb.mm(bankZ[:, 0:NB], wupz[:, cs], twa[:], True, True, ["wupb", "twa"], [kz])
            kb.mm(bankZ[:, NB:2 * NB], aupz[:, cs], twa[:], True, True, ["wupb", "twa"], [kz])
            kb.mm(bankG[:, 0:NB], gup1b[:, cs], sg1[:], True, False, ["gupb", "sg1"], [kg])
            kb.mm(bankG[:, 0:NB], gup2z[:, cs], sg2[:], False, True, ["gupb", "sg2"], [kg])
            yield
            thw, tha = Tb[0], Tb[1]
            kb.act(thw[:], bankZ[:, 0:NB], AF.Tanh, [kz, "dc"], [tk(0)], scale=0.5, bias=dc[:, 15 + ct:16 + ct])
            kb.act(tha[:], bankZ[:, NB:2 * NB], AF.Tanh, [kz, "dc"], [tk(1)], scale=0.5, bias=dc[:, 19 + ct:20 + ct])
            BK.put(bz)
            kk = Tb[4]
            kb.ts("pool", kk[:], k_, cols[:, 63 + ct:64 + ct], 0.0, ALU.mult, ALU.add, [kk_, "cols"], [tk(4)])
            yield
            kb.cp("act", gT[:, ct, :], bankG[:, 0:NB], [kg], [f"gT.{ct}"])
            BK.put(bg)
            kk2, k_kk2 = kk2_p[ch], f"kk2.{ch}"
            kb.act(kk2[:], kk[:], AF.Square, [tk(4)], [k_kk2])
            ld = Tb[0]
            kb.ts("dve", ld[:], thw[:], -CL, -CL, ALU.mult, ALU.add, [tk(0)], [tk(0)])
            yield
            bn_ = BK.get()
            bankN, kn = BK.f32(bn_), BK.key(bn_)
            kb.mm(bankN[:, 0:NB], bonesb[:], kk2[:], True, True, ["bonesb", k_kk2], [kn])
            cum = Tb[2]
            for c in range(2):
                ld_c, cum_c = ld[:, c * 128:(c + 1) * 128], cum[:, c * 128:(c + 1) * 128]
                P.op("dve", lambda e, cum_c=cum_c, ld_c=ld_c: e.tensor_tensor_scan(out=cum_c, data0=onesf, data1=ld_c, initial=0.0, op0=ALU.mult, op1=ALU.add),
                     [tk(0), "cqf"], [tk(2)])
            an = Tb[1]
            kb.ts("pool", an[:], tha[:], -0.5, -0.5, ALU.mult, ALU.add, [tk(1)], [tk(1)])
            yield
            sn = Tb[5]
            kb.act(sn[:], bankN[:, 0:NB], AF.Sqrt, [kn], [tk(5)])
            BK.put(bn_)
            cumex = Tb[3]
            kb.tt("pool", cumex[:], cum[:], ld[:], ALU.subtract, [tk(2), tk(0)], [tk(3)])
            yield
            ep, en, ex = Tb[6], Tb[7], Tb[8]
            kb.act(ep[:], cum[:], AF.Exp, [tk(2)], [tk(6)])
            kb.act(en[:], cum[:], AF.Exp, [tk(2)], [tk(7)], scale=-1.0)
            kb.ts("dve", sn[:], sn[:], 1e-12, None, ALU.max, None, [tk(5)], [tk(5)])
            tmp2 = Tb[9]
            kb.ts("pool", tmp2[:], an[:], dc[:, 23 + ct:24 + ct], dc[:, 27 + ct:28 + ct], ALU.mult, ALU.add, [tk(1), "dc"], [tk(9)])
            yield
            kb.act(ex[:], cumex[:], AF.Exp, [tk(3)], [tk(8)])
            P.op("dve", lambda e: e.reciprocal(out=sn[:], in_=sn[:]), [tk(5)], [tk(5)])
            k2 = Tb[9]
            kb.tt("pool", k2[:], k_, tmp2[:], ALU.mult, [kk_, tk(9)], [tk(9)])
            yield
            kkn = Tb[4]
            kb.tt("dve", kkn[:], kk[:], sn[:], ALU.mult, [tk(4), tk(5)], [tk(4)])
            c3 = lambda a: a.rearrange("p (c n) -> p c n", c=2)
            kb.cp("pool", PCt[:, ct, :], c3(ep[:])[:, :, 127], [tk(6)], [f"PCt.{ct}"])
            yield
            bn = Tb[5]
            kb.tt("pool", bn[:], kkn[:], an[:], ALU.mult, [tk(4), tk(1), tk(5)], [tk(5)])
            kb.tt("dve", KT[:, ct, :], k2[:], en[:], ALU.mult, [tk(9), tk(7)], [f"KT.{ct}"])
            yield
            for hh in range(2):
                pp = slice(64 * hh, 64 * hh + 64)
                kb.tt("dve", ARt[pp, ct, :, hh, 1, :], c3(r_)[pp], c3(ep[:])[pp], ALU.mult, [kr, tk(6)], [f"ARt.{ct}"])
                kb.tt("dve" if hh == 0 else "pool", ARt[pp, ct, :, hh, 0, :], c3(kkn[:])[pp], c3(ex[:])[pp], ALU.mult, [tk(4), tk(8)], [f"ARt.{ct}"])
                yield
            kb.tt("dve", BnT[:, ct, :], bn[:], en[:], ALU.mult, [tk(5), tk(7)], [f"BnT.{ct}"])
            kb.tt("pool", rkb[:, ct, :], r_, k2[:], ALU.mult, [kr, tk(9)], [f"rkb.{ct}"])
            yield

        def gen_tok(blk):
            for c in range(2):
                cs = slice(c * 128, (c + 1) * 128)
                for src, dst, nm, sk in ((KT, Ktok, "KT", "KT"), (BnT, Btok, "BnT", "BnT"), (vbf_p[blk % 2], Vtok, "vbf", f"vbf{blk % 2}")):
                    b = BK.get()
                    bankT, kt = BK.bf(b), BK.key(b)
                    for ct in range(4):
                        kb.tr(bankT[:, ct * 128:(ct + 1) * 128], src[:, ct, cs], [f"{sk}.{ct}"], [kt])
                    yield
                    kb.cpalt(dst[:, c, :], bankT[:, 0:512], [kt], [f"{nm}tok.{c}"])
                    BK.put(b)
                    yield

        def gen_F1(blk, c):
            cs = slice(c * 128, (c + 1) * 128)
            MT = MTs[c]
            for h in range(8):
                ct = h // 2
                b = BK.get()
                bankM, km = BK.f32(b), BK.key(b)
                rhsAR = ARt[:, ct, c, h % 2, :, :].rearrange("p a b -> p (a b)")
                kb.mm(bankM[:, 0:256], BnT[:, ct, cs], rhsAR, True, True, [f"BnT.{ct}", f"ARt.{ct}"], [km])
                kb.mm(bankM[:, 256:512], KT[:, ct, cs], rhsAR, True, True, [f"KT.{ct}", f"ARt.{ct}"], [km])
                yield
                if True:
                    kb.tt("dve", MT[:, h, :], bankM, mask4, ALU.mult, [km, "cqf"], [f"MT{c}.{h}"])
                    BK.put(b)
                else:
                    mr, kmr = mraw_p[(h // 2) % 2], f"mraw.{(h // 2) % 2}"
                    kb.cp("act", mr[:], bankM, [km], [kmr])
                    BK.put(b)
                    yield
                    kb.tt("pool", MT[:, h, :], mr[:], mask4b[:], ALU.mult, [kmr, "mask4b"], [f"MT{c}.{h}"])
                yield
            for g4 in range(2):
                b = BK.get()
                bankL, kl = BK.f32(b), BK.key(b)
                for hh in range(4):
                    h = 4 * g4 + hh
                    kb.mm(bankL[:, hh * 128:(hh + 1) * 128], ARt[:, h // 2, c, h % 2, 0, :], BnT[:, h // 2, cs], True, True,
                          [f"ARt.{h // 2}", f"BnT.{h // 2}"], [kl])
                yield
                kb.tt("dve", Lm[0][:, 4 * g4:4 * g4 + 4, :], bankL.rearrange("p (c n) -> p c n", c=4),
                      maskLs.unsqueeze(1).to_broadcast([128, 4, 128]), ALU.mult, [kl, "cqf"], [f"Lm0.{g4}"])
                BK.put(b)
                hs = slice(4 * g4, 4 * g4 + 4)
                mtk = [f"MT{c}.{h}" for h in range(4 * g4, 4 * g4 + 4)]
                kb.tt("dve", Xm[1][:, hs, :], MT[:, hs, 0:128], identf.unsqueeze(1).to_broadcast([128, 4, 128]), ALU.add,
                      mtk + ["cqf"], [f"Xm1.{g4}"])
                yield

        def gen_inv_group(c, g4):
            hs = slice(4 * g4, 4 * g4 + 4)
            v4 = lambda bank: bank.rearrange("p (c n) -> p c n", c=4)
            cur = 0
            for lvl in range(7):
                nxt = cur ^ 1
                kN, kL, kX = f"Nm{cur}.{g4}", f"Lm{cur}.{g4}", f"Xm{cur}.{g4}"
                if lvl == 0:
                    Ncur = MTs[c][:, :, 0:128]
                    kNl = [f"MT{c}.{h}" for h in range(4 * g4, 4 * g4 + 4)]
                else:
                    Ncur, kNl = Nm[cur], [kN]
                last = lvl == 6
                Xdst, kXd = (TTs[c], f"TT{c}.{g4}") if last else (Xm[nxt], f"Xm{nxt}.{g4}")
                bx = ba = bb = None
                if lvl >= 1:
                    bx = BK.get()
                    bankX, kbx = BK.f32(bx), BK.key(bx)
                    for hh in range(4):
                        h = 4 * g4 + hh
                        kb.mm(bankX[:, hh * 128:(hh + 1) * 128], Lm[cur][:, h, :], Xm[cur][:, h, :], True, True, [kL, kX], [kbx])
                if not last:
                    ba, bb = BK.get(), BK.get()
                    bankA, kba = BK.f32(ba), BK.key(ba)
                    bankB, kbb = BK.f32(bb), BK.key(bb)
                    for hh in range(4):
                        h = 4 * g4 + hh
                        kb.mm(bankA[:, hh * 128:(hh + 1) * 128], Lm[cur][:, h, :], Ncur[:, h, :], True, True, [kL] + kNl, [kba])
                    for hh in range(4):
                        h = 4 * g4 + hh
                        kb.mm(bankB[:, hh * 128:(hh + 1) * 128], Ncur[:, h, :], Lm[cur][:, h, :], True, True, [kL] + kNl, [kbb])
                yield
                if lvl >= 1:
                    kb.tt("dve", Xdst[:, hs, :], v4(bankX), Xm[cur][:, hs, :], ALU.add, [kbx, kX], [kXd])
                    BK.put(bx)
                else:
                    pass
                if not last:
                    kb.cp("act", Nm[nxt][:, hs, :], v4(bankA), [kba], [f"Nm{nxt}.{g4}"])
                    BK.put(ba)
                    if g4 == 0:
                        kb.cp("act", Lm[nxt][:, hs, :], v4(bankB), [kbb], [f"Lm{nxt}.{g4}"])
                    else:
                        kb.cp("dve", Lm[nxt][:, hs, :], v4(bankB), [kbb], [f"Lm{nxt}.{g4}"])
                    BK.put(bb)
                yield
                cur = nxt

        def gen_inv(blk, c):
            gs = [gen_inv_group(c, 0), gen_inv_group(c, 1)]
            while gs:
                for g in list(gs):
                    try:
                        next(g)
                    except StopIteration:
                        gs.remove(g)
                yield

        def gen_seqpost(blk, c):
            n_ch = blk * 2 + c
            cs = slice(c * 128, (c + 1) * 128)
            tok = slice(blk * NB + c * 128, blk * NB + (c + 1) * 128)
            MT, TT, RHSb, Ub = MTs[c], TTs[c], RHSs[c], Ubs[c]
            kTT = [f"TT{c}.0", f"TT{c}.1"]
            kRH, kUb = f"RHSb{c}", f"Ub{c}"
            br = BK.get()
            bankR, kr_ = BK.f32(br), BK.key(br)
            for h in range(8):
                ct = h // 2
                o = bankR[:, 64 * h:64 * h + 64]
                if n_ch > 0:
                    kb.mm(o, ARt[:, ct, c, h % 2, 0, :], Sbf[:, ct, :], True, False, [f"ARt.{ct}", "Sbf"], [kr_])
                kb.mm(o, MT[:, h, 256:384], Vtok[:, c, 64 * h:64 * h + 64], n_ch == 0, True, [f"MT{c}.{h}", f"vbftok.{c}"], [kr_])
            yield
            kb.cp("act", RHSb[:], bankR, [kr_], [kRH])
            BK.put(br)
            yield
            bu = BK.get()
            bankU, ku = BK.f32(bu), BK.key(bu)
            for h in range(8):
                kb.mm(bankU[:, 64 * h:64 * h + 64], TT[:, h, :], RHSb[:, 64 * h:64 * h + 64], True, True, kTT + [kRH], [ku])
            yield
            kb.cp("dve", Ub[:], bankU, [ku], [kUb])
            BK.put(bu)
            yield
            if n_ch < NT - 1:
                bs_ = BK.get()
                bankS, ks_ = BK.f32(bs_), BK.key(bs_)
                for ct in range(4):
                    o = bankS[:, ct * 128:(ct + 1) * 128]
                    kb.mm(o, Btok[:, c, ct * 128:(ct + 1) * 128], Ub[:, ct * 128:(ct + 1) * 128], True, False, [f"BnTtok.{c}", kUb], [ks_])
                    kb.mm(o, Ktok[:, c, ct * 128:(ct + 1) * 128], Vtok[:, c, ct * 128:(ct + 1) * 128], False, True, [f"KTtok.{c}", f"vbftok.{c}"], [ks_])
            by = BK.get()
            bankY, ky = BK.f32(by), BK.key(by)
            for h in range(8):
                ct = h // 2
                o = bankY[:, 64 * h:64 * h + 64]
                if n_ch > 0:
                    kb.mm(o, ARt[:, ct, c, h % 2, 1, :], Sbf[:, ct, :], True, False, [f"ARt.{ct}", "Sbf"], [ky])
                kb.mm(o, MT[:, h, 128:256], Ub[:, 64 * h:64 * h + 64], n_ch == 0, False, [f"MT{c}.{h}", kUb], [ky])
                kb.mm(o, MT[:, h, 384:512], Vtok[:, c, 64 * h:64 * h + 64], False, True, [f"MT{c}.{h}", f"vbftok.{c}"], [ky])
            yield
            if n_ch < NT - 1:
                for hh in range(2):
                    b0 = 64 * hh
                    kb.tt("dve", S32[b0:b0 + 64, :, :], bankS[b0:b0 + 64, :].rearrange("p (c n) -> p c n", c=4)[:, :, b0:b0 + 64],
                          S32[b0:b0 + 64, :, :], ALU.add, [ks_, "S32"], ["S32"])
                BK.put(bs_)
                kb.tt("dve", S32[:], S32[:], PCt[:, :, c].unsqueeze(2).to_broadcast([128, 4, 64]), ALU.mult,
                      ["S32"] + [f"PCt.{ct}" for ct in range(4)], ["S32"])
                kb.cp("act", Sbf[:], S32[:], ["S32"], ["Sbf"])
                yield
            k1, k2, ks, k3 = "cy32", "csq", "cstat", "cynb"
            kb.cp("act", y32[:], bankY, [ky], [k1])
            BK.put(by)
            yield
            v3 = lambda a: a.rearrange("p (h d) -> p h d", d=64)
            bc = lambda a: a.unsqueeze(2).to_broadcast([128, 8, 64])
            kb.red(stt_[:, 0:8], v3(y32[:]), ALU.add, [k1], [ks])
            kb.act(sq[:], y32[:], AF.Square, [k1], [k2])
            yield
            kb.red(stt_[:, 8:16], v3(sq[:]), ALU.add, [k2], [ks])
            yield
            kb.tt("dve", stt_[:, 24:32], stt_[:, 0:8], stt_[:, 0:8], ALU.mult, [ks], [ks])
            kb.ts("dve", stt_[:, 32:40], stt_[:, 8:16], 1.0 / 64, 64e-5, ALU.mult, ALU.add, [ks], [ks])
            yield
            kb.stt(stt_[:, 40:48], stt_[:, 24:32], -1.0 / 4096, stt_[:, 32:40], ALU.mult, ALU.add, [ks], [ks])
            yield
            kb.tt("pool", stt_[:, 48:56], stt_[:, 40:48], mhalf[:, 0:8], ALU.pow, [ks, "mhalf"], [ks])
            yield
            kb.stt(stt_[:, 56:64], stt_[:, 0:8], -1.0 / 64, stt_[:, 48:56], ALU.mult, ALU.mult, [ks], [ks])
            kb.tt("dve", v3(y32[:]), v3(y32[:]), bc(stt_[:, 48:56]), ALU.mult, [k1, ks], [k1])
            yield
            kb.tt("dve", v3(ynb[:]), v3(y32[:]), bc(stt_[:, 56:64]), ALU.add, [k1, ks], [k3])
            yield
            b2 = BK.get()
            bankT, kt = BK.bf(b2), BK.key(b2)
            for ct in range(4):
                kb.tr(bankT[:, ct * 128:(ct + 1) * 128], ynb[:, ct * 128:(ct + 1) * 128], [k3], [kt])
            bbo = BK.get()
            bankBo, kbo = BK.f32(bbo), BK.key(bbo)
            for ct in range(4):
                kb.mm(bankBo[:, ct * 128:(ct + 1) * 128], RKblk[:, ct, :], rkb[:, ct, cs], True, True, ["RKblk", f"rkb.{ct}"], [kbo])
            yield
            for ct in range(4):
                kb.act(yaff[:, ct, :], bankT[:, ct * 128:(ct + 1) * 128], AF.Identity, [kt, "cols"], ["cyaff"],
                       scale=cols[:, 75 + ct:76 + ct], bias=cols[:, 79 + ct:80 + ct])
            kb.tt("dve", t1b[:], bankBo.rearrange("p (c n) -> p c n", c=4), vbf_p[blk % 2][:, :, cs], ALU.mult,
                  [kbo] + [f"vbf{blk % 2}.{ct}" for ct in range(4)], ["ct1"])
            BK.put(b2)
            BK.put(bbo)
            yield
            kb.tt("dve", t1b[:], t1b[:], yaff[:], ALU.add, ["ct1", "cyaff"], ["ct1"])
            yield
            kb.tt("dve", hT[:, 0:4, tok], t1b[:], gT[:, :, cs], ALU.mult, ["ct1"] + [f"gT.{ct}" for ct in range(4)],
                  [f"yT.{n_ch}.1", f"hT.{n_ch // 4}"])
            yield

        def gen_CD(blk):
            yield from gen_lora(blk)
            for pair in range(2):
                gs = [gen_ct(0, blk, 2 * pair), gen_ct(1, blk, 2 * pair + 1)]
                while gs:
                    for g in list(gs):
                        try:
                            next(g)
                        except StopIteration:
                            gs.remove(g)
                    yield
            yield from gen_tok(blk)

        def run(g):
            for _ in g:
                pass

        run(gen_AB(0))
        run(gen_CD(0))
        for blk in range(NBLK):
            run(gen_F1(blk, 0))
            interleave([gen_inv(blk, 0), gen_F1(blk, 1)])
            interleave([gen_seqpost(blk, 0), gen_inv(blk, 1)])
            if blk + 1 < NBLK:
                interleave([gen_seqpost(blk, 1), gen_AB(blk + 1)])
                if blk + 1 == NBLK - 1 and "D" in L["stages"]:
                    wv = lambda w: w.rearrange("(kc p) n -> p kc n", p=128)
                    Wout = WA[:, 0:8192].rearrange("p (k n) -> p k n", k=KC)
                    WkvK = WA[:, 8192:16384].rearrange("p (k n) -> p k n", k=KC)
                    WkvV = hT[:, 4:8, :].rearrange("p a (b n) -> p (a b) n", n=1024)
                    kb.dma("pool", Wout, wv(L["w_out_d"]), [], ["WA.0", "Wk"])
                    kb.dma("pool", WkvK, wv(L["wkv_d"])[:, :, 0:1024], [], ["WA.1", "Wk"])
                    kb.dma("pool", WkvV, wv(L["wkv_d"])[:, :, 1024:2048], [], ["WkvV", "hTu"])
                run(gen_CD(blk + 1))
            else:
                run(gen_seqpost(blk, 1))
        P.barrier()
        P.flush()


def stage_D(nc, P, kb, L):
    cols, cqf, hT, yT, WA, mhalf, onesb, ss, BK = (L[k] for k in ("cols", "cqf", "hT", "yT", "WA", "mhalf", "onesb", "ss", "L_BK"))
    x_d, mem_d, gfin_d, out_d = L["x_d"], L["mem_d"], L["gfin_d"], L["out_d"]
    w_out_d, wq_d, wkv_d, wo_d, wupm_d, wdn_d = (L[k] for k in ("w_out_d", "wq_d", "wkv_d", "wo_d", "wupm_d", "wdn_d"))
    wview = lambda w: w.rearrange("(kc p) n -> p kc n", p=128)

    def run(g):
        for _ in g:
            pass

    with contextlib.ExitStack() as sd:
        sb = lambda n, s, d, st=sd: kb.sb(n, s, d, st=st)
        xres = sb("xres", [128, NT, D], F32)
        gfin = sb("gfin", [128, D], F32)
        kTm = sb("kTm", [128, KC, 256], BF16)
        vmem = sb("vmem", [128, 2, D], BF16)
        kmx = sb("kmx", [128, 4], F32)
        hb_p = [sb(f"dhb{i}", [128, D], BF16) for i in range(4)]
        sqj = sb("dsqj", [128, D], BF16)
        W0 = WA[:, 0:8192].rearrange("p (k n) -> p k n", k=KC)
        W1 = WA[:, 8192:16384].rearrange("p (k n) -> p k n", k=KC)
        Wout, WkvK = W0, W1
        WkvV = hT[:, 4:8, :].rearrange("p a (b n) -> p (a b) n", n=1024)
        xv = x_d.rearrange("(t p) d -> p t d", p=128)
        kb.dma("sp", xres[:, 0:4, :], xv[:, 0:4, :], [], [f"xres.{t}" for t in range(0, 4)])
        if "C" not in L["stages"]:
            kb.dma("pool", Wout, wview(w_out_d), [], ["WA.0"])
            kb.dma("pool", WkvK, wview(wkv_d)[:, :, 0:1024], [], ["WA.1"])
            kb.dma("pool", WkvV, wview(wkv_d)[:, :, 1024:2048], [], ["WkvV"])
        kb.dma("sp", gfin[:], gfin_d.partition_broadcast(128), [], ["gfin"])

        def gen_norm(tiles, src, gcol0, dst_fn, dkey, sscol0):
            for tl, t in enumerate(tiles):
                xa, xk = src(t)
                c = sscol0 + t
                xks = xk if isinstance(xk, list) else [xk]
                kb.act(sqj[:], xa, AF.Square, xks, ["sqj", f"ss{c}"], accum=ss[:, c:c + 1])
                yield
                kb.ts("dve", ss[:, c:c + 1], ss[:, c:c + 1], 1.0 / D, 1e-6, ALU.mult, ALU.add, [f"ss{c}"], [f"ss{c}"])
                yield
                kb.tt("pool", ss[:, c:c + 1], ss[:, c:c + 1], mhalf[:, 0:1], ALU.pow, [f"ss{c}", "mhalf"], [f"ss{c}"])
                yield
                kb.act(hb_p[tl][:], xa, AF.Copy, xks + [f"ss{c}"], [f"dhb.{tl}"], scale=ss[:, c:c + 1])
                yield
            n = len(tiles) * 128
            for kc in range(KC):
                b = BK.get()
                bankT, bk = BK.bf(b), BK.key(b)
                for tl in range(len(tiles)):
                    kb.tr(bankT[:, tl * 128:(tl + 1) * 128], hb_p[tl][:, kc * 128:(kc + 1) * 128], [f"dhb.{tl}"], [bk])
                yield
                o = dst_fn(kc)
                dk = dkey if isinstance(dkey, list) else [dkey]
                if kc % 2:
                    kb.act(o, bankT[:, 0:n], AF.Copy, [bk, "cols"], dk, scale=cols[:, gcol0 + kc:gcol0 + kc + 1])
                else:
                    kb.ts("dve", o, bankT[:, 0:n], cols[:, gcol0 + kc:gcol0 + kc + 1], None, ALU.mult, None, [bk, "cols"], dk)
                BK.put(b)
                yield

        oTb_t = sb("oTb", [128, KC * 512], BF16)
        aT_t = sb("aT_all", [128, 8 * 512], BF16)
        if True:
            memx = [oTb_t[:, 2048 * i:2048 * (i + 1)].bitcast(F32) for i in range(2)]
            mxk = [[f"oTb.{k}" for k in range(4 * i, 4 * i + 4)] for i in range(2)]
            memT = aT_t[:, 0:2048].rearrange("p (k n) -> p k n", k=KC)
            k2m = aT_t[:, 2048:4096].rearrange("p (k n) -> p k n", k=KC)
            kmT = [f"aT.{k}" for k in range(4)]
            kk2 = [f"aT.{k}" for k in range(4, 8)]
            for i in range(2):
                kb.dma("sp", memx[i], mem_d[i * 128:(i + 1) * 128, :], [], mxk[i])
            for i in range(1, 4):
                kb.dma("sp", xres[:, 4 * i:4 * i + 4, :], xv[:, 4 * i:4 * i + 4, :], [], [f"xres.{t}" for t in range(4 * i, 4 * i + 4)])

            dstate = {"v_done": False, "t3_done": False}

            def gen_memkv():
                yield from gen_norm([0, 1], lambda t: (memx[t], mxk[t]), 16, lambda kc: memT[:, kc, :], kmT, 16)
                for mt in range(2):
                    for hf in range(2):
                        b = BK.get()
                        bank, bk = BK.f32(b), BK.key(b)
                        for kc in range(KC):
                            kb.mm(bank, memT[:, kc, mt * 128:(mt + 1) * 128], WkvV[:, kc, hf * 512:(hf + 1) * 512],
                                  kc == 0, kc == KC - 1, ["WkvV"] + kmT, [bk])
                        yield
                        kb.cpalt(vmem[:, mt, hf * 512:(hf + 1) * 512], bank, [bk], ["vmem"])
                        BK.put(b)
                        yield
                dstate["v_done"] = True
                for ft in range(KC):
                    b = BK.get()
                    bank, bk = BK.f32(b), BK.key(b)
                    for kc in range(KC):
                        kb.mm(bank[:, 0:256], WkvK[:, kc, ft * 128:(ft + 1) * 128], memT[:, kc, :], kc == 0, kc == KC - 1, ["WA.1"] + kmT, [bk])
                    yield
                    kb.cpalt(kTm[:, ft, :], bank[:, 0:256], [bk], ["kTm"])
                    BK.put(b)
                    yield
                kb.act(k2m, kTm[:], AF.Square, ["kTm"], kk2)
                yield
                for h in range(4):
                    b = BK.get()
                    bank, bk = BK.f32(b), BK.key(b)
                    kb.mm(bank[:, 0:256], onesb[:], k2m[:, 2 * h, :], True, False, ["onesb"] + kk2, [bk])
                    kb.mm(bank[:, 0:256], onesb[:], k2m[:, 2 * h + 1, :], False, True, ["onesb"] + kk2, [bk])
                    yield
                    kb.red(kmx[:, h:h + 1], bank[:, 0:256], ALU.max, [bk], ["kmx"])
                    BK.put(b)
                    yield

            def gen_wout():
                for t in range(NT):
                    tok = slice(t * 128, (t + 1) * 128)
                    for hf in range(2):
                        b = BK.get()
                        bank, bk = BK.f32(b), BK.key(b)
                        for kc in range(KC):
                            ysrc = yT[:, kc, tok] if kc < 4 else hT[:, kc - 4, tok]
                            kb.mm(bank, ysrc, Wout[:, kc, hf * 512:(hf + 1) * 512], kc == 0, kc == KC - 1,
                                  [f"yT.{t}.0", f"yT.{t}.1", "WA.0", f"hT.{t // 4}"], [bk])
                        yield
                        xs = xres[:, t, hf * 512:(hf + 1) * 512]
                        kb.tt("dve", xs, bank, xs, ALU.add, [bk, f"xres.{t}"], [f"xres.{t}"])
                        BK.put(b)
                        yield
                    if t == 3:
                        dstate["t3_done"] = True

            srcx = lambda t: (xres[:, t, :], f"xres.{t}")

            def gen_norm0():
                while not (dstate["v_done"] and dstate["t3_done"]):
                    yield
                yield from gen_norm([0, 1, 2, 3], srcx, 8, lambda kc: hT[:, kc, 0:512], ["hT.0", "WkvV"], 0)

            interleave([gen_memkv(), gen_wout(), gen_norm0()])
            kb.dma("pool", W1, wview(wq_d), [], ["WA.1"])
            kb.dma("pool", W0, wview(wo_d), [], ["WA.0"])

        srcx = lambda t: (xres[:, t, :], f"xres.{t}")
        norm_x = lambda blk: gen_norm(list(range(4 * blk, 4 * blk + 4)), srcx, 8, lambda kc: hT[:, kc, blk * 512:(blk + 1) * 512], [f"hT.{blk}", "WkvV"], 0)
        norm_m = lambda blk: gen_norm(list(range(4 * blk, 4 * blk + 4)), srcx, 24, lambda kc: hT[:, kc, blk * 512:(blk + 1) * 512], f"hT.{blk}", 16)
        with contextlib.ExitStack() as s2:
            qTb = yT[:, 0:2, :].rearrange("p a (b n) -> p (a b) n", n=512)
            PT = yT[:, 2:4, :].rearrange("p a (b n) -> p (a b) n", n=512)
            oTb = oTb_t[:].rearrange("p (k n) -> p k n", k=KC)
            yall = [f"yT.{t}.0" for t in range(NT)]
            q2b_p = [sb(f"q2b{i}", [128, 2, 512], BF16, st=s2) for i in range(4)]
            nb_p = [sb(f"nb{i}", [128, 2], F32, st=s2) for i in range(4)]
            rc_p = [sb(f"rc{i}", [128, 512], F32, st=s2) for i in range(4)]

            def gen_head(sl, h):
                qk = [f"qTb.{2 * h}", f"qTb.{2 * h + 1}"]
                q2b, kq2 = q2b_p[sl], f"q2b.{sl}"
                kb.act(q2b[:], qTb[:, 2 * h:2 * h + 2, :], AF.Square, qk, [kq2])
                yield
                b = BK.get()
                bankQ, kq = BK.f32(b), BK.key(b)
                kb.mm(bankQ, onesb[:], q2b[:, 0, :], True, False, ["onesb", kq2], [kq])
                kb.mm(bankQ, onesb[:], q2b[:, 1, :], False, True, ["onesb", kq2], [kq])
                yield
                nb, kbs = nb_p[sl], f"nb.{sl}"
                kb.red(nb[:, 0:1], bankQ, ALU.max, [kq], [kbs])
                BK.put(b)
                yield
                kb.ts("dve", nb[:, 1:2], nb[:, 0:1], kmx[:, h:h + 1], -1.0 / 32, ALU.add, ALU.mult, [kbs, "kmx"], [kbs])
                yield
                for mt in range(2):
                    b = BK.get()
                    bankS, ksb = BK.f32(b), BK.key(b)
                    ms = slice(mt * 128, (mt + 1) * 128)
                    kb.mm(bankS, kTm[:, 2 * h, ms], qTb[:, 2 * h, :], True, False, ["kTm", qk[0]], [ksb])
                    kb.mm(bankS, kTm[:, 2 * h + 1, ms], qTb[:, 2 * h + 1, :], False, True, ["kTm", qk[1]], [ksb])
                    yield
                    kb.act(PT[:, 2 * h + mt, :], bankS, AF.Exp, [ksb, kbs], [f"PT.{2 * h + mt}"] + (yall if blk == 0 else []), scale=1.0 / 16, bias=nb[:, 1:2])
                    BK.put(b)
                    yield
                pk = [f"PT.{2 * h}", f"PT.{2 * h + 1}"]
                b = BK.get()
                bankRs, krs = BK.f32(b), BK.key(b)
                kb.mm(bankRs, onesb[:], PT[:, 2 * h, :], True, False, ["onesb", pk[0]], [krs])
                kb.mm(bankRs, onesb[:], PT[:, 2 * h + 1, :], False, True, ["onesb", pk[1]], [krs])
                yield
                rc, krc = rc_p[sl], f"rc.{sl}"
                P.op("dve", lambda e: e.reciprocal(out=rc[:], in_=bankRs), [krs], [krc])
                BK.put(b)
                yield
                for dt_ in range(2):
                    ft = 2 * h + dt_
                    b = BK.get()
                    bankO, kob = BK.f32(b), BK.key(b)
                    kb.mm(bankO, vmem[:, 0, ft * 128:(ft + 1) * 128], PT[:, 2 * h, :], True, False, ["vmem", pk[0]], [kob])
                    kb.mm(bankO, vmem[:, 1, ft * 128:(ft + 1) * 128], PT[:, 2 * h + 1, :], False, True, ["vmem", pk[1]], [kob])
                    yield
                    kb.tt("dve", oTb[:, ft, :], bankO, rc[:], ALU.mult, [kob, krc], [f"oTb.{ft}"])
                    BK.put(b)
                    yield

            def gen_attn(blk):
                bs = slice(blk * 512, (blk + 1) * 512)
                for ft in range(KC):
                    b = BK.get()
                    bank, bk = BK.f32(b), BK.key(b)
                    for kc in range(KC):
                        kb.mm(bank, W1[:, kc, ft * 128:(ft + 1) * 128], hT[:, kc, bs], kc == 0, kc == KC - 1, ["WA.1", f"hT.{blk}"], [bk])
                    yield
                    kb.cpalt(qTb[:, ft, :], bank, [bk], [f"qTb.{ft}"] + (yall if blk == 0 else []))
                    BK.put(b)
                    yield
                if blk == 3:
                    mlp_load(0)
                for pair in range(1):
                    gs = [gen_head(h, h) for h in range(4)]
                    while gs:
                        for g in list(gs):
                            try:
                                next(g)
                            except StopIteration:
                                gs.remove(g)
                        yield
                for tl in range(4):
                    t = blk * 4 + tl
                    for hf in range(2):
                        b = BK.get()
                        bank, bk = BK.f32(b), BK.key(b)
                        for kc in range(KC):
                            kb.mm(bank, oTb[:, kc, tl * 128:(tl + 1) * 128], W0[:, kc, hf * 512:(hf + 1) * 512], kc == 0, kc == KC - 1,
                                  [f"oTb.{kc}", "WA.0"], [bk])
                        yield
                        xs = xres[:, t, hf * 512:(hf + 1) * 512]
                        kb.tt("dve", xs, bank, xs, ALU.add, [bk, f"xres.{t}"], [f"xres.{t}"])
                        BK.put(b)
                        yield

            def norm_worker(blk):
                if blk + 1 < 4:
                    yield from norm_x(blk + 1)
                if blk - 1 >= 0:
                    yield from norm_m(blk - 1)

            s3 = s2
            aT_all = aT_t[:].rearrange("p (k n) -> p k n", k=8)
            rl_p = [sb(f"rl{i}", [128, 512], BF16, st=s3) for i in range(2)]
            o_p = [oTb_t[:, 2048 * i:2048 * (i + 1)].bitcast(F32) for i in range(2)]
            upv = wupm_d.rearrange("(kc p) n -> p kc n", p=128)
            dnv = wdn_d.rearrange("(f p) n -> p f n", p=128)
            ov = out_d.rearrange("(t p) d -> p t d", p=128)

            def mlp_w(j):
                s = (j + 1) % 2
                Wu = WA[:, s * 8192:s * 8192 + 4096].rearrange("p (k n) -> p k n", k=KC)
                Wd = WA[:, s * 8192 + 4096:(s + 1) * 8192].rearrange("p (f n) -> p f n", f=4)
                return s, Wu, Wd

            def mlp_load(j):
                s, Wu, Wd = mlp_w(j)
                kb.dma("pool", Wu, upv[:, :, j * 512:(j + 1) * 512], [], [f"WA.{s}"])
                kb.dma("pool", Wd, dnv[:, 4 * j:4 * j + 4, :], [], [f"WA.{s}"])

            for blk in range(4):
                interleave([gen_attn(blk), norm_worker(blk)])
            run(norm_m(3))

            NJ = 8
            ai = 0
            ri = 0
            for j in range(NJ):
                s, Wu, Wd = mlp_w(j)
                if j > 0:
                    mlp_load(j)
                for blk in range(4):
                    bs = slice(blk * 512, (blk + 1) * 512)
                    ab = (ai % 2) * 4
                    ai += 1
                    for fft in range(4):
                        b = BK.get()
                        bank, bk = BK.f32(b), BK.key(b)
                        for kc in range(KC):
                            kb.mm(bank, Wu[:, kc, fft * 128:(fft + 1) * 128], hT[:, kc, bs], kc == 0, kc == KC - 1, [f"WA.{s}", f"hT.{blk}"], [bk])
                        rl, krl = rl_p[ri % 2], f"rl.{ri % 2}"
                        ri += 1
                        kb.act(rl[:], bank, AF.Relu, [bk], [krl])
                        BK.put(b)
                        kb.tt("dve", aT_all[:, ab + fft, :], rl[:], rl[:], ALU.mult, [krl], [f"aT.{ab + fft}"])
                    for tl in range(4):
                        t = blk * 4 + tl
                        for hf in range(2):
                            b = BK.get()
                            bank, bk = BK.f32(b), BK.key(b)
                            for fft in range(4):
                                kb.mm(bank, aT_all[:, ab + fft, tl * 128:(tl + 1) * 128], Wd[:, fft, hf * 512:(hf + 1) * 512], fft == 0, fft == 3,
                                      [f"aT.{ab + fft}", f"WA.{s}"], [bk])
                            xs = xres[:, t, hf * 512:(hf + 1) * 512]
                            kb.tt("dve", xs, bank, xs, ALU.add, [bk, f"xres.{t}"], [f"xres.{t}"])
                            BK.put(b)
                    if j == NJ - 1:
                        for fb in ([blk - 1] if blk > 0 else []) + ([3] if blk == 3 else []):
                            for t in range(4 * fb, 4 * fb + 4):
                                c = 32 + t
                                kb.act(sqj[:], xres[:, t, :], AF.Square, [f"xres.{t}"], ["sqj", f"ss{c}"], accum=ss[:, c:c + 1])
                                kb.ts("dve", ss[:, c:c + 1], ss[:, c:c + 1], 1.0 / D, 1e-6, ALU.mult, ALU.add, [f"ss{c}"], [f"ss{c}"])
                                kb.tt("pool", ss[:, c:c + 1], ss[:, c:c + 1], mhalf[:, 0:1], ALU.pow, [f"ss{c}", "mhalf"], [f"ss{c}"])
                                ot, ko = o_p[t % 2], f"ot.{t % 2}"
                                kb.stt(ot, xres[:, t, :], ss[:, c:c + 1], gfin[:], ALU.mult, ALU.mult, [f"xres.{t}", f"ss{c}", "gfin"],
                                       [ko] + [f"oTb.{k}" for k in range(KC)])
                                kb.dma("sp", ov[:, t, :], ot, [ko], [f"out.{t}"])
            P.barrier()
            P.flush()


def _consts():
    cq = np.zeros((128, NCQ), np.float32)
    i = np.arange(128)
    cq[:, 0:128] = np.eye(128, dtype=np.float32)
    strict = (i[:, None] < i[None, :]).astype(np.float32)
    incl = (i[:, None] <= i[None, :]).astype(np.float32)
    cq[:, 128:256] = strict
    cq[:, 256:384] = incl
    cq[:, 384:512] = strict
    cq[:, 512:640] = incl
    cq[:, 640:768] = (i[:, None] > i[None, :]).astype(np.float32)
    cq[:, 768:896] = ((i[:, None] // 64) == (i[None, :] // 64)).astype(np.float32)
    cq[:, 896:1024] = 1.0
    gam = 1.0 - 2.0 ** (-5.0 - np.arange(8, dtype=np.float64))
    xi = gam[None, :] ** (i[:, None] + 1.0)
    kz = 0.125 * gam[None, :] ** (-(i[:, None] + 1.0))
    gC = np.zeros((128, 4))
    for ct in range(4):
        for p in range(128):
            gC[p, ct] = gam[2 * ct + p // 64] ** 128.0
    invf = (10000.0 ** (-np.arange(32, dtype=np.float32) / 32.0)).astype(np.float32)
    return cq, xi.astype(np.float32), kz.astype(np.float32), gC.astype(np.float32), np.broadcast_to(invf[None], (128, 32))


def _col(v):
    v = np.asarray(v, np.float32).reshape(-1)
    return v.reshape(-1, 128).T


def make_inputs(inp):
    cq, xi, kz, gC, invf = _consts()
    cols = np.zeros((128, NCOLS), np.float32)
    cols[:, 0:8] = _col(inp["norm_mix"][0])
    cols[:, 8:16] = _col(inp["norm_xattn"][0])
    cols[:, 16:24] = _col(inp["norm_mem"][0])
    cols[:, 24:32] = _col(inp["norm_mlp"][0])
    cols[:, 32:36] = _col(inp["ret_gn_w"][0])
    cols[:, 36:40] = _col(inp["ret_gn_b"][0])
    mu = np.asarray(inp["rwkv_mu"][0], np.float32)
    cols[:, 40:54] = _col(mu[:1792])
    cols[:, 54] = mu[1696:1824]
    cols[:, 55:59] = _col(inp["rwkv_w0"][0])
    cols[:, 59:63] = _col(inp["rwkv_a0"][0])
    cols[:, 63:67] = _col(inp["rwkv_k_k"][0])
    cols[:, 67:71] = _col(inp["rwkv_k_a"][0])
    cols[:, 71:75] = _col(inp["rwkv_r_k"][0])
    cols[:, 75:79] = _col(inp["rwkv_gn_w"][0])
    cols[:, 79:83] = _col(inp["rwkv_gn_b"][0])
    cols[:, 83:91] = xi
    cols[:, 91:99] = kz
    cols[:, 99:103] = gC
    cols[:, 103:135] = invf
    shared = {
        "cols": cols, "cq": cq,
        "gfin": np.ascontiguousarray(inp["norm_final"], np.float32),
        "w_in": np.ascontiguousarray(inp["w_in"][0]),
        "w_up": np.ascontiguousarray(inp["rwkv_w_up"][0]),
        "a_up": np.ascontiguousarray(inp["rwkv_a_up"][0]),
        "g_up": np.ascontiguousarray(inp["rwkv_g_up"][0]),
        "w_out": np.ascontiguousarray(inp["w_out"][0]),
        "wq": np.ascontiguousarray(inp["xattn_w_q"][0]),
        "wkv": np.ascontiguousarray(inp["xattn_w_kv"][0]),
        "wo": np.ascontiguousarray(inp["xattn_w_o"][0]),
        "mlp_up": np.ascontiguousarray(inp["mlp_w_up"][0]),
        "mlp_down": np.ascontiguousarray(inp["mlp_w_down"][0]),
    }
    maps = []
    for b in range(8):
        m = dict(shared)
        m["x"] = np.ascontiguousarray(inp["x"][b], np.float32)
        m["mem"] = np.ascontiguousarray(inp["mem"][b], np.float32)
        m["pos"] = np.ascontiguousarray(np.asarray(inp["positions"][b], np.int32).reshape(NT, 128).T)
        maps.append(m)
    return maps


_NC_CACHE = {}


def kernel(**inputs):
    inp = {k: np.asarray(v) for k, v in inputs.items()}
    maps = make_inputs(inp)
    if "nc" not in _NC_CACHE:
        _NC_CACHE["nc"] = build()
    res = run_bass_kernel_spmd(_NC_CACHE["nc"], maps, core_ids=list(range(8)))
    return np.stack([np.asarray(r["out"], np.float32) for r in res.results], axis=0)
```

```python
import contextlib
import math
import numpy as np
import concourse.bass as bass
import concourse.mybir as mybir
from concourse.bass_utils import run_bass_kernel_spmd

F32 = mybir.dt.float32
BF16 = mybir.dt.bfloat16
I32 = mybir.dt.int32
AF = mybir.ActivationFunctionType
ALU = mybir.AluOpType
AX = mybir.AxisListType

T = 2048
D = 1024
NT = 16
KC = 8
NCOLS = 135
NCQ = 1024
CUT = 99
COMPUTE = ("pe", "act", "dve", "pool")
ISSUERS = ("pe", "act", "dve", "pool", "sp")


class Prog:
    def __init__(self, nc, st, n_dma=12, self_sync=True):
        self.nc = nc
        self.n_dma = n_dma
        self.self_sync = self_sync
        self.chans = list(COMPUTE) + [f"d{i}" for i in range(n_dma)]
        self.sems = {c: st.enter_context(nc.semaphore(f"s_{c}")) for c in self.chans}
        self.streams = {e: [] for e in ISSUERS}
        self.count = {c: 0 for c in self.chans}
        self.clock = {e: {} for e in ISSUERS}
        self.snap = {}
        self.wr = {}
        self.rd = {}
        self.rr = 0
        self.nops = 0
        self.nwaits = 0

    @staticmethod
    def _need(needs, d):
        for c, n in d.items():
            if needs.get(c, 0) < n:
                needs[c] = n

    def op(self, eng, fn, reads=(), writes=(), dma=False):
        needs = {}
        for k in reads:
            self._need(needs, self.wr.get(k, {}))
        for k in writes:
            self._need(needs, self.wr.get(k, {}))
            self._need(needs, self.rd.get(k, {}))
        if dma:
            chan = f"d{self.rr % self.n_dma}"
            self.rr += 1
            if self.count[chan]:
                self._need(needs, {chan: self.count[chan]})
        else:
            chan = eng
        clk = self.clock[eng]
        waits = []
        for c, n in needs.items():
            if c == eng and (eng == "pe" or not self.self_sync):
                continue
            if clk.get(c, 0) < n:
                waits.append((c, n))
                for c2, n2 in self.snap[(c, n)].items():
                    if clk.get(c2, 0) < n2:
                        clk[c2] = n2
        self.nwaits += len(waits)
        self.nops += 1
        n_new = self.count[chan] + 1
        self.count[chan] = n_new
        s = dict(clk)
        s[chan] = n_new
        self.snap[(chan, n_new)] = s
        if eng == "pe" and chan == "pe":
            clk["pe"] = n_new
        wset = set(writes)
        for k in wset:
            self.wr[k] = {chan: n_new}
            self.rd[k] = {}
        for k in reads:
            if k in wset:
                continue
            self.rd.setdefault(k, {})[chan] = n_new
        self.streams[eng].append((waits, fn, chan))

    def barrier(self):
        allk = {c: n for c, n in self.count.items() if n}
        for e in ISSUERS:
            clk = self.clock[e]
            waits = []
            for c, n in allk.items():
                if c == e and e == "pe":
                    continue
                if clk.get(c, 0) < n:
                    waits.append((c, n))
            if waits:
                self.streams[e].append((waits, None, None))
        for e in ISSUERS:
            for c, n in allk.items():
                if self.clock[e].get(c, 0) < n:
                    self.clock[e][c] = n

    def flush(self):
        nc = self.nc
        streams = self.streams
        if not any(streams[e] for e in ISSUERS):
            return
        self.streams = {e: [] for e in ISSUERS}
        sems = self.sems

        def val(c, n):
            return n * 16 if c.startswith("d") else n

        def run(engname):
            def body(e):
                for waits, fn, chan in streams[engname]:
                    for c, n in waits:
                        e.wait_ge(sems[c], val(c, n))
                    if fn is not None:
                        fn(e).then_inc(sems[chan], 16 if chan.startswith("d") else 1)
            return body

        with nc.Block() as block:
            block.tensor(run("pe"))
            block.scalar(run("act"))
            block.vector(run("dve"))
            block.gpsimd(run("pool"))
            block.sync(run("sp"))


class Ring:
    def __init__(self, kb, name, n, shape, dtype, st=None, psum=False):
        st = st or kb.st
        alloc = kb.nc.psum_tensor if psum else kb.nc.sbuf_tensor
        self.name = name
        self.t = [st.enter_context(alloc(f"rg_{name}{i}", shape, dtype)) for i in range(n)]
        self.i = 0

    def next(self):
        i = self.i % len(self.t)
        self.i += 1
        return self.t[i], f"{self.name}.{i}"


class ViewRing:
    def __init__(self, aps, keys):
        self.t, self.k, self.i = aps, keys, 0

    def next(self):
        i = self.i % len(self.t)
        self.i += 1
        return self.t[i], self.k[i]


class Banks:
    def __init__(self, pp):
        import collections
        self.pp = pp
        self.free = collections.deque(range(8))

    def get(self):
        return self.free.popleft()

    def put(self, i):
        self.free.append(i)

    def f32(self, i):
        return self.pp[:, i, :]

    def bf(self, i):
        return self.pp[:, i, :].bitcast(BF16)

    @staticmethod
    def key(i):
        return f"ps{i}"


def interleave(gens):
    gens = list(gens)
    while gens:
        for g in list(gens):
            try:
                next(g)
            except StopIteration:
                gens.remove(g)


class KB:
    def __init__(self, nc, P, st):
        self.nc, self.P, self.st = nc, P, st
        self.flip = 0

    def sb(self, name, shape, dtype, st=None):
        return (st or self.st).enter_context(self.nc.sbuf_tensor("sb_" + name, shape, dtype))

    def mm(self, out, lhsT, rhs, start, stop, r, w):
        self.P.op("pe", lambda e: e.matmul(out, lhsT=lhsT, rhs=rhs, start=start, stop=stop), r, w)

    def tr(self, out, in_, r, w):
        idb = self.identb[:]
        self.P.op("pe", lambda e: e.transpose(out=out, in_=in_, identity=idb), list(r) + ["identb"], w)

    def act(self, out, in_, func, r, w, scale=1.0, bias=0.0, accum=None):
        kw = {}
        if accum is not None:
            kw["accum_out"] = accum
        self.P.op("act", lambda e: e.activation(out=out, in_=in_, func=func, bias=bias, scale=scale, **kw), r, w)

    def tt(self, eng, out, in0, in1, op, r, w):
        self.P.op(eng, lambda e: e.tensor_tensor(out=out, in0=in0, in1=in1, op=op), r, w)

    def ts(self, eng, out, in0, s1, s2, op0, op1, r, w):
        if s2 is None:
            self.P.op(eng, lambda e: e.tensor_scalar(out=out, in0=in0, scalar1=s1, scalar2=None, op0=op0), r, w)
        else:
            self.P.op(eng, lambda e: e.tensor_scalar(out=out, in0=in0, scalar1=s1, scalar2=s2, op0=op0, op1=op1), r, w)

    def stt(self, out, in0, sc, in1, op0, op1, r, w):
        self.P.op("dve", lambda e: e.scalar_tensor_tensor(out=out, in0=in0, scalar=sc, in1=in1, op0=op0, op1=op1), r, w)

    def cp(self, eng, out, in_, r, w):
        if eng == "act":
            self.P.op("act", lambda e: e.activation(out=out, in_=in_, func=AF.Copy), r, w)
        else:
            self.P.op(eng, lambda e: e.tensor_copy(out=out, in_=in_), r, w)

    def cpalt(self, out, in_, r, w):
        self.flip ^= 1
        self.cp("act" if self.flip else "dve", out, in_, r, w)

    def red(self, out, in_, op, r, w):
        self.P.op("dve", lambda e: e.tensor_reduce(out=out, in_=in_, axis=AX.X, op=op), r, w)

    def dma(self, eng, out, in_, r, w):
        self.P.op(eng, lambda e: e.dma_start(out=out, in_=in_), r, w, dma=True)


def build(stages="ABCD", dbg=False):
    nc = bass.Bass("TRN2", target_bir_lowering=False)

    def dram(n, s, d, kind="ExternalInput"):
        return nc.dram_tensor(n, s, d, kind=kind).ap()

    x_d = dram("x", [T, D], F32)
    mem_d = dram("mem", [256, D], F32)
    pos_d = dram("pos", [128, NT], I32)
    cols_d = dram("cols", [128, NCOLS], F32)
    cq_d = dram("cq", [128, NCQ], F32)
    gfin_d = dram("gfin", [D], F32)
    w_in_d = dram("w_in", [D, 3872], F32)
    wup_d = dram("w_up", [64, 512], F32)
    aup_d = dram("a_up", [64, 512], F32)
    gup_d = dram("g_up", [160, 512], F32)
    w_out_d = dram("w_out", [D, D], F32)
    wq_d = dram("wq", [D, D], F32)
    wkv_d = dram("wkv", [D, 2 * D], F32)
    wo_d = dram("wo", [D, D], F32)
    wupm_d = dram("mlp_up", [D, 4 * D], F32)
    wdn_d = dram("mlp_down", [4 * D, D], F32)
    out_d = dram("out", [T, D], F32, kind="ExternalOutput")
    if dbg:
        dbg_yT = dram("dbg_yT", [128, KC, T], BF16, kind="ExternalOutput")
        dbg_hT = dram("dbg_hT", [128, KC, T], BF16, kind="ExternalOutput")

    def wview(w):
        return w.rearrange("(kc p) n -> p kc n", p=128)

    with contextlib.ExitStack() as st:
        P = Prog(nc, st)
        kb = KB(nc, P, st)
        cols = kb.sb("cols", [128, NCOLS], F32)
        dc = kb.sb("dcols", [128, 40], F32)
        cqf = kb.sb("cqf", [128, NCQ], F32)
        identb = kb.sb("identb", [128, 128], BF16)
        bonesb = kb.sb("bonesb", [128, 128], BF16)
        onesb = kb.sb("onesb", [128, 128], BF16)
        mhalf = kb.sb("mhalf", [128, 256], F32)
        hT = kb.sb("hT", [128, KC, T], BF16)
        yT = kb.sb("yT", [128, 4, T], BF16)
        WA = kb.sb("WA", [128, 16384], BF16)
        ss = kb.sb("ss", [128, 3 * NT], F32)
        kb.identb = identb
        pp = st.enter_context(nc.psum_tensor("pp", [128, 8, 512], F32))
        L_BK = Banks(pp)
        psA = ViewRing([pp[:, i, :] for i in range(6)], [f"ps{i}" for i in range(6)])
        psT = ViewRing([pp[:, i, :].bitcast(BF16) for i in (6, 7)], ["ps6", "ps7"])
        mask4 = cqf[:, 128:640]
        maskI = cqf[:, 256:384]
        maskLs = cqf[:, 640:768]
        identf = cqf[:, 0:128]

        kb.dma("sp", cols[:], cols_d, [], ["cols"])
        kb.dma("sp", cqf[:], cq_d, [], ["cqf"])
        kb.dma("pool", identb[:], cq_d[:, 0:128], [], ["identb"])
        kb.dma("pool", bonesb[:], cq_d[:, 768:896], [], ["bonesb"])
        kb.dma("pool", onesb[:], cq_d[:, 896:1024], [], ["onesb"])
        P.op("dve", lambda e: e.memset(mhalf[:], -0.5), [], ["mhalf"])
        kb.ts("dve", dc[:, 0:15], cols[:, 40:55], -1.0, 1.0, ALU.mult, ALU.add, ["cols"], ["dc"])
        kb.ts("dve", dc[:, 15:19], cols[:, 55:59], 0.5, None, ALU.mult, None, ["cols"], ["dc"])
        kb.ts("dve", dc[:, 19:23], cols[:, 59:63], 0.5, None, ALU.mult, None, ["cols"], ["dc"])
        kb.ts("dve", dc[:, 23:27], cols[:, 67:71], -1.0, None, ALU.mult, None, ["cols"], ["dc"])
        kb.ts("dve", dc[:, 27:31], cols[:, 67:71], -1.0, 1.0, ALU.mult, ALU.add, ["cols"], ["dc"])

        def norm_T(src, nblk, tpb, gcol0, dst, dstkey, sscol0, hb, sqj):
            for blk in range(nblk):
                hbs = []
                for tl in range(tpb):
                    t = blk * tpb + tl
                    xa, xk = src(t)
                    c = sscol0 + t
                    kb.act(sqj[:], xa, AF.Square, [xk], ["sqj", f"ss{c}"], accum=ss[:, c:c + 1])
                    kb.ts("dve", ss[:, c:c + 1], ss[:, c:c + 1], 1.0 / D, 1e-6, ALU.mult, ALU.add, [f"ss{c}"], [f"ss{c}"])
                    kb.tt("pool", ss[:, c:c + 1], ss[:, c:c + 1], mhalf[:, 0:1], ALU.pow, [f"ss{c}", "mhalf"], [f"ss{c}"])
                    h, hk = hb.next()
                    kb.act(h[:], xa, AF.Copy, [xk, f"ss{c}"], [hk], scale=ss[:, c:c + 1])
                    hbs.append((h, hk))
                for kc in range(KC):
                    bank, bk = psT.next()
                    for tl, (h, hk) in enumerate(hbs):
                        kb.tr(bank[:, tl * 128:(tl + 1) * 128], h[:, kc * 128:(kc + 1) * 128], [hk], [bk])
                    n = tpb * 128
                    o = dst[:, kc, blk * n:(blk + 1) * n]
                    kb.flip ^= 1
                    if kb.flip:
                        kb.act(o, bank[:, 0:n], AF.Copy, [bk, "cols"], [dstkey(blk)], scale=cols[:, gcol0 + kc:gcol0 + kc + 1])
                    else:
                        kb.ts("dve", o, bank[:, 0:n], cols[:, gcol0 + kc:gcol0 + kc + 1], None, ALU.mult, None, [bk, "cols"], [dstkey(blk)])

        def load_w(dst, src, key, eng="pool"):
            kb.dma(eng, dst, src, [], [key])

        Wv8 = lambda ncol: WA[:, 0:KC * ncol].rearrange("p (k n) -> p k n", k=KC)

        Wr = Wv8(2048)
        BK = L_BK

        def acquire(n=1):
            spins = 0
            while len(BK.free) < n:
                spins += 1
                assert spins < 100000, "PSUM bank pool deadlock"
                yield
            return [BK.get() for _ in range(n)]

        def rr(gens):
            gens = list(gens)
            while gens:
                for g in list(gens):
                    try:
                        next(g)
                    except StopIteration:
                        gens.remove(g)
                yield

        def run(g):
            for _ in g:
                pass

        if "B" not in stages:
            with contextlib.ExitStack() as sa:
                xring = Ring(kb, "xr", 3, [128, D], F32, st=sa)
                hb = Ring(kb, "hb", 8, [128, D], BF16, st=sa)
                sqj = kb.sb("sqj", [128, D], BF16, st=sa)

                def srcx(t):
                    xa, xk = xring.next()
                    kb.dma("sp", xa[:], x_d[t * 128:(t + 1) * 128, :], [], [xk])
                    return xa[:], xk
                norm_T(srcx, 4, 4, 0, hT, lambda b: f"hT.{b}", 0, hb, sqj)
                P.barrier()
                P.flush()
            P.op("dve", lambda e: e.memset(yT[:, 0:4, :], 0.0), [], [f"yT.{t}.0" for t in range(NT)])
        else:
            for gi in range(4):
                load_w(Wr[:, :, gi * 512:(gi + 1) * 512], wview(w_in_d)[:, :, gi * 512:(gi + 1) * 512], f"W.{gi}")
            with contextlib.ExitStack() as sbk:
                cos_t = kb.sb("cos_t", [128, NT, 32], F32, st=sbk)
                sin_t = kb.sb("sin_t", [128, NT, 32], F32, st=sbk)
                srope = sbk
                if True:
                    posi = kb.sb("posi", [128, NT], I32, st=srope)
                    posf = kb.sb("posf", [128, NT], F32, st=srope)
                    ang = kb.sb("ang", [128, NT, 32], F32, st=srope)
                    ra = kb.sb("ra", [128, NT, 32], F32, st=srope)
                    rb_ = kb.sb("rb_", [128, NT, 32], F32, st=srope)
                    ri = kb.sb("ri", [128, NT, 32], I32, st=srope)
                    kb.dma("sp", posi[:], pos_d, [], ["posi"])
                    kb.cp("dve", posf[:], posi[:], ["posi"], ["posf"])
                    kb.tt("dve", ang[:], posf[:].unsqueeze(2).to_broadcast([128, NT, 32]),
                          cols[:, 103:135].unsqueeze(1).to_broadcast([128, NT, 32]), ALU.mult, ["posf", "cols"], ["ang"])
                    C1 = 6.28125
                    C2 = 2.0 * math.pi - C1
                    for tab, shift, nm in ((sin_t, 0.0, "sin"), (cos_t, 0.5 * math.pi, "cos")):
                        kb.ts("dve", ra[:], ang[:], shift, 1.0 / (2.0 * math.pi), ALU.add, ALU.mult, ["ang"], ["ra"])
                        kb.cp("dve", ri[:], ra[:], ["ra"], ["ri"])
                        kb.cp("dve", rb_[:], ri[:], ["ri"], ["rb"])
                        kb.ts("dve", ra[:], ang[:], shift, None, ALU.add, None, ["ang"], ["ra"])
                        kb.stt(ra[:], rb_[:], -C1, ra[:], ALU.mult, ALU.add, ["rb", "ra"], ["ra"])
                        kb.stt(ra[:], rb_[:], -C2, ra[:], ALU.mult, ALU.add, ["rb", "ra"], ["ra"])
                        kb.ts("dve", rb_[:], ra[:], math.pi, -2.0 * math.pi, ALU.is_gt, ALU.mult, ["ra"], ["rb"])
                        kb.tt("dve", ra[:], ra[:], rb_[:], ALU.add, ["ra", "rb"], ["ra"])
                        kb.ts("dve", rb_[:], ra[:], -math.pi, 2.0 * math.pi, ALU.is_lt, ALU.mult, ["ra"], ["rb"])
                        kb.tt("dve", ra[:], ra[:], rb_[:], ALU.add, ["ra", "rb"], ["ra"])
                        kb.act(tab[:], ra[:], AF.Sin, ["ra"], [nm])

                sb_ = lambda n, shp, d: kb.sb(n, shp, d, st=sbk)
                W = 2
                xr_p = [sb_(f"xr{i}", [128, D], F32) for i in range(2)]
                hb_p = [sb_(f"hb{i}", [128, D], BF16) for i in range(4)]
                sqj = sb_("sqj", [128, D], BF16)
                qT_p = [sb_(f"qT{i}", [128, 4, 2, 512], BF16) for i in range(2)]
                for i in range(2):
                    P.op("dve", lambda e, i=i: e.memset(qT_p[i][:], 0.0), [], [f"qT{i}.{j}" for j in range(4)])
                kT_p = [sb_(f"kT{i}", [128, 4, 512], BF16) for i in range(2)]
                ktok_p = [sb_(f"ktok{i}", [128, 4, 512], BF16) for i in range(2)]
                vtok_p = [sb_(f"vtok{i}", [128, 4, 512], BF16) for i in range(2)]
                gateT_p = [sb_(f"gateT{i}", [128, 4, 512], BF16) for i in range(2)]
                xs_p = [sb_(f"xs{i}", [128, 512], F32) for i in range(W)]
                A_p = [sb_(f"rA{i}", [128, 512], F32) for i in range(W)]
                B_p = [sb_(f"rB{i}", [128, 512], F32) for i in range(W)]
                qtok_p = [sb_(f"qtok{i}", [128, 512], BF16) for i in range(W)]
                th_p = [sb_(f"thr{i}", [128, 512], F32) for i in range(2)]
                sT_p = [sb_(f"sT{i}", [128, 8, 128], BF16) for i in range(W)]
                y32_p = [sb_(f"y32{i}", [128, 512], F32) for i in range(W)]
                sq_p = [sb_(f"sqr{i}", [128, 512], F32) for i in range(W)]
                ynb_p = [sb_(f"ynb{i}", [128, 512], BF16) for i in range(W)]
                yaff_p = [sb_(f"yaff{i}", [128, 2, 128], F32) for i in range(W)]
                stat_p = [sb_(f"stat{i}", [128, 64], F32) for i in range(W)]
                R32 = sb_("R32", [128, 4, 64], F32)
                Rbf = sb_("Rbf", [128, 5, 4, 64], BF16)
                P.op("dve", lambda e: e.memset(R32[:], 0.0), [], ["R32"])

                def gen_normA(blk):
                    for tl in range(4):
                        t = blk * 4 + tl
                        xa, xk = xr_p[t % 2], f"xr.{t % 2}"
                        kb.dma("sp", xa[:], x_d[t * 128:(t + 1) * 128, :], [], [xk])
                        kb.act(sqj[:], xa[:], AF.Square, [xk], ["sqj", f"ss{t}"], accum=ss[:, t:t + 1])
                        yield
                        kb.ts("dve", ss[:, t:t + 1], ss[:, t:t + 1], 1.0 / D, 1e-6, ALU.mult, ALU.add, [f"ss{t}"], [f"ss{t}"])
                        yield
                        kb.tt("pool", ss[:, t:t + 1], ss[:, t:t + 1], mhalf[:, 0:1], ALU.pow, [f"ss{t}", "mhalf"], [f"ss{t}"])
                        yield
                        kb.act(hb_p[tl][:], xa[:], AF.Copy, [xk, f"ss{t}"], [f"hb.{tl}"], scale=ss[:, t:t + 1])
                        yield
                    for kc in range(KC):
                        (b,) = yield from acquire(1)
                        bankT, bk = BK.bf(b), BK.key(b)
                        for tl in range(4):
                            kb.tr(bankT[:, tl * 128:(tl + 1) * 128], hb_p[tl][:, kc * 128:(kc + 1) * 128], [f"hb.{tl}"], [bk])
                        yield
                        o = hT[:, kc, blk * 512:(blk + 1) * 512]
                        if kc % 2:
                            kb.act(o, bankT[:, 0:512], AF.Copy, [bk, "cols"], [f"hT.{blk}"], scale=cols[:, kc:kc + 1])
                        else:
                            kb.ts("dve", o, bankT[:, 0:512], cols[:, kc:kc + 1], None, ALU.mult, None, [bk, "cols"], [f"hT.{blk}"])
                        BK.put(b)
                        yield

                def post_gen(slot, b, eps, wcol0, bcol0, finish):
                    bank, bk = BK.f32(b), BK.key(b)
                    y32, k1 = y32_p[slot], f"y32.{slot}"
                    kb.cp("act", y32[:], bank, [bk], [k1])
                    BK.put(b)
                    yield
                    stt_, ks = stat_p[slot], f"stat.{slot}"
                    v3 = lambda a: a.rearrange("p (h d) -> p h d", d=64)
                    bc = lambda a: a.unsqueeze(2).to_broadcast([128, 8, 64])
                    kb.red(stt_[:, 0:8], v3(y32[:]), ALU.add, [k1], [ks])
                    sq, k2 = sq_p[slot], f"sq.{slot}"
                    kb.act(sq[:], y32[:], AF.Square, [k1], [k2])
                    yield
                    kb.red(stt_[:, 8:16], v3(sq[:]), ALU.add, [k2], [ks])
                    yield
                    kb.tt("dve", stt_[:, 24:32], stt_[:, 0:8], stt_[:, 0:8], ALU.mult, [ks], [ks])
                    kb.ts("dve", stt_[:, 32:40], stt_[:, 8:16], 1.0 / 64, eps, ALU.mult, ALU.add, [ks], [ks])
                    yield
                    kb.stt(stt_[:, 40:48], stt_[:, 24:32], -1.0 / 4096, stt_[:, 32:40], ALU.mult, ALU.add, [ks], [ks])
                    yield
                    kb.tt("pool", stt_[:, 48:56], stt_[:, 40:48], mhalf[:, 0:8], ALU.pow, [ks, "mhalf"], [ks])
                    yield
                    kb.stt(stt_[:, 56:64], stt_[:, 0:8], -1.0 / 64, stt_[:, 48:56], ALU.mult, ALU.mult, [ks], [ks])
                    kb.tt("dve", v3(y32[:]), v3(y32[:]), bc(stt_[:, 48:56]), ALU.mult, [k1, ks], [k1])
                    yield
                    ynb, k3 = ynb_p[slot], f"ynb.{slot}"
                    kb.tt("dve", v3(ynb[:]), v3(y32[:]), bc(stt_[:, 56:64]), ALU.add, [k1, ks], [k3])
                    yield
                    (b2,) = yield from acquire(1)
                    bankT, kt = BK.bf(b2), BK.key(b2)
                    for ct in range(4):
                        kb.tr(bankT[:, ct * 128:(ct + 1) * 128], ynb[:, ct * 128:(ct + 1) * 128], [k3], [kt])
                    yield
                    for ct in range(4):
                        ya, k4 = yaff_p[slot][:, ct % 2, :], f"yaff.{slot}.{ct % 2}"
                        kb.act(ya, bankT[:, ct * 128:(ct + 1) * 128], AF.Identity, [kt, "cols"], [k4],
                               scale=cols[:, wcol0 + ct:wcol0 + ct + 1], bias=cols[:, bcol0 + ct:bcol0 + ct + 1])
                        finish(ct, ya, k4)
                        if ct == 3:
                            BK.put(b2)
                        yield

                def qk_chain(slot, blk, tl, wi):
                    par = blk % 2
                    t = blk * 4 + tl
                    tok = slice(t * 128, (t + 1) * 128)
                    hk = f"hT.{blk}"
                    tabcol = 83 if wi == 0 else 91
                    (b,) = yield from acquire(1)
                    bank, bk = BK.f32(b), BK.key(b)
                    for kc in range(KC):
                        kb.mm(bank, hT[:, kc, tok], Wr[:, kc, wi * 512:(wi + 1) * 512], kc == 0, kc == KC - 1, [hk, f"W.{wi}"], [bk])
                    yield
                    xs, kx = xs_p[slot], f"xs.{slot}"
                    kb.cp("act", xs[:], bank, [bk], [kx])
                    BK.put(b)
                    yield
                    A, ka = A_p[slot], f"rA.{slot}"
                    B, kbk = B_p[slot], f"rB.{slot}"
                    v4 = lambda a: a.rearrange("p (h two f) -> p h two f", two=2, f=32)
                    cs4 = cos_t[:, t, :].unsqueeze(1).unsqueeze(1).to_broadcast([128, 8, 2, 32])
                    sn3 = sin_t[:, t, :].unsqueeze(1).to_broadcast([128, 8, 32])
                    kb.tt("dve", v4(A[:]), v4(xs[:]), cs4, ALU.mult, [kx, "cos"], [ka])
                    kb.tt("pool", v4(B[:])[:, :, 0, :], v4(xs[:])[:, :, 1, :], sn3, ALU.mult, [kx, "sin"], [kbk])
                    kb.tt("pool", v4(B[:])[:, :, 1, :], v4(xs[:])[:, :, 0, :], sn3, ALU.mult, [kx, "sin"], [kbk])
                    yield
                    kb.tt("dve", v4(A[:])[:, :, 0, :], v4(A[:])[:, :, 0, :], v4(B[:])[:, :, 0, :], ALU.subtract, [ka, kbk], [ka])
                    kb.tt("dve", v4(A[:])[:, :, 1, :], v4(A[:])[:, :, 1, :], v4(B[:])[:, :, 1, :], ALU.add, [ka, kbk], [ka])
                    yield
                    scl = cols[:, tabcol:tabcol + 8].unsqueeze(2).to_broadcast([128, 8, 64])
                    A3 = A[:].rearrange("p (h d) -> p h d", d=64)
                    if wi == 0:
                        qtv, kq = qtok_p[slot][:], f"qtok.{slot}"
                    else:
                        qtv, kq = ktok_p[par][:, tl, :], f"ktok{par}.{tl}"
                    kb.tt("dve", qtv.rearrange("p (h d) -> p h d", d=64), A3, scl, ALU.mult, [ka, "cols"], [kq])
                    yield
                    (b2,) = yield from acquire(1)
                    bankT, kt = BK.bf(b2), BK.key(b2)
                    for ct in range(4):
                        kb.tr(bankT[:, ct * 128:(ct + 1) * 128], qtv[:, ct * 128:(ct + 1) * 128], [kq], [kt])
                    yield
                    bv = bankT[:, 0:512].rearrange("p (c n) -> p c n", c=4)
                    if wi == 0:
                        kb.cp("act", qT_p[par][0:64, :, 0, tl * 128:(tl + 1) * 128], bv[0:64], [kt], [f"qT{par}.{tl}"])
                        kb.cp("dve", qT_p[par][64:128, :, 1, tl * 128:(tl + 1) * 128], bv[64:128], [kt], [f"qT{par}.{tl}"])
                    else:
                        kb.cpalt(kT_p[par][:, :, tl * 128:(tl + 1) * 128], bv, [kt], [f"kT{par}.{tl}"])
                    BK.put(b2)
                    yield

                def v_chain(blk, tl):
                    par = blk % 2
                    t = blk * 4 + tl
                    tok = slice(t * 128, (t + 1) * 128)
                    (b,) = yield from acquire(1)
                    bank, bk = BK.f32(b), BK.key(b)
                    for kc in range(KC):
                        kb.mm(bank, hT[:, kc, tok], Wr[:, kc, 1024:1536], kc == 0, kc == KC - 1, [f"hT.{blk}", "W.2"], [bk])
                    yield
                    kb.cpalt(vtok_p[par][:, tl, :], bank, [bk], [f"vtok{par}.{tl}"])
                    BK.put(b)
                    yield

                def g_chain(blk, ct):
                    par = blk % 2
                    (b,) = yield from acquire(1)
                    bank, bk = BK.f32(b), BK.key(b)
                    for kc in range(KC):
                        kb.mm(bank, Wr[:, kc, 1536 + ct * 128:1536 + (ct + 1) * 128], hT[:, kc, blk * 512:(blk + 1) * 512],
                              kc == 0, kc == KC - 1, [f"hT.{blk}", "W.3"], [bk])
                    yield
                    th, kth = th_p[ct % 2], f"thr.{ct % 2}"
                    kb.act(th[:], bank, AF.Tanh, [bk], [kth], scale=0.5)
                    yield
                    kb.ts("pool", th[:], th[:], 0.5, 0.5, ALU.mult, ALU.add, [kth], [kth])
                    yield
                    kb.tt("dve", gateT_p[par][:, ct, :], th[:], bank, ALU.mult, [kth, bk], [f"gateT{par}.{ct}"])
                    BK.put(b)
                    yield

                def gen_proj(blk):
                    for k in range(4):
                        yield from rr([qk_chain((2 * k) % W, blk, k, 0), qk_chain((2 * k + 1) % W, blk, k, 1), v_chain(blk, k), g_chain(blk, k)])

                def chunk_chain(slot, blk, tl):
                    par = blk % 2
                    qT, kT_, vtok, gateT = qT_p[par], kT_p[par], vtok_p[par], gateT_p[par]
                    t = blk * 4 + tl
                    tok = slice(t * 128, (t + 1) * 128)
                    tl_ = slice(tl * 128, (tl + 1) * 128)
                    sT, ksT = sT_p[slot], f"sT.{slot}"
                    bs = yield from acquire(2)
                    for g4 in range(2):
                        bank, bk = BK.f32(bs[g4]), BK.key(bs[g4])
                        for hh in range(4):
                            h = 4 * g4 + hh
                            kb.mm(bank[:, hh * 128:(hh + 1) * 128], kT_[:, h // 2, tl_], qT[:, h // 2, h % 2, tl_], True, True,
                                  [f"kT{par}.{tl}", f"qT{par}.{tl}"], [bk])
                    yield
                    for g4 in range(2):
                        bank, bk = BK.f32(bs[g4]), BK.key(bs[g4])
                        kb.tt("dve", sT[:, 4 * g4:4 * g4 + 4, :], bank.rearrange("p (c n) -> p c n", c=4),
                              maskI.unsqueeze(1).to_broadcast([128, 4, 128]), ALU.mult, [bk, "cqf"], [ksT])
                        BK.put(bs[g4])
                        yield
                    (bo,) = yield from acquire(1)
                    bankO, ko = BK.f32(bo), BK.key(bo)
                    for h in range(8):
                        kb.mm(bankO[:, 64 * h:64 * h + 64], sT[:, h, :], vtok[:, tl, 64 * h:64 * h + 64], True, t == 0,
                              [ksT, f"vtok{par}.{tl}"], [ko])
                        if t > 0:
                            kb.mm(bankO[:, 64 * h:64 * h + 64], qT[:, h // 2, h % 2, tl_], Rbf[:, t % 5, h // 2, :], False, True,
                                  [f"qT{par}.{tl}", f"Rbf.{t % 5}"], [ko])
                    yield

                    def fin(ct, ya, k4):
                        kb.tt("dve", yT[:, ct, tok], ya, gateT[:, ct, tl_], ALU.mult, [k4, f"gateT{par}.{ct}"], [f"yT.{t}.0"])
                    yield from post_gen(slot, bo, 1e-5, 32, 36, fin)

                def gen_chunks(blk):
                    par = blk % 2
                    ktok, vtok = ktok_p[par], vtok_p[par]
                    for tl in range(4):
                        t = blk * 4 + tl
                        if t == NT - 1:
                            continue
                        (b,) = yield from acquire(1)
                        bankK, kk_ = BK.f32(b), BK.key(b)
                        for ct in range(4):
                            kb.mm(bankK[:, ct * 128:(ct + 1) * 128], ktok[:, tl, ct * 128:(ct + 1) * 128], vtok[:, tl, ct * 128:(ct + 1) * 128],
                                  True, True, [f"ktok{par}.{tl}", f"vtok{par}.{tl}"], [kk_])
                        yield
                        for hh in range(2):
                            b0 = 64 * hh
                            kb.tt("dve", R32[b0:b0 + 64, :, :], bankK[b0:b0 + 64, :].rearrange("p (c n) -> p c n", c=4)[:, :, b0:b0 + 64],
                                  R32[b0:b0 + 64, :, :], ALU.add, [kk_, "R32"], ["R32"])
                        BK.put(b)
                        yield
                        kb.tt("dve", R32[:], R32[:], cols[:, 99:103].unsqueeze(2).to_broadcast([128, 4, 64]), ALU.mult, ["R32", "cols"], ["R32"])
                        yield
                        kb.cp("act", Rbf[:, (t + 1) % 5, :, :], R32[:], ["R32"], [f"Rbf.{(t + 1) % 5}"])
                        yield
                    yield from rr([chunk_chain(0, blk, 0), chunk_chain(1, blk, 1)])
                    yield from rr([chunk_chain(0, blk, 2), chunk_chain(1, blk, 3)])

                run(gen_normA(0))
                interleave([gen_proj(0), gen_normA(1)])
                for blk in range(4):
                    if blk == 3 and "C" in stages:
                        Wk = Wv8(1824)
                        kb.dma("pool", Wk[:, :, :], wview(w_in_d)[:, :, 2048:3872], [], ["Wk", "W.0", "W.1", "W.2", "W.3"])
                    gs = [gen_chunks(blk)]
                    if blk + 1 < 4:
                        gs.append(gen_proj(blk + 1))
                    if blk + 2 < 4:
                        gs.append(gen_normA(blk + 2))
                    interleave(gs)
                if dbg:
                    kb.dma("sp", dbg_hT, hT[:], [f"hT.{b}" for b in range(4)], [])
                P.barrier()
                P.flush()

        if "C" in stages:
            stage_C(nc, P, kb, locals())
        else:
            P.op("dve", lambda e: e.memset(hT[:, 0:4, :], 0.0), [f"hT.{b}" for b in range(4)], [f"yT.{t}.1" for t in range(NT)])

        if dbg:
            kb.dma("sp", dbg_yT[:, 0:4, :], yT[:], [f"yT.{t}.0" for t in range(NT)], [])
            kb.dma("sp", dbg_yT[:, 4:8, :], hT[:, 0:4, :], [f"yT.{t}.1" for t in range(NT)], [])

        if "D" in stages:
            stage_D(nc, P, kb, locals())
        P.barrier()
        P.flush()
    return nc


def stage_C(nc, P, kb, L):
    cols, dc, cqf, hT, WA, mhalf, bonesb, BK = (L[k] for k in ("cols", "dc", "cqf", "hT", "WA", "mhalf", "bonesb", "L_BK"))
    wup_d, aup_d, gup_d, w_in_d = L["wup_d"], L["aup_d"], L["gup_d"], L["w_in_d"]
    mask4, maskLs, identf = L["mask4"], L["maskLs"], L["identf"]
    onesf = cqf[:, 896:1024]
    Wk = WA[:, 0:KC * 1824].rearrange("p (k n) -> p k n", k=KC)
    CL = 0.5 * math.exp(-0.5)
    NB = 256
    NBLK = T // NB
    with contextlib.ExitStack() as sc:
        if "B" not in L["stages"]:
            kb.dma("pool", Wk[:, :, :], w_in_d.rearrange("(kc p) n -> p kc n", p=128)[:, :, 2048:3872], [], ["Wk"])
        sb = lambda n, s, d: kb.sb(n, s, d, st=sc)
        wupz = sb("wupz", [128, 512], BF16)
        aupz = sb("aupz", [128, 512], BF16)
        gup1b = sb("gup1b", [128, 512], BF16)
        gup2z = sb("gup2z", [128, 512], BF16)
        for tz in (wupz, aupz, gup2z):
            P.op("dve", lambda e, tz=tz: e.memset(tz[:], 0.0), [], ["wupb", "gupb"])
        kb.dma("pool", wupz[0:64, :], wup_d, [], ["wupb"])
        kb.dma("pool", aupz[64:128, :], aup_d, [], ["wupb"])
        kb.dma("pool", gup1b[:], gup_d[0:128, :], [], ["gupb"])
        kb.dma("pool", gup2z[96:128, :], gup_d[128:160, :], [], ["gupb"])
        RKblk = sb("RKblk", [128, 4, 128], BF16)
        for ct in range(4):
            kb.ts("dve", RKblk[:, ct, :], cqf[:, 768:896], cols[:, 71 + ct:72 + ct], None, ALU.mult, None, ["cqf", "cols"], ["RKblk"])
        pT = sb("pT", [128, 15, NB + 1], F32)
        P.op("dve", lambda e: e.memset(pT[:], 0.0), [], [f"pT.{i}" for i in range(15)])
        carry = sb("carry", [128, 15], F32)
        NTMP = 10
        T_p = [[sb(f"T{c}_{i}", [128, NB], F32) for i in range(NTMP)] for c in range(2)]
        sh_p = [sb(f"shT{i}", [128, NB], F32) for i in range(2)]
        kk2_p = [sb(f"kk2{i}", [128, NB], BF16) for i in range(2)]
        twa = sb("twa", [128, NB], BF16)
        sg1 = sb("sg1", [128, NB], BF16)
        sg2 = sb("sg2", [128, NB], BF16)
        ARt = sb("ARt", [128, 4, 2, 2, 2, 128], BF16)
        P.op("dve", lambda e: e.memset(ARt[:], 0.0), [], [f"ARt.{i}" for i in range(4)])
        KT = sb("KT", [128, 4, NB], BF16)
        BnT = sb("BnT", [128, 4, NB], BF16)
        rkb = sb("rkb", [128, 4, NB], BF16)
        vbf_p = [sb(f"vbf{i}", [128, 4, NB], BF16) for i in range(2)]
        gT = sb("gT", [128, 4, NB], BF16)
        Ktok = sb("Ktok", [128, 2, 512], BF16)
        Btok = sb("Btok", [128, 2, 512], BF16)
        Vtok = sb("Vtok", [128, 2, 512], BF16)
        PCt = sb("PCt", [128, 4, 2], F32)
        MTs = [sb(f"MT{i}", [128, 8, 512], BF16) for i in range(2)]
        mraw_p = [sb(f"mraw{i}", [128, 512], BF16) for i in range(2)]
        mask4b = sb("mask4b", [128, 512], BF16)
        kb.dma("pool", mask4b[:], L["cq_d"][:, 128:640], [], ["mask4b"])
        Xm = [sb(f"Xm{i}", [128, 8, 128], BF16) for i in range(2)]
        Nm = [sb(f"Nm{i}", [128, 8, 128], BF16) for i in range(2)]
        Lm = [sb(f"Lm{i}", [128, 8, 128], BF16) for i in range(2)]
        TTs = [sb(f"TT{i}", [128, 8, 128], BF16) for i in range(2)]
        RHSs = [sb(f"RHSb{i}", [128, 512], BF16) for i in range(2)]
        Ubs = [sb(f"Ub{i}", [128, 512], BF16) for i in range(2)]
        S32 = sb("S32", [128, 4, 64], F32)
        Sbf = sb("Sbf", [128, 4, 64], BF16)
        P.op("dve", lambda e: e.memset(S32[:], 0.0), [], ["S32"])
        y32 = sb("cy32", [128, 512], F32)
        sq = sb("csq", [128, 512], F32)
        ynb = sb("cynb", [128, 512], BF16)
        yaff = sb("cyaff", [128, 4, 128], F32)
        t1b = sb("ct1", [128, 4, 128], F32)
        stt_ = sb("cstat", [128, 64], F32)

        def gen_AB(blk):
            tok0 = blk * NB
            hk = f"hT.{tok0 // 512}"
            for i0 in range(0, 15, 2):
                b = BK.get()
                bank, bk = BK.f32(b), BK.key(b)
                idxs = [i for i in (i0, i0 + 1) if i < 15]
                for j, idx in enumerate(idxs):
                    c0 = idx * 128 if idx < 14 else 1696
                    for kc in range(KC):
                        kb.mm(bank[:, j * NB:(j + 1) * NB], Wk[:, kc, c0:c0 + 128], hT[:, kc, tok0:tok0 + NB],
                              kc == 0, kc == KC - 1, [hk, "Wk"] + (["hTu"] if kc >= 4 else []), [bk])
                yield
                for j, idx in enumerate(idxs):
                    kb.cp("act", pT[:, idx, 1:NB + 1], bank[:, j * NB:(j + 1) * NB], [bk], [f"pT.{idx}"])
                BK.put(b)
                yield
            allp = [f"pT.{i}" for i in range(15)]
            kb.cp("dve", carry[:], pT[:, :, NB], allp, ["carry"])
            yield
            for idx in range(15):
                tmp, ktm = sh_p[idx % 2], f"shT.{idx % 2}"
                kb.act(tmp[:], pT[:, idx, 0:NB], AF.Copy, [f"pT.{idx}", "cols"], [ktm], scale=cols[:, 40 + idx:41 + idx])
                yield
                if 8 <= idx < 12:
                    kb.stt(vbf_p[blk % 2][:, idx - 8, :], pT[:, idx, 1:NB + 1], dc[:, idx:idx + 1], tmp[:], ALU.mult, ALU.add,
                           [f"pT.{idx}", "dc", ktm, "carry"], [f"vbf{blk % 2}.{idx - 8}"])
                else:
                    kb.stt(pT[:, idx, 1:NB + 1], pT[:, idx, 1:NB + 1], dc[:, idx:idx + 1], tmp[:], ALU.mult, ALU.add,
                           [f"pT.{idx}", "dc", ktm, "carry"], [f"pT.{idx}"])
                yield
            kb.cp("dve", pT[:, :, 0], carry[:], ["carry"], allp)
            yield

        ps = lambda idx: pT[:, idx, 1:NB + 1]

        def gen_lora(blk):
            kb.act(twa[0:64, :], pT[0:64, 12, 1:NB + 1], AF.Tanh, ["pT.12"], ["twa"])
            kb.cp("dve", twa[64:128, :], pT[64:128, 12, 1:NB + 1], ["pT.12"], ["twa"])
            yield
            th, kth = sh_p[0], "shT.0"
            kb.act(th[:], ps(13), AF.Tanh, ["pT.13"], [kth], scale=0.5)
            th2, kth2 = sh_p[1], "shT.1"
            kb.act(th2[:], ps(14), AF.Tanh, ["pT.14"], [kth2], scale=0.5)
            yield
            kb.ts("dve", sg1[:], th[:], 0.5, 0.5, ALU.mult, ALU.add, [kth], ["sg1"])
            kb.ts("pool", sg2[:], th2[:], 0.5, 0.5, ALU.mult, ALU.add, [kth2], ["sg2"])
            yield

        def gen_ct(ch, blk, ct):
            Tb = T_p[ch]
            tk = lambda i: f"T{ch}.{i}"
            cs = slice(ct * 128, (ct + 1) * 128)
            r_, k_, v_ = ps(ct), ps(4 + ct), ps(8 + ct)
            kr, kk_, kv = f"pT.{ct}", f"pT.{4 + ct}", f"pT.{8 + ct}"
            bz, bg = BK.get(), BK.get()
            bankZ, kz = BK.f32(bz), BK.key(bz)
            bankG, kg = BK.f32(bg), BK.key(bg)
            kb.mm(bankZ[:, 0:NB], wupz[:, cs], twa[:], True, True, ["wupb", "twa"], [kz])
            kb.mm(bankZ[:, NB:2 * NB], aupz[:, cs], twa[:], True, True, ["wupb", "twa"], [kz])
            kb.mm(bankG[:, 0:NB], gup1b[:, cs], sg1[:], True, False, ["gupb", "sg1"], [kg])
            kb.mm(bankG[:, 0:NB], gup2z[:, cs], sg2[:], False, True, ["gupb", "sg2"], [kg])
            yield
            thw, tha = Tb[0], Tb[1]
            kb.act(thw[:], bankZ[:, 0:NB], AF.Tanh, [kz, "dc"], [tk(0)], scale=0.5, bias=dc[:, 15 + ct:16 + ct])
            kb.act(tha[:], bankZ[:, NB:2 * NB], AF.Tanh, [kz, "dc"], [tk(1)], scale=0.5, bias=dc[:, 19 + ct:20 + ct])
            BK.put(bz)
            kk = Tb[4]
            kb.ts("pool", kk[:], k_, cols[:, 63 + ct:64 + ct], 0.0, ALU.mult, ALU.add, [kk_, "cols"], [tk(4)])
            yield
            kb.cp("act", gT[:, ct, :], bankG[:, 0:NB], [kg], [f"gT.{ct}"])
            BK.put(bg)
            kk2, k_kk2 = kk2_p[ch], f"kk2.{ch}"
            kb.act(kk2[:], kk[:], AF.Square, [tk(4)], [k_kk2])
            ld = Tb[0]
            kb.ts("dve", ld[:], thw[:], -CL, -CL, ALU.mult, ALU.add, [tk(0)], [tk(0)])
            yield
            bn_ = BK.get()
            bankN, kn = BK.f32(bn_), BK.key(bn_)
            kb.mm(bankN[:, 0:NB], bonesb[:], kk2[:], True, True, ["bonesb", k_kk2], [kn])
            cum = Tb[2]
            for c in range(2):
                ld_c, cum_c = ld[:, c * 128:(c + 1) * 128], cum[:, c * 128:(c + 1) * 128]
                P.op("dve", lambda e, cum_c=cum_c, ld_c=ld_c: e.tensor_tensor_scan(out=cum_c, data0=onesf, data1=ld_c, initial=0.0, op0=ALU.mult, op1=ALU.add),
                     [tk(0), "cqf"], [tk(2)])
            an = Tb[1]
            kb.ts("pool", an[:], tha[:], -0.5, -0.5, ALU.mult, ALU.add, [tk(1)], [tk(1)])
            yield
            sn = Tb[5]
            kb.act(sn[:], bankN[:, 0:NB], AF.Sqrt, [kn], [tk(5)])
            BK.put(bn_)
            cumex = Tb[3]
            kb.tt("pool", cumex[:], cum[:], ld[:], ALU.subtract, [tk(2), tk(0)], [tk(3)])
            yield
            ep, en, ex = Tb[6], Tb[7], Tb[8]
            kb.act(ep[:], cum[:], AF.Exp, [tk(2)], [tk(6)])
            kb.act(en[:], cum[:], AF.Exp, [tk(2)], [tk(7)], scale=-1.0)
            kb.ts("dve", sn[:], sn[:], 1e-12, None, ALU.max, None, [tk(5)], [tk(5)])
            tmp2 = Tb[9]
            kb.ts("pool", tmp2[:], an[:], dc[:, 23 + ct:24 + ct], dc[:, 27 + ct:28 + ct], ALU.mult, ALU.add, [tk(1), "dc"], [tk(9)])
            yield
            kb.act(ex[:], cumex[:], AF.Exp, [tk(3)], [tk(8)])
            P.op("dve", lambda e: e.reciprocal(out=sn[:], in_=sn[:]), [tk(5)], [tk(5)])
            k2 = Tb[9]
            kb.tt("pool", k2[:], k_, tmp2[:], ALU.mult, [kk_, tk(9)], [tk(9)])
            yield
            kkn = Tb[4]
            kb.tt("dve", kkn[:], kk[:], sn[:], ALU.mult, [tk(4), tk(5)], [tk(4)])
            c3 = lambda a: a.rearrange("p (c n) -> p c n", c=2)
            kb.cp("pool", PCt[:, ct, :], c3(ep[:])[:, :, 127], [tk(6)], [f"PCt.{ct}"])
            yield
            bn = Tb[5]
            kb.tt("pool", bn[:], kkn[:], an[:], ALU.mult, [tk(4), tk(1), tk(5)], [tk(5)])
            kb.tt("dve", KT[:, ct, :], k2[:], en[:], ALU.mult, [tk(9), tk(7)], [f"KT.{ct}"])
            yield
            for hh in range(2):
                pp = slice(64 * hh, 64 * hh + 64)
                kb.tt("dve", ARt[pp, ct, :, hh, 1, :], c3(r_)[pp], c3(ep[:])[pp], ALU.mult, [kr, tk(6)], [f"ARt.{ct}"])
                kb.tt("dve" if hh == 0 else "pool", ARt[pp, ct, :, hh, 0, :], c3(kkn[:])[pp], c3(ex[:])[pp], ALU.mult, [tk(4), tk(8)], [f"ARt.{ct}"])
                yield
            kb.tt("dve", BnT[:, ct, :], bn[:], en[:], ALU.mult, [tk(5), tk(7)], [f"BnT.{ct}"])
            kb.tt("pool", rkb[:, ct, :], r_, k2[:], ALU.mult, [kr, tk(9)], [f"rkb.{ct}"])
            yield

        def gen_tok(blk):
            for c in range(2):
                cs = slice(c * 128, (c + 1) * 128)
                for src, dst, nm, sk in ((KT, Ktok, "KT", "KT"), (BnT, Btok, "BnT", "BnT"), (vbf_p[blk % 2], Vtok, "vbf", f"vbf{blk % 2}")):
                    b = BK.get()
                    bankT, kt = BK.bf(b), BK.key(b)
                    for ct in range(4):
                        kb.tr(bankT[:, ct * 128:(ct + 1) * 128], src[:, ct, cs], [f"{sk}.{ct}"], [kt])
                    yield
                    kb.cpalt(dst[:, c, :], bankT[:, 0:512], [kt], [f"{nm}tok.{c}"])
                    BK.put(b)
                    yield

        def gen_F1(blk, c):
            cs = slice(c * 128, (c + 1) * 128)
            MT = MTs[c]
            for h in range(8):
                ct = h // 2
                b = BK.get()
                bankM, km = BK.f32(b), BK.key(b)
                rhsAR = ARt[:, ct, c, h % 2, :, :].rearrange("p a b -> p (a b)")
                kb.mm(bankM[:, 0:256], BnT[:, ct, cs], rhsAR, True, True, [f"BnT.{ct}", f"ARt.{ct}"], [km])
                kb.mm(bankM[:, 256:512], KT[:, ct, cs], rhsAR, True, True, [f"KT.{ct}", f"ARt.{ct}"], [km])
                yield
                if True:
                    kb.tt("dve", MT[:, h, :], bankM, mask4, ALU.mult, [km, "cqf"], [f"MT{c}.{h}"])
                    BK.put(b)
                else:
                    mr, kmr = mraw_p[(h // 2) % 2], f"mraw.{(h // 2) % 2}"
                    kb.cp("act", mr[:], bankM, [km], [kmr])
                    BK.put(b)
                    yield
                    kb.tt("pool", MT[:, h, :], mr[:], mask4b[:], ALU.mult, [kmr, "mask4b"], [f"MT{c}.{h}"])
                yield
            for g4 in range(2):
                b = BK.get()
                bankL, kl = BK.f32(b), BK.key(b)
                for hh in range(4):
                    h = 4 * g4 + hh
                    kb.mm(bankL[:, hh * 128:(hh + 1) * 128], ARt[:, h // 2, c, h % 2, 0, :], BnT[:, h // 2, cs], True, True,
                          [f"ARt.{h // 2}", f"BnT.{h // 2}"], [kl])
                yield
                kb.tt("dve", Lm[0][:, 4 * g4:4 * g4 + 4, :], bankL.rearrange("p (c n) -> p c n", c=4),
                      maskLs.unsqueeze(1).to_broadcast([128, 4, 128]), ALU.mult, [kl, "cqf"], [f"Lm0.{g4}"])
                BK.put(b)
                hs = slice(4 * g4, 4 * g4 + 4)
                mtk = [f"MT{c}.{h}" for h in range(4 * g4, 4 * g4 + 4)]
                kb.tt("dve", Xm[1][:, hs, :], MT[:, hs, 0:128], identf.unsqueeze(1).to_broadcast([128, 4, 128]), ALU.add,
                      mtk + ["cqf"], [f"Xm1.{g4}"])
                yield

        def gen_inv_group(c, g4):
            hs = slice(4 * g4, 4 * g4 + 4)
            v4 = lambda bank: bank.rearrange("p (c n) -> p c n", c=4)
            cur = 0
            for lvl in range(7):
                nxt = cur ^ 1
                kN, kL, kX = f"Nm{cur}.{g4}", f"Lm{cur}.{g4}", f"Xm{cur}.{g4}"
                if lvl == 0:
                    Ncur = MTs[c][:, :, 0:128]
                    kNl = [f"MT{c}.{h}" for h in range(4 * g4, 4 * g4 + 4)]
                else:
                    Ncur, kNl = Nm[cur], [kN]
                last = lvl == 6
                Xdst, kXd = (TTs[c], f"TT{c}.{g4}") if last else (Xm[nxt], f"Xm{nxt}.{g4}")
                bx = ba = bb = None
                if lvl >= 1:
                    bx = BK.get()
                    bankX, kbx = BK.f32(bx), BK.key(bx)
                    for hh in range(4):
                        h = 4 * g4 + hh
                        kb.mm(bankX[:, hh * 128:(hh + 1) * 128], Lm[cur][:, h, :], Xm[cur][:, h, :], True, True, [kL, kX], [kbx])
                if not last:
                    ba, bb = BK.get(), BK.get()
                    bankA, kba = BK.f32(ba), BK.key(ba)
                    bankB, kbb = BK.f32(bb), BK.key(bb)
                    for hh in range(4):
                        h = 4 * g4 + hh
                        kb.mm(bankA[:, hh * 128:(hh + 1) * 128], Lm[cur][:, h, :], Ncur[:, h, :], True, True, [kL] + kNl, [kba])
                    for hh in range(4):
                        h = 4 * g4 + hh
                        kb.mm(bankB[:, hh * 128:(hh + 1) * 128], Ncur[:, h, :], Lm[cur][:, h, :], True, True, [kL] + kNl, [kbb])
                yield
                if lvl >= 1:
                    kb.tt("dve", Xdst[:, hs, :], v4(bankX), Xm[cur][:, hs, :], ALU.add, [kbx, kX], [kXd])
                    BK.put(bx)
                else:
                    pass
                if not last:
                    kb.cp("act", Nm[nxt][:, hs, :], v4(bankA), [kba], [f"Nm{nxt}.{g4}"])
                    BK.put(ba)
                    if g4 == 0:
                        kb.cp("act", Lm[nxt][:, hs, :], v4(bankB), [kbb], [f"Lm{nxt}.{g4}"])
                    else:
                        kb.cp("dve", Lm[nxt][:, hs, :], v4(bankB), [kbb], [f"Lm{nxt}.{g4}"])
                    BK.put(bb)
                yield
                cur = nxt

        def gen_inv(blk, c):
            gs = [gen_inv_group(c, 0), gen_inv_group(c, 1)]
            while gs:
                for g in list(gs):
                    try:
                        next(g)
                    except StopIteration:
                        gs.remove(g)
                yield

        def gen_seqpost(blk, c):
            n_ch = blk * 2 + c
            cs = slice(c * 128, (c + 1) * 128)
            tok = slice(blk * NB + c * 128, blk * NB + (c + 1) * 128)
            MT, TT, RHSb, Ub = MTs[c], TTs[c], RHSs[c], Ubs[c]
            kTT = [f"TT{c}.0", f"TT{c}.1"]
            kRH, kUb = f"RHSb{c}", f"Ub{c}"
            br = BK.get()
            bankR, kr_ = BK.f32(br), BK.key(br)
            for h in range(8):
                ct = h // 2
                o = bankR[:, 64 * h:64 * h + 64]
                if n_ch > 0:
                    kb.mm(o, ARt[:, ct, c, h % 2, 0, :], Sbf[:, ct, :], True, False, [f"ARt.{ct}", "Sbf"], [kr_])
                kb.mm(o, MT[:, h, 256:384], Vtok[:, c, 64 * h:64 * h + 64], n_ch == 0, True, [f"MT{c}.{h}", f"vbftok.{c}"], [kr_])
            yield
            kb.cp("act", RHSb[:], bankR, [kr_], [kRH])
            BK.put(br)
            yield
            bu = BK.get()
            bankU, ku = BK.f32(bu), BK.key(bu)
            for h in range(8):
                kb.mm(bankU[:, 64 * h:64 * h + 64], TT[:, h, :], RHSb[:, 64 * h:64 * h + 64], True, True, kTT + [kRH], [ku])
            yield
            kb.cp("act", Ub[:], bankU, [ku], [kUb])
            BK.put(bu)
            yield
            if n_ch < NT - 1:
                bs_ = BK.get()
                bankS, ks_ = BK.f32(bs_), BK.key(bs_)
                for ct in range(4):
                    o = bankS[:, ct * 128:(ct + 1) * 128]
                    kb.mm(o, Btok[:, c, ct * 128:(ct + 1) * 128], Ub[:, ct * 128:(ct + 1) * 128], True, False, [f"BnTtok.{c}", kUb], [ks_])
                    kb.mm(o, Ktok[:, c, ct * 128:(ct + 1) * 128], Vtok[:, c, ct * 128:(ct + 1) * 128], False, True, [f"KTtok.{c}", f"vbftok.{c}"], [ks_])
            by = BK.get()
            bankY, ky = BK.f32(by), BK.key(by)
            for h in range(8):
                ct = h // 2
                o = bankY[:, 64 * h:64 * h + 64]
                if n_ch > 0:
                    kb.mm(o, ARt[:, ct, c, h % 2, 1, :], Sbf[:, ct, :], True, False, [f"ARt.{ct}", "Sbf"], [ky])
                kb.mm(o, MT[:, h, 128:256], Ub[:, 64 * h:64 * h + 64], n_ch == 0, False, [f"MT{c}.{h}", kUb], [ky])
                kb.mm(o, MT[:, h, 384:512], Vtok[:, c, 64 * h:64 * h + 64], False, True, [f"MT{c}.{h}", f"vbftok.{c}"], [ky])
            yield
            if n_ch < NT - 1:
                for hh in range(2):
                    b0 = 64 * hh
                    kb.tt("dve", S32[b0:b0 + 64, :, :], bankS[b0:b0 + 64, :].rearrange("p (c n) -> p c n", c=4)[:, :, b0:b0 + 64],
                          S32[b0:b0 + 64, :, :], ALU.add, [ks_, "S32"], ["S32"])
                BK.put(bs_)
                kb.tt("dve", S32[:], S32[:], PCt[:, :, c].unsqueeze(2).to_broadcast([128, 4, 64]), ALU.mult,
                      ["S32"] + [f"PCt.{ct}" for ct in range(4)], ["S32"])
                kb.cp("act", Sbf[:], S32[:], ["S32"], ["Sbf"])
                yield
            k1, k2, ks, k3 = "cy32", "csq", "cstat", "cynb"
            kb.cp("act", y32[:], bankY, [ky], [k1])
            BK.put(by)
            yield
            v3 = lambda a: a.rearrange("p (h d) -> p h d", d=64)
            bc = lambda a: a.unsqueeze(2).to_broadcast([128, 8, 64])
            kb.red(stt_[:, 0:8], v3(y32[:]), ALU.add, [k1], [ks])
            kb.act(sq[:], y32[:], AF.Square, [k1], [k2])
            yield
            kb.red(stt_[:, 8:16], v3(sq[:]), ALU.add, [k2], [ks])
            yield
            kb.tt("dve", stt_[:, 24:32], stt_[:, 0:8], stt_[:, 0:8], ALU.mult, [ks], [ks])
            kb.ts("dve", stt_[:, 32:40], stt_[:, 8:16], 1.0 / 64, 64e-5, ALU.mult, ALU.add, [ks], [ks])
            yield
            kb.stt(stt_[:, 40:48], stt_[:, 24:32], -1.0 / 4096, stt_[:, 32:40], ALU.mult, ALU.add, [ks], [ks])
            yield
            kb.tt("pool", stt_[:, 48:56], stt_[:, 40:48], mhalf[:, 0:8], ALU.pow, [ks, "mhalf"], [ks])
            yield
            kb.stt(stt_[:, 56:64], stt_[:, 0:8], -1.0 / 64, stt_[:, 48:56], ALU.mult, ALU.mult, [ks], [ks])
            kb.tt("dve", v3(y32[:]), v3(y32[:]), bc(stt_[:, 48:56]), ALU.mult, [k1, ks], [k1])
            yield
            kb.tt("dve", v3(ynb[:]), v3(y32[:]), bc(stt_[:, 56:64]), ALU.add, [k1, ks], [k3])
            yield
            b2 = BK.get()
            bankT, kt = BK.bf(b2), BK.key(b2)
            for ct in range(4):
                kb.tr(bankT[:, ct * 128:(ct + 1) * 128], ynb[:, ct * 128:(ct + 1) * 128], [k3], [kt])
            bbo = BK.get()
            bankBo, kbo = BK.f32(bbo), BK.key(bbo)
            for ct in range(4):
                kb.mm(bankBo[:, ct * 128:(ct + 1) * 128], RKblk[:, ct, :], rkb[:, ct, cs], True, True, ["RKblk", f"rkb.{ct}"], [kbo])
            yield
            for ct in range(4):
                kb.act(yaff[:, ct, :], bankT[:, ct * 128:(ct + 1) * 128], AF.Identity, [kt, "cols"], ["cyaff"],
                       scale=cols[:, 75 + ct:76 + ct], bias=cols[:, 79 + ct:80 + ct])
            kb.tt("dve", t1b[:], bankBo.rearrange("p (c n) -> p c n", c=4), vbf_p[blk % 2][:, :, cs], ALU.mult,
                  [kbo] + [f"vbf{blk % 2}.{ct}" for ct in range(4)], ["ct1"])
            BK.put(b2)
            BK.put(bbo)
            yield
            kb.tt("dve", t1b[:], t1b[:], yaff[:], ALU.add, ["ct1", "cyaff"], ["ct1"])
            yield
            kb.tt("dve", hT[:, 0:4, tok], t1b[:], gT[:, :, cs], ALU.mult, ["ct1"] + [f"gT.{ct}" for ct in range(4)],
                  [f"yT.{n_ch}.1", f"hT.{n_ch // 4}"])
            yield

        def gen_CD(blk):
            yield from gen_lora(blk)
            for pair in range(2):
                gs = [gen_ct(0, blk, 2 * pair), gen_ct(1, blk, 2 * pair + 1)]
                while gs:
                    for g in list(gs):
                        try:
                            next(g)
                        except StopIteration:
                            gs.remove(g)
                    yield
            yield from gen_tok(blk)

        def run(g):
            for _ in g:
                pass

        run(gen_AB(0))
        run(gen_CD(0))
        for blk in range(NBLK):
            run(gen_F1(blk, 0))
            interleave([gen_inv(blk, 0), gen_F1(blk, 1)])
            interleave([gen_seqpost(blk, 0), gen_inv(blk, 1)])
            if blk + 1 < NBLK:
                interleave([gen_seqpost(blk, 1), gen_AB(blk + 1)])
                if blk + 1 == NBLK - 1 and "D" in L["stages"]:
                    wv = lambda w: w.rearrange("(kc p) n -> p kc n", p=128)
                    Wout = WA[:, 0:8192].rearrange("p (k n) -> p k n", k=KC)
                    WkvK = WA[:, 8192:16384].rearrange("p (k n) -> p k n", k=KC)
                    WkvV = hT[:, 4:8, :].rearrange("p a (b n) -> p (a b) n", n=1024)
                    kb.dma("pool", Wout, wv(L["w_out_d"]), [], ["WA.0", "Wk"])
                    kb.dma("pool", WkvK, wv(L["wkv_d"])[:, :, 0:1024], [], ["WA.1", "Wk"])
                    kb.dma("pool", WkvV, wv(L["wkv_d"])[:, :, 1024:2048], [], ["WkvV", "hTu"])
                run(gen_CD(blk + 1))
            else:
                run(gen_seqpost(blk, 1))
        P.barrier()
        P.flush()


def stage_D(nc, P, kb, L):
    cols, cqf, hT, yT, WA, mhalf, onesb, ss, BK = (L[k] for k in ("cols", "cqf", "hT", "yT", "WA", "mhalf", "onesb", "ss", "L_BK"))
    x_d, mem_d, gfin_d, out_d = L["x_d"], L["mem_d"], L["gfin_d"], L["out_d"]
    w_out_d, wq_d, wkv_d, wo_d, wupm_d, wdn_d = (L[k] for k in ("w_out_d", "wq_d", "wkv_d", "wo_d", "wupm_d", "wdn_d"))
    wview = lambda w: w.rearrange("(kc p) n -> p kc n", p=128)

    def run(g):
        for _ in g:
            pass

    with contextlib.ExitStack() as sd:
        sb = lambda n, s, d, st=sd: kb.sb(n, s, d, st=st)
        xres = sb("xres", [128, NT, D], F32)
        gfin = sb("gfin", [128, D], F32)
        kTm = sb("kTm", [128, KC, 256], BF16)
        vmem = sb("vmem", [128, 2, D], BF16)
        kmx = sb("kmx", [128, 4], F32)
        hb_p = [sb(f"dhb{i}", [128, D], BF16) for i in range(4)]
        sqj = sb("dsqj", [128, D], BF16)
        W0 = WA[:, 0:8192].rearrange("p (k n) -> p k n", k=KC)
        W1 = WA[:, 8192:16384].rearrange("p (k n) -> p k n", k=KC)
        Wout, WkvK = W0, W1
        WkvV = hT[:, 4:8, :].rearrange("p a (b n) -> p (a b) n", n=1024)
        xv = x_d.rearrange("(t p) d -> p t d", p=128)
        kb.dma("sp", xres[:, 0:4, :], xv[:, 0:4, :], [], [f"xres.{t}" for t in range(0, 4)])
        if "C" not in L["stages"]:
            kb.dma("pool", Wout, wview(w_out_d), [], ["WA.0"])
            kb.dma("pool", WkvK, wview(wkv_d)[:, :, 0:1024], [], ["WA.1"])
            kb.dma("pool", WkvV, wview(wkv_d)[:, :, 1024:2048], [], ["WkvV"])
        kb.dma("sp", gfin[:], gfin_d.partition_broadcast(128), [], ["gfin"])

        def gen_norm(tiles, src, gcol0, dst_fn, dkey, sscol0):
            for tl, t in enumerate(tiles):
                xa, xk = src(t)
                c = sscol0 + t
                xks = xk if isinstance(xk, list) else [xk]
                kb.act(sqj[:], xa, AF.Square, xks, ["sqj", f"ss{c}"], accum=ss[:, c:c + 1])
                yield
                kb.ts("dve", ss[:, c:c + 1], ss[:, c:c + 1], 1.0 / D, 1e-6, ALU.mult, ALU.add, [f"ss{c}"], [f"ss{c}"])
                yield
                kb.tt("pool", ss[:, c:c + 1], ss[:, c:c + 1], mhalf[:, 0:1], ALU.pow, [f"ss{c}", "mhalf"], [f"ss{c}"])
                yield
                kb.act(hb_p[tl][:], xa, AF.Copy, xks + [f"ss{c}"], [f"dhb.{tl}"], scale=ss[:, c:c + 1])
                yield
            n = len(tiles) * 128
            for kc in range(KC):
                b = BK.get()
                bankT, bk = BK.bf(b), BK.key(b)
                for tl in range(len(tiles)):
                    kb.tr(bankT[:, tl * 128:(tl + 1) * 128], hb_p[tl][:, kc * 128:(kc + 1) * 128], [f"dhb.{tl}"], [bk])
                yield
                o = dst_fn(kc)
                dk = dkey if isinstance(dkey, list) else [dkey]
                if kc % 2:
                    kb.act(o, bankT[:, 0:n], AF.Copy, [bk, "cols"], dk, scale=cols[:, gcol0 + kc:gcol0 + kc + 1])
                else:
                    kb.ts("dve", o, bankT[:, 0:n], cols[:, gcol0 + kc:gcol0 + kc + 1], None, ALU.mult, None, [bk, "cols"], dk)
                BK.put(b)
                yield

        oTb_t = sb("oTb", [128, KC * 512], BF16)
        aT_t = sb("aT_all", [128, 8 * 512], BF16)
        if True:
            memx = [oTb_t[:, 2048 * i:2048 * (i + 1)].bitcast(F32) for i in range(2)]
            mxk = [[f"oTb.{k}" for k in range(4 * i, 4 * i + 4)] for i in range(2)]
            memT = aT_t[:, 0:2048].rearrange("p (k n) -> p k n", k=KC)
            k2m = aT_t[:, 2048:4096].rearrange("p (k n) -> p k n", k=KC)
            kmT = [f"aT.{k}" for k in range(4)]
            kk2 = [f"aT.{k}" for k in range(4, 8)]
            for i in range(2):
                kb.dma("sp", memx[i], mem_d[i * 128:(i + 1) * 128, :], [], mxk[i])
            for i in range(1, 4):
                kb.dma("sp", xres[:, 4 * i:4 * i + 4, :], xv[:, 4 * i:4 * i + 4, :], [], [f"xres.{t}" for t in range(4 * i, 4 * i + 4)])

            dstate = {"v_done": False, "t3_done": False}

            def gen_memkv():
                yield from gen_norm([0, 1], lambda t: (memx[t], mxk[t]), 16, lambda kc: memT[:, kc, :], kmT, 16)
                for mt in range(2):
                    for hf in range(2):
                        b = BK.get()
                        bank, bk = BK.f32(b), BK.key(b)
                        for kc in range(KC):
                            kb.mm(bank, memT[:, kc, mt * 128:(mt + 1) * 128], WkvV[:, kc, hf * 512:(hf + 1) * 512],
                                  kc == 0, kc == KC - 1, ["WkvV"] + kmT, [bk])
                        yield
                        kb.cpalt(vmem[:, mt, hf * 512:(hf + 1) * 512], bank, [bk], ["vmem"])
                        BK.put(b)
                        yield
                dstate["v_done"] = True
                for ft in range(KC):
                    b = BK.get()
                    bank, bk = BK.f32(b), BK.key(b)
                    for kc in range(KC):
                        kb.mm(bank[:, 0:256], WkvK[:, kc, ft * 128:(ft + 1) * 128], memT[:, kc, :], kc == 0, kc == KC - 1, ["WA.1"] + kmT, [bk])
                    yield
                    kb.cpalt(kTm[:, ft, :], bank[:, 0:256], [bk], ["kTm"])
                    BK.put(b)
                    yield
                kb.act(k2m, kTm[:], AF.Square, ["kTm"], kk2)
                yield
                for h in range(4):
                    b = BK.get()
                    bank, bk = BK.f32(b), BK.key(b)
                    kb.mm(bank[:, 0:256], onesb[:], k2m[:, 2 * h, :], True, False, ["onesb"] + kk2, [bk])
                    kb.mm(bank[:, 0:256], onesb[:], k2m[:, 2 * h + 1, :], False, True, ["onesb"] + kk2, [bk])
                    yield
                    kb.red(kmx[:, h:h + 1], bank[:, 0:256], ALU.max, [bk], ["kmx"])
                    BK.put(b)
                    yield

            def gen_wout():
                for t in range(NT):
                    tok = slice(t * 128, (t + 1) * 128)
                    for hf in range(2):
                        b = BK.get()
                        bank, bk = BK.f32(b), BK.key(b)
                        for kc in range(KC):
                            ysrc = yT[:, kc, tok] if kc < 4 else hT[:, kc - 4, tok]
                            kb.mm(bank, ysrc, Wout[:, kc, hf * 512:(hf + 1) * 512], kc == 0, kc == KC - 1,
                                  [f"yT.{t}.0", f"yT.{t}.1", "WA.0", f"hT.{t // 4}"], [bk])
                        yield
                        xs = xres[:, t, hf * 512:(hf + 1) * 512]
                        kb.tt("dve", xs, bank, xs, ALU.add, [bk, f"xres.{t}"], [f"xres.{t}"])
                        BK.put(b)
                        yield
                    if t == 3:
                        dstate["t3_done"] = True

            srcx = lambda t: (xres[:, t, :], f"xres.{t}")

            def gen_norm0():
                while not (dstate["v_done"] and dstate["t3_done"]):
                    yield
                yield from gen_norm([0, 1, 2, 3], srcx, 8, lambda kc: hT[:, kc, 0:512], ["hT.0", "WkvV"], 0)

            interleave([gen_memkv(), gen_wout(), gen_norm0()])
            kb.dma("pool", W1, wview(wq_d), [], ["WA.1"])
            kb.dma("pool", W0, wview(wo_d), [], ["WA.0"])

        srcx = lambda t: (xres[:, t, :], f"xres.{t}")
        norm_x = lambda blk: gen_norm(list(range(4 * blk, 4 * blk + 4)), srcx, 8, lambda kc: hT[:, kc, blk * 512:(blk + 1) * 512], [f"hT.{blk}", "WkvV"], 0)
        norm_m = lambda blk: gen_norm(list(range(4 * blk, 4 * blk + 4)), srcx, 24, lambda kc: hT[:, kc, blk * 512:(blk + 1) * 512], f"hT.{blk}", 16)
        with contextlib.ExitStack() as s2:
            qTb = yT[:, 0:2, :].rearrange("p a (b n) -> p (a b) n", n=512)
            PT = yT[:, 2:4, :].rearrange("p a (b n) -> p (a b) n", n=512)
            oTb = oTb_t[:].rearrange("p (k n) -> p k n", k=KC)
            yall = [f"yT.{t}.0" for t in range(NT)]
            q2b_p = [sb(f"q2b{i}", [128, 2, 512], BF16, st=s2) for i in range(4)]
            nb_p = [sb(f"nb{i}", [128, 2], F32, st=s2) for i in range(4)]
            rc_p = [sb(f"rc{i}", [128, 512], F32, st=s2) for i in range(4)]

            def gen_head(sl, h):
                qk = [f"qTb.{2 * h}", f"qTb.{2 * h + 1}"]
                q2b, kq2 = q2b_p[sl], f"q2b.{sl}"
                kb.act(q2b[:], qTb[:, 2 * h:2 * h + 2, :], AF.Square, qk, [kq2])
                yield
                b = BK.get()
                bankQ, kq = BK.f32(b), BK.key(b)
                kb.mm(bankQ, onesb[:], q2b[:, 0, :], True, False, ["onesb", kq2], [kq])
                kb.mm(bankQ, onesb[:], q2b[:, 1, :], False, True, ["onesb", kq2], [kq])
                yield
                nb, kbs = nb_p[sl], f"nb.{sl}"
                kb.red(nb[:, 0:1], bankQ, ALU.max, [kq], [kbs])
                BK.put(b)
                yield
                kb.ts("dve", nb[:, 1:2], nb[:, 0:1], kmx[:, h:h + 1], -1.0 / 32, ALU.add, ALU.mult, [kbs, "kmx"], [kbs])
                yield
                for mt in range(2):
                    b = BK.get()
                    bankS, ksb = BK.f32(b), BK.key(b)
                    ms = slice(mt * 128, (mt + 1) * 128)
                    kb.mm(bankS, kTm[:, 2 * h, ms], qTb[:, 2 * h, :], True, False, ["kTm", qk[0]], [ksb])
                    kb.mm(bankS, kTm[:, 2 * h + 1, ms], qTb[:, 2 * h + 1, :], False, True, ["kTm", qk[1]], [ksb])
                    yield
                    kb.act(PT[:, 2 * h + mt, :], bankS, AF.Exp, [ksb, kbs], [f"PT.{2 * h + mt}"] + (yall if blk == 0 else []), scale=1.0 / 16, bias=nb[:, 1:2])
                    BK.put(b)
                    yield
                pk = [f"PT.{2 * h}", f"PT.{2 * h + 1}"]
                b = BK.get()
                bankRs, krs = BK.f32(b), BK.key(b)
                kb.mm(bankRs, onesb[:], PT[:, 2 * h, :], True, False, ["onesb", pk[0]], [krs])
                kb.mm(bankRs, onesb[:], PT[:, 2 * h + 1, :], False, True, ["onesb", pk[1]], [krs])
                yield
                rc, krc = rc_p[sl], f"rc.{sl}"
                P.op("dve", lambda e: e.reciprocal(out=rc[:], in_=bankRs), [krs], [krc])
                BK.put(b)
                yield
                for dt_ in range(2):
                    ft = 2 * h + dt_
                    b = BK.get()
                    bankO, kob = BK.f32(b), BK.key(b)
                    kb.mm(bankO, vmem[:, 0, ft * 128:(ft + 1) * 128], PT[:, 2 * h, :], True, False, ["vmem", pk[0]], [kob])
                    kb.mm(bankO, vmem[:, 1, ft * 128:(ft + 1) * 128], PT[:, 2 * h + 1, :], False, True, ["vmem", pk[1]], [kob])
                    yield
                    kb.tt("dve", oTb[:, ft, :], bankO, rc[:], ALU.mult, [kob, krc], [f"oTb.{ft}"])
                    BK.put(b)
                    yield

            def gen_attn(blk):
                bs = slice(blk * 512, (blk + 1) * 512)
                for ft in range(KC):
                    b = BK.get()
                    bank, bk = BK.f32(b), BK.key(b)
                    for kc in range(KC):
                        kb.mm(bank, W1[:, kc, ft * 128:(ft + 1) * 128], hT[:, kc, bs], kc == 0, kc == KC - 1, ["WA.1", f"hT.{blk}"], [bk])
                    yield
                    kb.cpalt(qTb[:, ft, :], bank, [bk], [f"qTb.{ft}"] + (yall if blk == 0 else []))
                    BK.put(b)
                    yield
                if blk == 3:
                    mlp_load(0)
                for pair in range(1):
                    gs = [gen_head(h, h) for h in range(4)]
                    while gs:
                        for g in list(gs):
                            try:
                                next(g)
                            except StopIteration:
                                gs.remove(g)
                        yield
                for tl in range(4):
                    t = blk * 4 + tl
                    for hf in range(2):
                        b = BK.get()
                        bank, bk = BK.f32(b), BK.key(b)
                        for kc in range(KC):
                            kb.mm(bank, oTb[:, kc, tl * 128:(tl + 1) * 128], W0[:, kc, hf * 512:(hf + 1) * 512], kc == 0, kc == KC - 1,
                                  [f"oTb.{kc}", "WA.0"], [bk])
                        yield
                        xs = xres[:, t, hf * 512:(hf + 1) * 512]
                        kb.tt("dve", xs, bank, xs, ALU.add, [bk, f"xres.{t}"], [f"xres.{t}"])
                        BK.put(b)
                        yield

            def norm_worker(blk):
                if blk + 1 < 4:
                    yield from norm_x(blk + 1)
                if blk - 1 >= 0:
                    yield from norm_m(blk - 1)

            s3 = s2
            aT_all = aT_t[:].rearrange("p (k n) -> p k n", k=8)
            rl_p = [sb(f"rl{i}", [128, 512], BF16, st=s3) for i in range(2)]
            o_p = [oTb_t[:, 2048 * i:2048 * (i + 1)].bitcast(F32) for i in range(2)]
            upv = wupm_d.rearrange("(kc p) n -> p kc n", p=128)
            dnv = wdn_d.rearrange("(f p) n -> p f n", p=128)
            ov = out_d.rearrange("(t p) d -> p t d", p=128)

            def mlp_w(j):
                s = (j + 1) % 2
                Wu = WA[:, s * 8192:s * 8192 + 4096].rearrange("p (k n) -> p k n", k=KC)
                Wd = WA[:, s * 8192 + 4096:(s + 1) * 8192].rearrange("p (f n) -> p f n", f=4)
                return s, Wu, Wd

            def mlp_load(j):
                s, Wu, Wd = mlp_w(j)
                kb.dma("pool", Wu, upv[:, :, j * 512:(j + 1) * 512], [], [f"WA.{s}"])
                kb.dma("pool", Wd, dnv[:, 4 * j:4 * j + 4, :], [], [f"WA.{s}"])

            for blk in range(4):
                interleave([gen_attn(blk), norm_worker(blk)])
            run(norm_m(3))

            NJ = 8
            ai = 0
            ri = 0
            for j in range(NJ):
                s, Wu, Wd = mlp_w(j)
                if j > 0:
                    mlp_load(j)
                for blk in range(4):
                    bs = slice(blk * 512, (blk + 1) * 512)
                    ab = (ai % 2) * 4
                    ai += 1
                    for fft in range(4):
                        b = BK.get()
                        bank, bk = BK.f32(b), BK.key(b)
                        for kc in range(KC):
                            kb.mm(bank, Wu[:, kc, fft * 128:(fft + 1) * 128], hT[:, kc, bs], kc == 0, kc == KC - 1, [f"WA.{s}", f"hT.{blk}"], [bk])
                        rl, krl = rl_p[ri % 2], f"rl.{ri % 2}"
                        ri += 1
                        kb.act(rl[:], bank, AF.Relu, [bk], [krl])
                        BK.put(b)
                        kb.tt("dve", aT_all[:, ab + fft, :], rl[:], rl[:], ALU.mult, [krl], [f"aT.{ab + fft}"])
                    for tl in range(4):
                        t = blk * 4 + tl
                        for hf in range(2):
                            b = BK.get()
                            bank, bk = BK.f32(b), BK.key(b)
                            for fft in range(4):
                                kb.mm(bank, aT_all[:, ab + fft, tl * 128:(tl + 1) * 128], Wd[:, fft, hf * 512:(hf + 1) * 512], fft == 0, fft == 3,
                                      [f"aT.{ab + fft}", f"WA.{s}"], [bk])
                            xs = xres[:, t, hf * 512:(hf + 1) * 512]
                            kb.tt("dve", xs, bank, xs, ALU.add, [bk, f"xres.{t}"], [f"xres.{t}"])
                            BK.put(b)
                    if j == NJ - 1:
                        for fb in ([blk - 1] if blk > 0 else []) + ([3] if blk == 3 else []):
                            for t in range(4 * fb, 4 * fb + 4):
                                c = 32 + t
                                kb.act(sqj[:], xres[:, t, :], AF.Square, [f"xres.{t}"], ["sqj", f"ss{c}"], accum=ss[:, c:c + 1])
                                kb.ts("dve", ss[:, c:c + 1], ss[:, c:c + 1], 1.0 / D, 1e-6, ALU.mult, ALU.add, [f"ss{c}"], [f"ss{c}"])
                                kb.tt("pool", ss[:, c:c + 1], ss[:, c:c + 1], mhalf[:, 0:1], ALU.pow, [f"ss{c}", "mhalf"], [f"ss{c}"])
                                ot, ko = o_p[t % 2], f"ot.{t % 2}"
                                kb.stt(ot, xres[:, t, :], ss[:, c:c + 1], gfin[:], ALU.mult, ALU.mult, [f"xres.{t}", f"ss{c}", "gfin"],
                                       [ko] + [f"oTb.{k}" for k in range(KC)])
                                kb.dma("sp", ov[:, t, :], ot, [ko], [f"out.{t}"])
            P.barrier()
            P.flush()


def _consts():
    cq = np.zeros((128, NCQ), np.float32)
    i = np.arange(128)
    cq[:, 0:128] = np.eye(128, dtype=np.float32)
    strict = (i[:, None] < i[None, :]).astype(np.float32)
    incl = (i[:, None] <= i[None, :]).astype(np.float32)
    cq[:, 128:256] = strict
    cq[:, 256:384] = incl
    cq[:, 384:512] = strict
    cq[:, 512:640] = incl
    cq[:, 640:768] = (i[:, None] > i[None, :]).astype(np.float32)
    cq[:, 768:896] = ((i[:, None] // 64) == (i[None, :] // 64)).astype(np.float32)
    cq[:, 896:1024] = 1.0
    gam = 1.0 - 2.0 ** (-5.0 - np.arange(8, dtype=np.float64))
    xi = gam[None, :] ** (i[:, None] + 1.0)
    kz = 0.125 * gam[None, :] ** (-(i[:, None] + 1.0))
    gC = np.zeros((128, 4))
    for ct in range(4):
        for p in range(128):
            gC[p, ct] = gam[2 * ct + p // 64] ** 128.0
    invf = (10000.0 ** (-np.arange(32, dtype=np.float32) / 32.0)).astype(np.float32)
    return cq, xi.astype(np.float32), kz.astype(np.float32), gC.astype(np.float32), np.broadcast_to(invf[None], (128, 32))


def _col(v):
    v = np.asarray(v, np.float32).reshape(-1)
    return v.reshape(-1, 128).T


def make_inputs(inp):
    cq, xi, kz, gC, invf = _consts()
    cols = np.zeros((128, NCOLS), np.float32)
    cols[:, 0:8] = _col(inp["norm_mix"][0])
    cols[:, 8:16] = _col(inp["norm_xattn"][0])
    cols[:, 16:24] = _col(inp["norm_mem"][0])
    cols[:, 24:32] = _col(inp["norm_mlp"][0])
    cols[:, 32:36] = _col(inp["ret_gn_w"][0])
    cols[:, 36:40] = _col(inp["ret_gn_b"][0])
    mu = np.asarray(inp["rwkv_mu"][0], np.float32)
    cols[:, 40:54] = _col(mu[:1792])
    cols[:, 54] = mu[1696:1824]
    cols[:, 55:59] = _col(inp["rwkv_w0"][0])
    cols[:, 59:63] = _col(inp["rwkv_a0"][0])
    cols[:, 63:67] = _col(inp["rwkv_k_k"][0])
    cols[:, 67:71] = _col(inp["rwkv_k_a"][0])
    cols[:, 71:75] = _col(inp["rwkv_r_k"][0])
    cols[:, 75:79] = _col(inp["rwkv_gn_w"][0])
    cols[:, 79:83] = _col(inp["rwkv_gn_b"][0])
    cols[:, 83:91] = xi
    cols[:, 91:99] = kz
    cols[:, 99:103] = gC
    cols[:, 103:135] = invf
    shared = {
        "cols": cols, "cq": cq,
        "gfin": np.ascontiguousarray(inp["norm_final"], np.float32),
        "w_in": np.ascontiguousarray(inp["w_in"][0]),
        "w_up": np.ascontiguousarray(inp["rwkv_w_up"][0]),
        "a_up": np.ascontiguousarray(inp["rwkv_a_up"][0]),
        "g_up": np.ascontiguousarray(inp["rwkv_g_up"][0]),
        "w_out": np.ascontiguousarray(inp["w_out"][0]),
        "wq": np.ascontiguousarray(inp["xattn_w_q"][0]),
        "wkv": np.ascontiguousarray(inp["xattn_w_kv"][0]),
        "wo": np.ascontiguousarray(inp["xattn_w_o"][0]),
        "mlp_up": np.ascontiguousarray(inp["mlp_w_up"][0]),
        "mlp_down": np.ascontiguousarray(inp["mlp_w_down"][0]),
    }
    maps = []
    for b in range(8):
        m = dict(shared)
        m["x"] = np.ascontiguousarray(inp["x"][b], np.float32)
        m["mem"] = np.ascontiguousarray(inp["mem"][b], np.float32)
        m["pos"] = np.ascontiguousarray(np.asarray(inp["positions"][b], np.int32).reshape(NT, 128).T)
        maps.append(m)
    return maps


_NC_CACHE = {}


def kernel(**inputs):
    inp = {k: np.asarray(v) for k, v in inputs.items()}
    maps = make_inputs(inp)
    if "nc" not in _NC_CACHE:
        _NC_CACHE["nc"] = build()
    res = run_bass_kernel_spmd(_NC_CACHE["nc"], maps, core_ids=list(range(8)))
    return np.stack([np.asarray(r["out"], np.float32) for r in res.results], axis=0)
```
